# Optimizing a Trainium2 kernel written in Bass

```python
import math
import jax, jax.numpy as jnp
from jax import lax
import numpy as np

D_MODEL = 1024
BATCH = 16
SEQ = 2048
DEPTH = 2

DEEPNORM_ALPHA = (2 * DEPTH) ** 0.25
DEEPNORM_BETA = (8 * DEPTH) ** -0.25
LN_EPS = 1e-5
RMS_EPS = 1e-6
ADA_INIT = 0.1

CONV_CH = D_MODEL // 2
CONV_WIDTH = 31

GLA_HEADS = 4
GLA_V_WIDTH = D_MODEL // 2
GLA_K_WIDTH = GLA_V_WIDTH // 2
GLA_HEAD_K = GLA_K_WIDTH // GLA_HEADS
GLA_HEAD_V = GLA_V_WIDTH // GLA_HEADS
GLA_GATE_RANK = 16
GLA_GATE_TAU = 16.0
GLA_CHUNK = 64

AB_SPLITS = [2 * CONV_CH,
             2 * CONV_CH + GLA_K_WIDTH,
             2 * CONV_CH + 2 * GLA_K_WIDTH,
             2 * CONV_CH + 2 * GLA_K_WIDTH + GLA_V_WIDTH,
             2 * CONV_CH + 2 * GLA_K_WIDTH + 2 * GLA_V_WIDTH]
AB_IN = AB_SPLITS[-1] + GLA_GATE_RANK
AB_OUT = CONV_CH + GLA_V_WIDTH

MLA_HEADS = 8
MLA_NOPE = 128
MLA_ROPE = 64
MLA_V = 128
MLA_Q_LORA = 384
MLA_KV_LORA = 256
MLA_IN = MLA_Q_LORA + MLA_KV_LORA + MLA_ROPE
MLA_SCALE = (MLA_NOPE + MLA_ROPE) ** -0.5
ROPE_THETA = 10000.0
Q_BLOCK = 128

MOE_GROUPS = 4
MOE_EXPERTS_PER_GROUP = 8
MOE_EXPERTS = MOE_GROUPS * MOE_EXPERTS_PER_GROUP
MOE_TOPK = 2
MOE_FF = 256

kernel_name = "hybrid_conv_gla_mla_hmoe_deepnorm"


def layer_norm(x, g, b):
    xf = x.astype(jnp.float32)
    mu = jnp.mean(xf, axis=-1, keepdims=True)
    var = jnp.mean(jnp.square(xf - mu), axis=-1, keepdims=True)
    return ((xf - mu) * lax.rsqrt(var + LN_EPS) * g + b).astype(x.dtype)


def rms_norm(x, g):
    xf = x.astype(jnp.float32)
    return (xf * lax.rsqrt(jnp.mean(jnp.square(xf), axis=-1, keepdims=True) + RMS_EPS) * g).astype(x.dtype)


def apply_rope(x, cos, sin):
    xf = x.astype(jnp.float32)
    x1, x2 = jnp.split(xf, 2, axis=-1)
    return jnp.concatenate([x1 * cos - x2 * sin, x1 * sin + x2 * cos], axis=-1).astype(x.dtype)


def conformer_conv(u, conv_w, conv_b, ln_g, ln_b):
    a, gate = jnp.split(u, 2, axis=-1)
    h = a * jax.nn.sigmoid(gate)
    h = lax.conv_general_dilated(
        h, conv_w[:, None, :], window_strides=(1,), padding=[(CONV_WIDTH - 1, 0)],
        dimension_numbers=('NWC', 'WIO', 'NWC'), feature_group_count=CONV_CH) + conv_b
    return jax.nn.silu(layer_norm(h, ln_g, ln_b))


def gla_chunked(q, k, v, glog):
    B, S, H, DK = q.shape
    DV = v.shape[-1]
    C = GLA_CHUNK
    NC = S // C

    def chunks(t):
        return t.astype(jnp.float32).reshape(B, NC, C, H, t.shape[-1]).transpose(1, 0, 3, 2, 4)

    qc = chunks(q) * (DK ** -0.5)
    kc = chunks(k)
    vc = chunks(v)
    bc = jnp.cumsum(chunks(glog), axis=3)
    causal = jnp.tril(jnp.ones((C, C), dtype=bool))

    def step(state, inp):
        qi, ki, vi, bi = inp
        o_inter = jnp.einsum('bhik,bhkv->bhiv', qi * jnp.exp(bi), state)
        diff = bi[:, :, :, None, :] - bi[:, :, None, :, :]
        decay = jnp.exp(jnp.where(causal[:, :, None], diff, -jnp.inf))
        att = jnp.einsum('bhijk,bhjk->bhij', qi[:, :, :, None, :] * decay, ki)
        o = o_inter + jnp.einsum('bhij,bhjv->bhiv', att, vi)
        b_last = bi[:, :, -1, :]
        k_dec = ki * jnp.exp(b_last[:, :, None, :] - bi)
        state = jnp.exp(b_last)[..., None] * state + jnp.einsum('bhjk,bhjv->bhkv', k_dec, vi)
        return state, o

    state0 = jnp.zeros((B, H, DK, DV), jnp.float32)
    _, o = lax.scan(step, state0, (qc, kc, vc, bc))
    return o.transpose(1, 0, 3, 2, 4).reshape(B, S, H, DV)


def conv_gla_mixer(h, w_in, conv_w, conv_b, conv_ln_g, conv_ln_b, gate_w, gate_b, norm_g, w_out):
    B, S, _ = h.shape
    u = h @ w_in
    u_conv, q, k, v, r, g_low = jnp.split(u, AB_SPLITS, axis=-1)
    y_a = conformer_conv(u_conv, conv_w, conv_b, conv_ln_g, conv_ln_b)
    glog = jax.nn.log_sigmoid((g_low @ gate_w + gate_b).astype(jnp.float32)) / GLA_GATE_TAU
    o = gla_chunked(q.reshape(B, S, GLA_HEADS, GLA_HEAD_K),
                    k.reshape(B, S, GLA_HEADS, GLA_HEAD_K),
                    v.reshape(B, S, GLA_HEADS, GLA_HEAD_V),
                    glog.reshape(B, S, GLA_HEADS, GLA_HEAD_K))
    o = rms_norm(o, norm_g.reshape(GLA_HEADS, GLA_HEAD_V)).reshape(B, S, GLA_V_WIDTH)
    y_b = (o * jax.nn.silu(r.astype(jnp.float32))).astype(h.dtype)
    return jnp.concatenate([y_a.astype(h.dtype), y_b], axis=-1) @ w_out


def mla_mixer(h, cos, sin, w_in, q_norm_g, kv_norm_g, w_uq, w_ukv, w_out):
    B, S, _ = h.shape
    u = h @ w_in
    cq, ckv, k_rope = jnp.split(u, [MLA_Q_LORA, MLA_Q_LORA + MLA_KV_LORA], axis=-1)
    q = (rms_norm(cq, q_norm_g) @ w_uq).reshape(B, S, MLA_HEADS, MLA_NOPE + MLA_ROPE)
    kv = (rms_norm(ckv, kv_norm_g) @ w_ukv).reshape(B, S, MLA_HEADS, MLA_NOPE + MLA_V)
    q_nope, q_rope = jnp.split(q, [MLA_NOPE], axis=-1)
    k_nope, v = jnp.split(kv, [MLA_NOPE], axis=-1)
    q_rope = apply_rope(q_rope, cos[:, :, None, :], sin[:, :, None, :])
    k_rope = apply_rope(k_rope, cos, sin)
    nb = S // Q_BLOCK

    def blocks(t):
        return t.reshape(B, nb, Q_BLOCK, *t.shape[2:]).swapaxes(0, 1)

    key_idx = jnp.arange(S)

    def attend(args):
        qn, qr, blk = args
        s = (jnp.einsum('bqhd,bkhd->bhqk', qn, k_nope, preferred_element_type=jnp.float32)
             + jnp.einsum('bqhr,bkr->bhqk', qr, k_rope, preferred_element_type=jnp.float32)) * MLA_SCALE
        q_idx = blk * Q_BLOCK + jnp.arange(Q_BLOCK)
        s = jnp.where(key_idx[None, :] <= q_idx[:, None], s, -jnp.inf)
        p = jax.nn.softmax(s, axis=-1).astype(v.dtype)
        return jnp.einsum('bhqk,bkhv->bqhv', p, v)

    o = lax.map(attend, (blocks(q_nope), blocks(q_rope), jnp.arange(nb)))
    o = o.swapaxes(0, 1).reshape(B, S, MLA_HEADS * MLA_V)
    return o @ w_out


def hier_moe(h, w_group, b_group, w_router, b_router, w_gate, w_up, w_down):
    B, S, D = h.shape
    t = h.reshape(-1, D)
    T = t.shape[0]
    g_logits = jnp.dot(t, w_group, preferred_element_type=jnp.float32) + b_group
    g_prob = jax.nn.softmax(g_logits, axis=-1)
    g_idx = jnp.argmax(g_logits, axis=-1)
    g_w = jnp.take_along_axis(g_prob, g_idx[:, None], axis=-1)[:, 0]
    e_logits = (jnp.dot(t, w_router, preferred_element_type=jnp.float32) + b_router
                ).reshape(T, MOE_GROUPS, MOE_EXPERTS_PER_GROUP)
    e_sel = jnp.take_along_axis(e_logits, g_idx[:, None, None], axis=1)[:, 0]
    top_v, top_i = lax.top_k(e_sel, MOE_TOPK)
    top_w = jax.nn.softmax(top_v, axis=-1) * g_w[:, None]
    e_id = g_idx[:, None] * MOE_EXPERTS_PER_GROUP + top_i
    comb = jnp.sum(jax.nn.one_hot(e_id, MOE_EXPERTS, dtype=jnp.float32) * top_w[..., None], axis=1)
    y = jnp.zeros((T, D), jnp.float32)
    for e in range(MOE_EXPERTS):
        hid = jax.nn.silu(t @ w_gate[e]) * (t @ w_up[e])
        y = y + comb[:, e:e + 1] * jnp.dot(hid, w_down[e], preferred_element_type=jnp.float32)
    return y.astype(h.dtype).reshape(B, S, D)


def setup_inputs(seed: int = 0) -> dict:
    key = jax.random.key(seed)
    ks = iter(jax.random.split(key, 40))
    D = D_MODEL
    ne = (DEPTH + 1) // 2
    no = DEPTH // 2

    def nrm(shape, scale):
        return jax.random.normal(next(ks), shape, jnp.float32) * scale

    def gain(shape):
        return 1.0 + nrm(shape, 0.02)

    x = nrm((BATCH, SEQ, D), 1.0)
    c = nrm((BATCH, D), 1.0)
    offsets = jax.random.randint(next(ks), (BATCH, 1), 0, 4096)
    positions = (offsets + jnp.arange(SEQ)[None, :]).astype(jnp.int32)
    return {
        "x": x,
        "c": c,
        "positions": positions,
        "ada_w": nrm((DEPTH, D, 6 * D), ADA_INIT * D ** -0.5),
        "ada_b": nrm((DEPTH, 6 * D), 0.01),
        "ln_mix_g": gain((DEPTH, D)),
        "ln_mix_b": nrm((DEPTH, D), 0.02),
        "ln_ffn_g": gain((DEPTH, D)),
        "ln_ffn_b": nrm((DEPTH, D), 0.02),
        "ab_w_in": nrm((ne, D, AB_IN), D ** -0.5),
        "conv_w": nrm((ne, CONV_WIDTH, CONV_CH), CONV_WIDTH ** -0.5),
        "conv_b": nrm((ne, CONV_CH), 0.02),
        "conv_ln_g": gain((ne, CONV_CH)),
        "conv_ln_b": nrm((ne, CONV_CH), 0.02),
        "gla_gate_w": nrm((ne, GLA_GATE_RANK, GLA_K_WIDTH), GLA_GATE_RANK ** -0.5),
        "gla_gate_b": nrm((ne, GLA_K_WIDTH), 0.02),
        "gla_norm_g": gain((ne, GLA_V_WIDTH)),
        "ab_w_out": nrm((ne, AB_OUT, D), AB_OUT ** -0.5 * DEEPNORM_BETA),
        "mla_w_in": nrm((no, D, MLA_IN), D ** -0.5),
        "mla_q_norm_g": gain((no, MLA_Q_LORA)),
        "mla_kv_norm_g": gain((no, MLA_KV_LORA)),
        "mla_w_uq": nrm((no, MLA_Q_LORA, MLA_HEADS * (MLA_NOPE + MLA_ROPE)), MLA_Q_LORA ** -0.5),
        "mla_w_ukv": nrm((no, MLA_KV_LORA, MLA_HEADS * (MLA_NOPE + MLA_V)), MLA_KV_LORA ** -0.5),
        "mla_w_out": nrm((no, MLA_HEADS * MLA_V, D), (MLA_HEADS * MLA_V) ** -0.5 * DEEPNORM_BETA),
        "moe_w_group": nrm((DEPTH, D, MOE_GROUPS), D ** -0.5),
        "moe_b_group": nrm((DEPTH, MOE_GROUPS), 0.01),
        "moe_w_router": nrm((DEPTH, D, MOE_EXPERTS), D ** -0.5),
        "moe_b_router": nrm((DEPTH, MOE_EXPERTS), 0.01),
        "moe_w_gate": nrm((DEPTH, MOE_EXPERTS, D, MOE_FF), D ** -0.5),
        "moe_w_up": nrm((DEPTH, MOE_EXPERTS, D, MOE_FF), D ** -0.5),
        "moe_w_down": nrm((DEPTH, MOE_EXPERTS, MOE_FF, D), MOE_FF ** -0.5 * DEEPNORM_BETA),
    }


def reference(x, c, positions, ada_w, ada_b, ln_mix_g, ln_mix_b, ln_ffn_g, ln_ffn_b,
              ab_w_in, conv_w, conv_b, conv_ln_g, conv_ln_b, gla_gate_w, gla_gate_b,
              gla_norm_g, ab_w_out, mla_w_in, mla_q_norm_g, mla_kv_norm_g, mla_w_uq,
              mla_w_ukv, mla_w_out, moe_w_group, moe_b_group, moe_w_router, moe_b_router,
              moe_w_gate, moe_w_up, moe_w_down):
    inv_freq = 1.0 / (ROPE_THETA ** (jnp.arange(0, MLA_ROPE, 2, dtype=jnp.float32) / MLA_ROPE))
    ang = positions.astype(jnp.float32)[..., None] * inv_freq
    cos, sin = jnp.cos(ang), jnp.sin(ang)
    c_act = jax.nn.silu(c)

    for layer in range(DEPTH):
        mod = (c_act @ ada_w[layer] + ada_b[layer])[:, None, :]
        sh_m, sc_m, g_m, sh_f, sc_f, g_f = jnp.split(mod, 6, axis=-1)
        i = layer // 2

        h = x * (1.0 + sc_m) + sh_m
        if layer % 2 == 0:
            mix = conv_gla_mixer(h, ab_w_in[i], conv_w[i], conv_b[i], conv_ln_g[i], conv_ln_b[i],
                                 gla_gate_w[i], gla_gate_b[i], gla_norm_g[i], ab_w_out[i])
        else:
            mix = mla_mixer(h, cos, sin, mla_w_in[i], mla_q_norm_g[i], mla_kv_norm_g[i],
                            mla_w_uq[i], mla_w_ukv[i], mla_w_out[i])
        x = layer_norm(DEEPNORM_ALPHA * x + (1.0 + g_m) * mix, ln_mix_g[layer], ln_mix_b[layer])

        h = x * (1.0 + sc_f) + sh_f
        ffn = hier_moe(h, moe_w_group[layer], moe_b_group[layer], moe_w_router[layer],
                       moe_b_router[layer], moe_w_gate[layer], moe_w_up[layer], moe_w_down[layer])
        x = layer_norm(DEEPNORM_ALPHA * x + (1.0 + g_f) * ffn, ln_ffn_g[layer], ln_ffn_b[layer])
    return x
```

```python
from contextlib import ExitStack
import math
import numpy as np
import concourse.bass as bass
import concourse.mybir as mybir
from concourse.bass_utils import run_bass_kernel_spmd

F32 = mybir.dt.float32
BF16 = mybir.dt.bfloat16
I32 = mybir.dt.int32
AF = mybir.ActivationFunctionType
ALU = mybir.AluOpType
AX = mybir.AxisListType

NCORES = 8
D = 1024
S_LEN = 2048
NT = 16
ALPHA = 4.0 ** 0.25
LN_EPS = 1e-5
RMS_EPS = 1e-6
MLA_SCALE = 192.0 ** -0.5
TWO_PI = 2.0 * math.pi

COMPUTE = ("pe", "act", "dve", "pool")


class Tile:
    __slots__ = ("ap", "name", "w", "rs", "semkey")

    def __init__(self, ap, name, semkey=None):
        self.ap = ap
        self.name = name
        self.w = None
        self.rs = []
        self.semkey = semkey or name

    def __getitem__(self, idx):
        return self.ap[idx]


class Op:
    __slots__ = ("eng", "fn", "deps", "signal", "sigidx", "pos", "semkey", "semval", "ndma")

    def __init__(self, eng, fn):
        self.eng = eng
        self.fn = fn
        self.deps = []
        self.signal = False
        self.sigidx = 0
        self.pos = 0
        self.semkey = None
        self.semval = 0
        self.ndma = 0


class Sched:
    def __init__(self, nc, stack):
        self.nc = nc
        self.stack = stack
        self.ops = {e: [] for e in ("pe", "act", "dve", "pool", "sp")}
        self.semcnt = {}
        self.uid = 0
        self.fence_deps = []
        self.fence_pending = set()
        self.dma_since = []

    def sbuf(self, name, shape, dtype, st=None):
        self.uid += 1
        t = (st or self.stack).enter_context(self.nc.sbuf_tensor("%s_%d" % (name, self.uid), list(shape), dtype))
        return Tile(t, name)

    def psum(self, name, shape, dtype=F32, st=None):
        self.uid += 1
        t = (st or self.stack).enter_context(self.nc.psum_tensor("%s_%d" % (name, self.uid), list(shape), dtype))
        return Tile(t, name)

    def _add(self, eng, fn, reads, writes, semkey=None, ndma=0):
        op = Op(eng, fn)
        lst = self.ops[eng]
        op.pos = len(lst)
        deps = []
        if eng in self.fence_pending:
            self.fence_pending.discard(eng)
            deps.extend(self.fence_deps)
        for t in reads:
            if t.w is not None:
                deps.append(t.w)
        for t in writes:
            if t.w is not None:
                deps.append(t.w)
            deps.extend(t.rs)
        seen = set()
        for d in deps:
            if id(d) in seen or d is op:
                continue
            seen.add(id(d))
            if d.semkey is None and d.eng == eng:
                if eng == "pe" or eng == "sp":
                    continue
            op.deps.append(d)
            if d.semkey is None:
                d.signal = True
        for t in reads:
            if semkey is None:
                t.rs = [r for r in t.rs if not (r.semkey is None and r.eng == eng)]
            t.rs.append(op)
        for t in writes:
            t.w = op
            t.rs = []
        if semkey is not None:
            op.semkey = semkey
            op.ndma = ndma
            self.semcnt[semkey] = self.semcnt.get(semkey, 0) + 16 * ndma
            op.semval = self.semcnt[semkey]
            self.dma_since.append(op)
        lst.append(op)
        return op

    def op(self, eng, fn, reads=(), writes=()):
        return self._add(eng, fn, list(reads), list(writes))

    def dma(self, eng, fn, reads=(), writes=(), sync=None, n=1):
        return self._add(eng, fn, list(reads), list(writes), semkey=sync.semkey, ndma=n)

    def fence(self):
        deps = []
        for lst in self.ops.values():
            for d in reversed(lst):
                if d.semkey is None:
                    deps.append(d)
                    break
        deps = deps + self.dma_since
        self.dma_since = []
        self.fence_deps = deps
        self.fence_pending = set(self.ops.keys())

    def emit(self, final_keys=()):
        nc = self.nc
        stack = self.stack
        esem = {e: stack.enter_context(nc.semaphore("es_" + e)) for e in COMPUTE}
        dsem = {k: stack.enter_context(nc.semaphore("ds_%d" % i)) for i, k in enumerate(sorted(self.semcnt))}
        for e in COMPUTE:
            c = 0
            for op in self.ops[e]:
                if op.signal and op.semkey is None:
                    c += 1
                    op.sigidx = c
        block = stack.enter_context(nc.Block())

        def run(ename, engine):
            waited = {}
            for op in self.ops[ename]:
                need = {}
                for d in op.deps:
                    if d.semkey is not None:
                        s, v, key = dsem[d.semkey], d.semval, "d" + d.semkey
                    else:
                        s, v, key = esem[d.eng], d.sigidx, "e" + d.eng
                    if key not in need or need[key][1] < v:
                        need[key] = (s, v)
                for key, (s, v) in need.items():
                    if waited.get(key, 0) >= v:
                        continue
                    waited[key] = v
                    engine.wait_ge(s, v)
                r = op.fn(engine)
                if op.semkey is not None:
                    if not isinstance(r, (list, tuple)):
                        r = [r]
                    assert len(r) == op.ndma, (len(r), op.ndma)
                    for ins in r:
                        ins.then_inc(dsem[op.semkey], 16)
                elif op.signal:
                    r.then_inc(esem[ename], 1)
            if ename == "sp":
                for k in final_keys:
                    engine.wait_ge(dsem[k], self.semcnt[k])

        @block.sync
        def _(sync):
            run("sp", sync)

        @block.tensor
        def _(tensor):
            run("pe", tensor)

        @block.scalar
        def _(scalar):
            run("act", scalar)

        @block.vector
        def _(vector):
            run("dve", vector)

        @block.gpsimd
        def _(gpsimd):
            run("pool", gpsimd)


class Rot:
    def __init__(self, tiles):
        self.tiles = tiles
        self.i = 0

    def next(self):
        t = self.tiles[self.i % len(self.tiles)]
        self.i += 1
        return t


C_ID, C_TRI, C_SU, C_ONE, C_INVF, C_SGN, C_NHALF, C_NPI, C_REPS, NCONST = 0, 128, 256, 384, 512, 513, 514, 515, 516, 517


C2_LT, C2_THR, C2_VROW, C2_EROW, C2_VAL, NC2 = 0, 1024, 1032, 1080, 1112, 1144


def make_consts2():
    c = np.zeros((128, NC2), np.float32)
    p = np.arange(128)
    e = np.arange(32)
    c[:, C2_LT:C2_LT + 1024] = (e[None, :] < e[:, None]).astype(np.float32).reshape(1, 1024)
    c[:, C2_THR:C2_THR + 8] = 256.0 * np.arange(8)[None, :]
    c[:, C2_VROW:C2_VROW + 48] = np.arange(48)[None, :]
    c[:, C2_EROW:C2_EROW + 32] = e[None, :] * 128.0 + p[:, None] - 1048576.0
    kt = np.arange(32)
    c[:, C2_VAL:C2_VAL + 32] = (kt // 16)[None, :] * 2048.0 + (kt % 16)[None, :] * 128.0 + p[:, None]
    return c


def make_consts():
    c = np.zeros((128, NCONST), np.float32)
    p = np.arange(128)
    c[:, C_ID:C_ID + 128] = np.eye(128, dtype=np.float32)
    c[:, C_TRI:C_TRI + 128] = (p[:, None] <= p[None, :]).astype(np.float32)
    c[:, C_SU:C_SU + 128] = (p[:, None] > p[None, :]).astype(np.float32)
    c[:, C_ONE:C_ONE + 128] = 1.0
    inv_freq = (1.0 / (np.float32(10000.0) ** (np.arange(0, 64, 2, dtype=np.float32) / np.float32(64)))).astype(np.float32)
    c[:64, C_INVF] = inv_freq[p[:64] % 32]
    c[:, C_SGN] = np.where(p < 32, -1.0, 1.0)
    c[:, C_NHALF] = -0.5
    c[:, C_NPI] = -math.pi
    c[:, C_REPS] = RMS_EPS
    return c


def build_nc(nseq=2, stop_after=None, dbg=9):
    nc = bass.Bass("TRN2", target_bir_lowering=False)

    def din(name, shape, dt=F32):
        return nc.dram_tensor(name, list(shape), dt, kind="ExternalInput").ap()

    x_d = din("x", [2, S_LEN, D])
    c_d = din("c", [2, D])
    pos_d = din("pos", [2, S_LEN], I32)
    const_d = din("consts", [128, NCONST])
    ada_w_d = din("ada_w", [2, D, 6 * D])
    ada_b_d = din("ada_b", [2, 6 * D])
    ln_d = din("ln_gb", [2, 4, D])
    ab_w_in_d = din("ab_w_in", [D, 2576])
    conv_w_d = din("conv_w", [31, 512])
    pv_d = din("pvec", [40, 128])
    gate_w_d = din("gla_gate_w", [16, 256])
    gate_b_d = din("gla_gate_b", [1, 256])
    gnorm_d = din("gla_norm_g", [1, 512])
    ab_w_out_d = din("ab_w_out", [D, D])
    mla_w_in_d = din("mla_w_in", [D, 768])
    mla_w_uq_d = din("mla_w_uq", [384, 2048])
    mla_w_ukv_d = din("mla_w_ukv", [256, 2048])
    mla_w_out_d = din("mla_w_out", [D, D])
    wr_d = din("moe_wr", [2, D, 36])
    br_d = din("moe_br", [2, 36])
    wall_ds = [din("moe_wall%d" % i, [32 * 128, 6144]) for i in range(2)]
    const2_d = din("consts2", [128, NC2])
    htok_d = nc.dram_tensor("htok_scr", [S_LEN, D], BF16).ap()
    ys_d = nc.dram_tensor("ys_scr", [2 * S_LEN, D], F32).ap()
    tab_d = nc.dram_tensor("tab_scr", [96 * 128, 1], I32).ap()
    out_d = nc.dram_tensor("out", [2, S_LEN, D], F32, kind="ExternalOutput").ap()

    with ExitStack() as st:
        S = Sched(nc, st)
        global LAST_SCHED
        LAST_SCHED = S
        DR = Tile(None, "dram_in")
        DO = Tile(None, "dram_out")
        TABF = Tile(None, "tabf")
        TABS = Tile(None, "tabs")
        BC = {}
        for bv in (4095, 2047, 96 * 128 - 1):
            BC[bv] = nc.alloc_register(mybir.EngineType.Pool, "bc%d" % bv)
            S.op("pool", lambda e, bv=bv: e.reg_mov(BC[bv], bv))

        X = S.sbuf("X", [128, NT, D], F32)
        XT = [Tile(X.ap[:, t, :], "X%d" % t) for t in range(NT)]
        HT = S.sbuf("HT", [128, 8, S_LEN], BF16)
        CONST = S.sbuf("CONST", [128, NCONST], F32)
        IDB = S.sbuf("IDB", [128, 128], BF16)
        TRIB = S.sbuf("TRIB", [128, 128], BF16)
        ONEB = S.sbuf("ONEB", [128, 128], BF16)
        MOD = S.sbuf("MOD", [128, 2, 48, 2], F32)
        G1B = S.sbuf("G1B", [128, D], F32)
        LNG = S.sbuf("LNG", [128, D], F32)
        LNB = S.sbuf("LNB", [128, D], F32)
        PVT = S.sbuf("PVT", [128, 40], F32)
        MV = S.sbuf("MV", [128, NT, 2], F32)
        RSTD = S.sbuf("RSTD", [128, NT], F32)
        NHALF = S.sbuf("NHALF", [128, 512], F32)

        ident = CONST[:, C_ID:C_ID + 128]
        tri = CONST[:, C_TRI:C_TRI + 128]
        su = CONST[:, C_SU:C_SU + 128]
        ones = CONST[:, C_ONE:C_ONE + 128]

        with ExitStack() as ph:
            S.dma("sp", lambda e: e.dma_start(out=CONST[:, :], in_=const_d[:, :]), reads=[DR], writes=[CONST], sync=CONST)
            S.op("dve", lambda e: e.tensor_copy(out=IDB[:, :], in_=ident), reads=[CONST], writes=[IDB])
            S.op("dve", lambda e: e.tensor_copy(out=TRIB[:, :], in_=tri), reads=[CONST], writes=[TRIB])
            S.op("dve", lambda e: e.tensor_copy(out=ONEB[:, :], in_=ones), reads=[CONST], writes=[ONEB])
            S.op("pool", lambda e: e.memset(NHALF[:, :], -0.5), writes=[NHALF])
            STG = S.sbuf("STG", [128, 128], F32, st=ph)
            S.op("dve", lambda e: e.memset(STG[:, :], 0.0), writes=[STG])
            S.dma("sp", lambda e: [e.dma_start(out=STG[0:40, :], in_=pv_d[:, :]),
                                   e.dma_start(out=STG[64:80, :], in_=c_d.rearrange("s (c p) -> (s c) p", p=128)),
                                   ], reads=[DR], writes=[STG], sync=STG, n=2)
            pp = S.psum("pp_setup", [128, 512], F32, st=ph)
            pp2 = S.psum("pp_setup2", [128, 512], F32, st=ph)
            S.op("pe", lambda e: e.transpose(pp[:, 0:128], STG[:, :], ident), reads=[STG, CONST], writes=[pp])
            S.op("dve", lambda e: e.tensor_copy(out=PVT[:, :], in_=pp[:, 0:40]), reads=[pp], writes=[PVT])
            CACT = S.sbuf("CACT", [128, 2, 8], BF16, st=ph)
            S.op("act", lambda e: e.activation(out=CACT[:, :, :].rearrange("p s c -> p (s c)"), in_=pp[:, 64:80], func=AF.Silu), reads=[pp], writes=[CACT])
            STB = S.sbuf("STB", [128, 128], F32, st=ph)
            S.op("dve", lambda e: e.memset(STB[:, :], 0.0), writes=[STB])
            S.dma("sp", lambda e: e.dma_start(out=STB[0:96, :], in_=ada_b_d.rearrange("l (j p) -> (l j) p", p=128)), reads=[DR], writes=[STB], sync=STB)
            S.op("pe", lambda e: e.transpose(pp2[:, 0:128], STB[:, :], ident), reads=[STB, CONST], writes=[pp2])
            ADABT = S.sbuf("ADABT", [128, 96], F32, st=ph)
            S.op("dve", lambda e: e.tensor_copy(out=ADABT[:, :], in_=pp2[:, 0:96]), reads=[pp2], writes=[ADABT])
            AW = [S.sbuf("AW%d" % i, [128, 8, 512], BF16, st=ph) for i in range(3)]
            modp = S.psum("modp", [128, 2, 48, 2], F32, st=ph)
            for l in range(2):
                for blk in range(12):
                    aw = AW[(l * 12 + blk) % 3]
                    src = ada_w_d[l].rearrange("(c p) n -> p c n", p=128)[:, :, blk * 512:(blk + 1) * 512]
                    S.dma("pool", lambda e, aw=aw, src=src: e.dma_start(out=aw[:, :, :], in_=src), reads=[DR], writes=[aw], sync=aw)
                    for jj in range(4):
                        j = blk * 4 + jj
                        for kc in range(8):
                            S.op("pe", lambda e, aw=aw, jj=jj, kc=kc, l=l, j=j: e.matmul(
                                modp[:, l, j, :], lhsT=aw[:, kc, jj * 128:(jj + 1) * 128], rhs=CACT[:, :, kc],
                                start=(kc == 0), stop=(kc == 7)), reads=[aw, CACT], writes=[modp])
            for l in range(2):
                for s in range(2):
                    S.op("dve", lambda e, l=l, s=s: e.tensor_tensor(out=MOD[:, l, :, s], in0=modp[:, l, :, s], in1=ADABT[:, l * 48:(l + 1) * 48], op=ALU.add),
                         reads=[modp, ADABT], writes=[MOD])
            for l in range(2):
                for j0 in (8, 32):
                    S.op("dve", lambda e, l=l, j0=j0: e.tensor_scalar(out=MOD[:, l, j0:j0 + 8, :], in0=MOD[:, l, j0:j0 + 8, :], scalar1=1.0, scalar2=1.0 / ALPHA, op0=ALU.add, op1=ALU.mult),
                         reads=[MOD], writes=[MOD])
                for j0 in (16, 40):
                    S.op("dve", lambda e, l=l, j0=j0: e.tensor_scalar(out=MOD[:, l, j0:j0 + 8, :], in0=MOD[:, l, j0:j0 + 8, :], scalar1=1.0, scalar2=None, op0=ALU.add),
                         reads=[MOD], writes=[MOD])
            S.fence()

        def load_ln(l, which, scaled):
            S.dma("sp", lambda e: [e.dma_start(out=LNG[:, :], in_=ln_d[l, 2 * which:2 * which + 1, :].partition_broadcast(128)),
                                   e.dma_start(out=LNB[:, :], in_=ln_d[l, 2 * which + 1:2 * which + 2, :].partition_broadcast(128))],
                  reads=[DR], writes=[LNG, LNB], sync=LNG, n=2)
            if scaled:
                S.op("dve", lambda e: e.tensor_scalar(out=LNG[:, :], in0=LNG[:, :], scalar1=ALPHA, scalar2=None, op0=ALU.mult), reads=[LNG], writes=[LNG])
                S.op("dve", lambda e: e.tensor_scalar(out=LNB[:, :], in0=LNB[:, :], scalar1=ALPHA, scalar2=None, op0=ALU.mult), reads=[LNB], writes=[LNB])

        def build_g1b(l, j0, s, pp_ts, scratch, dst=None):
            dst = G1B if dst is None else dst
            for c in range(8):
                S.op("dve", lambda e, c=c: e.tensor_scalar(out=scratch[:, c, :], in0=ident, scalar1=MOD[:, l, j0 + c, s:s + 1], scalar2=None, op0=ALU.mult),
                     reads=[CONST, MOD], writes=[scratch])
            for hf in range(2):
                pp_t = pp_ts[hf]
                for c4 in range(4):
                    S.op("pe", lambda e, hf=hf, c4=c4, pp_t=pp_t: e.matmul(pp_t[:, c4 * 128:(c4 + 1) * 128], lhsT=ones, rhs=scratch[:, hf * 4 + c4, :], start=True, stop=True),
                         reads=[CONST, scratch], writes=[pp_t])
                S.op("act", lambda e, hf=hf, pp_t=pp_t: e.activation(out=dst[:, hf * 512:(hf + 1) * 512], in_=pp_t[:, :], func=AF.Copy), reads=[pp_t], writes=[dst])

        def transpose_tile(t, l, jsc, jsh, s, tp_rot, dst_fn, dst_tile, f32_dst=None):
            for hlf in range(2):
                tp = tp_rot.next()
                for c4 in range(4):
                    c = hlf * 4 + c4
                    S.op("pe", lambda e, tp=tp, c=c, c4=c4: e.transpose(tp[:, c4 * 128:(c4 + 1) * 128], XT[t][:, c * 128:(c + 1) * 128], ident),
                         reads=[XT[t], CONST], writes=[tp])
                for c4 in range(4):
                    c = hlf * 4 + c4
                    o_ap = dst_fn(c) if f32_dst is None else f32_dst[:, c, :]
                    o_t = dst_tile if f32_dst is None else f32_dst
                    if c % 2 == 0:
                        S.op("dve", lambda e, tp=tp, c=c, c4=c4, o_ap=o_ap: e.tensor_scalar(
                            out=o_ap, in0=tp[:, c4 * 128:(c4 + 1) * 128], scalar1=MOD[:, l, jsc + c, s:s + 1], scalar2=MOD[:, l, jsh + c, s:s + 1],
                            op0=ALU.mult, op1=ALU.add), reads=[tp, MOD], writes=[o_t])
                    else:
                        S.op("act", lambda e, tp=tp, c=c, c4=c4, o_ap=o_ap: e.activation(
                            out=o_ap, in_=tp[:, c4 * 128:(c4 + 1) * 128], func=AF.Identity, scale=MOD[:, l, jsc + c, s:s + 1], bias=MOD[:, l, jsh + c, s:s + 1]),
                            reads=[tp, MOD], writes=[o_t])

        LN_STATS = S.sbuf("STATS", [128, NT, 2, 6], F32)
        LN_VE = S.sbuf("VE", [128, NT], F32)
        LN_NMR = S.sbuf("NMR", [128, NT], F32)

        def layer_norm_all():
            STATS, VE, NMR = LN_STATS, LN_VE, LN_NMR
            for t in range(NT):
                for h2 in range(2):
                    S.op("dve", lambda e, t=t, h2=h2: e.bn_stats(out=STATS[:, t, h2, :], in_=XT[t][:, h2 * 512:(h2 + 1) * 512]), reads=[XT[t]], writes=[STATS])
                S.op("dve", lambda e, t=t: e.bn_aggr(out=MV[:, t, :], in_=STATS[:, t, :, :].rearrange("p a b -> p (a b)")), reads=[STATS], writes=[MV])
            S.op("dve", lambda e: e.tensor_scalar(out=VE[:, :], in0=MV[:, :, 1], scalar1=LN_EPS, scalar2=None, op0=ALU.add), reads=[MV], writes=[VE])
            S.op("pool", lambda e: e.tensor_tensor(out=RSTD[:, :], in0=VE[:, :], in1=NHALF[:, 0:NT], op=ALU.pow), reads=[VE, NHALF], writes=[RSTD])
            S.op("dve", lambda e: e.scalar_tensor_tensor(out=NMR[:, :], in0=MV[:, :, 0], scalar=-1.0, in1=RSTD[:, :], op0=ALU.mult, op1=ALU.mult), reads=[MV, RSTD], writes=[NMR])
            for t in range(NT):
                S.op("act", lambda e, t=t: e.activation(out=XT[t][:, :], in_=XT[t][:, :], func=AF.Identity, scale=RSTD[:, t:t + 1], bias=NMR[:, t:t + 1]),
                     reads=[XT[t], NMR, RSTD], writes=[XT[t]])
                S.op("dve", lambda e, t=t: e.tensor_tensor(out=XT[t][:, :], in0=XT[t][:, :], in1=LNG[:, :], op=ALU.mult), reads=[XT[t], LNG], writes=[XT[t]])
                S.op("pool", lambda e, t=t: e.tensor_tensor(out=XT[t][:, :], in0=XT[t][:, :], in1=LNB[:, :], op=ALU.add), reads=[XT[t], LNB], writes=[XT[t]])

        LN_STt = [Tile(LN_STATS.ap[:, t], "LNST%d" % t) for t in range(NT)]
        MVt = [Tile(MV.ap[:, t, :], "MV%d" % t) for t in range(NT)]
        VEt = [Tile(LN_VE.ap[:, t:t + 1], "VE%d" % t) for t in range(NT)]
        RSTDt = [Tile(RSTD.ap[:, t:t + 1], "RSTD%d" % t) for t in range(NT)]
        NMRt = [Tile(LN_NMR.ap[:, t:t + 1], "NMR%d" % t) for t in range(NT)]

        class LNPipe:
            def __init__(self, act_stats=False, junk=None):
                self.q = []
                self.act_stats = act_stats
                self.junk = junk

            def s1(self, t):
                st_, mv, ve, rstd = LN_STt[t], MVt[t], VEt[t], RSTDt[t]
                if self.act_stats:
                    junk = self.junk
                    S.op("act", lambda e: e.activation(out=junk[:, :], in_=XT[t][:, :], func=AF.Copy, accum_out=st_[:, 0, 0:1]), reads=[XT[t]], writes=[junk, st_])
                    S.op("act", lambda e: e.activation(out=junk[:, :], in_=XT[t][:, :], func=AF.Square, accum_out=st_[:, 0, 1:2]), reads=[XT[t]], writes=[junk, st_])
                    S.op("dve", lambda e: e.tensor_scalar(out=mv[:, 0:1], in0=st_[:, 0, 0:1], scalar1=1.0 / D, scalar2=None, op0=ALU.mult), reads=[st_], writes=[mv])
                    S.op("dve", lambda e: e.tensor_tensor(out=mv[:, 1:2], in0=mv[:, 0:1], in1=mv[:, 0:1], op=ALU.mult), reads=[mv], writes=[mv])
                    S.op("dve", lambda e: e.scalar_tensor_tensor(out=ve[:, :], in0=st_[:, 0, 1:2], scalar=1.0 / D, in1=mv[:, 1:2], op0=ALU.mult, op1=ALU.subtract), reads=[st_, mv], writes=[ve])
                    S.op("dve", lambda e: e.tensor_scalar(out=ve[:, :], in0=ve[:, :], scalar1=LN_EPS, scalar2=None, op0=ALU.add), reads=[ve], writes=[ve])
                else:
                    for h2 in range(2):
                        S.op("dve", lambda e, h2=h2: e.bn_stats(out=st_[:, h2, :], in_=XT[t][:, h2 * 512:(h2 + 1) * 512]), reads=[XT[t]], writes=[st_])
                    S.op("dve", lambda e: e.bn_aggr(out=mv[:, :], in_=st_[:, :, :].rearrange("p a b -> p (a b)")), reads=[st_], writes=[mv])
                    S.op("dve", lambda e: e.tensor_scalar(out=ve[:, :], in0=mv[:, 1:2], scalar1=LN_EPS, scalar2=None, op0=ALU.add), reads=[mv], writes=[ve])
                S.op("pool", lambda e: e.tensor_tensor(out=rstd[:, :], in0=ve[:, :], in1=NHALF[:, 0:1], op=ALU.pow), reads=[ve, NHALF], writes=[rstd])

            def s2(self, t):
                mv, rstd, nmr = MVt[t], RSTDt[t], NMRt[t]
                S.op("dve", lambda e: e.scalar_tensor_tensor(out=nmr[:, :], in0=mv[:, 0:1], scalar=-1.0, in1=rstd[:, :], op0=ALU.mult, op1=ALU.mult), reads=[mv, rstd], writes=[nmr])
                S.op("act", lambda e: e.activation(out=XT[t][:, :], in_=XT[t][:, :], func=AF.Identity, scale=rstd[:, :], bias=nmr[:, :]), reads=[XT[t], nmr, rstd], writes=[XT[t]])

            def s3(self, t):
                S.op("dve", lambda e: e.tensor_tensor(out=XT[t][:, :], in0=XT[t][:, :], in1=LNG[:, :], op=ALU.mult), reads=[XT[t], LNG], writes=[XT[t]])
                S.op("pool", lambda e: e.tensor_tensor(out=XT[t][:, :], in0=XT[t][:, :], in1=LNB[:, :], op=ALU.add), reads=[XT[t], LNB], writes=[XT[t]])

            def push(self, t):
                self.q.append([t, 0])
                self.step()

            def step(self):
                for ent in list(self.q):
                    if ent[1] == 0:
                        self.s1(ent[0])
                    elif ent[1] == 1:
                        self.s2(ent[0])
                    else:
                        self.s3(ent[0])
                    ent[1] += 1
                self.q = [en for en in self.q if en[1] < 3]

            def flush(self):
                while self.q:
                    self.step()

        NV, NJ = 48, 96
        BIG = 1048576.0

        def moe_phase(l, s, last):
            jsh, jsc, jg = 24, 32, 40
            wall_d = wall_ds[l]
            IOA = bass.IndirectOffsetOnAxis
            with ExitStack() as ph:
                LG = S.sbuf("LG", [128, NT, 36], F32, st=ph)
                P12 = S.sbuf("P12", [128, 2, NT], F32, st=ph)
                IDXY = S.sbuf("IDXY", [128, NJ], I32, st=ph)
                IDXG = S.sbuf("IDXG", [128, NJ], I32, st=ph)
                WIDX = S.sbuf("WIDX", [128, NV], I32, st=ph)
                NW, NH = 3, 4

                with ExitStack() as ph1:
                    WR = S.sbuf("WR", [128, 8, 36], F32, st=ph1)
                    RB = S.sbuf("RB", [128, 36], F32, st=ph1)
                    H32 = Rot([S.sbuf("H32_%d" % i, [128, 8, 128], F32, st=ph1) for i in range(3)])
                    SCB = S.sbuf("SCB", [128, D], F32, st=ph1)
                    SHB = S.sbuf("SHB", [128, D], F32, st=ph1)
                    TM32 = Rot([S.sbuf("TM32_%d" % i, [128, D], F32, st=ph1) for i in range(2)])
                    HB = Rot([S.sbuf("HB_%d" % i, [128, D], BF16, st=ph1) for i in range(2)])
                    tp_rot = Rot([S.psum("tp%d" % i, [128, 512], F32, st=ph1) for i in range(4)])
                    lg_rot = Rot([S.psum("lgp%d" % i, [128, 512], F32, st=ph1) for i in range(2)])
                    scrs = [S.sbuf("scr%d" % i, [128, 8, 128], F32, st=ph1) for i in range(2)]
                    S.dma("sp", lambda e: [e.dma_start(out=WR[:, :, :], in_=wr_d[l].rearrange("(c p) n -> p c n", p=128)),
                                           e.dma_start(out=RB[:, :], in_=br_d[l:l + 1, :].partition_broadcast(128))],
                          reads=[DR], writes=[WR, RB], sync=WR, n=2)
                    build_g1b(l, jsc, s, tp_rot.tiles[0:2], scrs[0], dst=SCB)
                    build_g1b(l, jsh, s, tp_rot.tiles[2:4], scrs[1], dst=SHB)
                    build_g1b(l, jg, s, lg_rot.tiles, scrs[0])
                    load_ln(l, 1, not last)
                    h32s = {}

                    def router(t):
                        h32 = h32s.pop(t)
                        lgp = lg_rot.next()
                        for c in range(8):
                            S.op("pe", lambda e, c=c: e.matmul(lgp[:, 0:36], lhsT=h32[:, c, :], rhs=WR[:, c, :], start=(c == 0), stop=(c == 7)),
                                 reads=[h32, WR], writes=[lgp])
                        S.op("dve", lambda e: e.tensor_tensor(out=LG[:, t, :], in0=lgp[:, 0:36], in1=RB[:, :], op=ALU.add), reads=[lgp, RB], writes=[LG])

                    for t in range(NT):
                        h32 = H32.next()
                        h32s[t] = h32
                        transpose_tile(t, l, jsc, jsh, s, tp_rot, None, None, f32_dst=h32)
                        tm = TM32.next()
                        hb = HB.next()
                        S.op("dve", lambda e, t=t, tm=tm: e.tensor_tensor(out=tm[:, :], in0=XT[t][:, :], in1=SCB[:, :], op=ALU.mult), reads=[XT[t], SCB], writes=[tm])
                        S.op("pool", lambda e, tm=tm, hb=hb: e.tensor_tensor(out=hb[:, :], in0=tm[:, :], in1=SHB[:, :], op=ALU.add), reads=[tm, SHB], writes=[hb])
                        S.dma("sp", lambda e, t=t, hb=hb: e.dma_start(out=htok_d[t * 128:(t + 1) * 128, :], in_=hb[:, :]), reads=[hb], writes=[], sync=hb)
                        if t >= 1:
                            router(t - 1)
                    router(NT - 1)
                    S.fence()
                with ExitStack() as ph1:
                    def sb(name, shape, dt=F32):
                        return S.sbuf(name, shape, dt, st=ph1)
                    C2 = sb("C2", [128, NC2])
                    S.dma("sp", lambda e: e.dma_start(out=C2[:, :], in_=const2_d[:, :]), reads=[DR], writes=[C2], sync=C2)
                    GMAX = sb("GMAX", [128, NT]); DG_ = sb("DGL", [128, NT, 4]); GE = sb("GE", [128, NT, 4]); GS = sb("GS", [128, NT])
                    GW = sb("GW", [128, NT]); PEN = sb("PEN", [128, NT, 4]); EM = sb("EM", [128, NT, 32]); M1 = sb("M1", [128, NT])
                    OH1 = sb("OH1", [128, NT, 32]); EM2 = sb("EM2", [128, NT, 32]); M2 = sb("M2", [128, NT]); OH2 = sb("OH2", [128, NT, 32])
                    DM = sb("DM", [128, NT]); P1 = sb("P1", [128, NT]); P2 = sb("P2", [128, NT])
                    GL = LG[:, :, 0:4]
                    EL = LG[:, :, 4:36]
                    V = lambda f, r, w: S.op("dve", f, reads=r, writes=w)
                    V(lambda e: e.tensor_reduce(out=GMAX[:, :], in_=GL, axis=AX.X, op=ALU.max), [LG], [GMAX])
                    V(lambda e: e.tensor_tensor(out=DG_[:, :, :], in0=GL, in1=GMAX[:, :].unsqueeze(2).to_broadcast([128, NT, 4]), op=ALU.subtract), [LG, GMAX], [DG_])
                    S.op("act", lambda e: e.activation(out=GE[:, :, :], in_=DG_[:, :, :], func=AF.Exp), reads=[DG_], writes=[GE])
                    V(lambda e: e.tensor_reduce(out=GS[:, :], in_=GE[:, :, :], axis=AX.X, op=ALU.add), [GE], [GS])
                    V(lambda e: e.reciprocal(out=GW[:, :], in_=GS[:, :]), [GS], [GW])
                    V(lambda e: e.tensor_scalar(out=PEN[:, :, :], in0=DG_[:, :, :], scalar1=0.0, scalar2=-1e30, op0=ALU.is_lt, op1=ALU.mult), [DG_], [PEN])
                    V(lambda e: e.tensor_tensor(out=EM[:, :, :].rearrange("p t (g j) -> p t g j", g=4), in0=EL.rearrange("p t (g j) -> p t g j", g=4),
                                                in1=PEN[:, :, :].unsqueeze(3).to_broadcast([128, NT, 4, 8]), op=ALU.add), [LG, PEN], [EM])
                    V(lambda e: e.tensor_reduce(out=M1[:, :], in_=EM[:, :, :], axis=AX.X, op=ALU.max), [EM], [M1])
                    V(lambda e: e.tensor_tensor(out=OH1[:, :, :], in0=EM[:, :, :], in1=M1[:, :].unsqueeze(2).to_broadcast([128, NT, 32]), op=ALU.is_equal), [EM, M1], [OH1])
                    V(lambda e: e.scalar_tensor_tensor(out=EM2[:, :, :], in0=OH1[:, :, :], scalar=-1e30, in1=EM[:, :, :], op0=ALU.mult, op1=ALU.add), [OH1, EM], [EM2])
                    V(lambda e: e.tensor_reduce(out=M2[:, :], in_=EM2[:, :, :], axis=AX.X, op=ALU.max), [EM2], [M2])
                    V(lambda e: e.tensor_tensor(out=OH2[:, :, :], in0=EM2[:, :, :], in1=M2[:, :].unsqueeze(2).to_broadcast([128, NT, 32]), op=ALU.is_equal), [EM2, M2], [OH2])
                    V(lambda e: e.tensor_tensor(out=DM[:, :], in0=M2[:, :], in1=M1[:, :], op=ALU.subtract), [M1, M2], [DM])
                    S.op("act", lambda e: e.activation(out=DM[:, :], in_=DM[:, :], func=AF.Exp), reads=[DM], writes=[DM])
                    V(lambda e: e.tensor_scalar(out=DM[:, :], in0=DM[:, :], scalar1=1.0, scalar2=None, op0=ALU.add), [DM], [DM])
                    V(lambda e: e.reciprocal(out=P1[:, :], in_=DM[:, :]), [DM], [P1])
                    V(lambda e: e.tensor_scalar(out=P2[:, :], in0=P1[:, :], scalar1=-1.0, scalar2=1.0, op0=ALU.mult, op1=ALU.add), [P1], [P2])
                    V(lambda e: e.tensor_tensor(out=P12[:, 0, :], in0=P1[:, :], in1=GW[:, :], op=ALU.mult), [P1, GW], [P12])
                    V(lambda e: e.tensor_tensor(out=P12[:, 1, :], in0=P2[:, :], in1=GW[:, :], op=ALU.mult), [P2, GW], [P12])
                    M = sb("M", [128, NT, 32]); MB = sb("MB", [128, NT, 32], BF16)
                    EXC = sb("EXC", [128, NT, 32]); TOT = sb("TOT", [128, NT, 32]); OFFS = sb("OFFS", [128, NT + 1, 32])
                    ip = S.psum("ip", [128, 512], F32, st=ph1)
                    tpp = S.psum("tpp", [128, 512], F32, st=ph1)
                    V(lambda e: e.tensor_tensor(out=M[:, :, :], in0=OH1[:, :, :], in1=OH2[:, :, :], op=ALU.add), [OH1, OH2], [M])
                    V(lambda e: e.tensor_copy(out=MB[:, :, :], in_=M[:, :, :]), [M], [MB])
                    S.op("pe", lambda e: e.matmul(ip[:, :], lhsT=TRIB[:, :], rhs=MB[:, :, :].rearrange("p t e -> p (t e)"), start=True, stop=True), reads=[TRIB, MB], writes=[ip])
                    S.op("pe", lambda e: e.matmul(tpp[:, :], lhsT=ONEB[:, :], rhs=MB[:, :, :].rearrange("p t e -> p (t e)"), start=True, stop=True), reads=[ONEB, MB], writes=[tpp])
                    V(lambda e: e.tensor_tensor(out=EXC[:, :, :], in0=ip[:, :].rearrange("p (t e) -> p t e", e=32), in1=M[:, :, :], op=ALU.subtract), [ip, M], [EXC])
                    S.op("act", lambda e: e.activation(out=TOT[:, :, :], in_=tpp[:, :].rearrange("p (t e) -> p t e", e=32), func=AF.Copy), reads=[tpp], writes=[TOT])
                    V(lambda e: e.memset(OFFS[:, 0, :], 0.0), [], [OFFS])
                    for t in range(1, NT + 1):
                        V(lambda e, t=t: e.tensor_tensor(out=OFFS[:, t, :], in0=OFFS[:, t - 1, :], in1=TOT[:, t - 1, :], op=ALU.add), [OFFS, TOT], [OFFS])
                    TMP8 = sb("TMP8", [128, 32, 8]); NVv = sb("NVv", [128, 32]); TMP32 = sb("TMP32", [128, 32, 32]); VB = sb("VB", [128, 32]); VE = sb("VE", [128, 32]); BASE = sb("BASE", [128, 32])
                    V(lambda e: e.tensor_tensor(out=TMP8[:, :, :], in0=C2[:, C2_THR:C2_THR + 8].unsqueeze(1).to_broadcast([128, 32, 8]),
                                                in1=OFFS[:, NT, :].unsqueeze(2).to_broadcast([128, 32, 8]), op=ALU.is_lt), [C2, OFFS], [TMP8])
                    V(lambda e: e.tensor_reduce(out=NVv[:, :], in_=TMP8[:, :, :], axis=AX.X, op=ALU.add), [TMP8], [NVv])
                    V(lambda e: e.tensor_tensor(out=TMP32[:, :, :], in0=C2[:, C2_LT:C2_LT + 1024].rearrange("p (a b) -> p a b", a=32),
                                                in1=NVv[:, :].unsqueeze(1).to_broadcast([128, 32, 32]), op=ALU.mult), [C2, NVv], [TMP32])
                    V(lambda e: e.tensor_reduce(out=VB[:, :], in_=TMP32[:, :, :], axis=AX.X, op=ALU.add), [TMP32], [VB])
                    V(lambda e: e.tensor_tensor(out=VE[:, :], in0=VB[:, :], in1=NVv[:, :], op=ALU.add), [VB, NVv], [VE])
                    V(lambda e: e.tensor_scalar(out=BASE[:, :], in0=VB[:, :], scalar1=256.0, scalar2=None, op0=ALU.mult), [VB], [BASE])
                    T1 = sb("T1r", [128, NT, 32]); POS = sb("POS", [128, 2, NT]); Q = sb("Q", [128, 2, NT]); QI = sb("QI", [128, 2, NT], I32); QF = sb("QF", [128, 2, NT])
                    CORR = sb("CORR", [128, 2, NT]); PM = sb("PM", [128, 2, NT]); DEST = sb("DEST", [128, 2, NT]); DESTI = sb("DESTI", [128, 2, NT], I32)
                    VALI = sb("VALI", [128, 2, NT], I32); FILLF = sb("FILLF", [128, NJ]); FILLI = sb("FILLI", [128, NJ], I32); IF = sb("IF", [128, NJ]); IGE = sb("IGE", [128, NJ])
                    V(lambda e: e.tensor_tensor(out=EXC[:, :, :], in0=EXC[:, :, :], in1=OFFS[:, 0:NT, :], op=ALU.add), [EXC, OFFS], [EXC])
                    V(lambda e: e.tensor_tensor(out=EXC[:, :, :], in0=EXC[:, :, :], in1=BASE[:, :].unsqueeze(1).to_broadcast([128, NT, 32]), op=ALU.add), [EXC, BASE], [EXC])
                    for k, OH in ((0, OH1), (1, OH2)):
                        V(lambda e, OH=OH: e.tensor_tensor(out=T1[:, :, :], in0=OH[:, :, :], in1=EXC[:, :, :], op=ALU.mult), [OH, EXC], [T1])
                        V(lambda e, k=k: e.tensor_reduce(out=POS[:, k, :], in_=T1[:, :, :], axis=AX.X, op=ALU.add), [T1], [POS])
                    V(lambda e: e.tensor_scalar(out=Q[:, :, :], in0=POS[:, :, :], scalar1=1.0 / 128, scalar2=None, op0=ALU.mult), [POS], [Q])
                    V(lambda e: e.tensor_copy(out=QI[:, :, :], in_=Q[:, :, :]), [Q], [QI])
                    V(lambda e: e.tensor_copy(out=QF[:, :, :], in_=QI[:, :, :]), [QI], [QF])
                    V(lambda e: e.tensor_tensor(out=CORR[:, :, :], in0=QF[:, :, :], in1=Q[:, :, :], op=ALU.is_gt), [QF, Q], [CORR])
                    V(lambda e: e.tensor_tensor(out=QF[:, :, :], in0=QF[:, :, :], in1=CORR[:, :, :], op=ALU.subtract), [QF, CORR], [QF])
                    V(lambda e: e.scalar_tensor_tensor(out=PM[:, :, :], in0=QF[:, :, :], scalar=-128.0, in1=POS[:, :, :], op0=ALU.mult, op1=ALU.add), [QF, POS], [PM])
                    V(lambda e: e.scalar_tensor_tensor(out=DEST[:, :, :], in0=PM[:, :, :], scalar=float(NJ), in1=QF[:, :, :], op0=ALU.mult, op1=ALU.add), [PM, QF], [DEST])
                    V(lambda e: e.tensor_copy(out=DESTI[:, :, :], in_=DEST[:, :, :]), [DEST], [DESTI])
                    V(lambda e: e.tensor_copy(out=VALI[:, :, :], in_=C2[:, C2_VAL:C2_VAL + 32].rearrange("p (k t) -> p k t", k=2)), [C2], [VALI])
                    V(lambda e: e.memset(FILLF[:, :], BIG), [], [FILLF])
                    V(lambda e: e.tensor_copy(out=FILLI[:, :], in_=FILLF[:, :]), [FILLF], [FILLI])
                    tab_v = tab_d.rearrange("(p j) o -> p (j o)", p=128)
                    S.dma("sp", lambda e: e.dma_start(out=tab_v, in_=FILLI[:, :]), reads=[FILLI], writes=[TABF], sync=TABF)
                    last_sc = None
                    for k in range(2):
                        for t in range(NT):
                            last_sc = S.dma("pool", lambda e, k=k, t=t: e.indirect_dma_start(out=tab_d[:, :], out_offset=IOA(ap=DESTI[:, k, t:t + 1], axis=0), in_=VALI[:, k, t:t + 1], in_offset=None,
                                                                                            bounds_check=BC[NJ * 128 - 1], oob_is_err=False), reads=[DESTI, VALI, TABF], writes=[], sync=TABS)
                    VA = sb("VA", [128, NV, 32]); VBm = sb("VBm", [128, NV, 32]); WF = sb("WF", [128, NV])
                    vrow = C2[:, C2_VROW:C2_VROW + NV].unsqueeze(2).to_broadcast([128, NV, 32])
                    V(lambda e: e.tensor_tensor(out=VA[:, :, :], in0=vrow, in1=VB[:, :].unsqueeze(1).to_broadcast([128, NV, 32]), op=ALU.is_ge), [C2, VB], [VA])
                    V(lambda e: e.tensor_tensor(out=VBm[:, :, :], in0=vrow, in1=VE[:, :].unsqueeze(1).to_broadcast([128, NV, 32]), op=ALU.is_lt), [C2, VE], [VBm])
                    V(lambda e: e.tensor_tensor(out=VA[:, :, :], in0=VA[:, :, :], in1=VBm[:, :, :], op=ALU.mult), [VA, VBm], [VA])
                    V(lambda e: e.tensor_tensor(out=VA[:, :, :], in0=VA[:, :, :], in1=C2[:, C2_EROW:C2_EROW + 32].unsqueeze(1).to_broadcast([128, NV, 32]), op=ALU.mult), [VA, C2], [VA])
                    V(lambda e: e.tensor_reduce(out=WF[:, :], in_=VA[:, :, :], axis=AX.X, op=ALU.add), [VA], [WF])
                    V(lambda e: e.tensor_scalar(out=WF[:, :], in0=WF[:, :], scalar1=BIG, scalar2=None, op0=ALU.add), [WF], [WF])
                    V(lambda e: e.tensor_copy(out=WIDX[:, :], in_=WF[:, :]), [WF], [WIDX])
                    TABS.w = last_sc
                    S.dma("sp", lambda e: e.dma_start(out=IDXY[:, :], in_=tab_v), reads=[TABS], writes=[IDXY], sync=IDXY)
                    TABS.w = None
                    V(lambda e: e.tensor_copy(out=IF[:, :], in_=IDXY[:, :]), [IDXY], [IF])
                    V(lambda e: e.tensor_single_scalar(out=IGE[:, :], in_=IF[:, :], scalar=2048.0, op=ALU.is_ge), [IF], [IGE])
                    V(lambda e: e.scalar_tensor_tensor(out=IF[:, :], in0=IGE[:, :], scalar=-2048.0, in1=IF[:, :], op0=ALU.mult, op1=ALU.add), [IGE, IF], [IF])
                    V(lambda e: e.tensor_copy(out=IDXG[:, :], in_=IF[:, :]), [IF], [IDXG])
                    S.fence()
                with ExitStack() as ph2:
                    W = [S.sbuf("W%d" % i, [128, 6144], BF16, st=ph2) for i in range(NW)]
                    HS = [S.sbuf("HS%d" % i, [128, D], BF16, st=ph2) for i in range(NH)]
                    for hs in HS:
                        S.op("dve", lambda e, hs=hs: e.memset(hs[:, :], 0.0), writes=[hs])

                    def issue_w(v):
                        w = W[v % NW]
                        S.dma("pool", lambda e: e.indirect_dma_start(out=w[:, :], out_offset=None, in_=wall_d[:, :], in_offset=IOA(ap=WIDX[:, v:v + 1], axis=0),
                                                                     bounds_check=BC[4095], oob_is_err=False), reads=[DR, WIDX, w], writes=[w], sync=w)

                    for v in range(NW):
                        issue_w(v)
                    HST = Rot([S.sbuf("HST%d" % i, [128, 8, 128], BF16, st=ph2) for i in range(2)])
                    SG = Rot([S.sbuf("SG%d" % i, [128, 256], F32, st=ph2) for i in range(2)])
                    HID = Rot([S.sbuf("HID%d" % i, [128, 256], BF16, st=ph2) for i in range(3)])
                    HIDT = Rot([S.sbuf("HIDT%d" % i, [128, 2, 128], BF16, st=ph2) for i in range(3)])
                    YSB = Rot([S.sbuf("YSB%d" % i, [128, D], F32, st=ph2) for i in range(3)])
                    xT_rot = Rot([S.psum("xTp%d" % i, [128, 8, 128], BF16, st=ph2) for i in range(1)])
                    gu_rot = Rot([S.psum("gu%d" % i, [128, 512], F32, st=ph2) for i in range(2)])
                    hT_rot = Rot([S.psum("hTp%d" % i, [128, 2, 128], BF16, st=ph2) for i in range(2)])
                    y_rot = Rot([S.psum("yp%d" % i, [128, 512], F32, st=ph2) for i in range(3)])
                    state = {}

                    def issue_gather(j):
                        hs = HS[j % NH]
                        S.dma("pool", lambda e: e.indirect_dma_start(out=hs[:, :], out_offset=None, in_=htok_d[:, :], in_offset=IOA(ap=IDXG[:, j:j + 1], axis=0),
                                                                     bounds_check=BC[S_LEN - 1], oob_is_err=False), reads=[DR, IDXG, hs], writes=[hs], sync=hs)

                    def do_T8(j):
                        hs = HS[j % NH]
                        xp = xT_rot.next()
                        for c in range(8):
                            S.op("pe", lambda e, c=c: e.transpose(xp[:, c, :], hs[:, c * 128:(c + 1) * 128], IDB[:, :]), reads=[hs, IDB], writes=[xp])
                        hst = HST.next()
                        S.op("act", lambda e: e.activation(out=hst[:, :, :], in_=xp[:, :, :], func=AF.Copy), reads=[xp], writes=[hst])
                        state[j] = [hst, None, None]

                    def do_GU(j):
                        w = W[(j // 2) % NW]
                        hst = state[j][0]
                        gp = gu_rot.next()
                        for c in range(8):
                            S.op("pe", lambda e, c=c: e.matmul(gp[:, :], lhsT=hst[:, c, :], rhs=w[:, c * 512:(c + 1) * 512], start=(c == 0), stop=(c == 7)),
                                 reads=[hst, w], writes=[gp])
                        sg = SG.next()
                        hid = HID.next()
                        S.op("act", lambda e: e.activation(out=sg[:, :], in_=gp[:, 0:256], func=AF.Silu), reads=[gp], writes=[sg])
                        S.op("dve", lambda e: e.tensor_tensor(out=hid[:, :], in0=gp[:, 256:512], in1=sg[:, :], op=ALU.mult), reads=[gp, sg], writes=[hid])
                        state[j][1] = hid

                    def do_HT(j):
                        hid = state[j][1]
                        hp = hT_rot.next()
                        for c in range(2):
                            S.op("pe", lambda e, c=c: e.transpose(hp[:, c, :], hid[:, c * 128:(c + 1) * 128], IDB[:, :]), reads=[hid, IDB], writes=[hp])
                        hT = HIDT.next()
                        S.op("act", lambda e: e.activation(out=hT[:, :, :], in_=hp[:, :, :], func=AF.Copy), reads=[hp], writes=[hT])
                        state[j][2] = hT

                    def do_D(j):
                        w = W[(j // 2) % NW]
                        hT = state.pop(j)[2]
                        ysb = YSB.next()
                        for hf in range(2):
                            yp = y_rot.next()
                            for c in range(2):
                                S.op("pe", lambda e, hf=hf, c=c, yp=yp: e.matmul(yp[:, :], lhsT=hT[:, c, :], rhs=w[:, 4096 + c * 1024 + hf * 512:4096 + c * 1024 + (hf + 1) * 512],
                                                                                 start=(c == 0), stop=(c == 1)), reads=[hT, w], writes=[yp])
                            S.op("dve", lambda e, yp=yp, hf=hf: e.tensor_tensor(out=ysb[:, hf * 512:(hf + 1) * 512], in0=yp[:, :], in1=G1B[:, hf * 512:(hf + 1) * 512], op=ALU.mult),
                                 reads=[yp, G1B], writes=[ysb])
                        S.dma("pool", lambda e: e.indirect_dma_start(out=ys_d[:, :], out_offset=IOA(ap=IDXY[:, j:j + 1], axis=0), in_=ysb[:, :], in_offset=None,
                                                                     bounds_check=BC[2 * S_LEN - 1], oob_is_err=False), reads=[ysb, IDXY], writes=[], sync=ysb)

                    for j in range(NH):
                        issue_gather(j)
                    do_T8(0)
                    issue_gather(NH)
                    for i in range(NJ + 2):
                        if i + 1 < NJ:
                            do_T8(i + 1)
                            if i + 1 + NH < NJ:
                                issue_gather(i + 1 + NH)
                        if i < NJ:
                            do_GU(i)
                        if 1 <= i <= NJ:
                            do_HT(i - 1)
                        if 2 <= i:
                            do_D(i - 2)
                            if (i - 2) % 2 == 1:
                                nv_ = (i - 2) // 2 + NW
                                if nv_ < NV:
                                    issue_w(nv_)
                    S.fence()
                with ExitStack() as ph3:
                    Y0 = Rot([S.sbuf("Y0_%d" % i, [128, D], F32, st=ph3) for i in range(3)])
                    Y1 = Rot([S.sbuf("Y1_%d" % i, [128, D], F32, st=ph3) for i in range(3)])
                    LJ = S.sbuf("LNJ", [128, D], BF16, st=ph3)
                    lnp = LNPipe(act_stats=True, junk=LJ)
                    for t in range(NT):
                        y0 = Y0.next(); y1 = Y1.next()
                        S.dma("sp", lambda e, t=t, y0=y0: e.dma_start(out=y0[:, :], in_=ys_d[t * 128:(t + 1) * 128, :]), reads=[DR], writes=[y0], sync=y0)
                        S.dma("sp", lambda e, t=t, y1=y1: e.dma_start(out=y1[:, :], in_=ys_d[S_LEN + t * 128:S_LEN + (t + 1) * 128, :]), reads=[DR], writes=[y1], sync=y1)
                        S.op("dve", lambda e, t=t, y0=y0: e.scalar_tensor_tensor(out=XT[t][:, :], in0=y0[:, :], scalar=P12[:, 0, t:t + 1], in1=XT[t][:, :], op0=ALU.mult, op1=ALU.add),
                             reads=[y0, P12, XT[t]], writes=[XT[t]])
                        S.op("dve", lambda e, t=t, y1=y1: e.scalar_tensor_tensor(out=XT[t][:, :], in0=y1[:, :], scalar=P12[:, 1, t:t + 1], in1=XT[t][:, :], op0=ALU.mult, op1=ALU.add),
                             reads=[y1, P12, XT[t]], writes=[XT[t]])
                        lnp.push(t)
                    lnp.flush()
                    S.fence()

        def l0_mixer(s):
            l = 0
            jsh, jsc, jg = 0, 8, 16
            CATA = [Tile(HT.ap[:, 0:4, B * 512:(B + 1) * 512], "CATA%d" % B) for B in range(4)]
            CATB = [Tile(HT.ap[:, 4:8, t * 128:(t + 1) * 128], "CATB%d" % t) for t in range(NT)]
            load_ln(l, 0, True)
            with ExitStack() as p12:
                HCT = S.sbuf("HCT", [128, 4, 2080], BF16, st=p12)
                S.op("pool", lambda e: e.memset(HCT[:, :, 0:30], 0.0), writes=[HCT])
                with ExitStack() as p1:
                    WINC = S.sbuf("WINC", [128, 8, 1024], BF16, st=p1)
                    S.dma("pool", lambda e: e.dma_start(out=WINC[:, :, :], in_=ab_w_in_d.rearrange("(c p) n -> p c n", p=128)[:, :, 0:1024]),
                          reads=[DR], writes=[WINC], sync=WINC)
                    HTB = Rot([S.sbuf("HTB%d" % i, [128, 8, 512], BF16, st=p1) for i in range(2)])
                    SIG = Rot([S.sbuf("SIG%d" % i, [128, 512], F32, st=p1) for i in range(2)])
                    tp_rot = Rot([S.psum("tp%d" % i, [128, 512], F32, st=p1) for i in range(4)])
                    ag_rot = Rot([S.psum("ag%d" % i, [128, 512], F32, st=p1) for i in range(4)])
                    scr = S.sbuf("scr", [128, 8, 128], F32, st=p1)
                    build_g1b(l, jg, s, ag_rot.tiles[0:2], scr)
                    for B in range(4):
                        htb = HTB.next()
                        for i in range(4):
                            transpose_tile(4 * B + i, l, jsc, jsh, s, tp_rot, lambda c, i=i, htb=htb: htb[:, c, i * 128:(i + 1) * 128], htb)
                        for cc in range(4):
                            a_p = ag_rot.next()
                            g_p = ag_rot.next()
                            for kc in range(8):
                                S.op("pe", lambda e, kc=kc, cc=cc, a_p=a_p, htb=htb: e.matmul(a_p[:, :], lhsT=WINC[:, kc, cc * 128:(cc + 1) * 128], rhs=htb[:, kc, :], start=(kc == 0), stop=(kc == 7)),
                                     reads=[WINC, htb], writes=[a_p])
                            for kc in range(8):
                                S.op("pe", lambda e, kc=kc, cc=cc, g_p=g_p, htb=htb: e.matmul(g_p[:, :], lhsT=WINC[:, kc, 512 + cc * 128:512 + (cc + 1) * 128], rhs=htb[:, kc, :], start=(kc == 0), stop=(kc == 7)),
                                     reads=[WINC, htb], writes=[g_p])
                            sig = SIG.next()
                            S.op("act", lambda e, sig=sig, g_p=g_p: e.activation(out=sig[:, :], in_=g_p[:, :], func=AF.Sigmoid), reads=[g_p], writes=[sig])
                            S.op("dve", lambda e, sig=sig, a_p=a_p, cc=cc, B=B: e.tensor_tensor(out=HCT[:, cc, 30 + B * 512:30 + (B + 1) * 512], in0=a_p[:, :], in1=sig[:, :], op=ALU.mult),
                                 reads=[a_p, sig], writes=[HCT])
                    S.fence()
                if stop_after == "l0a":
                    return
                with ExitStack() as p2:
                    CWS = S.sbuf("CWS", [32, 512], F32, st=p2)
                    CW = S.sbuf("CW", [128, 4, 31], F32, st=p2)
                    DG = [S.sbuf("DG%d" % cc, [128, 31, 128], BF16, st=p2) for cc in range(4)]
                    Y = S.sbuf("Y", [128, 4, 512], F32, st=p2)
                    YSQ = S.sbuf("YSQ", [128, 4, 512], F32, st=p2)
                    MEAN = S.sbuf("MEAN", [128, 512], F32, st=p2)
                    MSQ = S.sbuf("MSQ", [128, 512], F32, st=p2)
                    VAR = S.sbuf("VAR", [128, 512], F32, st=p2)
                    RS = S.sbuf("RS", [128, 512], F32, st=p2)
                    T1 = Rot([S.sbuf("T1_%d" % i, [128, 512], F32, st=p2) for i in range(2)])
                    y_ps = [S.psum("yps%d" % i, [128, 512], F32, st=p2) for i in range(4)]
                    st_rot = Rot([S.psum("stp%d" % i, [128, 512], F32, st=p2) for i in range(2)])
                    S.dma("sp", lambda e: e.dma_start(out=CWS[0:31, :], in_=conv_w_d[:, :]), reads=[DR], writes=[CWS], sync=CWS)
                    for cc in range(4):
                        pt = st_rot.next()
                        S.op("pe", lambda e, cc=cc, pt=pt: e.transpose(pt[:, 0:31], CWS[0:31, cc * 128:(cc + 1) * 128], CONST[0:31, C_ID:C_ID + 31]), reads=[CWS, CONST], writes=[pt])
                        S.op("dve", lambda e, cc=cc, pt=pt: e.tensor_copy(out=CW[:, cc, :], in_=pt[:, 0:31]), reads=[pt], writes=[CW])
                    for cc in range(4):
                        for j in range(31):
                            S.op("dve", lambda e, cc=cc, j=j: e.tensor_scalar(out=DG[cc][:, j, :], in0=ident, scalar1=CW[:, cc, j:j + 1], scalar2=None, op0=ALU.mult),
                                 reads=[CONST, CW], writes=[DG[cc]])
                    for B in range(4):
                        for cc in range(4):
                            for j in range(31):
                                S.op("pe", lambda e, cc=cc, j=j, B=B: e.matmul(y_ps[cc][:, :], lhsT=DG[cc][:, j, :], rhs=HCT[:, cc, B * 512 + j:B * 512 + j + 512], start=(j == 0), stop=(j == 30)),
                                     reads=[DG[cc], HCT], writes=[y_ps[cc]])
                            S.op("act", lambda e, cc=cc: e.activation(out=Y[:, cc, :], in_=y_ps[cc][:, :], func=AF.Identity, bias=PVT[:, cc:cc + 1]), reads=[y_ps[cc], PVT], writes=[Y])
                            S.op("act", lambda e, cc=cc: e.activation(out=YSQ[:, cc, :], in_=y_ps[cc][:, :], func=AF.Square, bias=PVT[:, cc:cc + 1]), reads=[y_ps[cc], PVT], writes=[YSQ])
                        mean_ps = st_rot.next()
                        msq_ps = st_rot.next()
                        for cc in range(4):
                            S.op("pe", lambda e, cc=cc, mean_ps=mean_ps: e.matmul(mean_ps[:, :], lhsT=ones, rhs=Y[:, cc, :], start=(cc == 0), stop=(cc == 3)), reads=[CONST, Y], writes=[mean_ps])
                        for cc in range(4):
                            S.op("pe", lambda e, cc=cc, msq_ps=msq_ps: e.matmul(msq_ps[:, :], lhsT=ones, rhs=YSQ[:, cc, :], start=(cc == 0), stop=(cc == 3)), reads=[CONST, YSQ], writes=[msq_ps])
                        S.op("act", lambda e, mean_ps=mean_ps: e.activation(out=MEAN[:, :], in_=mean_ps[:, :], func=AF.Copy, scale=1.0 / 512), reads=[mean_ps], writes=[MEAN])
                        S.op("dve", lambda e: e.tensor_tensor(out=MSQ[:, :], in0=MEAN[:, :], in1=MEAN[:, :], op=ALU.mult), reads=[MEAN], writes=[MSQ])
                        S.op("dve", lambda e, msq_ps=msq_ps: e.scalar_tensor_tensor(out=VAR[:, :], in0=msq_ps[:, :], scalar=1.0 / 512, in1=MSQ[:, :], op0=ALU.mult, op1=ALU.subtract),
                             reads=[msq_ps, MSQ], writes=[VAR])
                        S.op("act", lambda e: e.activation(out=VAR[:, :], in_=VAR[:, :], func=AF.Ln, bias=LN_EPS), reads=[VAR], writes=[VAR])
                        S.op("act", lambda e: e.activation(out=RS[:, :], in_=VAR[:, :], func=AF.Exp, scale=-0.5), reads=[VAR], writes=[RS])
                        for cc in range(4):
                            t1 = T1.next()
                            S.op("dve", lambda e, cc=cc, t1=t1: e.tensor_tensor(out=t1[:, :], in0=Y[:, cc, :], in1=MEAN[:, :], op=ALU.subtract), reads=[Y, MEAN], writes=[t1])
                            S.op("dve", lambda e, cc=cc, t1=t1: e.tensor_tensor(out=t1[:, :], in0=t1[:, :], in1=RS[:, :], op=ALU.mult), reads=[t1, RS], writes=[t1])
                            S.op("act", lambda e, cc=cc, t1=t1, B=B: e.activation(out=CATA[B][:, cc, :], in_=t1[:, :], func=AF.Silu, scale=PVT[:, 4 + cc:5 + cc], bias=PVT[:, 8 + cc:9 + cc]),
                                 reads=[t1, PVT], writes=[CATA[B]])
                    S.fence()
            if stop_after == "l0b":
                return
            with ExitStack() as p3:
                def sb(name, shape, dt=F32):
                    return S.sbuf(name, shape, dt, st=p3)
                WING = sb("WING", [128, 8, 1552], BF16)
                WOUT = sb("WOUT", [128, 8, D], BF16)
                S.dma("pool", lambda e: e.dma_start(out=WING[:, :, :], in_=ab_w_in_d.rearrange("(c p) n -> p c n", p=128)[:, :, 1024:2576]), reads=[DR], writes=[WING], sync=WING)
                S.dma("pool", lambda e: e.dma_start(out=WOUT[:, :, :], in_=ab_w_out_d.rearrange("(c p) n -> p c n", p=128)), reads=[DR], writes=[WOUT], sync=WOUT)
                S.op("pool", lambda e: e.tensor_tensor(out=WOUT[:, :, :], in0=WOUT[:, :, :], in1=G1B[:, :].unsqueeze(1).to_broadcast([128, 8, D]), op=ALU.mult), reads=[WOUT, G1B], writes=[WOUT])
                GWS = sb("GWS", [32, 256])
                GWB = sb("GWB", [32, 256], BF16)
                S.op("dve", lambda e: e.memset(GWS[:, :], 0.0), writes=[GWS])
                S.dma("sp", lambda e: [e.dma_start(out=GWS[0:16, :], in_=gate_w_d[:, :]), e.dma_start(out=GWS[16:17, :], in_=gate_b_d[:, :])], reads=[DR], writes=[GWS], sync=GWS, n=2)
                S.op("dve", lambda e: e.tensor_copy(out=GWB[:, :], in_=GWS[:, :]), reads=[GWS], writes=[GWB])
                GLT = Rot([sb("GLT%d" % i, [32, 512], BF16) for i in range(2)])
                for g_ in GLT.tiles:
                    S.op("dve", lambda e, g_=g_: e.memset(g_[:, :], 1.0), writes=[g_])
                NG = sb("NG", [128, 512])
                S.dma("sp", lambda e: e.dma_start(out=NG[:, :], in_=gnorm_d[0:1, :].partition_broadcast(128)), reads=[DR], writes=[NG], sync=NG)
                S32 = sb("S32", [128, 2, 128])
                SBF = sb("SBF", [128, 2, 128], BF16)
                S.op("dve", lambda e: e.memset(S32[:, :, :], 0.0), writes=[S32])
                S.op("dve", lambda e: e.memset(SBF[:, :, :], 0.0), writes=[SBF])
                S32h = [Tile(S32.ap[(h % 2) * 64:(h % 2) * 64 + 64, h // 2, :], "S32h%d" % h) for h in range(4)]
                SBFh = [Tile(SBF.ap[(h % 2) * 64:(h % 2) * 64 + 64, h // 2, :], "SBFh%d" % h) for h in range(4)]
                for h in range(4):
                    S32h[h].w = S32.w
                    SBFh[h].w = SBF.w
                HTB = sb("HTB", [128, 8, 512], BF16)
                QT = sb("QT", [128, 2, 512])
                KT = sb("KT", [128, 2, 512])
                R2 = lambda name, shape, dt=F32: Rot([sb("%s%d" % (name, i), shape, dt) for i in range(2)])
                VB = R2("VB", [128, 512], BF16); RG = R2("RG", [128, 512]); KTOK = R2("KTOK", [128, 256]); E1 = R2("E1", [128, 256]); SP = R2("SP", [128, 256])
                EB = R2("EB", [128, 2, 128]); ENB = R2("ENB", [128, 2, 128]); EDEC = R2("EDEC", [128, 256])
                QS = R2("QS", [128, 4, 128], BF16); KS = R2("KS", [128, 2, 128], BF16); KDEC = R2("KDEC", [128, 256], BF16)
                ATM = Rot([sb("ATM%d" % i, [128, 128], BF16) for i in range(4)])
                SS = R2("SS", [128, 4]); RS4 = R2("RS4", [128, 4]); YB = R2("YB", [128, 512], BF16)
                JUNK = sb("JUNK", [128, 128], BF16)
                for q_ in QS.tiles:
                    S.op("pool", lambda e, q_=q_: e.memset(q_[:, :, :], 0.0), writes=[q_])
                pool_rot = Rot([S.psum("pl%d" % i, [128, 512], F32, st=p3) for i in range(4)])
                o_ps = S.psum("o_ps", [128, 512], F32, st=p3)
                sn_ps = S.psum("sn_ps", [128, 4, 128], F32, st=p3)
                att_ps = S.psum("att_ps", [128, 4, 128], F32, st=p3)
                ybT_ps = S.psum("ybT", [128, 8, 128], BF16, st=p3)
                tp_rot = pool_rot

                def proj(dst_ps, cols, htb_ap, M=None):
                    pass

                gl_of = {}
                stA = {}

                def gla_prologue(B):
                    for i in range(4):
                        transpose_tile(4 * B + i, l, jsc, jsh, s, tp_rot, lambda c, i=i: HTB[:, c, i * 128:(i + 1) * 128], HTB)
                    for c2 in range(2):
                        for (dst, off) in ((QT, 0), (KT, 256)):
                            pp_ = pool_rot.next()
                            for kc in range(8):
                                S.op("pe", lambda e, kc=kc, c2=c2, off=off, pp_=pp_: e.matmul(pp_[:, :], lhsT=WING[:, kc, off + c2 * 128:off + (c2 + 1) * 128], rhs=HTB[:, kc, :], start=(kc == 0), stop=(kc == 7)),
                                     reads=[WING, HTB], writes=[pp_])
                            S.op("act", lambda e, dst=dst, c2=c2, pp_=pp_: e.activation(out=dst[:, c2, :], in_=pp_[:, :], func=AF.Copy), reads=[pp_], writes=[dst])
                    gl = GLT.next()
                    pp_ = pool_rot.next()
                    for kc in range(8):
                        S.op("pe", lambda e, kc=kc, pp_=pp_: e.matmul(pp_[0:16, :], lhsT=WING[:, kc, 1536:1552], rhs=HTB[:, kc, :], start=(kc == 0), stop=(kc == 7)), reads=[WING, HTB], writes=[pp_])
                    S.op("act", lambda e, gl=gl, pp_=pp_: e.activation(out=gl[0:16, :], in_=pp_[0:16, :], func=AF.Copy), reads=[pp_], writes=[gl])
                    gl_of[B] = gl

                def gla_a0(t):
                    B, i = t // 4, t % 4
                    gl = gl_of[B]
                    tc = slice(i * 128, (i + 1) * 128)
                    d = dict(tc=tc, vb=VB.next(), rg=RG.next(), ktok=KTOK.next(), e1=E1.next(), sp=SP.next(), eb=EB.next(), enb=ENB.next(), edec=EDEC.next(),
                             qs=QS.next(), ks=KS.next(), kdec=KDEC.next(), ss=SS.next(), rs4=RS4.next(), yb=YB.next())
                    stA[t] = d
                    e1, sp = d["e1"], d["sp"]
                    zp = pool_rot.next()
                    S.op("pe", lambda e: e.matmul(zp[:, 0:256], lhsT=gl[0:32, tc], rhs=GWB[0:32, :], start=True, stop=True), reads=[gl, GWB], writes=[zp])
                    S.op("act", lambda e: e.activation(out=e1[:, :], in_=zp[:, 0:256], func=AF.Exp, scale=-1.0), reads=[zp], writes=[e1])
                    S.op("act", lambda e: e.activation(out=sp[:, :], in_=e1[:, :], func=AF.Ln, bias=1.0), reads=[e1], writes=[sp])

                def gla_a1(t):
                    d = stA[t]
                    tc, vb, rg, ktok = d["tc"], d["vb"], d["rg"], d["ktok"]
                    kp = pool_rot.next()
                    for kc in range(8):
                        S.op("pe", lambda e, kc=kc: e.matmul(kp[:, 0:256], lhsT=HTB[:, kc, tc], rhs=WING[:, kc, 256:512], start=(kc == 0), stop=(kc == 7)), reads=[WING, HTB], writes=[kp])
                    S.op("dve", lambda e: e.tensor_copy(out=ktok[:, :], in_=kp[:, 0:256]), reads=[kp], writes=[ktok])
                    vp = pool_rot.next()
                    for kc in range(8):
                        S.op("pe", lambda e, kc=kc: e.matmul(vp[:, :], lhsT=HTB[:, kc, tc], rhs=WING[:, kc, 512:1024], start=(kc == 0), stop=(kc == 7)), reads=[WING, HTB], writes=[vp])
                    S.op("act", lambda e: e.activation(out=vb[:, :], in_=vp[:, :], func=AF.Copy), reads=[vp], writes=[vb])
                    rp = pool_rot.next()
                    for kc in range(8):
                        S.op("pe", lambda e, kc=kc: e.matmul(rp[:, :], lhsT=HTB[:, kc, tc], rhs=WING[:, kc, 1024:1536], start=(kc == 0), stop=(kc == 7)), reads=[WING, HTB], writes=[rp])
                    S.op("act", lambda e: e.activation(out=rg[:, :], in_=rp[:, :], func=AF.Silu), reads=[rp], writes=[rg])
                    S.op("pool", lambda e: e.tensor_tensor(out=rg[:, :], in0=rg[:, :], in1=NG[:, :], op=ALU.mult), reads=[rg, NG], writes=[rg])

                def gla_a2(t):
                    d = stA[t]
                    tc, sp, eb, enb, edec, qs, ks, kdec, ktok = d["tc"], d["sp"], d["eb"], d["enb"], d["edec"], d["qs"], d["ks"], d["kdec"], d["ktok"]
                    revp = pool_rot.next()
                    for c2 in range(2):
                        S.op("pe", lambda e, c2=c2: e.matmul(revp[:, 256 + c2 * 128:256 + (c2 + 1) * 128], lhsT=sp[:, c2 * 128:(c2 + 1) * 128], rhs=tri, start=True, stop=True), reads=[sp, CONST], writes=[revp])
                    S.op("pe", lambda e: e.matmul(revp[:, 0:256], lhsT=su, rhs=sp[:, :], start=True, stop=True), reads=[sp, CONST], writes=[revp])
                    S.op("act", lambda e: e.activation(out=eb[:, :, :], in_=revp[:, 256:512].rearrange("p (a b) -> p a b", a=2), func=AF.Exp, scale=-1.0 / 16), reads=[revp], writes=[eb])
                    S.op("act", lambda e: e.activation(out=enb[:, :, :], in_=revp[:, 256:512].rearrange("p (a b) -> p a b", a=2), func=AF.Exp, scale=1.0 / 16), reads=[revp], writes=[enb])
                    S.op("act", lambda e: e.activation(out=edec[:, :], in_=revp[:, 0:256], func=AF.Exp, scale=-1.0 / 16), reads=[revp], writes=[edec])
                    for h in range(4):
                        c2, hp = h // 2, (h % 2) * 64
                        S.op("dve", lambda e, h=h, c2=c2, hp=hp: e.scalar_tensor_tensor(out=qs[hp:hp + 64, h, :], in0=QT[hp:hp + 64, c2, tc], scalar=0.125, in1=eb[hp:hp + 64, c2, :], op0=ALU.mult, op1=ALU.mult),
                             reads=[QT, eb], writes=[qs])
                    S.op("dve", lambda e: e.tensor_tensor(out=ks[:, :, :], in0=KT[:, :, tc], in1=enb[:, :, :], op=ALU.mult), reads=[KT, enb], writes=[ks])
                    S.op("dve", lambda e: e.tensor_tensor(out=kdec[:, :], in0=ktok[:, :], in1=edec[:, :], op=ALU.mult), reads=[ktok, edec], writes=[kdec])

                def gla_b0(t):
                    d = stA[t]
                    qs, ks = d["qs"], d["ks"]
                    for h in range(4):
                        c2 = h // 2
                        S.op("pe", lambda e, c2=c2, h=h: e.matmul(att_ps[:, h, :], lhsT=ks[:, c2, :], rhs=qs[:, h, :], start=True, stop=True), reads=[ks, qs], writes=[att_ps])
                    atms = []
                    for h in range(4):
                        atm = ATM.next()
                        S.op("dve", lambda e, atm=atm, h=h: e.tensor_tensor(out=atm[:, :], in0=att_ps[:, h, :], in1=tri, op=ALU.mult), reads=[att_ps, CONST], writes=[atm])
                        atms.append(atm)
                    d["atms"] = atms

                def gla_b1(t):
                    d = stA[t]
                    vb, rg, eb, qs, kdec, ss, rs4, yb, atms = d["vb"], d["rg"], d["eb"], d["qs"], d["kdec"], d["ss"], d["rs4"], d["yb"], d["atms"]
                    for h in range(4):
                        c2 = h // 2
                        hc = slice(h * 128, (h + 1) * 128)
                        atm = atms[h]
                        S.op("pe", lambda e, c2=c2, hc=hc, h=h: e.matmul(o_ps[:, hc], lhsT=qs[:, h, :], rhs=SBF[:, c2, :], start=True, stop=False), reads=[qs, SBFh[2 * c2], SBFh[2 * c2 + 1]], writes=[o_ps])
                        S.op("pe", lambda e, atm=atm, hc=hc: e.matmul(o_ps[:, hc], lhsT=atm[:, :], rhs=vb[:, hc], start=False, stop=True), reads=[atm, vb], writes=[o_ps])
                    for h in range(4):
                        c2 = h // 2
                        hc = slice(h * 128, (h + 1) * 128)
                        S.op("pe", lambda e, c2=c2, hc=hc, h=h: e.matmul(sn_ps[:, h, :], lhsT=kdec[:, c2 * 128:(c2 + 1) * 128], rhs=vb[:, hc], start=True, stop=True), reads=[kdec, vb], writes=[sn_ps])
                    for h in range(4):
                        c2, hp = h // 2, (h % 2) * 64
                        S.op("dve", lambda e, c2=c2, hp=hp, h=h: e.scalar_tensor_tensor(out=S32[hp:hp + 64, c2, :], in0=S32[hp:hp + 64, c2, :], scalar=eb[hp:hp + 64, c2, 127:128],
                                                                                         in1=sn_ps[hp:hp + 64, h, :], op0=ALU.mult, op1=ALU.add), reads=[S32h[h], eb, sn_ps], writes=[S32h[h]])
                        S.op("pool", lambda e, c2=c2, hp=hp: e.tensor_copy(out=SBF[hp:hp + 64, c2, :], in_=S32[hp:hp + 64, c2, :]), reads=[S32h[h]], writes=[SBFh[h]])
                    for h in range(4):
                        hc = slice(h * 128, (h + 1) * 128)
                        S.op("act", lambda e, hc=hc, h=h: e.activation(out=JUNK[:, :], in_=o_ps[:, hc], func=AF.Square, accum_out=ss[:, h:h + 1]), reads=[o_ps], writes=[JUNK, ss])
                    S.op("dve", lambda e: e.tensor_scalar(out=ss[:, :], in0=ss[:, :], scalar1=1.0 / 128, scalar2=RMS_EPS, op0=ALU.mult, op1=ALU.add), reads=[ss], writes=[ss])
                    S.op("pool", lambda e: e.tensor_tensor(out=rs4[:, :], in0=ss[:, :], in1=NHALF[:, 0:4], op=ALU.pow), reads=[ss, NHALF], writes=[rs4])
                    for h in range(4):
                        hc = slice(h * 128, (h + 1) * 128)
                        S.op("dve", lambda e, hc=hc, h=h: e.scalar_tensor_tensor(out=yb[:, hc], in0=o_ps[:, hc], scalar=rs4[:, h:h + 1], in1=rg[:, hc], op0=ALU.mult, op1=ALU.mult),
                             reads=[o_ps, rs4, rg], writes=[yb])

                def gla_b2a(t):
                    yb = stA[t]["yb"]
                    for h in range(4):
                        S.op("pe", lambda e, h=h: e.transpose(ybT_ps[:, h, :], yb[:, h * 128:(h + 1) * 128], IDB[:, :]), reads=[yb, IDB], writes=[ybT_ps])
                    S.op("act", lambda e: e.activation(out=CATB[t][:, :, :], in_=ybT_ps[:, 0:4, :], func=AF.Copy), reads=[ybT_ps], writes=[CATB[t]])

                def gla_b2b(t):
                    stA.pop(t)
                    B = t // 4
                    for hf in range(2):
                        mp = pool_rot.next()
                        for kc in range(8):
                            S.op("pe", lambda e, kc=kc, hf=hf, mp=mp: e.matmul(mp[:, :], lhsT=HT[:, kc, t * 128:(t + 1) * 128], rhs=WOUT[:, kc, hf * 512:(hf + 1) * 512], start=(kc == 0), stop=(kc == 7)),
                                 reads=[CATA[B], CATB[t], WOUT], writes=[mp])
                        S.op("dve", lambda e, hf=hf, mp=mp: e.tensor_tensor(out=XT[t][:, hf * 512:(hf + 1) * 512], in0=mp[:, :], in1=XT[t][:, hf * 512:(hf + 1) * 512], op=ALU.add),
                             reads=[mp, XT[t]], writes=[XT[t]])
                    lnp.push(t)

                def gla_a_all(t):
                    gla_a0(t); gla_a1(t); gla_a2(t)

                lnp = LNPipe()
                gla_prologue(0)
                gla_a_all(0)
                for t in range(NT + 1):
                    nxt = t + 1 if t + 1 < NT else None
                    if nxt is not None and nxt % 4 == 0:
                        gla_prologue(nxt // 4)
                    if nxt is not None:
                        gla_a0(nxt)
                    if t < NT:
                        gla_b0(t)
                    if t >= 1:
                        gla_b2a(t - 1)
                    if nxt is not None:
                        gla_a1(nxt)
                        gla_a2(nxt)
                    if t < NT:
                        gla_b1(t)
                    if t >= 1:
                        gla_b2b(t - 1)
                lnp.flush()
                S.fence()

        def l1_mixer(s):
            l = 1
            jsh, jsc, jg = 0, 8, 16
            CQB = [Tile(HT.ap[:, 0:6, B * 512:(B + 1) * 512], "CQB%d" % B) for B in range(4)]
            load_ln(l, 0, True)
            with ExitStack() as pa:
                CS1 = S.sbuf("CS1", [64, S_LEN], F32, st=pa)
                CS2 = S.sbuf("CS2", [64, S_LEN], F32, st=pa)
                with ExitStack() as pr:
                    POSI = S.sbuf("POSI", [64, S_LEN], I32, st=pr)
                    ANG = S.sbuf("ANG", [64, S_LEN], F32, st=pr)
                    TT = S.sbuf("TT", [64, S_LEN], F32, st=pr)
                    TI = S.sbuf("TI", [64, S_LEN], I32, st=pr)
                    FR = S.sbuf("FR", [64, S_LEN], F32, st=pr)
                    MK = S.sbuf("MK", [64, S_LEN], F32, st=pr)
                    S.dma("sp", lambda e: e.dma_start(out=POSI[:, :], in_=pos_d[s:s + 1, :].partition_broadcast(64)), reads=[DR], writes=[POSI], sync=POSI)
                    S.op("dve", lambda e: e.tensor_copy(out=ANG[:, :], in_=POSI[:, :]), reads=[POSI], writes=[ANG])
                    S.op("dve", lambda e: e.tensor_scalar(out=ANG[:, :], in0=ANG[:, :], scalar1=CONST[0:64, C_INVF:C_INVF + 1], scalar2=None, op0=ALU.mult), reads=[ANG, CONST], writes=[ANG])
                    for (dst, shift, sgn) in ((CS1, 0.75, False), (CS2, 0.5, True)):
                        S.op("dve", lambda e, shift=shift: e.tensor_scalar(out=TT[:, :], in0=ANG[:, :], scalar1=1.0 / TWO_PI, scalar2=shift, op0=ALU.mult, op1=ALU.add), reads=[ANG], writes=[TT])
                        S.op("dve", lambda e: e.tensor_copy(out=TI[:, :], in_=TT[:, :]), reads=[TT], writes=[TI])
                        S.op("dve", lambda e: e.tensor_copy(out=FR[:, :], in_=TI[:, :]), reads=[TI], writes=[FR])
                        S.op("dve", lambda e: e.tensor_tensor(out=FR[:, :], in0=TT[:, :], in1=FR[:, :], op=ALU.subtract), reads=[TT, FR], writes=[FR])
                        S.op("dve", lambda e: e.tensor_single_scalar(out=MK[:, :], in_=FR[:, :], scalar=0.0, op=ALU.is_lt), reads=[FR], writes=[MK])
                        S.op("dve", lambda e: e.tensor_tensor(out=FR[:, :], in0=FR[:, :], in1=MK[:, :], op=ALU.add), reads=[FR, MK], writes=[FR])
                        S.op("dve", lambda e: e.tensor_single_scalar(out=MK[:, :], in_=FR[:, :], scalar=1.0, op=ALU.is_ge), reads=[FR], writes=[MK])
                        S.op("dve", lambda e: e.tensor_tensor(out=FR[:, :], in0=FR[:, :], in1=MK[:, :], op=ALU.subtract), reads=[FR, MK], writes=[FR])
                        S.op("act", lambda e, dst=dst: e.activation(out=dst[:, :], in_=FR[:, :], func=AF.Sin, scale=TWO_PI, bias=CONST[0:64, C_NPI:C_NPI + 1]), reads=[FR, CONST], writes=[dst])
                        if sgn:
                            S.op("dve", lambda e, dst=dst: e.tensor_scalar(out=dst[:, :], in0=dst[:, :], scalar1=CONST[0:64, C_SGN:C_SGN + 1], scalar2=None, op0=ALU.mult), reads=[dst, CONST], writes=[dst])
                    S.fence()
                WUQ = S.sbuf("WUQ", [128, 3, 2048], BF16, st=pa)
                WUKV = S.sbuf("WUKV", [128, 2, 2048], BF16, st=pa)
                S.dma("pool", lambda e: [e.dma_start(out=WUQ[:, :, 0:1024], in_=mla_w_uq_d.rearrange("(c p) n -> p c n", p=128)[:, :, 0:1024]),
                                         e.dma_start(out=WUQ[:, :, 1024:2048], in_=mla_w_uq_d.rearrange("(c p) n -> p c n", p=128)[:, :, 1024:2048])],
                      reads=[DR], writes=[WUQ], sync=WUQ, n=2)
                S.dma("pool", lambda e: [e.dma_start(out=WUKV[:, :, 0:1024], in_=mla_w_ukv_d.rearrange("(c p) n -> p c n", p=128)[:, :, 0:1024]),
                                         e.dma_start(out=WUKV[:, :, 1024:2048], in_=mla_w_ukv_d.rearrange("(c p) n -> p c n", p=128)[:, :, 1024:2048])],
                      reads=[DR], writes=[WUKV], sync=WUKV, n=2)
                with ExitStack() as p1:
                    def sb(name, shape, dt=F32):
                        return S.sbuf(name, shape, dt, st=p1)
                    WIN1 = sb("WIN1", [128, 8, 768], BF16)
                    S.dma("pool", lambda e: e.dma_start(out=WIN1[:, :, :], in_=mla_w_in_d.rearrange("(c p) n -> p c n", p=128)), reads=[DR], writes=[WIN1], sync=WIN1)
                    HTB = sb("HTB1", [128, 8, 512], BF16)
                    UT = sb("UT", [128, 5, 512])
                    SQ = Rot([sb("SQ%d" % i, [128, 512]) for i in range(2)])
                    RQ = sb("RQ", [128, 512]); RKV = sb("RKV", [128, 512])
                    T1 = sb("T1a", [64, 512]); T2 = sb("T2a", [64, 512])
                    scr = sb("scr1", [128, 8, 128])
                    pool_rot = Rot([S.psum("pla%d" % i, [128, 512], F32, st=p1) for i in range(6)])
                    ssq = [S.psum("ssq%d" % i, [128, 512], F32, st=p1) for i in range(2)]
                    build_g1b(l, jg, s, pool_rot.tiles[0:2], scr)
                    for B in range(4):
                        bc = slice(B * 512, (B + 1) * 512)
                        for i in range(4):
                            transpose_tile(4 * B + i, l, jsc, jsh, s, pool_rot, lambda c, i=i: HTB[:, c, i * 128:(i + 1) * 128], HTB)
                        for j in range(5):
                            up = pool_rot.next()
                            for kc in range(8):
                                S.op("pe", lambda e, kc=kc, j=j, up=up: e.matmul(up[:, :], lhsT=WIN1[:, kc, j * 128:(j + 1) * 128], rhs=HTB[:, kc, :], start=(kc == 0), stop=(kc == 7)), reads=[WIN1, HTB], writes=[up])
                            S.op("act", lambda e, j=j, up=up: e.activation(out=UT[:, j, :], in_=up[:, :], func=AF.Copy), reads=[up], writes=[UT])
                            sq = SQ.next()
                            S.op("act", lambda e, sq=sq, up=up: e.activation(out=sq[:, :], in_=up[:, :], func=AF.Square), reads=[up], writes=[sq])
                            sp_ = ssq[0] if j < 3 else ssq[1]
                            S.op("pe", lambda e, sq=sq, sp_=sp_, j=j: e.matmul(sp_[:, :], lhsT=ones, rhs=sq[:, :], start=(j in (0, 3)), stop=(j in (2, 4))), reads=[CONST, sq], writes=[sp_])
                        for (dst, sp_, n_) in ((RQ, ssq[0], 384.0), (RKV, ssq[1], 256.0)):
                            S.op("act", lambda e, dst=dst, sp_=sp_, n_=n_: e.activation(out=dst[:, :], in_=sp_[:, :], func=AF.Ln, scale=1.0 / n_, bias=CONST[:, C_REPS:C_REPS + 1]), reads=[sp_, CONST], writes=[dst])
                            S.op("act", lambda e, dst=dst: e.activation(out=dst[:, :], in_=dst[:, :], func=AF.Exp, scale=-0.5), reads=[dst], writes=[dst])
                        for j in range(5):
                            rr = RQ if j < 3 else RKV
                            S.op("dve", lambda e, j=j, rr=rr, bc=bc: e.scalar_tensor_tensor(out=HT[:, j, bc], in0=UT[:, j, :], scalar=PVT[:, 12 + j:13 + j], in1=rr[:, :], op0=ALU.mult, op1=ALU.mult),
                                 reads=[UT, PVT, rr], writes=[CQB[B]])
                        a_p = pool_rot.next()
                        b_p = pool_rot.next()
                        for (pp_, off) in ((a_p, 640), (b_p, 704)):
                            for kc in range(8):
                                S.op("pe", lambda e, kc=kc, pp_=pp_, off=off: e.matmul(pp_[0:64, :], lhsT=WIN1[:, kc, off:off + 64], rhs=HTB[:, kc, :], start=(kc == 0), stop=(kc == 7)), reads=[WIN1, HTB], writes=[pp_])
                        S.op("dve", lambda e, a_p=a_p, bc=bc: e.tensor_tensor(out=T1[:, :], in0=a_p[0:64, :], in1=CS1[:, bc], op=ALU.mult), reads=[a_p, CS1], writes=[T1])
                        S.op("dve", lambda e, b_p=b_p, bc=bc: e.tensor_tensor(out=T2[:, :], in0=b_p[0:64, :], in1=CS2[:, bc], op=ALU.mult), reads=[b_p, CS2], writes=[T2])
                        S.op("pool", lambda e, bc=bc: e.tensor_tensor(out=HT[0:64, 5, bc], in0=T1[:, :], in1=T2[:, :], op=ALU.add), reads=[T1, T2], writes=[CQB[B]])
                    S.fence()
                if stop_after == "l1a":
                    return
                with ExitStack() as p2:
                    def sb(name, shape, dt=F32):
                        return S.sbuf(name, shape, dt, st=p2)
                    WOUT = sb("WOUT1", [128, 8, D], BF16)
                    S.dma("pool", lambda e: e.dma_start(out=WOUT[:, :, :], in_=mla_w_out_d.rearrange("(c p) n -> p c n", p=128)), reads=[DR], writes=[WOUT], sync=WOUT)
                    S.op("pool", lambda e: e.tensor_tensor(out=WOUT[:, :, :], in0=WOUT[:, :, :], in1=G1B[:, :].unsqueeze(1).to_broadcast([128, 8, D]), op=ALU.mult), reads=[WOUT, G1B], writes=[WOUT])
                    QN = sb("QN", [128, S_LEN], BF16); QR = sb("QR", [64, S_LEN], BF16); KN = sb("KN", [128, S_LEN], BF16); VT = sb("VT", [128, NT, 128], BF16)
                    PT = Rot([sb("PT%d" % i, [128, 512], BF16) for i in range(5)])
                    RINV = sb("RINV", [128, 512])
                    OTH = Rot([sb("OTH%d" % i, [128, S_LEN], BF16) for i in range(2)])
                    T1 = sb("T1b", [64, 512]); T2 = sb("T2b", [64, 512])
                    pool_rot = Rot([S.psum("plb%d" % i, [128, 512], F32, st=p2) for i in range(3)])
                    st_rot = Rot([S.psum("stb%d" % i, [128, 512], F32, st=p2) for i in range(3)])
                    o_ps = S.psum("o1_ps", [128, 512], F32, st=p2)
                    r_ps = S.psum("r1_ps", [128, 512], F32, st=p2)
                    lnp = LNPipe()
                    for h in range(8 if dbg >= 2 else 0):
                        hb = h * 256
                        for B in range(4):
                            bc = slice(B * 512, (B + 1) * 512)
                            p_ = pool_rot.next()
                            for k3 in range(3):
                                S.op("pe", lambda e, k3=k3, p_=p_, bc=bc, hb=hb: e.matmul(p_[:, :], lhsT=WUQ[:, k3, hb:hb + 128], rhs=HT[:, k3, bc], start=(k3 == 0), stop=(k3 == 2)), reads=[WUQ, CQB[B]], writes=[p_])
                            S.op("act", lambda e, p_=p_, bc=bc: e.activation(out=QN[:, bc], in_=p_[:, :], func=AF.Copy), reads=[p_], writes=[QN])
                            a_p = pool_rot.next()
                            b_p = pool_rot.next()
                            for (pp_, off) in ((a_p, hb + 128), (b_p, hb + 192)):
                                for k3 in range(3):
                                    S.op("pe", lambda e, k3=k3, pp_=pp_, off=off, bc=bc: e.matmul(pp_[0:64, :], lhsT=WUQ[:, k3, off:off + 64], rhs=HT[:, k3, bc], start=(k3 == 0), stop=(k3 == 2)), reads=[WUQ, CQB[B]], writes=[pp_])
                            S.op("dve", lambda e, a_p=a_p, bc=bc: e.tensor_tensor(out=T1[:, :], in0=a_p[0:64, :], in1=CS1[:, bc], op=ALU.mult), reads=[a_p, CS1], writes=[T1])
                            S.op("dve", lambda e, b_p=b_p, bc=bc: e.tensor_tensor(out=T2[:, :], in0=b_p[0:64, :], in1=CS2[:, bc], op=ALU.mult), reads=[b_p, CS2], writes=[T2])
                            S.op("pool", lambda e, bc=bc: e.tensor_tensor(out=QR[:, bc], in0=T1[:, :], in1=T2[:, :], op=ALU.add), reads=[T1, T2], writes=[QR])
                            p_ = pool_rot.next()
                            for k2 in range(2):
                                S.op("pe", lambda e, k2=k2, p_=p_, bc=bc, hb=hb: e.matmul(p_[:, :], lhsT=WUKV[:, k2, hb:hb + 128], rhs=HT[:, 3 + k2, bc], start=(k2 == 0), stop=(k2 == 1)), reads=[WUKV, CQB[B]], writes=[p_])
                            S.op("act", lambda e, p_=p_, bc=bc: e.activation(out=KN[:, bc], in_=p_[:, :], func=AF.Copy), reads=[p_], writes=[KN])
                            p_ = pool_rot.next()
                            for i in range(4):
                                t = 4 * B + i
                                for k2 in range(2):
                                    S.op("pe", lambda e, k2=k2, p_=p_, i=i, t=t, hb=hb: e.matmul(p_[:, i * 128:(i + 1) * 128], lhsT=HT[:, 3 + k2, t * 128:(t + 1) * 128], rhs=WUKV[:, k2, hb + 128:hb + 256], start=(k2 == 0), stop=(k2 == 1)),
                                         reads=[WUKV, CQB[B]], writes=[p_])
                            S.op("act", lambda e, p_=p_, B=B: e.activation(out=VT[:, 4 * B:4 * B + 4, :], in_=p_[:, :].rearrange("p (a b) -> p a b", a=4), func=AF.Copy), reads=[p_], writes=[VT])
                        oth = OTH.next()
                        steps = [(Q, kt) for Q in range(4) for kt in range(4 * Q + 4)]
                        pend = {}

                        def do_st(Q, kt):
                            m = kt - 4 * Q if kt >= 4 * Q else 0
                            c0 = m * 128
                            st_ = st_rot.next()
                            qc = slice(Q * 512 + c0, (Q + 1) * 512)
                            kc_ = slice(kt * 128, (kt + 1) * 128)
                            S.op("pe", lambda e: e.matmul(st_[:, c0:512], lhsT=KN[:, kc_], rhs=QN[:, qc], start=True, stop=False), reads=[KN, QN], writes=[st_])
                            S.op("pe", lambda e: e.matmul(st_[:, c0:512], lhsT=HT[0:64, 5, kc_], rhs=QR[0:64, qc], start=False, stop=True), reads=[CQB[kt // 4], QR], writes=[st_])
                            pt = PT.next()
                            S.op("act", lambda e: e.activation(out=pt[:, c0:512], in_=st_[:, c0:512], func=AF.Exp, scale=MLA_SCALE), reads=[st_], writes=[pt])
                            if kt >= 4 * Q:
                                S.op("pool", lambda e: e.tensor_tensor(out=pt[:, c0:c0 + 128], in0=pt[:, c0:c0 + 128], in1=TRIB[:, :], op=ALU.mult), reads=[pt, TRIB], writes=[pt])
                            pend[(Q, kt)] = (pt, c0)

                        def do_pv(Q, kt, oth=oth):
                            pt, c0 = pend.pop((Q, kt))
                            last = (kt == 4 * Q + 3)
                            S.op("pe", lambda e: e.matmul(o_ps[:, c0:512], lhsT=VT[:, kt, :], rhs=pt[:, c0:512], start=(kt == 0), stop=last), reads=[VT, pt], writes=[o_ps])
                            S.op("pe", lambda e: e.matmul(r_ps[:, c0:512], lhsT=ONEB[:, :], rhs=pt[:, c0:512], start=(kt == 0), stop=last), reads=[ONEB, pt], writes=[r_ps])
                            if last:
                                qc = slice(Q * 512, (Q + 1) * 512)
                                S.op("dve", lambda e: e.reciprocal(out=RINV[:, :], in_=r_ps[:, :]), reads=[r_ps], writes=[RINV])
                                S.op("dve", lambda e: e.tensor_tensor(out=oth[:, qc], in0=o_ps[:, :], in1=RINV[:, :], op=ALU.mult), reads=[o_ps, RINV], writes=[oth])

                        for i_, (Q, kt) in enumerate(steps):
                            do_st(Q, kt)
                            if i_ >= 2:
                                do_pv(*steps[i_ - 2])
                        do_pv(*steps[-2])
                        do_pv(*steps[-1])
                        for t in range(NT):
                            for hf in range(2):
                                mp = pool_rot.next()
                                S.op("pe", lambda e, t=t, hf=hf, mp=mp, h=h, oth=oth: e.matmul(mp[:, :], lhsT=oth[:, t * 128:(t + 1) * 128], rhs=WOUT[:, h, hf * 512:(hf + 1) * 512], start=True, stop=True), reads=[oth, WOUT], writes=[mp])
                                S.op("dve", lambda e, t=t, hf=hf, mp=mp: e.tensor_tensor(out=XT[t][:, hf * 512:(hf + 1) * 512], in0=mp[:, :], in1=XT[t][:, hf * 512:(hf + 1) * 512], op=ALU.add),
                                     reads=[mp, XT[t]], writes=[XT[t]])
                            if h == 7:
                                lnp.push(t)
                    lnp.flush()
                    S.fence()

        for s in range(nseq):
            for t in range(NT):
                S.dma("sp", lambda e, t=t, s=s: e.dma_start(out=XT[t][:, :], in_=x_d[s, t * 128:(t + 1) * 128, :]), reads=[DR], writes=[XT[t]], sync=XT[t])
                S.op("act", lambda e, t=t: e.activation(out=XT[t][:, :], in_=XT[t][:, :], func=AF.Copy, scale=ALPHA), reads=[XT[t]], writes=[XT[t]])
            if stop_after == "moe0":
                moe_phase(0, s, last=True)
            elif stop_after in ("xm1only", "l1a"):
                l1_mixer(s)
            elif stop_after != "load":
                l0_mixer(s)
                if stop_after not in ("xm0", "l0a", "l0b"):
                    moe_phase(0, s, last=False)
                    if stop_after != "xf0":
                        l1_mixer(s)
                        if stop_after != "xm1":
                            moe_phase(1, s, last=True)
            for t in range(NT):
                S.dma("sp", lambda e, t=t, s=s: e.dma_start(out=out_d[s, t * 128:(t + 1) * 128, :], in_=XT[t][:, :]), reads=[XT[t]], writes=[DO], sync=XT[t])
            S.fence()
        S.emit(final_keys=["X%d" % t for t in range(NT)])
    return nc


def prep_shared(inp):
    f = lambda a: np.ascontiguousarray(np.asarray(a, dtype=np.float32))
    sh = {}
    sh["consts"] = make_consts()
    sh["ada_w"] = f(inp["ada_w"])
    sh["ada_b"] = f(inp["ada_b"])
    sh["ln_gb"] = f(np.stack([inp["ln_mix_g"], inp["ln_mix_b"], inp["ln_ffn_g"], inp["ln_ffn_b"]], axis=1))
    sh["ab_w_in"] = f(inp["ab_w_in"][0])
    sh["conv_w"] = f(inp["conv_w"][0])
    pv = np.zeros((40, 128), np.float32)
    pv[0:4] = np.asarray(inp["conv_b"][0]).reshape(4, 128)
    pv[4:8] = np.asarray(inp["conv_ln_g"][0]).reshape(4, 128)
    pv[8:12] = np.asarray(inp["conv_ln_b"][0]).reshape(4, 128)
    pv[12:15] = np.asarray(inp["mla_q_norm_g"][0]).reshape(3, 128)
    pv[15:17] = np.asarray(inp["mla_kv_norm_g"][0]).reshape(2, 128)
    sh["pvec"] = pv
    sh["gla_gate_w"] = f(inp["gla_gate_w"][0])
    sh["gla_gate_b"] = f(inp["gla_gate_b"][0]).reshape(1, 256)
    sh["gla_norm_g"] = f(inp["gla_norm_g"][0]).reshape(1, 512)
    sh["ab_w_out"] = f(inp["ab_w_out"][0])
    w_in = np.asarray(inp["mla_w_in"][0], dtype=np.float32)
    sh["mla_w_in"] = f(np.concatenate([w_in, w_in[:, 672:704], w_in[:, 640:672]], axis=1))
    wq = np.asarray(inp["mla_w_uq"][0], dtype=np.float32).reshape(384, 8, 192)
    sh["mla_w_uq"] = f(np.concatenate([wq, wq[:, :, 160:192], wq[:, :, 128:160]], axis=2).reshape(384, 2048))
    sh["mla_w_ukv"] = f(inp["mla_w_ukv"][0])
    sh["mla_w_out"] = f(inp["mla_w_out"][0])
    sh["moe_wr"] = f(np.concatenate([inp["moe_w_group"], inp["moe_w_router"]], axis=2))
    sh["moe_br"] = f(np.concatenate([inp["moe_b_group"], inp["moe_b_router"]], axis=1))
    sh["consts2"] = make_consts2()
    wg = np.asarray(inp["moe_w_gate"], dtype=np.float32).reshape(2, 32, 8, 128, 256).transpose(0, 1, 3, 2, 4)
    wu = np.asarray(inp["moe_w_up"], dtype=np.float32).reshape(2, 32, 8, 128, 256).transpose(0, 1, 3, 2, 4)
    wd = np.asarray(inp["moe_w_down"], dtype=np.float32).reshape(2, 32, 2, 128, 1024).transpose(0, 1, 3, 2, 4)
    wall = np.concatenate([np.concatenate([wg, wu], axis=4).reshape(2, 32, 128, 4096), wd.reshape(2, 32, 128, 2048)], axis=3)
    for i in range(2):
        sh["moe_wall%d" % i] = f(wall[i].reshape(32 * 128, 6144))
    return sh


def kernel(**inputs):
    sh = prep_shared(inputs)
    x = np.asarray(inputs["x"], dtype=np.float32)
    c = np.asarray(inputs["c"], dtype=np.float32)
    pos = np.asarray(inputs["positions"], dtype=np.int32)
    nc = build_nc()
    in_maps = []
    for i in range(NCORES):
        m = dict(sh)
        m["x"] = np.ascontiguousarray(x[2 * i:2 * i + 2])
        m["c"] = np.ascontiguousarray(c[2 * i:2 * i + 2])
        m["pos"] = np.ascontiguousarray(pos[2 * i:2 * i + 2])
        in_maps.append(m)
    res = run_bass_kernel_spmd(nc, in_maps, core_ids=list(range(NCORES)))
    return np.concatenate([r["out"] for r in res.results], axis=0).astype(np.float32)
```

```python
from contextlib import ExitStack
import math
import numpy as np
import concourse.bass as bass
import concourse.mybir as mybir
from concourse.bass_utils import run_bass_kernel_spmd

F32 = mybir.dt.float32
BF16 = mybir.dt.bfloat16
I32 = mybir.dt.int32
AF = mybir.ActivationFunctionType
ALU = mybir.AluOpType
AX = mybir.AxisListType

NCORES = 8
D = 1024
S_LEN = 2048
NT = 16
ALPHA = 4.0 ** 0.25
LN_EPS = 1e-5
RMS_EPS = 1e-6
MLA_SCALE = 192.0 ** -0.5
TWO_PI = 2.0 * math.pi

COMPUTE = ("pe", "act", "dve", "pool")


class Tile:
    __slots__ = ("ap", "name", "w", "rs", "semkey")

    def __init__(self, ap, name, semkey=None):
        self.ap = ap
        self.name = name
        self.w = None
        self.rs = []
        self.semkey = semkey or name

    def __getitem__(self, idx):
        return self.ap[idx]


class Op:
    __slots__ = ("eng", "fn", "deps", "signal", "sigidx", "pos", "semkey", "semval", "ndma")

    def __init__(self, eng, fn):
        self.eng = eng
        self.fn = fn
        self.deps = []
        self.signal = False
        self.sigidx = 0
        self.pos = 0
        self.semkey = None
        self.semval = 0
        self.ndma = 0


class Sched:
    def __init__(self, nc, stack):
        self.nc = nc
        self.stack = stack
        self.ops = {e: [] for e in ("pe", "act", "dve", "pool", "sp")}
        self.semcnt = {}
        self.uid = 0
        self.fence_deps = []
        self.fence_pending = set()
        self.dma_since = []

    def sbuf(self, name, shape, dtype, st=None):
        self.uid += 1
        t = (st or self.stack).enter_context(self.nc.sbuf_tensor("%s_%d" % (name, self.uid), list(shape), dtype))
        return Tile(t, name)

    def psum(self, name, shape, dtype=F32, st=None):
        self.uid += 1
        t = (st or self.stack).enter_context(self.nc.psum_tensor("%s_%d" % (name, self.uid), list(shape), dtype))
        return Tile(t, name)

    def _add(self, eng, fn, reads, writes, semkey=None, ndma=0):
        op = Op(eng, fn)
        lst = self.ops[eng]
        op.pos = len(lst)
        deps = []
        if eng in self.fence_pending:
            self.fence_pending.discard(eng)
            deps.extend(self.fence_deps)
        for t in reads:
            if t.w is not None:
                deps.append(t.w)
        for t in writes:
            if t.w is not None:
                deps.append(t.w)
            deps.extend(t.rs)
        seen = set()
        for d in deps:
            if id(d) in seen or d is op:
                continue
            seen.add(id(d))
            if d.semkey is None and d.eng == eng:
                if eng == "pe" or eng == "sp":
                    continue
            op.deps.append(d)
            if d.semkey is None:
                d.signal = True
        for t in reads:
            if semkey is None:
                t.rs = [r for r in t.rs if not (r.semkey is None and r.eng == eng)]
            t.rs.append(op)
        for t in writes:
            t.w = op
            t.rs = []
        if semkey is not None:
            op.semkey = semkey
            op.ndma = ndma
            self.semcnt[semkey] = self.semcnt.get(semkey, 0) + 16 * ndma
            op.semval = self.semcnt[semkey]
            self.dma_since.append(op)
        lst.append(op)
        return op

    def op(self, eng, fn, reads=(), writes=()):
        return self._add(eng, fn, list(reads), list(writes))

    def dma(self, eng, fn, reads=(), writes=(), sync=None, n=1):
        return self._add(eng, fn, list(reads), list(writes), semkey=sync.semkey, ndma=n)

    def fence(self):
        deps = []
        for lst in self.ops.values():
            for d in reversed(lst):
                if d.semkey is None:
                    deps.append(d)
                    break
        deps = deps + self.dma_since
        self.dma_since = []
        self.fence_deps = deps
        self.fence_pending = set(self.ops.keys())

    def emit(self, final_keys=()):
        nc = self.nc
        stack = self.stack
        esem = {e: stack.enter_context(nc.semaphore("es_" + e)) for e in COMPUTE}
        dsem = {k: stack.enter_context(nc.semaphore("ds_%d" % i)) for i, k in enumerate(sorted(self.semcnt))}
        for e in COMPUTE:
            c = 0
            for op in self.ops[e]:
                if op.signal and op.semkey is None:
                    c += 1
                    op.sigidx = c
        block = stack.enter_context(nc.Block())

        def run(ename, engine):
            waited = {}
            for op in self.ops[ename]:
                need = {}
                for d in op.deps:
                    if d.semkey is not None:
                        s, v, key = dsem[d.semkey], d.semval, "d" + d.semkey
                    else:
                        s, v, key = esem[d.eng], d.sigidx, "e" + d.eng
                    if key not in need or need[key][1] < v:
                        need[key] = (s, v)
                for key, (s, v) in need.items():
                    if waited.get(key, 0) >= v:
                        continue
                    waited[key] = v
                    engine.wait_ge(s, v)
                r = op.fn(engine)
                if op.semkey is not None:
                    if not isinstance(r, (list, tuple)):
                        r = [r]
                    assert len(r) == op.ndma, (len(r), op.ndma)
                    for ins in r:
                        ins.then_inc(dsem[op.semkey], 16)
                elif op.signal:
                    r.then_inc(esem[ename], 1)
            if ename == "sp":
                for k in final_keys:
                    engine.wait_ge(dsem[k], self.semcnt[k])

        @block.sync
        def _(sync):
            run("sp", sync)

        @block.tensor
        def _(tensor):
            run("pe", tensor)

        @block.scalar
        def _(scalar):
            run("act", scalar)

        @block.vector
        def _(vector):
            run("dve", vector)

        @block.gpsimd
        def _(gpsimd):
            run("pool", gpsimd)


class Rot:
    def __init__(self, tiles):
        self.tiles = tiles
        self.i = 0

    def next(self):
        t = self.tiles[self.i % len(self.tiles)]
        self.i += 1
        return t


C_ID, C_TRI, C_SU, C_ONE, C_INVF, C_SGN, C_NHALF, C_NPI, C_REPS, NCONST = 0, 128, 256, 384, 512, 513, 514, 515, 516, 517


C2_LT, C2_THR, C2_VROW, C2_EROW, C2_VAL, NC2 = 0, 1024, 1032, 1080, 1112, 1144


def make_consts2():
    c = np.zeros((128, NC2), np.float32)
    p = np.arange(128)
    e = np.arange(32)
    c[:, C2_LT:C2_LT + 1024] = (e[None, :] < e[:, None]).astype(np.float32).reshape(1, 1024)
    c[:, C2_THR:C2_THR + 8] = 256.0 * np.arange(8)[None, :]
    c[:, C2_VROW:C2_VROW + 48] = np.arange(48)[None, :]
    c[:, C2_EROW:C2_EROW + 32] = e[None, :] * 128.0 + p[:, None] - 8192.0
    kt = np.arange(32)
    c[:, C2_VAL:C2_VAL + 32] = (kt // 16)[None, :] * 2048.0 + (kt % 16)[None, :] * 128.0 + p[:, None]
    return c


def make_consts():
    c = np.zeros((128, NCONST), np.float32)
    p = np.arange(128)
    c[:, C_ID:C_ID + 128] = np.eye(128, dtype=np.float32)
    c[:, C_TRI:C_TRI + 128] = (p[:, None] <= p[None, :]).astype(np.float32)
    c[:, C_SU:C_SU + 128] = (p[:, None] > p[None, :]).astype(np.float32)
    c[:, C_ONE:C_ONE + 128] = 1.0
    inv_freq = (1.0 / (np.float32(10000.0) ** (np.arange(0, 64, 2, dtype=np.float32) / np.float32(64)))).astype(np.float32)
    c[:64, C_INVF] = inv_freq[p[:64] % 32]
    c[:, C_SGN] = np.where(p < 32, -1.0, 1.0)
    c[:, C_NHALF] = -0.5
    c[:, C_NPI] = -math.pi
    c[:, C_REPS] = RMS_EPS
    return c


def build_nc(nseq=2, stop_after=None, dbg=9):
    nc = bass.Bass("TRN2", target_bir_lowering=False)

    def din(name, shape, dt=F32):
        return nc.dram_tensor(name, list(shape), dt, kind="ExternalInput").ap()

    x_d = din("x", [2, S_LEN, D])
    c_d = din("c", [2, D])
    pos_d = din("pos", [2, S_LEN], I32)
    const_d = din("consts", [128, NCONST])
    ada_w_d = din("ada_w", [2, D, 6 * D])
    ada_b_d = din("ada_b", [2, 6 * D])
    ln_d = din("ln_gb", [2, 4, D])
    ab_w_in_d = din("ab_w_in", [D, 2576])
    conv_w_d = din("conv_w", [31, 512])
    pv_d = din("pvec", [40, 128])
    gate_w_d = din("gla_gate_w", [16, 256])
    gate_b_d = din("gla_gate_b", [1, 256])
    gnorm_d = din("gla_norm_g", [1, 512])
    ab_w_out_d = din("ab_w_out", [D, D])
    mla_w_in_d = din("mla_w_in", [D, 768])
    mla_w_uq_d = din("mla_w_uq", [384, 2048])
    mla_w_ukv_d = din("mla_w_ukv", [256, 2048])
    mla_w_out_d = din("mla_w_out", [D, D])
    wr_d = din("moe_wr", [2, D, 36])
    br_d = din("moe_br", [2, 36])
    wall_ds = [din("moe_wall%d" % i, [32 * 128, 6144]) for i in range(2)]
    const2_d = din("consts2", [128, NC2])
    htok_d = nc.dram_tensor("htok_scr", [S_LEN, D], BF16).ap()
    ys_d = nc.dram_tensor("ys_scr", [2 * S_LEN, D], F32).ap()
    tab_d = nc.dram_tensor("tab_scr", [96 * 128, 1], I32).ap()
    out_d = nc.dram_tensor("out", [2, S_LEN, D], F32, kind="ExternalOutput").ap()

    with ExitStack() as st:
        S = Sched(nc, st)
        global LAST_SCHED
        LAST_SCHED = S
        DR = Tile(None, "dram_in")
        DO = Tile(None, "dram_out")
        TABF = Tile(None, "tabf")
        TABS = Tile(None, "tabs")
        BC = {}
        for bv in (4095, 2047, 96 * 128 - 1):
            BC[bv] = nc.alloc_register(mybir.EngineType.Pool, "bc%d" % bv)
            S.op("pool", lambda e, bv=bv: e.reg_mov(BC[bv], bv))

        X = S.sbuf("X", [128, NT, D], F32)
        XT = [Tile(X.ap[:, t, :], "X%d" % t) for t in range(NT)]
        HT = S.sbuf("HT", [128, 8, S_LEN], BF16)
        CONST = S.sbuf("CONST", [128, NCONST], F32)
        IDB = S.sbuf("IDB", [128, 128], BF16)
        TRIB = S.sbuf("TRIB", [128, 128], BF16)
        ONEB = S.sbuf("ONEB", [128, 128], BF16)
        MOD = S.sbuf("MOD", [128, 2, 48, 2], F32)
        G1B = S.sbuf("G1B", [128, D], F32)
        LNG = S.sbuf("LNG", [128, D], F32)
        LNB = S.sbuf("LNB", [128, D], F32)
        PVT = S.sbuf("PVT", [128, 40], F32)
        MV = S.sbuf("MV", [128, NT, 2], F32)
        RSTD = S.sbuf("RSTD", [128, NT], F32)
        NHALF = S.sbuf("NHALF", [128, 512], F32)

        ident = CONST[:, C_ID:C_ID + 128]
        tri = CONST[:, C_TRI:C_TRI + 128]
        su = CONST[:, C_SU:C_SU + 128]
        ones = CONST[:, C_ONE:C_ONE + 128]

        with ExitStack() as ph:
            S.dma("sp", lambda e: e.dma_start(out=CONST[:, :], in_=const_d[:, :]), reads=[DR], writes=[CONST], sync=CONST)
            S.op("dve", lambda e: e.tensor_copy(out=IDB[:, :], in_=ident), reads=[CONST], writes=[IDB])
            S.op("dve", lambda e: e.tensor_copy(out=TRIB[:, :], in_=tri), reads=[CONST], writes=[TRIB])
            S.op("dve", lambda e: e.tensor_copy(out=ONEB[:, :], in_=ones), reads=[CONST], writes=[ONEB])
            S.op("pool", lambda e: e.memset(NHALF[:, :], -0.5), writes=[NHALF])
            STG = S.sbuf("STG", [128, 128], F32, st=ph)
            S.op("dve", lambda e: e.memset(STG[:, :], 0.0), writes=[STG])
            S.dma("sp", lambda e: [e.dma_start(out=STG[0:40, :], in_=pv_d[:, :]),
                                   e.dma_start(out=STG[64:80, :], in_=c_d.rearrange("s (c p) -> (s c) p", p=128)),
                                   ], reads=[DR], writes=[STG], sync=STG, n=2)
            pp = S.psum("pp_setup", [128, 512], F32, st=ph)
            pp2 = S.psum("pp_setup2", [128, 512], F32, st=ph)
            S.op("pe", lambda e: e.transpose(pp[:, 0:128], STG[:, :], ident), reads=[STG, CONST], writes=[pp])
            S.op("dve", lambda e: e.tensor_copy(out=PVT[:, :], in_=pp[:, 0:40]), reads=[pp], writes=[PVT])
            CACT = S.sbuf("CACT", [128, 2, 8], BF16, st=ph)
            S.op("act", lambda e: e.activation(out=CACT[:, :, :].rearrange("p s c -> p (s c)"), in_=pp[:, 64:80], func=AF.Silu), reads=[pp], writes=[CACT])
            STB = S.sbuf("STB", [128, 128], F32, st=ph)
            S.op("dve", lambda e: e.memset(STB[:, :], 0.0), writes=[STB])
            S.dma("sp", lambda e: e.dma_start(out=STB[0:96, :], in_=ada_b_d.rearrange("l (j p) -> (l j) p", p=128)), reads=[DR], writes=[STB], sync=STB)
            S.op("pe", lambda e: e.transpose(pp2[:, 0:128], STB[:, :], ident), reads=[STB, CONST], writes=[pp2])
            ADABT = S.sbuf("ADABT", [128, 96], F32, st=ph)
            S.op("dve", lambda e: e.tensor_copy(out=ADABT[:, :], in_=pp2[:, 0:96]), reads=[pp2], writes=[ADABT])
            AW = [S.sbuf("AW%d" % i, [128, 8, 512], BF16, st=ph) for i in range(3)]
            modp = S.psum("modp", [128, 2, 48, 2], F32, st=ph)
            for l in range(2):
                for blk in range(12):
                    aw = AW[(l * 12 + blk) % 3]
                    src = ada_w_d[l].rearrange("(c p) n -> p c n", p=128)[:, :, blk * 512:(blk + 1) * 512]
                    S.dma("pool", lambda e, aw=aw, src=src: e.dma_start(out=aw[:, :, :], in_=src), reads=[DR], writes=[aw], sync=aw)
                    for jj in range(4):
                        j = blk * 4 + jj
                        for kc in range(8):
                            S.op("pe", lambda e, aw=aw, jj=jj, kc=kc, l=l, j=j: e.matmul(
                                modp[:, l, j, :], lhsT=aw[:, kc, jj * 128:(jj + 1) * 128], rhs=CACT[:, :, kc],
                                start=(kc == 0), stop=(kc == 7)), reads=[aw, CACT], writes=[modp])
            for l in range(2):
                for s in range(2):
                    S.op("dve", lambda e, l=l, s=s: e.tensor_tensor(out=MOD[:, l, :, s], in0=modp[:, l, :, s], in1=ADABT[:, l * 48:(l + 1) * 48], op=ALU.add),
                         reads=[modp, ADABT], writes=[MOD])
            for l in range(2):
                for j0 in (8, 32):
                    S.op("dve", lambda e, l=l, j0=j0: e.tensor_scalar(out=MOD[:, l, j0:j0 + 8, :], in0=MOD[:, l, j0:j0 + 8, :], scalar1=1.0, scalar2=1.0 / ALPHA, op0=ALU.add, op1=ALU.mult),
                         reads=[MOD], writes=[MOD])
                for j0 in (16, 40):
                    S.op("dve", lambda e, l=l, j0=j0: e.tensor_scalar(out=MOD[:, l, j0:j0 + 8, :], in0=MOD[:, l, j0:j0 + 8, :], scalar1=1.0, scalar2=None, op0=ALU.add),
                         reads=[MOD], writes=[MOD])
            S.fence()

        def load_ln(l, which, scaled):
            S.dma("sp", lambda e: [e.dma_start(out=LNG[:, :], in_=ln_d[l, 2 * which:2 * which + 1, :].partition_broadcast(128)),
                                   e.dma_start(out=LNB[:, :], in_=ln_d[l, 2 * which + 1:2 * which + 2, :].partition_broadcast(128))],
                  reads=[DR], writes=[LNG, LNB], sync=LNG, n=2)
            if scaled:
                S.op("dve", lambda e: e.tensor_scalar(out=LNG[:, :], in0=LNG[:, :], scalar1=ALPHA, scalar2=None, op0=ALU.mult), reads=[LNG], writes=[LNG])
                S.op("dve", lambda e: e.tensor_scalar(out=LNB[:, :], in0=LNB[:, :], scalar1=ALPHA, scalar2=None, op0=ALU.mult), reads=[LNB], writes=[LNB])

        def build_g1b(l, j0, s, pp_ts, scratch, dst=None):
            dst = G1B if dst is None else dst
            for c in range(8):
                S.op("dve", lambda e, c=c: e.tensor_scalar(out=scratch[:, c, :], in0=ident, scalar1=MOD[:, l, j0 + c, s:s + 1], scalar2=None, op0=ALU.mult),
                     reads=[CONST, MOD], writes=[scratch])
            for hf in range(2):
                pp_t = pp_ts[hf]
                for c4 in range(4):
                    S.op("pe", lambda e, hf=hf, c4=c4, pp_t=pp_t: e.matmul(pp_t[:, c4 * 128:(c4 + 1) * 128], lhsT=ones, rhs=scratch[:, hf * 4 + c4, :], start=True, stop=True),
                         reads=[CONST, scratch], writes=[pp_t])
                S.op("act", lambda e, hf=hf, pp_t=pp_t: e.activation(out=dst[:, hf * 512:(hf + 1) * 512], in_=pp_t[:, :], func=AF.Copy), reads=[pp_t], writes=[dst])

        def transpose_tile(t, l, jsc, jsh, s, tp_rot, dst_fn, dst_tile, f32_dst=None):
            for hlf in range(2):
                tp = tp_rot.next()
                for c4 in range(4):
                    c = hlf * 4 + c4
                    S.op("pe", lambda e, tp=tp, c=c, c4=c4: e.transpose(tp[:, c4 * 128:(c4 + 1) * 128], XT[t][:, c * 128:(c + 1) * 128], ident),
                         reads=[XT[t], CONST], writes=[tp])
                for c4 in range(4):
                    c = hlf * 4 + c4
                    o_ap = dst_fn(c) if f32_dst is None else f32_dst[:, c, :]
                    o_t = dst_tile if f32_dst is None else f32_dst
                    if c % 2 == 0:
                        S.op("dve", lambda e, tp=tp, c=c, c4=c4, o_ap=o_ap: e.tensor_scalar(
                            out=o_ap, in0=tp[:, c4 * 128:(c4 + 1) * 128], scalar1=MOD[:, l, jsc + c, s:s + 1], scalar2=MOD[:, l, jsh + c, s:s + 1],
                            op0=ALU.mult, op1=ALU.add), reads=[tp, MOD], writes=[o_t])
                    else:
                        S.op("act", lambda e, tp=tp, c=c, c4=c4, o_ap=o_ap: e.activation(
                            out=o_ap, in_=tp[:, c4 * 128:(c4 + 1) * 128], func=AF.Identity, scale=MOD[:, l, jsc + c, s:s + 1], bias=MOD[:, l, jsh + c, s:s + 1]),
                            reads=[tp, MOD], writes=[o_t])

        LN_STATS = S.sbuf("STATS", [128, NT, 2, 6], F32)
        LN_VE = S.sbuf("VE", [128, NT], F32)
        LN_NMR = S.sbuf("NMR", [128, NT], F32)

        def layer_norm_all():
            STATS, VE, NMR = LN_STATS, LN_VE, LN_NMR
            for t in range(NT):
                for h2 in range(2):
                    S.op("dve", lambda e, t=t, h2=h2: e.bn_stats(out=STATS[:, t, h2, :], in_=XT[t][:, h2 * 512:(h2 + 1) * 512]), reads=[XT[t]], writes=[STATS])
                S.op("dve", lambda e, t=t: e.bn_aggr(out=MV[:, t, :], in_=STATS[:, t, :, :].rearrange("p a b -> p (a b)")), reads=[STATS], writes=[MV])
            S.op("dve", lambda e: e.tensor_scalar(out=VE[:, :], in0=MV[:, :, 1], scalar1=LN_EPS, scalar2=None, op0=ALU.add), reads=[MV], writes=[VE])
            S.op("pool", lambda e: e.tensor_tensor(out=RSTD[:, :], in0=VE[:, :], in1=NHALF[:, 0:NT], op=ALU.pow), reads=[VE, NHALF], writes=[RSTD])
            S.op("dve", lambda e: e.scalar_tensor_tensor(out=NMR[:, :], in0=MV[:, :, 0], scalar=-1.0, in1=RSTD[:, :], op0=ALU.mult, op1=ALU.mult), reads=[MV, RSTD], writes=[NMR])
            for t in range(NT):
                S.op("act", lambda e, t=t: e.activation(out=XT[t][:, :], in_=XT[t][:, :], func=AF.Identity, scale=RSTD[:, t:t + 1], bias=NMR[:, t:t + 1]),
                     reads=[XT[t], NMR, RSTD], writes=[XT[t]])
                S.op("dve", lambda e, t=t: e.tensor_tensor(out=XT[t][:, :], in0=XT[t][:, :], in1=LNG[:, :], op=ALU.mult), reads=[XT[t], LNG], writes=[XT[t]])
                S.op("pool", lambda e, t=t: e.tensor_tensor(out=XT[t][:, :], in0=XT[t][:, :], in1=LNB[:, :], op=ALU.add), reads=[XT[t], LNB], writes=[XT[t]])

        LN_STt = [Tile(LN_STATS.ap[:, t], "LNST%d" % t) for t in range(NT)]
        MVt = [Tile(MV.ap[:, t, :], "MV%d" % t) for t in range(NT)]
        VEt = [Tile(LN_VE.ap[:, t:t + 1], "VE%d" % t) for t in range(NT)]
        RSTDt = [Tile(RSTD.ap[:, t:t + 1], "RSTD%d" % t) for t in range(NT)]
        NMRt = [Tile(LN_NMR.ap[:, t:t + 1], "NMR%d" % t) for t in range(NT)]

        class LNPipe:
            def __init__(self, act_stats=False, junk=None):
                self.q = []
                self.act_stats = act_stats
                self.junk = junk

            def s1(self, t):
                st_, mv, ve, rstd = LN_STt[t], MVt[t], VEt[t], RSTDt[t]
                if self.act_stats:
                    junk = self.junk
                    S.op("act", lambda e: e.activation(out=junk[:, :], in_=XT[t][:, :], func=AF.Copy, accum_out=st_[:, 0, 0:1]), reads=[XT[t]], writes=[junk, st_])
                    S.op("act", lambda e: e.activation(out=junk[:, :], in_=XT[t][:, :], func=AF.Square, accum_out=st_[:, 0, 1:2]), reads=[XT[t]], writes=[junk, st_])
                    S.op("dve", lambda e: e.tensor_scalar(out=mv[:, 0:1], in0=st_[:, 0, 0:1], scalar1=1.0 / D, scalar2=None, op0=ALU.mult), reads=[st_], writes=[mv])
                    S.op("dve", lambda e: e.tensor_tensor(out=mv[:, 1:2], in0=mv[:, 0:1], in1=mv[:, 0:1], op=ALU.mult), reads=[mv], writes=[mv])
                    S.op("dve", lambda e: e.scalar_tensor_tensor(out=ve[:, :], in0=st_[:, 0, 1:2], scalar=1.0 / D, in1=mv[:, 1:2], op0=ALU.mult, op1=ALU.subtract), reads=[st_, mv], writes=[ve])
                    S.op("dve", lambda e: e.tensor_scalar(out=ve[:, :], in0=ve[:, :], scalar1=LN_EPS, scalar2=None, op0=ALU.add), reads=[ve], writes=[ve])
                else:
                    for h2 in range(2):
                        S.op("dve", lambda e, h2=h2: e.bn_stats(out=st_[:, h2, :], in_=XT[t][:, h2 * 512:(h2 + 1) * 512]), reads=[XT[t]], writes=[st_])
                    S.op("dve", lambda e: e.bn_aggr(out=mv[:, :], in_=st_[:, :, :].rearrange("p a b -> p (a b)")), reads=[st_], writes=[mv])
                    S.op("dve", lambda e: e.tensor_scalar(out=ve[:, :], in0=mv[:, 1:2], scalar1=LN_EPS, scalar2=None, op0=ALU.add), reads=[mv], writes=[ve])
                S.op("pool", lambda e: e.tensor_tensor(out=rstd[:, :], in0=ve[:, :], in1=NHALF[:, 0:1], op=ALU.pow), reads=[ve, NHALF], writes=[rstd])

            def s2(self, t):
                mv, rstd, nmr = MVt[t], RSTDt[t], NMRt[t]
                S.op("dve", lambda e: e.scalar_tensor_tensor(out=nmr[:, :], in0=mv[:, 0:1], scalar=-1.0, in1=rstd[:, :], op0=ALU.mult, op1=ALU.mult), reads=[mv, rstd], writes=[nmr])
                S.op("act", lambda e: e.activation(out=XT[t][:, :], in_=XT[t][:, :], func=AF.Identity, scale=rstd[:, :], bias=nmr[:, :]), reads=[XT[t], nmr, rstd], writes=[XT[t]])

            def s3(self, t):
                S.op("dve", lambda e: e.tensor_tensor(out=XT[t][:, :], in0=XT[t][:, :], in1=LNG[:, :], op=ALU.mult), reads=[XT[t], LNG], writes=[XT[t]])
                S.op("pool", lambda e: e.tensor_tensor(out=XT[t][:, :], in0=XT[t][:, :], in1=LNB[:, :], op=ALU.add), reads=[XT[t], LNB], writes=[XT[t]])

            def push(self, t):
                self.q.append([t, 0])
                self.step()

            def step(self):
                for ent in list(self.q):
                    if ent[1] == 0:
                        self.s1(ent[0])
                    elif ent[1] == 1:
                        self.s2(ent[0])
                    else:
                        self.s3(ent[0])
                    ent[1] += 1
                self.q = [en for en in self.q if en[1] < 3]

            def flush(self):
                while self.q:
                    self.step()

        NV, NJ = 48, 96
        BIG = 65536.0
        BIGW = 8192.0

        def moe_phase(l, s, last):
            jsh, jsc, jg = 24, 32, 40
            wall_d = wall_ds[l]
            IOA = bass.IndirectOffsetOnAxis
            with ExitStack() as ph:
                LG = S.sbuf("LG", [128, NT, 36], F32, st=ph)
                P12 = S.sbuf("P12", [128, 2, NT], F32, st=ph)
                IDXY = S.sbuf("IDXY", [128, NJ], I32, st=ph)
                IDXG = S.sbuf("IDXG", [128, NJ], I32, st=ph)
                WIDX = S.sbuf("WIDX", [128, NV], I32, st=ph)
                NW, NH = 3, 4

                with ExitStack() as ph1:
                    WR = S.sbuf("WR", [128, 8, 36], F32, st=ph1)
                    RB = S.sbuf("RB", [128, 36], F32, st=ph1)
                    H32 = Rot([S.sbuf("H32_%d" % i, [128, 8, 128], F32, st=ph1) for i in range(3)])
                    SCB = S.sbuf("SCB", [128, D], F32, st=ph1)
                    SHB = S.sbuf("SHB", [128, D], F32, st=ph1)
                    TM32 = Rot([S.sbuf("TM32_%d" % i, [128, D], F32, st=ph1) for i in range(2)])
                    HB = Rot([S.sbuf("HB_%d" % i, [128, D], BF16, st=ph1) for i in range(2)])
                    tp_rot = Rot([S.psum("tp%d" % i, [128, 512], F32, st=ph1) for i in range(4)])
                    lg_rot = Rot([S.psum("lgp%d" % i, [128, 512], F32, st=ph1) for i in range(2)])
                    scrs = [S.sbuf("scr%d" % i, [128, 8, 128], F32, st=ph1) for i in range(2)]
                    S.dma("sp", lambda e: [e.dma_start(out=WR[:, :, :], in_=wr_d[l].rearrange("(c p) n -> p c n", p=128)),
                                           e.dma_start(out=RB[:, :], in_=br_d[l:l + 1, :].partition_broadcast(128))],
                          reads=[DR], writes=[WR, RB], sync=WR, n=2)
                    build_g1b(l, jsc, s, tp_rot.tiles[0:2], scrs[0], dst=SCB)
                    build_g1b(l, jsh, s, tp_rot.tiles[2:4], scrs[1], dst=SHB)
                    build_g1b(l, jg, s, lg_rot.tiles, scrs[0])
                    load_ln(l, 1, not last)
                    h32s = {}

                    def router(t):
                        h32 = h32s.pop(t)
                        lgp = lg_rot.next()
                        for c in range(8):
                            S.op("pe", lambda e, c=c: e.matmul(lgp[:, 0:36], lhsT=h32[:, c, :], rhs=WR[:, c, :], start=(c == 0), stop=(c == 7)),
                                 reads=[h32, WR], writes=[lgp])
                        S.op("dve", lambda e: e.tensor_tensor(out=LG[:, t, :], in0=lgp[:, 0:36], in1=RB[:, :], op=ALU.add), reads=[lgp, RB], writes=[LG])

                    for t in range(NT):
                        h32 = H32.next()
                        h32s[t] = h32
                        transpose_tile(t, l, jsc, jsh, s, tp_rot, None, None, f32_dst=h32)
                        tm = TM32.next()
                        hb = HB.next()
                        S.op("dve", lambda e, t=t, tm=tm: e.tensor_tensor(out=tm[:, :], in0=XT[t][:, :], in1=SCB[:, :], op=ALU.mult), reads=[XT[t], SCB], writes=[tm])
                        S.op("pool", lambda e, tm=tm, hb=hb: e.tensor_tensor(out=hb[:, :], in0=tm[:, :], in1=SHB[:, :], op=ALU.add), reads=[tm, SHB], writes=[hb])
                        S.dma("sp", lambda e, t=t, hb=hb: e.dma_start(out=htok_d[t * 128:(t + 1) * 128, :], in_=hb[:, :]), reads=[hb], writes=[], sync=hb)
                        if t >= 1:
                            router(t - 1)
                    router(NT - 1)
                    S.fence()
                with ExitStack() as ph1:
                    def sb(name, shape, dt=F32):
                        return S.sbuf(name, shape, dt, st=ph1)
                    C2 = sb("C2", [128, NC2])
                    S.dma("sp", lambda e: e.dma_start(out=C2[:, :], in_=const2_d[:, :]), reads=[DR], writes=[C2], sync=C2)
                    GMAX = sb("GMAX", [128, NT]); DG_ = sb("DGL", [128, NT, 4]); GE = sb("GE", [128, NT, 4]); GS = sb("GS", [128, NT])
                    GW = sb("GW", [128, NT]); PEN = sb("PEN", [128, NT, 4]); EM = sb("EM", [128, NT, 32]); M1 = sb("M1", [128, NT])
                    OH1 = sb("OH1", [128, NT, 32]); EM2 = sb("EM2", [128, NT, 32]); M2 = sb("M2", [128, NT]); OH2 = sb("OH2", [128, NT, 32])
                    DM = sb("DM", [128, NT]); P1 = sb("P1", [128, NT]); P2 = sb("P2", [128, NT])
                    GL = LG[:, :, 0:4]
                    EL = LG[:, :, 4:36]
                    V = lambda f, r, w: S.op("dve", f, reads=r, writes=w)
                    V(lambda e: e.tensor_reduce(out=GMAX[:, :], in_=GL, axis=AX.X, op=ALU.max), [LG], [GMAX])
                    V(lambda e: e.tensor_tensor(out=DG_[:, :, :], in0=GL, in1=GMAX[:, :].unsqueeze(2).to_broadcast([128, NT, 4]), op=ALU.subtract), [LG, GMAX], [DG_])
                    S.op("act", lambda e: e.activation(out=GE[:, :, :], in_=DG_[:, :, :], func=AF.Exp), reads=[DG_], writes=[GE])
                    V(lambda e: e.tensor_reduce(out=GS[:, :], in_=GE[:, :, :], axis=AX.X, op=ALU.add), [GE], [GS])
                    V(lambda e: e.reciprocal(out=GW[:, :], in_=GS[:, :]), [GS], [GW])
                    V(lambda e: e.tensor_scalar(out=PEN[:, :, :], in0=DG_[:, :, :], scalar1=0.0, scalar2=-1e30, op0=ALU.is_lt, op1=ALU.mult), [DG_], [PEN])
                    V(lambda e: e.tensor_tensor(out=EM[:, :, :].rearrange("p t (g j) -> p t g j", g=4), in0=EL.rearrange("p t (g j) -> p t g j", g=4),
                                                in1=PEN[:, :, :].unsqueeze(3).to_broadcast([128, NT, 4, 8]), op=ALU.add), [LG, PEN], [EM])
                    V(lambda e: e.tensor_reduce(out=M1[:, :], in_=EM[:, :, :], axis=AX.X, op=ALU.max), [EM], [M1])
                    V(lambda e: e.tensor_tensor(out=OH1[:, :, :], in0=EM[:, :, :], in1=M1[:, :].unsqueeze(2).to_broadcast([128, NT, 32]), op=ALU.is_equal), [EM, M1], [OH1])
                    V(lambda e: e.scalar_tensor_tensor(out=EM2[:, :, :], in0=OH1[:, :, :], scalar=-1e30, in1=EM[:, :, :], op0=ALU.mult, op1=ALU.add), [OH1, EM], [EM2])
                    V(lambda e: e.tensor_reduce(out=M2[:, :], in_=EM2[:, :, :], axis=AX.X, op=ALU.max), [EM2], [M2])
                    V(lambda e: e.tensor_tensor(out=OH2[:, :, :], in0=EM2[:, :, :], in1=M2[:, :].unsqueeze(2).to_broadcast([128, NT, 32]), op=ALU.is_equal), [EM2, M2], [OH2])
                    V(lambda e: e.tensor_tensor(out=DM[:, :], in0=M2[:, :], in1=M1[:, :], op=ALU.subtract), [M1, M2], [DM])
                    S.op("act", lambda e: e.activation(out=DM[:, :], in_=DM[:, :], func=AF.Exp), reads=[DM], writes=[DM])
                    V(lambda e: e.tensor_scalar(out=DM[:, :], in0=DM[:, :], scalar1=1.0, scalar2=None, op0=ALU.add), [DM], [DM])
                    V(lambda e: e.reciprocal(out=P1[:, :], in_=DM[:, :]), [DM], [P1])
                    V(lambda e: e.tensor_scalar(out=P2[:, :], in0=P1[:, :], scalar1=-1.0, scalar2=1.0, op0=ALU.mult, op1=ALU.add), [P1], [P2])
                    V(lambda e: e.tensor_tensor(out=P12[:, 0, :], in0=P1[:, :], in1=GW[:, :], op=ALU.mult), [P1, GW], [P12])
                    V(lambda e: e.tensor_tensor(out=P12[:, 1, :], in0=P2[:, :], in1=GW[:, :], op=ALU.mult), [P2, GW], [P12])
                    M = sb("M", [128, NT, 32]); MB = sb("MB", [128, NT, 32], BF16)
                    EXC = sb("EXC", [128, NT, 32]); TOT = sb("TOT", [128, NT, 32]); OFFS = sb("OFFS", [128, NT + 1, 32])
                    ip = S.psum("ip", [128, 512], F32, st=ph1)
                    tpp = S.psum("tpp", [128, 512], F32, st=ph1)
                    V(lambda e: e.tensor_tensor(out=M[:, :, :], in0=OH1[:, :, :], in1=OH2[:, :, :], op=ALU.add), [OH1, OH2], [M])
                    V(lambda e: e.tensor_copy(out=MB[:, :, :], in_=M[:, :, :]), [M], [MB])
                    S.op("pe", lambda e: e.matmul(ip[:, :], lhsT=TRIB[:, :], rhs=MB[:, :, :].rearrange("p t e -> p (t e)"), start=True, stop=True), reads=[TRIB, MB], writes=[ip])
                    S.op("pe", lambda e: e.matmul(tpp[:, :], lhsT=ONEB[:, :], rhs=MB[:, :, :].rearrange("p t e -> p (t e)"), start=True, stop=True), reads=[ONEB, MB], writes=[tpp])
                    V(lambda e: e.tensor_tensor(out=EXC[:, :, :], in0=ip[:, :].rearrange("p (t e) -> p t e", e=32), in1=M[:, :, :], op=ALU.subtract), [ip, M], [EXC])
                    S.op("act", lambda e: e.activation(out=TOT[:, :, :], in_=tpp[:, :].rearrange("p (t e) -> p t e", e=32), func=AF.Copy), reads=[tpp], writes=[TOT])
                    V(lambda e: e.memset(OFFS[:, 0, :], 0.0), [], [OFFS])
                    for t in range(1, NT + 1):
                        V(lambda e, t=t: e.tensor_tensor(out=OFFS[:, t, :], in0=OFFS[:, t - 1, :], in1=TOT[:, t - 1, :], op=ALU.add), [OFFS, TOT], [OFFS])
                    TMP8 = sb("TMP8", [128, 32, 8]); NVv = sb("NVv", [128, 32]); TMP32 = sb("TMP32", [128, 32, 32]); VB = sb("VB", [128, 32]); VE = sb("VE", [128, 32]); BASE = sb("BASE", [128, 32])
                    V(lambda e: e.tensor_tensor(out=TMP8[:, :, :], in0=C2[:, C2_THR:C2_THR + 8].unsqueeze(1).to_broadcast([128, 32, 8]),
                                                in1=OFFS[:, NT, :].unsqueeze(2).to_broadcast([128, 32, 8]), op=ALU.is_lt), [C2, OFFS], [TMP8])
                    V(lambda e: e.tensor_reduce(out=NVv[:, :], in_=TMP8[:, :, :], axis=AX.X, op=ALU.add), [TMP8], [NVv])
                    V(lambda e: e.tensor_tensor(out=TMP32[:, :, :], in0=C2[:, C2_LT:C2_LT + 1024].rearrange("p (a b) -> p a b", a=32),
                                                in1=NVv[:, :].unsqueeze(1).to_broadcast([128, 32, 32]), op=ALU.mult), [C2, NVv], [TMP32])
                    V(lambda e: e.tensor_reduce(out=VB[:, :], in_=TMP32[:, :, :], axis=AX.X, op=ALU.add), [TMP32], [VB])
                    V(lambda e: e.tensor_tensor(out=VE[:, :], in0=VB[:, :], in1=NVv[:, :], op=ALU.add), [VB, NVv], [VE])
                    V(lambda e: e.tensor_scalar(out=BASE[:, :], in0=VB[:, :], scalar1=256.0, scalar2=None, op0=ALU.mult), [VB], [BASE])
                    T1 = sb("T1r", [128, NT, 32]); POS = sb("POS", [128, 2, NT]); Q = sb("Q", [128, 2, NT]); QI = sb("QI", [128, 2, NT], I32); QF = sb("QF", [128, 2, NT])
                    CORR = sb("CORR", [128, 2, NT]); PM = sb("PM", [128, 2, NT]); DEST = sb("DEST", [128, 2, NT]); DESTI = sb("DESTI", [128, 2, NT], I32)
                    VALI = sb("VALI", [128, 2, NT], I32); FILLF = sb("FILLF", [128, NJ]); FILLI = sb("FILLI", [128, NJ], I32); IF = sb("IF", [128, NJ]); IGE = sb("IGE", [128, NJ])
                    V(lambda e: e.tensor_tensor(out=EXC[:, :, :], in0=EXC[:, :, :], in1=OFFS[:, 0:NT, :], op=ALU.add), [EXC, OFFS], [EXC])
                    V(lambda e: e.tensor_tensor(out=EXC[:, :, :], in0=EXC[:, :, :], in1=BASE[:, :].unsqueeze(1).to_broadcast([128, NT, 32]), op=ALU.add), [EXC, BASE], [EXC])
                    for k, OH in ((0, OH1), (1, OH2)):
                        V(lambda e, OH=OH: e.tensor_tensor(out=T1[:, :, :], in0=OH[:, :, :], in1=EXC[:, :, :], op=ALU.mult), [OH, EXC], [T1])
                        V(lambda e, k=k: e.tensor_reduce(out=POS[:, k, :], in_=T1[:, :, :], axis=AX.X, op=ALU.add), [T1], [POS])
                    V(lambda e: e.tensor_scalar(out=Q[:, :, :], in0=POS[:, :, :], scalar1=1.0 / 128, scalar2=None, op0=ALU.mult), [POS], [Q])
                    V(lambda e: e.tensor_copy(out=QI[:, :, :], in_=Q[:, :, :]), [Q], [QI])
                    V(lambda e: e.tensor_copy(out=QF[:, :, :], in_=QI[:, :, :]), [QI], [QF])
                    V(lambda e: e.tensor_tensor(out=CORR[:, :, :], in0=QF[:, :, :], in1=Q[:, :, :], op=ALU.is_gt), [QF, Q], [CORR])
                    V(lambda e: e.tensor_tensor(out=QF[:, :, :], in0=QF[:, :, :], in1=CORR[:, :, :], op=ALU.subtract), [QF, CORR], [QF])
                    V(lambda e: e.scalar_tensor_tensor(out=PM[:, :, :], in0=QF[:, :, :], scalar=-128.0, in1=POS[:, :, :], op0=ALU.mult, op1=ALU.add), [QF, POS], [PM])
                    V(lambda e: e.scalar_tensor_tensor(out=DEST[:, :, :], in0=PM[:, :, :], scalar=float(NJ), in1=QF[:, :, :], op0=ALU.mult, op1=ALU.add), [PM, QF], [DEST])
                    V(lambda e: e.tensor_copy(out=DESTI[:, :, :], in_=DEST[:, :, :]), [DEST], [DESTI])
                    V(lambda e: e.tensor_copy(out=VALI[:, :, :], in_=C2[:, C2_VAL:C2_VAL + 32].rearrange("p (k t) -> p k t", k=2)), [C2], [VALI])
                    V(lambda e: e.memset(FILLF[:, :], BIG), [], [FILLF])
                    V(lambda e: e.tensor_copy(out=FILLI[:, :], in_=FILLF[:, :]), [FILLF], [FILLI])
                    tab_v = tab_d.rearrange("(p j) o -> p (j o)", p=128)
                    S.dma("sp", lambda e: e.dma_start(out=tab_v, in_=FILLI[:, :]), reads=[FILLI], writes=[TABF], sync=TABF)
                    last_sc = None
                    for k in range(2):
                        for t in range(NT):
                            last_sc = S.dma("pool", lambda e, k=k, t=t: e.indirect_dma_start(out=tab_d[:, :], out_offset=IOA(ap=DESTI[:, k, t:t + 1], axis=0), in_=VALI[:, k, t:t + 1], in_offset=None,
                                                                                            bounds_check=BC[NJ * 128 - 1], oob_is_err=False), reads=[DESTI, VALI, TABF], writes=[], sync=TABS)
                    VA = sb("VA", [128, NV, 32]); VBm = sb("VBm", [128, NV, 32]); WF = sb("WF", [128, NV])
                    vrow = C2[:, C2_VROW:C2_VROW + NV].unsqueeze(2).to_broadcast([128, NV, 32])
                    V(lambda e: e.tensor_tensor(out=VA[:, :, :], in0=vrow, in1=VB[:, :].unsqueeze(1).to_broadcast([128, NV, 32]), op=ALU.is_ge), [C2, VB], [VA])
                    V(lambda e: e.tensor_tensor(out=VBm[:, :, :], in0=vrow, in1=VE[:, :].unsqueeze(1).to_broadcast([128, NV, 32]), op=ALU.is_lt), [C2, VE], [VBm])
                    V(lambda e: e.tensor_tensor(out=VA[:, :, :], in0=VA[:, :, :], in1=VBm[:, :, :], op=ALU.mult), [VA, VBm], [VA])
                    V(lambda e: e.tensor_tensor(out=VA[:, :, :], in0=VA[:, :, :], in1=C2[:, C2_EROW:C2_EROW + 32].unsqueeze(1).to_broadcast([128, NV, 32]), op=ALU.mult), [VA, C2], [VA])
                    V(lambda e: e.tensor_reduce(out=WF[:, :], in_=VA[:, :, :], axis=AX.X, op=ALU.add), [VA], [WF])
                    V(lambda e: e.tensor_scalar(out=WF[:, :], in0=WF[:, :], scalar1=BIGW, scalar2=None, op0=ALU.add), [WF], [WF])
                    V(lambda e: e.tensor_copy(out=WIDX[:, :], in_=WF[:, :]), [WF], [WIDX])
                    TABS.w = last_sc
                    S.dma("sp", lambda e: e.dma_start(out=IDXY[:, :], in_=tab_v), reads=[TABS], writes=[IDXY], sync=IDXY)
                    TABS.w = None
                    V(lambda e: e.tensor_copy(out=IF[:, :], in_=IDXY[:, :]), [IDXY], [IF])
                    V(lambda e: e.tensor_single_scalar(out=IGE[:, :], in_=IF[:, :], scalar=2048.0, op=ALU.is_ge), [IF], [IGE])
                    V(lambda e: e.scalar_tensor_tensor(out=IF[:, :], in0=IGE[:, :], scalar=-2048.0, in1=IF[:, :], op0=ALU.mult, op1=ALU.add), [IGE, IF], [IF])
                    V(lambda e: e.tensor_copy(out=IDXG[:, :], in_=IF[:, :]), [IF], [IDXG])
                    S.fence()
                with ExitStack() as ph2:
                    W = [S.sbuf("W%d" % i, [128, 6144], BF16, st=ph2) for i in range(NW)]
                    HS = [S.sbuf("HS%d" % i, [128, D], BF16, st=ph2) for i in range(NH)]
                    for hs in HS:
                        S.op("dve", lambda e, hs=hs: e.memset(hs[:, :], 0.0), writes=[hs])

                    def issue_w(v):
                        w = W[v % NW]
                        S.dma("pool", lambda e: e.indirect_dma_start(out=w[:, :], out_offset=None, in_=wall_d[:, :], in_offset=IOA(ap=WIDX[:, v:v + 1], axis=0),
                                                                     bounds_check=BC[4095], oob_is_err=False), reads=[DR, WIDX, w], writes=[w], sync=w)

                    for v in range(NW):
                        issue_w(v)
                    HST = Rot([S.sbuf("HST%d" % i, [128, 8, 128], BF16, st=ph2) for i in range(2)])
                    SG = Rot([S.sbuf("SG%d" % i, [128, 256], F32, st=ph2) for i in range(2)])
                    HID = Rot([S.sbuf("HID%d" % i, [128, 256], BF16, st=ph2) for i in range(3)])
                    HIDT = Rot([S.sbuf("HIDT%d" % i, [128, 2, 128], BF16, st=ph2) for i in range(3)])
                    YSB = Rot([S.sbuf("YSB%d" % i, [128, D], F32, st=ph2) for i in range(3)])
                    xT_rot = Rot([S.psum("xTp%d" % i, [128, 8, 128], BF16, st=ph2) for i in range(2)])
                    gu_rot = Rot([S.psum("gu%d" % i, [128, 512], F32, st=ph2) for i in range(2)])
                    hT_rot = Rot([S.psum("hTp%d" % i, [128, 2, 128], BF16, st=ph2) for i in range(1)])
                    y_rot = Rot([S.psum("yp%d" % i, [128, 512], F32, st=ph2) for i in range(3)])
                    state = {}

                    def issue_gather(j):
                        hs = HS[j % NH]
                        S.dma("pool", lambda e: e.indirect_dma_start(out=hs[:, :], out_offset=None, in_=htok_d[:, :], in_offset=IOA(ap=IDXG[:, j:j + 1], axis=0),
                                                                     bounds_check=BC[S_LEN - 1], oob_is_err=False), reads=[DR, IDXG, hs], writes=[hs], sync=hs)

                    def do_T8(j):
                        hs = HS[j % NH]
                        xp = xT_rot.next()
                        for c in range(8):
                            S.op("pe", lambda e, c=c: e.transpose(xp[:, c, :], hs[:, c * 128:(c + 1) * 128], IDB[:, :]), reads=[hs, IDB], writes=[xp])
                        hst = HST.next()
                        S.op("act", lambda e: e.activation(out=hst[:, :, :], in_=xp[:, :, :], func=AF.Copy), reads=[xp], writes=[hst])
                        state[j] = [hst, None, None]

                    def do_GU(j):
                        w = W[(j // 2) % NW]
                        hst = state[j][0]
                        gp = gu_rot.next()
                        for c in range(8):
                            S.op("pe", lambda e, c=c: e.matmul(gp[:, :], lhsT=hst[:, c, :], rhs=w[:, c * 512:(c + 1) * 512], start=(c == 0), stop=(c == 7)),
                                 reads=[hst, w], writes=[gp])
                        sg = SG.next()
                        hid = HID.next()
                        S.op("act", lambda e: e.activation(out=sg[:, :], in_=gp[:, 0:256], func=AF.Silu), reads=[gp], writes=[sg])
                        S.op("dve", lambda e: e.tensor_tensor(out=hid[:, :], in0=gp[:, 256:512], in1=sg[:, :], op=ALU.mult), reads=[gp, sg], writes=[hid])
                        state[j][1] = hid

                    def do_HT(j):
                        hid = state[j][1]
                        hp = hT_rot.next()
                        for c in range(2):
                            S.op("pe", lambda e, c=c: e.transpose(hp[:, c, :], hid[:, c * 128:(c + 1) * 128], IDB[:, :]), reads=[hid, IDB], writes=[hp])
                        hT = HIDT.next()
                        S.op("act", lambda e: e.activation(out=hT[:, :, :], in_=hp[:, :, :], func=AF.Copy), reads=[hp], writes=[hT])
                        state[j][2] = hT

                    def do_D(j):
                        w = W[(j // 2) % NW]
                        hT = state.pop(j)[2]
                        ysb = YSB.next()
                        for hf in range(2):
                            yp = y_rot.next()
                            for c in range(2):
                                S.op("pe", lambda e, hf=hf, c=c, yp=yp: e.matmul(yp[:, :], lhsT=hT[:, c, :], rhs=w[:, 4096 + c * 1024 + hf * 512:4096 + c * 1024 + (hf + 1) * 512],
                                                                                 start=(c == 0), stop=(c == 1)), reads=[hT, w], writes=[yp])
                            S.op("dve", lambda e, yp=yp, hf=hf: e.tensor_tensor(out=ysb[:, hf * 512:(hf + 1) * 512], in0=yp[:, :], in1=G1B[:, hf * 512:(hf + 1) * 512], op=ALU.mult),
                                 reads=[yp, G1B], writes=[ysb])
                        S.dma("pool", lambda e: e.indirect_dma_start(out=ys_d[:, :], out_offset=IOA(ap=IDXY[:, j:j + 1], axis=0), in_=ysb[:, :], in_offset=None,
                                                                     bounds_check=BC[2 * S_LEN - 1], oob_is_err=False), reads=[ysb, IDXY], writes=[], sync=ysb)

                    for j in range(NH):
                        issue_gather(j)
                    do_T8(0)
                    issue_gather(NH)
                    for i in range(NJ + 2):
                        if i + 1 < NJ:
                            do_T8(i + 1)
                            if i + 1 + NH < NJ:
                                issue_gather(i + 1 + NH)
                        if i < NJ:
                            do_GU(i)
                        if 1 <= i <= NJ:
                            do_HT(i - 1)
                        if 2 <= i:
                            do_D(i - 2)
                            if (i - 2) % 2 == 1:
                                nv_ = (i - 2) // 2 + NW
                                if nv_ < NV:
                                    issue_w(nv_)
                    S.fence()
                with ExitStack() as ph3:
                    Y0 = Rot([S.sbuf("Y0_%d" % i, [128, D], F32, st=ph3) for i in range(3)])
                    Y1 = Rot([S.sbuf("Y1_%d" % i, [128, D], F32, st=ph3) for i in range(3)])
                    LJ = S.sbuf("LNJ", [128, D], BF16, st=ph3)
                    lnp = LNPipe(act_stats=True, junk=LJ)
                    for t in range(NT):
                        y0 = Y0.next(); y1 = Y1.next()
                        S.dma("sp", lambda e, t=t, y0=y0: e.dma_start(out=y0[:, :], in_=ys_d[t * 128:(t + 1) * 128, :]), reads=[DR], writes=[y0], sync=y0)
                        S.dma("sp", lambda e, t=t, y1=y1: e.dma_start(out=y1[:, :], in_=ys_d[S_LEN + t * 128:S_LEN + (t + 1) * 128, :]), reads=[DR], writes=[y1], sync=y1)
                        S.op("dve", lambda e, t=t, y0=y0: e.scalar_tensor_tensor(out=XT[t][:, :], in0=y0[:, :], scalar=P12[:, 0, t:t + 1], in1=XT[t][:, :], op0=ALU.mult, op1=ALU.add),
                             reads=[y0, P12, XT[t]], writes=[XT[t]])
                        S.op("dve", lambda e, t=t, y1=y1: e.scalar_tensor_tensor(out=XT[t][:, :], in0=y1[:, :], scalar=P12[:, 1, t:t + 1], in1=XT[t][:, :], op0=ALU.mult, op1=ALU.add),
                             reads=[y1, P12, XT[t]], writes=[XT[t]])
                        lnp.push(t)
                    lnp.flush()
                    S.fence()

        def l0_mixer(s):
            l = 0
            jsh, jsc, jg = 0, 8, 16
            CATA = [Tile(HT.ap[:, 0:4, B * 512:(B + 1) * 512], "CATA%d" % B) for B in range(4)]
            CATB = [Tile(HT.ap[:, 4:8, t * 128:(t + 1) * 128], "CATB%d" % t) for t in range(NT)]
            load_ln(l, 0, True)
            with ExitStack() as p12:
                HCT = S.sbuf("HCT", [128, 4, 2080], BF16, st=p12)
                S.op("pool", lambda e: e.memset(HCT[:, :, 0:30], 0.0), writes=[HCT])
                with ExitStack() as p1:
                    WINC = S.sbuf("WINC", [128, 8, 1024], BF16, st=p1)
                    S.dma("pool", lambda e: e.dma_start(out=WINC[:, :, :], in_=ab_w_in_d.rearrange("(c p) n -> p c n", p=128)[:, :, 0:1024]),
                          reads=[DR], writes=[WINC], sync=WINC)
                    HTB = Rot([S.sbuf("HTB%d" % i, [128, 8, 512], BF16, st=p1) for i in range(2)])
                    SIG = Rot([S.sbuf("SIG%d" % i, [128, 512], F32, st=p1) for i in range(2)])
                    tp_rot = Rot([S.psum("tp%d" % i, [128, 512], F32, st=p1) for i in range(4)])
                    ag_rot = Rot([S.psum("ag%d" % i, [128, 512], F32, st=p1) for i in range(4)])
                    scr = S.sbuf("scr", [128, 8, 128], F32, st=p1)
                    build_g1b(l, jg, s, ag_rot.tiles[0:2], scr)
                    for B in range(4):
                        htb = HTB.next()
                        for i in range(4):
                            transpose_tile(4 * B + i, l, jsc, jsh, s, tp_rot, lambda c, i=i, htb=htb: htb[:, c, i * 128:(i + 1) * 128], htb)
                        for cc in range(4):
                            a_p = ag_rot.next()
                            g_p = ag_rot.next()
                            for kc in range(8):
                                S.op("pe", lambda e, kc=kc, cc=cc, a_p=a_p, htb=htb: e.matmul(a_p[:, :], lhsT=WINC[:, kc, cc * 128:(cc + 1) * 128], rhs=htb[:, kc, :], start=(kc == 0), stop=(kc == 7)),
                                     reads=[WINC, htb], writes=[a_p])
                            for kc in range(8):
                                S.op("pe", lambda e, kc=kc, cc=cc, g_p=g_p, htb=htb: e.matmul(g_p[:, :], lhsT=WINC[:, kc, 512 + cc * 128:512 + (cc + 1) * 128], rhs=htb[:, kc, :], start=(kc == 0), stop=(kc == 7)),
                                     reads=[WINC, htb], writes=[g_p])
                            sig = SIG.next()
                            S.op("act", lambda e, sig=sig, g_p=g_p: e.activation(out=sig[:, :], in_=g_p[:, :], func=AF.Sigmoid), reads=[g_p], writes=[sig])
                            S.op("dve", lambda e, sig=sig, a_p=a_p, cc=cc, B=B: e.tensor_tensor(out=HCT[:, cc, 30 + B * 512:30 + (B + 1) * 512], in0=a_p[:, :], in1=sig[:, :], op=ALU.mult),
                                 reads=[a_p, sig], writes=[HCT])
                    S.fence()
                if stop_after == "l0a":
                    return
                with ExitStack() as p2:
                    CWS = S.sbuf("CWS", [32, 512], F32, st=p2)
                    CW = S.sbuf("CW", [128, 4, 31], F32, st=p2)
                    DG = [S.sbuf("DG%d" % cc, [128, 31, 128], BF16, st=p2) for cc in range(4)]
                    Y = S.sbuf("Y", [128, 4, 512], F32, st=p2)
                    YSQ = S.sbuf("YSQ", [128, 4, 512], F32, st=p2)
                    MEAN = S.sbuf("MEAN", [128, 512], F32, st=p2)
                    MSQ = S.sbuf("MSQ", [128, 512], F32, st=p2)
                    VAR = S.sbuf("VAR", [128, 512], F32, st=p2)
                    RS = S.sbuf("RS", [128, 512], F32, st=p2)
                    T1 = Rot([S.sbuf("T1_%d" % i, [128, 512], F32, st=p2) for i in range(2)])
                    y_ps = [S.psum("yps%d" % i, [128, 512], F32, st=p2) for i in range(4)]
                    st_rot = Rot([S.psum("stp%d" % i, [128, 512], F32, st=p2) for i in range(2)])
                    S.dma("sp", lambda e: e.dma_start(out=CWS[0:31, :], in_=conv_w_d[:, :]), reads=[DR], writes=[CWS], sync=CWS)
                    for cc in range(4):
                        pt = st_rot.next()
                        S.op("pe", lambda e, cc=cc, pt=pt: e.transpose(pt[:, 0:31], CWS[0:31, cc * 128:(cc + 1) * 128], CONST[0:31, C_ID:C_ID + 31]), reads=[CWS, CONST], writes=[pt])
                        S.op("dve", lambda e, cc=cc, pt=pt: e.tensor_copy(out=CW[:, cc, :], in_=pt[:, 0:31]), reads=[pt], writes=[CW])
                    for cc in range(4):
                        for j in range(31):
                            S.op("dve", lambda e, cc=cc, j=j: e.tensor_scalar(out=DG[cc][:, j, :], in0=ident, scalar1=CW[:, cc, j:j + 1], scalar2=None, op0=ALU.mult),
                                 reads=[CONST, CW], writes=[DG[cc]])
                    for B in range(4):
                        for cc in range(4):
                            for j in range(31):
                                S.op("pe", lambda e, cc=cc, j=j, B=B: e.matmul(y_ps[cc][:, :], lhsT=DG[cc][:, j, :], rhs=HCT[:, cc, B * 512 + j:B * 512 + j + 512], start=(j == 0), stop=(j == 30)),
                                     reads=[DG[cc], HCT], writes=[y_ps[cc]])
                            S.op("act", lambda e, cc=cc: e.activation(out=Y[:, cc, :], in_=y_ps[cc][:, :], func=AF.Identity, bias=PVT[:, cc:cc + 1]), reads=[y_ps[cc], PVT], writes=[Y])
                            S.op("act", lambda e, cc=cc: e.activation(out=YSQ[:, cc, :], in_=y_ps[cc][:, :], func=AF.Square, bias=PVT[:, cc:cc + 1]), reads=[y_ps[cc], PVT], writes=[YSQ])
                        mean_ps = st_rot.next()
                        msq_ps = st_rot.next()
                        for cc in range(4):
                            S.op("pe", lambda e, cc=cc, mean_ps=mean_ps: e.matmul(mean_ps[:, :], lhsT=ones, rhs=Y[:, cc, :], start=(cc == 0), stop=(cc == 3)), reads=[CONST, Y], writes=[mean_ps])
                        for cc in range(4):
                            S.op("pe", lambda e, cc=cc, msq_ps=msq_ps: e.matmul(msq_ps[:, :], lhsT=ones, rhs=YSQ[:, cc, :], start=(cc == 0), stop=(cc == 3)), reads=[CONST, YSQ], writes=[msq_ps])
                        S.op("act", lambda e, mean_ps=mean_ps: e.activation(out=MEAN[:, :], in_=mean_ps[:, :], func=AF.Copy, scale=1.0 / 512), reads=[mean_ps], writes=[MEAN])
                        S.op("dve", lambda e: e.tensor_tensor(out=MSQ[:, :], in0=MEAN[:, :], in1=MEAN[:, :], op=ALU.mult), reads=[MEAN], writes=[MSQ])
                        S.op("dve", lambda e, msq_ps=msq_ps: e.scalar_tensor_tensor(out=VAR[:, :], in0=msq_ps[:, :], scalar=1.0 / 512, in1=MSQ[:, :], op0=ALU.mult, op1=ALU.subtract),
                             reads=[msq_ps, MSQ], writes=[VAR])
                        S.op("act", lambda e: e.activation(out=VAR[:, :], in_=VAR[:, :], func=AF.Ln, bias=LN_EPS), reads=[VAR], writes=[VAR])
                        S.op("act", lambda e: e.activation(out=RS[:, :], in_=VAR[:, :], func=AF.Exp, scale=-0.5), reads=[VAR], writes=[RS])
                        for cc in range(4):
                            t1 = T1.next()
                            S.op("dve", lambda e, cc=cc, t1=t1: e.tensor_tensor(out=t1[:, :], in0=Y[:, cc, :], in1=MEAN[:, :], op=ALU.subtract), reads=[Y, MEAN], writes=[t1])
                            S.op("dve", lambda e, cc=cc, t1=t1: e.tensor_tensor(out=t1[:, :], in0=t1[:, :], in1=RS[:, :], op=ALU.mult), reads=[t1, RS], writes=[t1])
                            S.op("act", lambda e, cc=cc, t1=t1, B=B: e.activation(out=CATA[B][:, cc, :], in_=t1[:, :], func=AF.Silu, scale=PVT[:, 4 + cc:5 + cc], bias=PVT[:, 8 + cc:9 + cc]),
                                 reads=[t1, PVT], writes=[CATA[B]])
                    S.fence()
            if stop_after == "l0b":
                return
            with ExitStack() as p3:
                def sb(name, shape, dt=F32):
                    return S.sbuf(name, shape, dt, st=p3)
                WING = sb("WING", [128, 8, 1552], BF16)
                WOUT = sb("WOUT", [128, 8, D], BF16)
                S.dma("pool", lambda e: e.dma_start(out=WING[:, :, :], in_=ab_w_in_d.rearrange("(c p) n -> p c n", p=128)[:, :, 1024:2576]), reads=[DR], writes=[WING], sync=WING)
                S.dma("pool", lambda e: e.dma_start(out=WOUT[:, :, :], in_=ab_w_out_d.rearrange("(c p) n -> p c n", p=128)), reads=[DR], writes=[WOUT], sync=WOUT)
                S.op("pool", lambda e: e.tensor_tensor(out=WOUT[:, :, :], in0=WOUT[:, :, :], in1=G1B[:, :].unsqueeze(1).to_broadcast([128, 8, D]), op=ALU.mult), reads=[WOUT, G1B], writes=[WOUT])
                GWS = sb("GWS", [32, 256])
                GWB = sb("GWB", [32, 256], BF16)
                S.op("dve", lambda e: e.memset(GWS[:, :], 0.0), writes=[GWS])
                S.dma("sp", lambda e: [e.dma_start(out=GWS[0:16, :], in_=gate_w_d[:, :]), e.dma_start(out=GWS[16:17, :], in_=gate_b_d[:, :])], reads=[DR], writes=[GWS], sync=GWS, n=2)
                S.op("dve", lambda e: e.tensor_copy(out=GWB[:, :], in_=GWS[:, :]), reads=[GWS], writes=[GWB])
                GLT = Rot([sb("GLT%d" % i, [32, 512], BF16) for i in range(2)])
                for g_ in GLT.tiles:
                    S.op("dve", lambda e, g_=g_: e.memset(g_[:, :], 1.0), writes=[g_])
                NG = sb("NG", [128, 512])
                S.dma("sp", lambda e: e.dma_start(out=NG[:, :], in_=gnorm_d[0:1, :].partition_broadcast(128)), reads=[DR], writes=[NG], sync=NG)
                S32 = sb("S32", [128, 2, 128])
                SBF = sb("SBF", [128, 2, 128], BF16)
                S.op("dve", lambda e: e.memset(S32[:, :, :], 0.0), writes=[S32])
                S.op("dve", lambda e: e.memset(SBF[:, :, :], 0.0), writes=[SBF])
                S32h = [Tile(S32.ap[(h % 2) * 64:(h % 2) * 64 + 64, h // 2, :], "S32h%d" % h) for h in range(4)]
                SBFh = [Tile(SBF.ap[(h % 2) * 64:(h % 2) * 64 + 64, h // 2, :], "SBFh%d" % h) for h in range(4)]
                for h in range(4):
                    S32h[h].w = S32.w
                    SBFh[h].w = SBF.w
                HTB = sb("HTB", [128, 8, 512], BF16)
                QT = sb("QT", [128, 2, 512])
                KT = sb("KT", [128, 2, 512])
                R2 = lambda name, shape, dt=F32: Rot([sb("%s%d" % (name, i), shape, dt) for i in range(2)])
                VB = R2("VB", [128, 512], BF16); RG = R2("RG", [128, 512]); KTOK = R2("KTOK", [128, 256]); E1 = R2("E1", [128, 256]); SP = R2("SP", [128, 256])
                EB = R2("EB", [128, 2, 128]); ENB = R2("ENB", [128, 2, 128]); EDEC = R2("EDEC", [128, 256])
                QS = R2("QS", [128, 4, 128], BF16); KS = R2("KS", [128, 2, 128], BF16); KDEC = R2("KDEC", [128, 256], BF16)
                ATM = Rot([sb("ATM%d" % i, [128, 128], BF16) for i in range(4)])
                SS = R2("SS", [128, 4]); RS4 = R2("RS4", [128, 4]); YB = R2("YB", [128, 512], BF16)
                JUNK = sb("JUNK", [128, 128], BF16)
                for q_ in QS.tiles:
                    S.op("pool", lambda e, q_=q_: e.memset(q_[:, :, :], 0.0), writes=[q_])
                pool_rot = Rot([S.psum("pl%d" % i, [128, 512], F32, st=p3) for i in range(4)])
                o_ps = S.psum("o_ps", [128, 512], F32, st=p3)
                sn_ps = S.psum("sn_ps", [128, 4, 128], F32, st=p3)
                att_ps = S.psum("att_ps", [128, 4, 128], F32, st=p3)
                ybT_ps = S.psum("ybT", [128, 8, 128], BF16, st=p3)
                tp_rot = pool_rot

                def proj(dst_ps, cols, htb_ap, M=None):
                    pass

                gl_of = {}
                stA = {}

                def gla_prologue(B):
                    for i in range(4):
                        transpose_tile(4 * B + i, l, jsc, jsh, s, tp_rot, lambda c, i=i: HTB[:, c, i * 128:(i + 1) * 128], HTB)
                    for c2 in range(2):
                        for (dst, off) in ((QT, 0), (KT, 256)):
                            pp_ = pool_rot.next()
                            for kc in range(8):
                                S.op("pe", lambda e, kc=kc, c2=c2, off=off, pp_=pp_: e.matmul(pp_[:, :], lhsT=WING[:, kc, off + c2 * 128:off + (c2 + 1) * 128], rhs=HTB[:, kc, :], start=(kc == 0), stop=(kc == 7)),
                                     reads=[WING, HTB], writes=[pp_])
                            S.op("act", lambda e, dst=dst, c2=c2, pp_=pp_: e.activation(out=dst[:, c2, :], in_=pp_[:, :], func=AF.Copy), reads=[pp_], writes=[dst])
                    gl = GLT.next()
                    pp_ = pool_rot.next()
                    for kc in range(8):
                        S.op("pe", lambda e, kc=kc, pp_=pp_: e.matmul(pp_[0:16, :], lhsT=WING[:, kc, 1536:1552], rhs=HTB[:, kc, :], start=(kc == 0), stop=(kc == 7)), reads=[WING, HTB], writes=[pp_])
                    S.op("act", lambda e, gl=gl, pp_=pp_: e.activation(out=gl[0:16, :], in_=pp_[0:16, :], func=AF.Copy), reads=[pp_], writes=[gl])
                    gl_of[B] = gl

                def gla_a0(t):
                    B, i = t // 4, t % 4
                    gl = gl_of[B]
                    tc = slice(i * 128, (i + 1) * 128)
                    d = dict(tc=tc, vb=VB.next(), rg=RG.next(), ktok=KTOK.next(), e1=E1.next(), sp=SP.next(), eb=EB.next(), enb=ENB.next(), edec=EDEC.next(),
                             qs=QS.next(), ks=KS.next(), kdec=KDEC.next(), ss=SS.next(), rs4=RS4.next(), yb=YB.next())
                    stA[t] = d
                    e1, sp = d["e1"], d["sp"]
                    zp = pool_rot.next()
                    S.op("pe", lambda e: e.matmul(zp[:, 0:256], lhsT=gl[0:32, tc], rhs=GWB[0:32, :], start=True, stop=True), reads=[gl, GWB], writes=[zp])
                    S.op("act", lambda e: e.activation(out=e1[:, :], in_=zp[:, 0:256], func=AF.Exp, scale=-1.0), reads=[zp], writes=[e1])
                    S.op("act", lambda e: e.activation(out=sp[:, :], in_=e1[:, :], func=AF.Ln, bias=1.0), reads=[e1], writes=[sp])

                def gla_a1(t):
                    d = stA[t]
                    tc, vb, rg, ktok = d["tc"], d["vb"], d["rg"], d["ktok"]
                    kp = pool_rot.next()
                    for kc in range(8):
                        S.op("pe", lambda e, kc=kc: e.matmul(kp[:, 0:256], lhsT=HTB[:, kc, tc], rhs=WING[:, kc, 256:512], start=(kc == 0), stop=(kc == 7)), reads=[WING, HTB], writes=[kp])
                    S.op("dve", lambda e: e.tensor_copy(out=ktok[:, :], in_=kp[:, 0:256]), reads=[kp], writes=[ktok])
                    vp = pool_rot.next()
                    for kc in range(8):
                        S.op("pe", lambda e, kc=kc: e.matmul(vp[:, :], lhsT=HTB[:, kc, tc], rhs=WING[:, kc, 512:1024], start=(kc == 0), stop=(kc == 7)), reads=[WING, HTB], writes=[vp])
                    S.op("act", lambda e: e.activation(out=vb[:, :], in_=vp[:, :], func=AF.Copy), reads=[vp], writes=[vb])
                    rp = pool_rot.next()
                    for kc in range(8):
                        S.op("pe", lambda e, kc=kc: e.matmul(rp[:, :], lhsT=HTB[:, kc, tc], rhs=WING[:, kc, 1024:1536], start=(kc == 0), stop=(kc == 7)), reads=[WING, HTB], writes=[rp])
                    S.op("act", lambda e: e.activation(out=rg[:, :], in_=rp[:, :], func=AF.Silu), reads=[rp], writes=[rg])
                    S.op("pool", lambda e: e.tensor_tensor(out=rg[:, :], in0=rg[:, :], in1=NG[:, :], op=ALU.mult), reads=[rg, NG], writes=[rg])

                def gla_a2(t):
                    d = stA[t]
                    tc, sp, eb, enb, edec, qs, ks, kdec, ktok = d["tc"], d["sp"], d["eb"], d["enb"], d["edec"], d["qs"], d["ks"], d["kdec"], d["ktok"]
                    revp = pool_rot.next()
                    for c2 in range(2):
                        S.op("pe", lambda e, c2=c2: e.matmul(revp[:, 256 + c2 * 128:256 + (c2 + 1) * 128], lhsT=sp[:, c2 * 128:(c2 + 1) * 128], rhs=tri, start=True, stop=True), reads=[sp, CONST], writes=[revp])
                    S.op("pe", lambda e: e.matmul(revp[:, 0:256], lhsT=su, rhs=sp[:, :], start=True, stop=True), reads=[sp, CONST], writes=[revp])
                    S.op("act", lambda e: e.activation(out=eb[:, :, :], in_=revp[:, 256:512].rearrange("p (a b) -> p a b", a=2), func=AF.Exp, scale=-1.0 / 16), reads=[revp], writes=[eb])
                    S.op("act", lambda e: e.activation(out=enb[:, :, :], in_=revp[:, 256:512].rearrange("p (a b) -> p a b", a=2), func=AF.Exp, scale=1.0 / 16), reads=[revp], writes=[enb])
                    S.op("act", lambda e: e.activation(out=edec[:, :], in_=revp[:, 0:256], func=AF.Exp, scale=-1.0 / 16), reads=[revp], writes=[edec])
                    for h in range(4):
                        c2, hp = h // 2, (h % 2) * 64
                        S.op("dve", lambda e, h=h, c2=c2, hp=hp: e.scalar_tensor_tensor(out=qs[hp:hp + 64, h, :], in0=QT[hp:hp + 64, c2, tc], scalar=0.125, in1=eb[hp:hp + 64, c2, :], op0=ALU.mult, op1=ALU.mult),
                             reads=[QT, eb], writes=[qs])
                    S.op("dve", lambda e: e.tensor_tensor(out=ks[:, :, :], in0=KT[:, :, tc], in1=enb[:, :, :], op=ALU.mult), reads=[KT, enb], writes=[ks])
                    S.op("dve", lambda e: e.tensor_tensor(out=kdec[:, :], in0=ktok[:, :], in1=edec[:, :], op=ALU.mult), reads=[ktok, edec], writes=[kdec])

                def gla_b0(t):
                    d = stA[t]
                    qs, ks = d["qs"], d["ks"]
                    for h in range(4):
                        c2 = h // 2
                        S.op("pe", lambda e, c2=c2, h=h: e.matmul(att_ps[:, h, :], lhsT=ks[:, c2, :], rhs=qs[:, h, :], start=True, stop=True), reads=[ks, qs], writes=[att_ps])
                    atms = []
                    for h in range(4):
                        atm = ATM.next()
                        S.op("dve", lambda e, atm=atm, h=h: e.tensor_tensor(out=atm[:, :], in0=att_ps[:, h, :], in1=tri, op=ALU.mult), reads=[att_ps, CONST], writes=[atm])
                        atms.append(atm)
                    d["atms"] = atms

                def gla_b1(t):
                    d = stA[t]
                    vb, rg, eb, qs, kdec, ss, rs4, yb, atms = d["vb"], d["rg"], d["eb"], d["qs"], d["kdec"], d["ss"], d["rs4"], d["yb"], d["atms"]
                    for h in range(4):
                        c2 = h // 2
                        hc = slice(h * 128, (h + 1) * 128)
                        atm = atms[h]
                        S.op("pe", lambda e, c2=c2, hc=hc, h=h: e.matmul(o_ps[:, hc], lhsT=qs[:, h, :], rhs=SBF[:, c2, :], start=True, stop=False), reads=[qs, SBFh[2 * c2], SBFh[2 * c2 + 1]], writes=[o_ps])
                        S.op("pe", lambda e, atm=atm, hc=hc: e.matmul(o_ps[:, hc], lhsT=atm[:, :], rhs=vb[:, hc], start=False, stop=True), reads=[atm, vb], writes=[o_ps])
                    for h in range(4):
                        c2 = h // 2
                        hc = slice(h * 128, (h + 1) * 128)
                        S.op("pe", lambda e, c2=c2, hc=hc, h=h: e.matmul(sn_ps[:, h, :], lhsT=kdec[:, c2 * 128:(c2 + 1) * 128], rhs=vb[:, hc], start=True, stop=True), reads=[kdec, vb], writes=[sn_ps])
                    for h in range(4):
                        c2, hp = h // 2, (h % 2) * 64
                        S.op("dve", lambda e, c2=c2, hp=hp, h=h: e.scalar_tensor_tensor(out=S32[hp:hp + 64, c2, :], in0=S32[hp:hp + 64, c2, :], scalar=eb[hp:hp + 64, c2, 127:128],
                                                                                         in1=sn_ps[hp:hp + 64, h, :], op0=ALU.mult, op1=ALU.add), reads=[S32h[h], eb, sn_ps], writes=[S32h[h]])
                        S.op("pool", lambda e, c2=c2, hp=hp: e.tensor_copy(out=SBF[hp:hp + 64, c2, :], in_=S32[hp:hp + 64, c2, :]), reads=[S32h[h]], writes=[SBFh[h]])
                    for h in range(4):
                        hc = slice(h * 128, (h + 1) * 128)
                        S.op("act", lambda e, hc=hc, h=h: e.activation(out=JUNK[:, :], in_=o_ps[:, hc], func=AF.Square, accum_out=ss[:, h:h + 1]), reads=[o_ps], writes=[JUNK, ss])
                    S.op("dve", lambda e: e.tensor_scalar(out=ss[:, :], in0=ss[:, :], scalar1=1.0 / 128, scalar2=RMS_EPS, op0=ALU.mult, op1=ALU.add), reads=[ss], writes=[ss])
                    S.op("pool", lambda e: e.tensor_tensor(out=rs4[:, :], in0=ss[:, :], in1=NHALF[:, 0:4], op=ALU.pow), reads=[ss, NHALF], writes=[rs4])
                    for h in range(4):
                        hc = slice(h * 128, (h + 1) * 128)
                        S.op("dve", lambda e, hc=hc, h=h: e.scalar_tensor_tensor(out=yb[:, hc], in0=o_ps[:, hc], scalar=rs4[:, h:h + 1], in1=rg[:, hc], op0=ALU.mult, op1=ALU.mult),
                             reads=[o_ps, rs4, rg], writes=[yb])

                def gla_b2a(t):
                    yb = stA[t]["yb"]
                    for h in range(4):
                        S.op("pe", lambda e, h=h: e.transpose(ybT_ps[:, h, :], yb[:, h * 128:(h + 1) * 128], IDB[:, :]), reads=[yb, IDB], writes=[ybT_ps])
                    S.op("act", lambda e: e.activation(out=CATB[t][:, :, :], in_=ybT_ps[:, 0:4, :], func=AF.Copy), reads=[ybT_ps], writes=[CATB[t]])

                def gla_b2b(t):
                    stA.pop(t)
                    B = t // 4
                    for hf in range(2):
                        mp = pool_rot.next()
                        for kc in range(8):
                            S.op("pe", lambda e, kc=kc, hf=hf, mp=mp: e.matmul(mp[:, :], lhsT=HT[:, kc, t * 128:(t + 1) * 128], rhs=WOUT[:, kc, hf * 512:(hf + 1) * 512], start=(kc == 0), stop=(kc == 7)),
                                 reads=[CATA[B], CATB[t], WOUT], writes=[mp])
                        S.op("dve", lambda e, hf=hf, mp=mp: e.tensor_tensor(out=XT[t][:, hf * 512:(hf + 1) * 512], in0=mp[:, :], in1=XT[t][:, hf * 512:(hf + 1) * 512], op=ALU.add),
                             reads=[mp, XT[t]], writes=[XT[t]])
                    lnp.push(t)

                def gla_a_all(t):
                    gla_a0(t); gla_a1(t); gla_a2(t)

                lnp = LNPipe()
                gla_prologue(0)
                gla_a_all(0)
                for t in range(NT + 1):
                    nxt = t + 1 if t + 1 < NT else None
                    if nxt is not None and nxt % 4 == 0:
                        gla_prologue(nxt // 4)
                    if nxt is not None:
                        gla_a0(nxt)
                    if t < NT:
                        gla_b0(t)
                    if t >= 1:
                        gla_b2a(t - 1)
                    if nxt is not None:
                        gla_a1(nxt)
                        gla_a2(nxt)
                    if t < NT:
                        gla_b1(t)
                    if t >= 1:
                        gla_b2b(t - 1)
                lnp.flush()
                S.fence()

        def l1_mixer(s):
            l = 1
            jsh, jsc, jg = 0, 8, 16
            CQB = [Tile(HT.ap[:, 0:6, B * 512:(B + 1) * 512], "CQB%d" % B) for B in range(4)]
            load_ln(l, 0, True)
            with ExitStack() as pa:
                CS1 = S.sbuf("CS1", [64, S_LEN], F32, st=pa)
                CS2 = S.sbuf("CS2", [64, S_LEN], F32, st=pa)
                with ExitStack() as pr:
                    POSI = S.sbuf("POSI", [64, S_LEN], I32, st=pr)
                    ANG = S.sbuf("ANG", [64, S_LEN], F32, st=pr)
                    TT = S.sbuf("TT", [64, S_LEN], F32, st=pr)
                    TI = S.sbuf("TI", [64, S_LEN], I32, st=pr)
                    FR = S.sbuf("FR", [64, S_LEN], F32, st=pr)
                    MK = S.sbuf("MK", [64, S_LEN], F32, st=pr)
                    S.dma("sp", lambda e: e.dma_start(out=POSI[:, :], in_=pos_d[s:s + 1, :].partition_broadcast(64)), reads=[DR], writes=[POSI], sync=POSI)
                    S.op("dve", lambda e: e.tensor_copy(out=ANG[:, :], in_=POSI[:, :]), reads=[POSI], writes=[ANG])
                    S.op("dve", lambda e: e.tensor_scalar(out=ANG[:, :], in0=ANG[:, :], scalar1=CONST[0:64, C_INVF:C_INVF + 1], scalar2=None, op0=ALU.mult), reads=[ANG, CONST], writes=[ANG])
                    for (dst, shift, sgn) in ((CS1, 0.75, False), (CS2, 0.5, True)):
                        S.op("dve", lambda e, shift=shift: e.tensor_scalar(out=TT[:, :], in0=ANG[:, :], scalar1=1.0 / TWO_PI, scalar2=shift, op0=ALU.mult, op1=ALU.add), reads=[ANG], writes=[TT])
                        S.op("dve", lambda e: e.tensor_copy(out=TI[:, :], in_=TT[:, :]), reads=[TT], writes=[TI])
                        S.op("dve", lambda e: e.tensor_copy(out=FR[:, :], in_=TI[:, :]), reads=[TI], writes=[FR])
                        S.op("dve", lambda e: e.tensor_tensor(out=FR[:, :], in0=TT[:, :], in1=FR[:, :], op=ALU.subtract), reads=[TT, FR], writes=[FR])
                        S.op("dve", lambda e: e.tensor_single_scalar(out=MK[:, :], in_=FR[:, :], scalar=0.0, op=ALU.is_lt), reads=[FR], writes=[MK])
                        S.op("dve", lambda e: e.tensor_tensor(out=FR[:, :], in0=FR[:, :], in1=MK[:, :], op=ALU.add), reads=[FR, MK], writes=[FR])
                        S.op("dve", lambda e: e.tensor_single_scalar(out=MK[:, :], in_=FR[:, :], scalar=1.0, op=ALU.is_ge), reads=[FR], writes=[MK])
                        S.op("dve", lambda e: e.tensor_tensor(out=FR[:, :], in0=FR[:, :], in1=MK[:, :], op=ALU.subtract), reads=[FR, MK], writes=[FR])
                        S.op("act", lambda e, dst=dst: e.activation(out=dst[:, :], in_=FR[:, :], func=AF.Sin, scale=TWO_PI, bias=CONST[0:64, C_NPI:C_NPI + 1]), reads=[FR, CONST], writes=[dst])
                        if sgn:
                            S.op("dve", lambda e, dst=dst: e.tensor_scalar(out=dst[:, :], in0=dst[:, :], scalar1=CONST[0:64, C_SGN:C_SGN + 1], scalar2=None, op0=ALU.mult), reads=[dst, CONST], writes=[dst])
                    S.fence()
                WUQ = S.sbuf("WUQ", [128, 3, 2048], BF16, st=pa)
                WUKV = S.sbuf("WUKV", [128, 2, 2048], BF16, st=pa)
                S.dma("pool", lambda e: [e.dma_start(out=WUQ[:, :, 0:1024], in_=mla_w_uq_d.rearrange("(c p) n -> p c n", p=128)[:, :, 0:1024]),
                                         e.dma_start(out=WUQ[:, :, 1024:2048], in_=mla_w_uq_d.rearrange("(c p) n -> p c n", p=128)[:, :, 1024:2048])],
                      reads=[DR], writes=[WUQ], sync=WUQ, n=2)
                S.dma("pool", lambda e: [e.dma_start(out=WUKV[:, :, 0:1024], in_=mla_w_ukv_d.rearrange("(c p) n -> p c n", p=128)[:, :, 0:1024]),
                                         e.dma_start(out=WUKV[:, :, 1024:2048], in_=mla_w_ukv_d.rearrange("(c p) n -> p c n", p=128)[:, :, 1024:2048])],
                      reads=[DR], writes=[WUKV], sync=WUKV, n=2)
                with ExitStack() as p1:
                    def sb(name, shape, dt=F32):
                        return S.sbuf(name, shape, dt, st=p1)
                    WIN1 = sb("WIN1", [128, 8, 768], BF16)
                    S.dma("pool", lambda e: e.dma_start(out=WIN1[:, :, :], in_=mla_w_in_d.rearrange("(c p) n -> p c n", p=128)), reads=[DR], writes=[WIN1], sync=WIN1)
                    HTB = sb("HTB1", [128, 8, 512], BF16)
                    UT = sb("UT", [128, 5, 512])
                    SQ = Rot([sb("SQ%d" % i, [128, 512]) for i in range(2)])
                    RQ = sb("RQ", [128, 512]); RKV = sb("RKV", [128, 512])
                    T1 = sb("T1a", [64, 512]); T2 = sb("T2a", [64, 512])
                    scr = sb("scr1", [128, 8, 128])
                    pool_rot = Rot([S.psum("pla%d" % i, [128, 512], F32, st=p1) for i in range(6)])
                    ssq = [S.psum("ssq%d" % i, [128, 512], F32, st=p1) for i in range(2)]
                    build_g1b(l, jg, s, pool_rot.tiles[0:2], scr)
                    for B in range(4):
                        bc = slice(B * 512, (B + 1) * 512)
                        for i in range(4):
                            transpose_tile(4 * B + i, l, jsc, jsh, s, pool_rot, lambda c, i=i: HTB[:, c, i * 128:(i + 1) * 128], HTB)
                        for j in range(5):
                            up = pool_rot.next()
                            for kc in range(8):
                                S.op("pe", lambda e, kc=kc, j=j, up=up: e.matmul(up[:, :], lhsT=WIN1[:, kc, j * 128:(j + 1) * 128], rhs=HTB[:, kc, :], start=(kc == 0), stop=(kc == 7)), reads=[WIN1, HTB], writes=[up])
                            S.op("act", lambda e, j=j, up=up: e.activation(out=UT[:, j, :], in_=up[:, :], func=AF.Copy), reads=[up], writes=[UT])
                            sq = SQ.next()
                            S.op("act", lambda e, sq=sq, up=up: e.activation(out=sq[:, :], in_=up[:, :], func=AF.Square), reads=[up], writes=[sq])
                            sp_ = ssq[0] if j < 3 else ssq[1]
                            S.op("pe", lambda e, sq=sq, sp_=sp_, j=j: e.matmul(sp_[:, :], lhsT=ones, rhs=sq[:, :], start=(j in (0, 3)), stop=(j in (2, 4))), reads=[CONST, sq], writes=[sp_])
                        for (dst, sp_, n_) in ((RQ, ssq[0], 384.0), (RKV, ssq[1], 256.0)):
                            S.op("act", lambda e, dst=dst, sp_=sp_, n_=n_: e.activation(out=dst[:, :], in_=sp_[:, :], func=AF.Ln, scale=1.0 / n_, bias=CONST[:, C_REPS:C_REPS + 1]), reads=[sp_, CONST], writes=[dst])
                            S.op("act", lambda e, dst=dst: e.activation(out=dst[:, :], in_=dst[:, :], func=AF.Exp, scale=-0.5), reads=[dst], writes=[dst])
                        for j in range(5):
                            rr = RQ if j < 3 else RKV
                            S.op("dve", lambda e, j=j, rr=rr, bc=bc: e.scalar_tensor_tensor(out=HT[:, j, bc], in0=UT[:, j, :], scalar=PVT[:, 12 + j:13 + j], in1=rr[:, :], op0=ALU.mult, op1=ALU.mult),
                                 reads=[UT, PVT, rr], writes=[CQB[B]])
                        a_p = pool_rot.next()
                        b_p = pool_rot.next()
                        for (pp_, off) in ((a_p, 640), (b_p, 704)):
                            for kc in range(8):
                                S.op("pe", lambda e, kc=kc, pp_=pp_, off=off: e.matmul(pp_[0:64, :], lhsT=WIN1[:, kc, off:off + 64], rhs=HTB[:, kc, :], start=(kc == 0), stop=(kc == 7)), reads=[WIN1, HTB], writes=[pp_])
                        S.op("dve", lambda e, a_p=a_p, bc=bc: e.tensor_tensor(out=T1[:, :], in0=a_p[0:64, :], in1=CS1[:, bc], op=ALU.mult), reads=[a_p, CS1], writes=[T1])
                        S.op("dve", lambda e, b_p=b_p, bc=bc: e.tensor_tensor(out=T2[:, :], in0=b_p[0:64, :], in1=CS2[:, bc], op=ALU.mult), reads=[b_p, CS2], writes=[T2])
                        S.op("pool", lambda e, bc=bc: e.tensor_tensor(out=HT[0:64, 5, bc], in0=T1[:, :], in1=T2[:, :], op=ALU.add), reads=[T1, T2], writes=[CQB[B]])
                    S.fence()
                if stop_after == "l1a":
                    return
                with ExitStack() as p2:
                    def sb(name, shape, dt=F32):
                        return S.sbuf(name, shape, dt, st=p2)
                    WOUT = sb("WOUT1", [128, 8, D], BF16)
                    S.dma("pool", lambda e: e.dma_start(out=WOUT[:, :, :], in_=mla_w_out_d.rearrange("(c p) n -> p c n", p=128)), reads=[DR], writes=[WOUT], sync=WOUT)
                    S.op("pool", lambda e: e.tensor_tensor(out=WOUT[:, :, :], in0=WOUT[:, :, :], in1=G1B[:, :].unsqueeze(1).to_broadcast([128, 8, D]), op=ALU.mult), reads=[WOUT, G1B], writes=[WOUT])
                    QN = sb("QN", [128, S_LEN], BF16); QR = sb("QR", [64, S_LEN], BF16); KN = sb("KN", [128, S_LEN], BF16); VT = sb("VT", [128, NT, 128], BF16)
                    PT = Rot([sb("PT%d" % i, [128, 512], BF16) for i in range(5)])
                    RINV = sb("RINV", [128, 512])
                    OTH = Rot([sb("OTH%d" % i, [128, S_LEN], BF16) for i in range(2)])
                    T1 = sb("T1b", [64, 512]); T2 = sb("T2b", [64, 512])
                    pool_rot = Rot([S.psum("plb%d" % i, [128, 512], F32, st=p2) for i in range(3)])
                    st_rot = Rot([S.psum("stb%d" % i, [128, 512], F32, st=p2) for i in range(3)])
                    o_ps = S.psum("o1_ps", [128, 512], F32, st=p2)
                    r_ps = S.psum("r1_ps", [128, 512], F32, st=p2)
                    lnp = LNPipe()
                    for h in range(8 if dbg >= 2 else 0):
                        hb = h * 256
                        for B in range(4):
                            bc = slice(B * 512, (B + 1) * 512)
                            p_ = pool_rot.next()
                            for k3 in range(3):
                                S.op("pe", lambda e, k3=k3, p_=p_, bc=bc, hb=hb: e.matmul(p_[:, :], lhsT=WUQ[:, k3, hb:hb + 128], rhs=HT[:, k3, bc], start=(k3 == 0), stop=(k3 == 2)), reads=[WUQ, CQB[B]], writes=[p_])
                            S.op("act", lambda e, p_=p_, bc=bc: e.activation(out=QN[:, bc], in_=p_[:, :], func=AF.Copy), reads=[p_], writes=[QN])
                            a_p = pool_rot.next()
                            b_p = pool_rot.next()
                            for (pp_, off) in ((a_p, hb + 128), (b_p, hb + 192)):
                                for k3 in range(3):
                                    S.op("pe", lambda e, k3=k3, pp_=pp_, off=off, bc=bc: e.matmul(pp_[0:64, :], lhsT=WUQ[:, k3, off:off + 64], rhs=HT[:, k3, bc], start=(k3 == 0), stop=(k3 == 2)), reads=[WUQ, CQB[B]], writes=[pp_])
                            S.op("dve", lambda e, a_p=a_p, bc=bc: e.tensor_tensor(out=T1[:, :], in0=a_p[0:64, :], in1=CS1[:, bc], op=ALU.mult), reads=[a_p, CS1], writes=[T1])
                            S.op("dve", lambda e, b_p=b_p, bc=bc: e.tensor_tensor(out=T2[:, :], in0=b_p[0:64, :], in1=CS2[:, bc], op=ALU.mult), reads=[b_p, CS2], writes=[T2])
                            S.op("pool", lambda e, bc=bc: e.tensor_tensor(out=QR[:, bc], in0=T1[:, :], in1=T2[:, :], op=ALU.add), reads=[T1, T2], writes=[QR])
                            p_ = pool_rot.next()
                            for k2 in range(2):
                                S.op("pe", lambda e, k2=k2, p_=p_, bc=bc, hb=hb: e.matmul(p_[:, :], lhsT=WUKV[:, k2, hb:hb + 128], rhs=HT[:, 3 + k2, bc], start=(k2 == 0), stop=(k2 == 1)), reads=[WUKV, CQB[B]], writes=[p_])
                            S.op("act", lambda e, p_=p_, bc=bc: e.activation(out=KN[:, bc], in_=p_[:, :], func=AF.Copy), reads=[p_], writes=[KN])
                            p_ = pool_rot.next()
                            for i in range(4):
                                t = 4 * B + i
                                for k2 in range(2):
                                    S.op("pe", lambda e, k2=k2, p_=p_, i=i, t=t, hb=hb: e.matmul(p_[:, i * 128:(i + 1) * 128], lhsT=HT[:, 3 + k2, t * 128:(t + 1) * 128], rhs=WUKV[:, k2, hb + 128:hb + 256], start=(k2 == 0), stop=(k2 == 1)),
                                         reads=[WUKV, CQB[B]], writes=[p_])
                            S.op("act", lambda e, p_=p_, B=B: e.activation(out=VT[:, 4 * B:4 * B + 4, :], in_=p_[:, :].rearrange("p (a b) -> p a b", a=4), func=AF.Copy), reads=[p_], writes=[VT])
                        oth = OTH.next()
                        steps = [(Q, kt) for Q in range(4) for kt in range(4 * Q + 4)]
                        pend = {}

                        def do_st(Q, kt):
                            m = kt - 4 * Q if kt >= 4 * Q else 0
                            c0 = m * 128
                            st_ = st_rot.next()
                            qc = slice(Q * 512 + c0, (Q + 1) * 512)
                            kc_ = slice(kt * 128, (kt + 1) * 128)
                            S.op("pe", lambda e: e.matmul(st_[:, c0:512], lhsT=KN[:, kc_], rhs=QN[:, qc], start=True, stop=False), reads=[KN, QN], writes=[st_])
                            S.op("pe", lambda e: e.matmul(st_[:, c0:512], lhsT=HT[0:64, 5, kc_], rhs=QR[0:64, qc], start=False, stop=True), reads=[CQB[kt // 4], QR], writes=[st_])
                            pt = PT.next()
                            S.op("act", lambda e: e.activation(out=pt[:, c0:512], in_=st_[:, c0:512], func=AF.Exp, scale=MLA_SCALE), reads=[st_], writes=[pt])
                            if kt >= 4 * Q:
                                S.op("pool", lambda e: e.tensor_tensor(out=pt[:, c0:c0 + 128], in0=pt[:, c0:c0 + 128], in1=TRIB[:, :], op=ALU.mult), reads=[pt, TRIB], writes=[pt])
                            pend[(Q, kt)] = (pt, c0)

                        def do_pv(Q, kt, oth=oth):
                            pt, c0 = pend.pop((Q, kt))
                            last = (kt == 4 * Q + 3)
                            S.op("pe", lambda e: e.matmul(o_ps[:, c0:512], lhsT=VT[:, kt, :], rhs=pt[:, c0:512], start=(kt == 0), stop=last), reads=[VT, pt], writes=[o_ps])
                            S.op("pe", lambda e: e.matmul(r_ps[:, c0:512], lhsT=ONEB[:, :], rhs=pt[:, c0:512], start=(kt == 0), stop=last), reads=[ONEB, pt], writes=[r_ps])
                            if last:
                                qc = slice(Q * 512, (Q + 1) * 512)
                                S.op("dve", lambda e: e.reciprocal(out=RINV[:, :], in_=r_ps[:, :]), reads=[r_ps], writes=[RINV])
                                S.op("dve", lambda e: e.tensor_tensor(out=oth[:, qc], in0=o_ps[:, :], in1=RINV[:, :], op=ALU.mult), reads=[o_ps, RINV], writes=[oth])

                        for i_, (Q, kt) in enumerate(steps):
                            do_st(Q, kt)
                            if i_ >= 2:
                                do_pv(*steps[i_ - 2])
                        do_pv(*steps[-2])
                        do_pv(*steps[-1])
                        for t in range(NT):
                            for hf in range(2):
                                mp = pool_rot.next()
                                S.op("pe", lambda e, t=t, hf=hf, mp=mp, h=h, oth=oth: e.matmul(mp[:, :], lhsT=oth[:, t * 128:(t + 1) * 128], rhs=WOUT[:, h, hf * 512:(hf + 1) * 512], start=True, stop=True), reads=[oth, WOUT], writes=[mp])
                                S.op("dve", lambda e, t=t, hf=hf, mp=mp: e.tensor_tensor(out=XT[t][:, hf * 512:(hf + 1) * 512], in0=mp[:, :], in1=XT[t][:, hf * 512:(hf + 1) * 512], op=ALU.add),
                                     reads=[mp, XT[t]], writes=[XT[t]])
                            if h == 7:
                                lnp.push(t)
                    lnp.flush()
                    S.fence()

        for s in range(nseq):
            for t in range(NT):
                S.dma("sp", lambda e, t=t, s=s: e.dma_start(out=XT[t][:, :], in_=x_d[s, t * 128:(t + 1) * 128, :]), reads=[DR], writes=[XT[t]], sync=XT[t])
                S.op("act", lambda e, t=t: e.activation(out=XT[t][:, :], in_=XT[t][:, :], func=AF.Copy, scale=ALPHA), reads=[XT[t]], writes=[XT[t]])
            if stop_after == "moe0":
                moe_phase(0, s, last=True)
            elif stop_after in ("xm1only", "l1a"):
                l1_mixer(s)
            elif stop_after != "load":
                l0_mixer(s)
                if stop_after not in ("xm0", "l0a", "l0b"):
                    moe_phase(0, s, last=False)
                    if stop_after != "xf0":
                        l1_mixer(s)
                        if stop_after != "xm1":
                            moe_phase(1, s, last=True)
            for t in range(NT):
                S.dma("sp", lambda e, t=t, s=s: e.dma_start(out=out_d[s, t * 128:(t + 1) * 128, :], in_=XT[t][:, :]), reads=[XT[t]], writes=[DO], sync=XT[t])
            S.fence()
        S.emit(final_keys=["X%d" % t for t in range(NT)])
    return nc


def prep_shared(inp):
    f = lambda a: np.ascontiguousarray(np.asarray(a, dtype=np.float32))
    sh = {}
    sh["consts"] = make_consts()
    sh["ada_w"] = f(inp["ada_w"])
    sh["ada_b"] = f(inp["ada_b"])
    sh["ln_gb"] = f(np.stack([inp["ln_mix_g"], inp["ln_mix_b"], inp["ln_ffn_g"], inp["ln_ffn_b"]], axis=1))
    sh["ab_w_in"] = f(inp["ab_w_in"][0])
    sh["conv_w"] = f(inp["conv_w"][0])
    pv = np.zeros((40, 128), np.float32)
    pv[0:4] = np.asarray(inp["conv_b"][0]).reshape(4, 128)
    pv[4:8] = np.asarray(inp["conv_ln_g"][0]).reshape(4, 128)
    pv[8:12] = np.asarray(inp["conv_ln_b"][0]).reshape(4, 128)
    pv[12:15] = np.asarray(inp["mla_q_norm_g"][0]).reshape(3, 128)
    pv[15:17] = np.asarray(inp["mla_kv_norm_g"][0]).reshape(2, 128)
    sh["pvec"] = pv
    sh["gla_gate_w"] = f(inp["gla_gate_w"][0])
    sh["gla_gate_b"] = f(inp["gla_gate_b"][0]).reshape(1, 256)
    sh["gla_norm_g"] = f(inp["gla_norm_g"][0]).reshape(1, 512)
    sh["ab_w_out"] = f(inp["ab_w_out"][0])
    w_in = np.asarray(inp["mla_w_in"][0], dtype=np.float32)
    sh["mla_w_in"] = f(np.concatenate([w_in, w_in[:, 672:704], w_in[:, 640:672]], axis=1))
    wq = np.asarray(inp["mla_w_uq"][0], dtype=np.float32).reshape(384, 8, 192)
    sh["mla_w_uq"] = f(np.concatenate([wq, wq[:, :, 160:192], wq[:, :, 128:160]], axis=2).reshape(384, 2048))
    sh["mla_w_ukv"] = f(inp["mla_w_ukv"][0])
    sh["mla_w_out"] = f(inp["mla_w_out"][0])
    sh["moe_wr"] = f(np.concatenate([inp["moe_w_group"], inp["moe_w_router"]], axis=2))
    sh["moe_br"] = f(np.concatenate([inp["moe_b_group"], inp["moe_b_router"]], axis=1))
    sh["consts2"] = make_consts2()
    wg = np.asarray(inp["moe_w_gate"], dtype=np.float32).reshape(2, 32, 8, 128, 256).transpose(0, 1, 3, 2, 4)
    wu = np.asarray(inp["moe_w_up"], dtype=np.float32).reshape(2, 32, 8, 128, 256).transpose(0, 1, 3, 2, 4)
    wd = np.asarray(inp["moe_w_down"], dtype=np.float32).reshape(2, 32, 2, 128, 1024).transpose(0, 1, 3, 2, 4)
    wall = np.concatenate([np.concatenate([wg, wu], axis=4).reshape(2, 32, 128, 4096), wd.reshape(2, 32, 128, 2048)], axis=3)
    for i in range(2):
        sh["moe_wall%d" % i] = f(wall[i].reshape(32 * 128, 6144))
    return sh


def kernel(**inputs):
    sh = prep_shared(inputs)
    x = np.asarray(inputs["x"], dtype=np.float32)
    c = np.asarray(inputs["c"], dtype=np.float32)
    pos = np.asarray(inputs["positions"], dtype=np.int32)
    nc = build_nc()
    in_maps = []
    for i in range(NCORES):
        m = dict(sh)
        m["x"] = np.ascontiguousarray(x[2 * i:2 * i + 2])
        m["c"] = np.ascontiguousarray(c[2 * i:2 * i + 2])
        m["pos"] = np.ascontiguousarray(pos[2 * i:2 * i + 2])
        in_maps.append(m)
    res = run_bass_kernel_spmd(nc, in_maps, core_ids=list(range(NCORES)))
    return np.concatenate([r["out"] for r in res.results], axis=0).astype(np.float32)
```

```python
from contextlib import ExitStack
import math
import numpy as np
import concourse.bass as bass
import concourse.mybir as mybir
from concourse.bass_utils import run_bass_kernel_spmd

F32 = mybir.dt.float32
BF16 = mybir.dt.bfloat16
I32 = mybir.dt.int32
AF = mybir.ActivationFunctionType
ALU = mybir.AluOpType
AX = mybir.AxisListType

NCORES = 8
D = 1024
S_LEN = 2048
NT = 16
ALPHA = 4.0 ** 0.25
LN_EPS = 1e-5
RMS_EPS = 1e-6
MLA_SCALE = 192.0 ** -0.5
TWO_PI = 2.0 * math.pi

COMPUTE = ("pe", "act", "dve", "pool")


class Tile:
    __slots__ = ("ap", "name", "w", "rs", "semkey")

    def __init__(self, ap, name, semkey=None):
        self.ap = ap
        self.name = name
        self.w = None
        self.rs = []
        self.semkey = semkey or name

    def __getitem__(self, idx):
        return self.ap[idx]


class Op:
    __slots__ = ("eng", "fn", "deps", "signal", "sigidx", "pos", "semkey", "semval", "ndma")

    def __init__(self, eng, fn):
        self.eng = eng
        self.fn = fn
        self.deps = []
        self.signal = False
        self.sigidx = 0
        self.pos = 0
        self.semkey = None
        self.semval = 0
        self.ndma = 0


class Sched:
    def __init__(self, nc, stack):
        self.nc = nc
        self.stack = stack
        self.ops = {e: [] for e in ("pe", "act", "dve", "pool", "sp")}
        self.semcnt = {}
        self.uid = 0
        self.fence_deps = []
        self.fence_pending = set()
        self.dma_since = []

    def sbuf(self, name, shape, dtype, st=None):
        self.uid += 1
        t = (st or self.stack).enter_context(self.nc.sbuf_tensor("%s_%d" % (name, self.uid), list(shape), dtype))
        return Tile(t, name)

    def psum(self, name, shape, dtype=F32, st=None):
        self.uid += 1
        t = (st or self.stack).enter_context(self.nc.psum_tensor("%s_%d" % (name, self.uid), list(shape), dtype))
        return Tile(t, name)

    def _add(self, eng, fn, reads, writes, semkey=None, ndma=0):
        op = Op(eng, fn)
        lst = self.ops[eng]
        op.pos = len(lst)
        deps = []
        if eng in self.fence_pending:
            self.fence_pending.discard(eng)
            deps.extend(self.fence_deps)
        for t in reads:
            if t.w is not None:
                deps.append(t.w)
        for t in writes:
            if t.w is not None:
                deps.append(t.w)
            deps.extend(t.rs)
        seen = set()
        for d in deps:
            if id(d) in seen or d is op:
                continue
            seen.add(id(d))
            if d.semkey is None and d.eng == eng:
                if eng == "pe" or eng == "sp":
                    continue
            op.deps.append(d)
            if d.semkey is None:
                d.signal = True
        for t in reads:
            if semkey is None:
                t.rs = [r for r in t.rs if not (r.semkey is None and r.eng == eng)]
            t.rs.append(op)
        for t in writes:
            t.w = op
            t.rs = []
        if semkey is not None:
            op.semkey = semkey
            op.ndma = ndma
            self.semcnt[semkey] = self.semcnt.get(semkey, 0) + 16 * ndma
            op.semval = self.semcnt[semkey]
            self.dma_since.append(op)
        lst.append(op)
        return op

    def op(self, eng, fn, reads=(), writes=()):
        return self._add(eng, fn, list(reads), list(writes))

    def dma(self, eng, fn, reads=(), writes=(), sync=None, n=1):
        return self._add(eng, fn, list(reads), list(writes), semkey=sync.semkey, ndma=n)

    def fence(self):
        deps = []
        for lst in self.ops.values():
            for d in reversed(lst):
                if d.semkey is None:
                    deps.append(d)
                    break
        deps = deps + self.dma_since
        self.dma_since = []
        self.fence_deps = deps
        self.fence_pending = set(self.ops.keys())

    def emit(self, final_keys=()):
        nc = self.nc
        stack = self.stack
        esem = {e: stack.enter_context(nc.semaphore("es_" + e)) for e in COMPUTE}
        dsem = {k: stack.enter_context(nc.semaphore("ds_%d" % i)) for i, k in enumerate(sorted(self.semcnt))}
        for e in COMPUTE:
            c = 0
            for op in self.ops[e]:
                if op.signal and op.semkey is None:
                    c += 1
                    op.sigidx = c
        block = stack.enter_context(nc.Block())

        def run(ename, engine):
            waited = {}
            for op in self.ops[ename]:
                need = {}
                for d in op.deps:
                    if d.semkey is not None:
                        s, v, key = dsem[d.semkey], d.semval, "d" + d.semkey
                    else:
                        s, v, key = esem[d.eng], d.sigidx, "e" + d.eng
                    if key not in need or need[key][1] < v:
                        need[key] = (s, v)
                for key, (s, v) in need.items():
                    if waited.get(key, 0) >= v:
                        continue
                    waited[key] = v
                    engine.wait_ge(s, v)
                r = op.fn(engine)
                if op.semkey is not None:
                    if not isinstance(r, (list, tuple)):
                        r = [r]
                    assert len(r) == op.ndma, (len(r), op.ndma)
                    for ins in r:
                        ins.then_inc(dsem[op.semkey], 16)
                elif op.signal:
                    r.then_inc(esem[ename], 1)
            if ename == "sp":
                for k in final_keys:
                    engine.wait_ge(dsem[k], self.semcnt[k])

        @block.sync
        def _(sync):
            run("sp", sync)

        @block.tensor
        def _(tensor):
            run("pe", tensor)

        @block.scalar
        def _(scalar):
            run("act", scalar)

        @block.vector
        def _(vector):
            run("dve", vector)

        @block.gpsimd
        def _(gpsimd):
            run("pool", gpsimd)


class Rot:
    def __init__(self, tiles):
        self.tiles = tiles
        self.i = 0

    def next(self):
        t = self.tiles[self.i % len(self.tiles)]
        self.i += 1
        return t


C_ID, C_TRI, C_SU, C_ONE, C_INVF, C_SGN, C_NHALF, C_NPI, C_REPS, NCONST = 0, 128, 256, 384, 512, 513, 514, 515, 516, 517


C2_LT, C2_THR, C2_VROW, C2_EROW, C2_VAL, NC2 = 0, 1024, 1032, 1080, 1112, 1144


def make_consts2():
    c = np.zeros((128, NC2), np.float32)
    p = np.arange(128)
    e = np.arange(32)
    c[:, C2_LT:C2_LT + 1024] = (e[None, :] < e[:, None]).astype(np.float32).reshape(1, 1024)
    c[:, C2_THR:C2_THR + 8] = 256.0 * np.arange(8)[None, :]
    c[:, C2_VROW:C2_VROW + 48] = np.arange(48)[None, :]
    c[:, C2_EROW:C2_EROW + 32] = e[None, :] * 128.0 + p[:, None] - 8192.0
    kt = np.arange(32)
    c[:, C2_VAL:C2_VAL + 32] = (kt // 16)[None, :] * 2048.0 + (kt % 16)[None, :] * 128.0 + p[:, None]
    return c


def make_consts():
    c = np.zeros((128, NCONST), np.float32)
    p = np.arange(128)
    c[:, C_ID:C_ID + 128] = np.eye(128, dtype=np.float32)
    c[:, C_TRI:C_TRI + 128] = (p[:, None] <= p[None, :]).astype(np.float32)
    c[:, C_SU:C_SU + 128] = (p[:, None] > p[None, :]).astype(np.float32)
    c[:, C_ONE:C_ONE + 128] = 1.0
    inv_freq = (1.0 / (np.float32(10000.0) ** (np.arange(0, 64, 2, dtype=np.float32) / np.float32(64)))).astype(np.float32)
    c[:64, C_INVF] = inv_freq[p[:64] % 32]
    c[:, C_SGN] = np.where(p < 32, -1.0, 1.0)
    c[:, C_NHALF] = -0.5
    c[:, C_NPI] = -math.pi
    c[:, C_REPS] = RMS_EPS
    return c


def build_nc(nseq=2, stop_after=None, dbg=9):
    nc = bass.Bass("TRN2", target_bir_lowering=False)

    def din(name, shape, dt=F32):
        return nc.dram_tensor(name, list(shape), dt, kind="ExternalInput").ap()

    x_d = din("x", [2, S_LEN, D])
    c_d = din("c", [2, D])
    pos_d = din("pos", [2, S_LEN], I32)
    const_d = din("consts", [128, NCONST])
    ada_w_d = din("ada_w", [2, D, 6 * D])
    ada_b_d = din("ada_b", [2, 6 * D])
    ln_d = din("ln_gb", [2, 4, D])
    ab_w_in_d = din("ab_w_in", [D, 2576])
    conv_w_d = din("conv_w", [31, 512])
    pv_d = din("pvec", [40, 128])
    gate_w_d = din("gla_gate_w", [16, 256])
    gate_b_d = din("gla_gate_b", [1, 256])
    gnorm_d = din("gla_norm_g", [1, 512])
    ab_w_out_d = din("ab_w_out", [D, D])
    mla_w_in_d = din("mla_w_in", [D, 768])
    mla_w_uq_d = din("mla_w_uq", [384, 2048])
    mla_w_ukv_d = din("mla_w_ukv", [256, 2048])
    mla_w_out_d = din("mla_w_out", [D, D])
    wr_d = din("moe_wr", [2, D, 36])
    br_d = din("moe_br", [2, 36])
    wall_ds = [din("moe_wall%d" % i, [32 * 128, 6144]) for i in range(2)]
    const2_d = din("consts2", [128, NC2])
    htok_d = nc.dram_tensor("htok_scr", [S_LEN, D], BF16).ap()
    ys_d = nc.dram_tensor("ys_scr", [2 * S_LEN, D], F32).ap()
    tab_d = nc.dram_tensor("tab_scr", [96 * 128, 1], I32).ap()
    out_d = nc.dram_tensor("out", [2, S_LEN, D], F32, kind="ExternalOutput").ap()

    with ExitStack() as st:
        S = Sched(nc, st)
        global LAST_SCHED
        LAST_SCHED = S
        DR = Tile(None, "dram_in")
        DO = Tile(None, "dram_out")
        TABF = Tile(None, "tabf")
        TABS = Tile(None, "tabs")
        BC = {}
        for bv in (4095, 2047, 96 * 128 - 1):
            BC[bv] = nc.alloc_register(mybir.EngineType.Pool, "bc%d" % bv)
            S.op("pool", lambda e, bv=bv: e.reg_mov(BC[bv], bv))

        X = S.sbuf("X", [128, NT, D], F32)
        XT = [Tile(X.ap[:, t, :], "X%d" % t) for t in range(NT)]
        HT = S.sbuf("HT", [128, 8, S_LEN], BF16)
        CONST = S.sbuf("CONST", [128, NCONST], F32)
        IDB = S.sbuf("IDB", [128, 128], BF16)
        TRIB = S.sbuf("TRIB", [128, 128], BF16)
        ONEB = S.sbuf("ONEB", [128, 128], BF16)
        MOD = S.sbuf("MOD", [128, 2, 48, 2], F32)
        G1B = S.sbuf("G1B", [128, D], F32)
        LNG = S.sbuf("LNG", [128, D], F32)
        LNB = S.sbuf("LNB", [128, D], F32)
        PVT = S.sbuf("PVT", [128, 40], F32)
        MV = S.sbuf("MV", [128, NT, 2], F32)
        RSTD = S.sbuf("RSTD", [128, NT], F32)
        NHALF = S.sbuf("NHALF", [128, 512], F32)

        ident = CONST[:, C_ID:C_ID + 128]
        tri = CONST[:, C_TRI:C_TRI + 128]
        su = CONST[:, C_SU:C_SU + 128]
        ones = CONST[:, C_ONE:C_ONE + 128]

        with ExitStack() as ph:
            S.dma("sp", lambda e: e.dma_start(out=CONST[:, :], in_=const_d[:, :]), reads=[DR], writes=[CONST], sync=CONST)
            S.op("dve", lambda e: e.tensor_copy(out=IDB[:, :], in_=ident), reads=[CONST], writes=[IDB])
            S.op("dve", lambda e: e.tensor_copy(out=TRIB[:, :], in_=tri), reads=[CONST], writes=[TRIB])
            S.op("dve", lambda e: e.tensor_copy(out=ONEB[:, :], in_=ones), reads=[CONST], writes=[ONEB])
            S.op("pool", lambda e: e.memset(NHALF[:, :], -0.5), writes=[NHALF])
            STG = S.sbuf("STG", [128, 128], F32, st=ph)
            S.op("dve", lambda e: e.memset(STG[:, :], 0.0), writes=[STG])
            S.dma("sp", lambda e: [e.dma_start(out=STG[0:40, :], in_=pv_d[:, :]),
                                   e.dma_start(out=STG[64:80, :], in_=c_d.rearrange("s (c p) -> (s c) p", p=128)),
                                   ], reads=[DR], writes=[STG], sync=STG, n=2)
            pp = S.psum("pp_setup", [128, 512], F32, st=ph)
            pp2 = S.psum("pp_setup2", [128, 512], F32, st=ph)
            S.op("pe", lambda e: e.transpose(pp[:, 0:128], STG[:, :], ident), reads=[STG, CONST], writes=[pp])
            S.op("dve", lambda e: e.tensor_copy(out=PVT[:, :], in_=pp[:, 0:40]), reads=[pp], writes=[PVT])
            CACT = S.sbuf("CACT", [128, 2, 8], BF16, st=ph)
            S.op("act", lambda e: e.activation(out=CACT[:, :, :].rearrange("p s c -> p (s c)"), in_=pp[:, 64:80], func=AF.Silu), reads=[pp], writes=[CACT])
            STB = S.sbuf("STB", [128, 128], F32, st=ph)
            S.op("dve", lambda e: e.memset(STB[:, :], 0.0), writes=[STB])
            S.dma("sp", lambda e: e.dma_start(out=STB[0:96, :], in_=ada_b_d.rearrange("l (j p) -> (l j) p", p=128)), reads=[DR], writes=[STB], sync=STB)
            S.op("pe", lambda e: e.transpose(pp2[:, 0:128], STB[:, :], ident), reads=[STB, CONST], writes=[pp2])
            ADABT = S.sbuf("ADABT", [128, 96], F32, st=ph)
            S.op("dve", lambda e: e.tensor_copy(out=ADABT[:, :], in_=pp2[:, 0:96]), reads=[pp2], writes=[ADABT])
            AW = [S.sbuf("AW%d" % i, [128, 8, 512], BF16, st=ph) for i in range(3)]
            modp = S.psum("modp", [128, 2, 48, 2], F32, st=ph)
            for l in range(2):
                for blk in range(12):
                    aw = AW[(l * 12 + blk) % 3]
                    src = ada_w_d[l].rearrange("(c p) n -> p c n", p=128)[:, :, blk * 512:(blk + 1) * 512]
                    S.dma("pool", lambda e, aw=aw, src=src: e.dma_start(out=aw[:, :, :], in_=src), reads=[DR], writes=[aw], sync=aw)
                    for jj in range(4):
                        j = blk * 4 + jj
                        for kc in range(8):
                            S.op("pe", lambda e, aw=aw, jj=jj, kc=kc, l=l, j=j: e.matmul(
                                modp[:, l, j, :], lhsT=aw[:, kc, jj * 128:(jj + 1) * 128], rhs=CACT[:, :, kc],
                                start=(kc == 0), stop=(kc == 7)), reads=[aw, CACT], writes=[modp])
            for l in range(2):
                for s in range(2):
                    S.op("dve", lambda e, l=l, s=s: e.tensor_tensor(out=MOD[:, l, :, s], in0=modp[:, l, :, s], in1=ADABT[:, l * 48:(l + 1) * 48], op=ALU.add),
                         reads=[modp, ADABT], writes=[MOD])
            for l in range(2):
                for j0 in (8, 32):
                    S.op("dve", lambda e, l=l, j0=j0: e.tensor_scalar(out=MOD[:, l, j0:j0 + 8, :], in0=MOD[:, l, j0:j0 + 8, :], scalar1=1.0, scalar2=1.0 / ALPHA, op0=ALU.add, op1=ALU.mult),
                         reads=[MOD], writes=[MOD])
                for j0 in (16, 40):
                    S.op("dve", lambda e, l=l, j0=j0: e.tensor_scalar(out=MOD[:, l, j0:j0 + 8, :], in0=MOD[:, l, j0:j0 + 8, :], scalar1=1.0, scalar2=None, op0=ALU.add),
                         reads=[MOD], writes=[MOD])
            S.fence()

        def load_ln(l, which, scaled):
            S.dma("sp", lambda e: [e.dma_start(out=LNG[:, :], in_=ln_d[l, 2 * which:2 * which + 1, :].partition_broadcast(128)),
                                   e.dma_start(out=LNB[:, :], in_=ln_d[l, 2 * which + 1:2 * which + 2, :].partition_broadcast(128))],
                  reads=[DR], writes=[LNG, LNB], sync=LNG, n=2)
            if scaled:
                S.op("dve", lambda e: e.tensor_scalar(out=LNG[:, :], in0=LNG[:, :], scalar1=ALPHA, scalar2=None, op0=ALU.mult), reads=[LNG], writes=[LNG])
                S.op("dve", lambda e: e.tensor_scalar(out=LNB[:, :], in0=LNB[:, :], scalar1=ALPHA, scalar2=None, op0=ALU.mult), reads=[LNB], writes=[LNB])

        def build_g1b(l, j0, s, pp_ts, scratch, dst=None):
            dst = G1B if dst is None else dst
            for c in range(8):
                S.op("dve", lambda e, c=c: e.tensor_scalar(out=scratch[:, c, :], in0=ident, scalar1=MOD[:, l, j0 + c, s:s + 1], scalar2=None, op0=ALU.mult),
                     reads=[CONST, MOD], writes=[scratch])
            for hf in range(2):
                pp_t = pp_ts[hf]
                for c4 in range(4):
                    S.op("pe", lambda e, hf=hf, c4=c4, pp_t=pp_t: e.matmul(pp_t[:, c4 * 128:(c4 + 1) * 128], lhsT=ones, rhs=scratch[:, hf * 4 + c4, :], start=True, stop=True),
                         reads=[CONST, scratch], writes=[pp_t])
                S.op("act", lambda e, hf=hf, pp_t=pp_t: e.activation(out=dst[:, hf * 512:(hf + 1) * 512], in_=pp_t[:, :], func=AF.Copy), reads=[pp_t], writes=[dst])

        def transpose_tile(t, l, jsc, jsh, s, tp_rot, dst_fn, dst_tile, f32_dst=None):
            for hlf in range(2):
                tp = tp_rot.next()
                for c4 in range(4):
                    c = hlf * 4 + c4
                    S.op("pe", lambda e, tp=tp, c=c, c4=c4: e.transpose(tp[:, c4 * 128:(c4 + 1) * 128], XT[t][:, c * 128:(c + 1) * 128], ident),
                         reads=[XT[t], CONST], writes=[tp])
                for c4 in range(4):
                    c = hlf * 4 + c4
                    o_ap = dst_fn(c) if f32_dst is None else f32_dst[:, c, :]
                    o_t = dst_tile if f32_dst is None else f32_dst
                    if c % 2 == 0:
                        S.op("dve", lambda e, tp=tp, c=c, c4=c4, o_ap=o_ap: e.tensor_scalar(
                            out=o_ap, in0=tp[:, c4 * 128:(c4 + 1) * 128], scalar1=MOD[:, l, jsc + c, s:s + 1], scalar2=MOD[:, l, jsh + c, s:s + 1],
                            op0=ALU.mult, op1=ALU.add), reads=[tp, MOD], writes=[o_t])
                    else:
                        S.op("act", lambda e, tp=tp, c=c, c4=c4, o_ap=o_ap: e.activation(
                            out=o_ap, in_=tp[:, c4 * 128:(c4 + 1) * 128], func=AF.Identity, scale=MOD[:, l, jsc + c, s:s + 1], bias=MOD[:, l, jsh + c, s:s + 1]),
                            reads=[tp, MOD], writes=[o_t])

        LN_STATS = S.sbuf("STATS", [128, NT, 2, 6], F32)
        LN_VE = S.sbuf("VE", [128, NT], F32)
        LN_NMR = S.sbuf("NMR", [128, NT], F32)

        def layer_norm_all():
            STATS, VE, NMR = LN_STATS, LN_VE, LN_NMR
            for t in range(NT):
                for h2 in range(2):
                    S.op("dve", lambda e, t=t, h2=h2: e.bn_stats(out=STATS[:, t, h2, :], in_=XT[t][:, h2 * 512:(h2 + 1) * 512]), reads=[XT[t]], writes=[STATS])
                S.op("dve", lambda e, t=t: e.bn_aggr(out=MV[:, t, :], in_=STATS[:, t, :, :].rearrange("p a b -> p (a b)")), reads=[STATS], writes=[MV])
            S.op("dve", lambda e: e.tensor_scalar(out=VE[:, :], in0=MV[:, :, 1], scalar1=LN_EPS, scalar2=None, op0=ALU.add), reads=[MV], writes=[VE])
            S.op("pool", lambda e: e.tensor_tensor(out=RSTD[:, :], in0=VE[:, :], in1=NHALF[:, 0:NT], op=ALU.pow), reads=[VE, NHALF], writes=[RSTD])
            S.op("dve", lambda e: e.scalar_tensor_tensor(out=NMR[:, :], in0=MV[:, :, 0], scalar=-1.0, in1=RSTD[:, :], op0=ALU.mult, op1=ALU.mult), reads=[MV, RSTD], writes=[NMR])
            for t in range(NT):
                S.op("act", lambda e, t=t: e.activation(out=XT[t][:, :], in_=XT[t][:, :], func=AF.Identity, scale=RSTD[:, t:t + 1], bias=NMR[:, t:t + 1]),
                     reads=[XT[t], NMR, RSTD], writes=[XT[t]])
                S.op("dve", lambda e, t=t: e.tensor_tensor(out=XT[t][:, :], in0=XT[t][:, :], in1=LNG[:, :], op=ALU.mult), reads=[XT[t], LNG], writes=[XT[t]])
                S.op("pool", lambda e, t=t: e.tensor_tensor(out=XT[t][:, :], in0=XT[t][:, :], in1=LNB[:, :], op=ALU.add), reads=[XT[t], LNB], writes=[XT[t]])

        LN_STt = [Tile(LN_STATS.ap[:, t], "LNST%d" % t) for t in range(NT)]
        MVt = [Tile(MV.ap[:, t, :], "MV%d" % t) for t in range(NT)]
        VEt = [Tile(LN_VE.ap[:, t:t + 1], "VE%d" % t) for t in range(NT)]
        RSTDt = [Tile(RSTD.ap[:, t:t + 1], "RSTD%d" % t) for t in range(NT)]
        NMRt = [Tile(LN_NMR.ap[:, t:t + 1], "NMR%d" % t) for t in range(NT)]

        class LNPipe:
            def __init__(self, act_stats=False, junk=None):
                self.q = []
                self.act_stats = act_stats
                self.junk = junk

            def s1(self, t):
                st_, mv, ve, rstd = LN_STt[t], MVt[t], VEt[t], RSTDt[t]
                if self.act_stats:
                    junk = self.junk
                    S.op("act", lambda e: e.activation(out=junk[:, :], in_=XT[t][:, :], func=AF.Copy, accum_out=st_[:, 0, 0:1]), reads=[XT[t]], writes=[junk, st_])
                    S.op("act", lambda e: e.activation(out=junk[:, :], in_=XT[t][:, :], func=AF.Square, accum_out=st_[:, 0, 1:2]), reads=[XT[t]], writes=[junk, st_])
                    S.op("dve", lambda e: e.tensor_scalar(out=mv[:, 0:1], in0=st_[:, 0, 0:1], scalar1=1.0 / D, scalar2=None, op0=ALU.mult), reads=[st_], writes=[mv])
                    S.op("dve", lambda e: e.tensor_tensor(out=mv[:, 1:2], in0=mv[:, 0:1], in1=mv[:, 0:1], op=ALU.mult), reads=[mv], writes=[mv])
                    S.op("dve", lambda e: e.scalar_tensor_tensor(out=ve[:, :], in0=st_[:, 0, 1:2], scalar=1.0 / D, in1=mv[:, 1:2], op0=ALU.mult, op1=ALU.subtract), reads=[st_, mv], writes=[ve])
                    S.op("dve", lambda e: e.tensor_scalar(out=ve[:, :], in0=ve[:, :], scalar1=LN_EPS, scalar2=None, op0=ALU.add), reads=[ve], writes=[ve])
                else:
                    for h2 in range(2):
                        S.op("dve", lambda e, h2=h2: e.bn_stats(out=st_[:, h2, :], in_=XT[t][:, h2 * 512:(h2 + 1) * 512]), reads=[XT[t]], writes=[st_])
                    S.op("dve", lambda e: e.bn_aggr(out=mv[:, :], in_=st_[:, :, :].rearrange("p a b -> p (a b)")), reads=[st_], writes=[mv])
                    S.op("dve", lambda e: e.tensor_scalar(out=ve[:, :], in0=mv[:, 1:2], scalar1=LN_EPS, scalar2=None, op0=ALU.add), reads=[mv], writes=[ve])
                S.op("pool", lambda e: e.tensor_tensor(out=rstd[:, :], in0=ve[:, :], in1=NHALF[:, 0:1], op=ALU.pow), reads=[ve, NHALF], writes=[rstd])

            def s2(self, t):
                mv, rstd, nmr = MVt[t], RSTDt[t], NMRt[t]
                S.op("dve", lambda e: e.scalar_tensor_tensor(out=nmr[:, :], in0=mv[:, 0:1], scalar=-1.0, in1=rstd[:, :], op0=ALU.mult, op1=ALU.mult), reads=[mv, rstd], writes=[nmr])
                S.op("act", lambda e: e.activation(out=XT[t][:, :], in_=XT[t][:, :], func=AF.Identity, scale=rstd[:, :], bias=nmr[:, :]), reads=[XT[t], nmr, rstd], writes=[XT[t]])

            def s3(self, t):
                S.op("dve", lambda e: e.tensor_tensor(out=XT[t][:, :], in0=XT[t][:, :], in1=LNG[:, :], op=ALU.mult), reads=[XT[t], LNG], writes=[XT[t]])
                S.op("pool", lambda e: e.tensor_tensor(out=XT[t][:, :], in0=XT[t][:, :], in1=LNB[:, :], op=ALU.add), reads=[XT[t], LNB], writes=[XT[t]])

            def push(self, t):
                self.q.append([t, 0])
                self.step()

            def step(self):
                for ent in list(self.q):
                    if ent[1] == 0:
                        self.s1(ent[0])
                    elif ent[1] == 1:
                        self.s2(ent[0])
                    else:
                        self.s3(ent[0])
                    ent[1] += 1
                self.q = [en for en in self.q if en[1] < 3]

            def flush(self):
                while self.q:
                    self.step()

        NV, NJ = 48, 96
        BIG = 65536.0
        BIGW = 8192.0

        def moe_phase(l, s, last):
            jsh, jsc, jg = 24, 32, 40
            wall_d = wall_ds[l]
            IOA = bass.IndirectOffsetOnAxis
            with ExitStack() as ph:
                LG = S.sbuf("LG", [128, NT, 36], F32, st=ph)
                P12 = S.sbuf("P12", [128, 2, NT], F32, st=ph)
                IDXY = S.sbuf("IDXY", [128, NJ], I32, st=ph)
                IDXG = S.sbuf("IDXG", [128, NJ], I32, st=ph)
                WIDX = S.sbuf("WIDX", [128, NV], I32, st=ph)
                NW, NH = 3, 4

                with ExitStack() as ph1:
                    WR = S.sbuf("WR", [128, 8, 36], F32, st=ph1)
                    RB = S.sbuf("RB", [128, 36], F32, st=ph1)
                    H32 = Rot([S.sbuf("H32_%d" % i, [128, 8, 128], F32, st=ph1) for i in range(3)])
                    SCB = S.sbuf("SCB", [128, D], F32, st=ph1)
                    SHB = S.sbuf("SHB", [128, D], F32, st=ph1)
                    TM32 = Rot([S.sbuf("TM32_%d" % i, [128, D], F32, st=ph1) for i in range(2)])
                    HB = Rot([S.sbuf("HB_%d" % i, [128, D], BF16, st=ph1) for i in range(2)])
                    tp_rot = Rot([S.psum("tp%d" % i, [128, 512], F32, st=ph1) for i in range(4)])
                    lg_rot = Rot([S.psum("lgp%d" % i, [128, 512], F32, st=ph1) for i in range(2)])
                    scrs = [S.sbuf("scr%d" % i, [128, 8, 128], F32, st=ph1) for i in range(2)]
                    S.dma("sp", lambda e: [e.dma_start(out=WR[:, :, :], in_=wr_d[l].rearrange("(c p) n -> p c n", p=128)),
                                           e.dma_start(out=RB[:, :], in_=br_d[l:l + 1, :].partition_broadcast(128))],
                          reads=[DR], writes=[WR, RB], sync=WR, n=2)
                    build_g1b(l, jsc, s, tp_rot.tiles[0:2], scrs[0], dst=SCB)
                    build_g1b(l, jsh, s, tp_rot.tiles[2:4], scrs[1], dst=SHB)
                    build_g1b(l, jg, s, lg_rot.tiles, scrs[0])
                    load_ln(l, 1, not last)
                    h32s = {}

                    def router(t):
                        h32 = h32s.pop(t)
                        lgp = lg_rot.next()
                        for c in range(8):
                            S.op("pe", lambda e, c=c: e.matmul(lgp[:, 0:36], lhsT=h32[:, c, :], rhs=WR[:, c, :], start=(c == 0), stop=(c == 7)),
                                 reads=[h32, WR], writes=[lgp])
                        S.op("dve", lambda e: e.tensor_tensor(out=LG[:, t, :], in0=lgp[:, 0:36], in1=RB[:, :], op=ALU.add), reads=[lgp, RB], writes=[LG])

                    for t in range(NT):
                        h32 = H32.next()
                        h32s[t] = h32
                        transpose_tile(t, l, jsc, jsh, s, tp_rot, None, None, f32_dst=h32)
                        tm = TM32.next()
                        hb = HB.next()
                        S.op("dve", lambda e, t=t, tm=tm: e.tensor_tensor(out=tm[:, :], in0=XT[t][:, :], in1=SCB[:, :], op=ALU.mult), reads=[XT[t], SCB], writes=[tm])
                        S.op("pool", lambda e, tm=tm, hb=hb: e.tensor_tensor(out=hb[:, :], in0=tm[:, :], in1=SHB[:, :], op=ALU.add), reads=[tm, SHB], writes=[hb])
                        S.dma("sp", lambda e, t=t, hb=hb: e.dma_start(out=htok_d[t * 128:(t + 1) * 128, :], in_=hb[:, :]), reads=[hb], writes=[], sync=hb)
                        if t >= 1:
                            router(t - 1)
                    router(NT - 1)
                    S.fence()
                with ExitStack() as ph1:
                    def sb(name, shape, dt=F32):
                        return S.sbuf(name, shape, dt, st=ph1)
                    C2 = sb("C2", [128, NC2])
                    S.dma("sp", lambda e: e.dma_start(out=C2[:, :], in_=const2_d[:, :]), reads=[DR], writes=[C2], sync=C2)
                    GMAX = sb("GMAX", [128, NT]); DG_ = sb("DGL", [128, NT, 4]); GE = sb("GE", [128, NT, 4]); GS = sb("GS", [128, NT])
                    GW = sb("GW", [128, NT]); PEN = sb("PEN", [128, NT, 4]); EM = sb("EM", [128, NT, 32]); M1 = sb("M1", [128, NT])
                    OH1 = sb("OH1", [128, NT, 32]); EM2 = sb("EM2", [128, NT, 32]); M2 = sb("M2", [128, NT]); OH2 = sb("OH2", [128, NT, 32])
                    DM = sb("DM", [128, NT]); P1 = sb("P1", [128, NT]); P2 = sb("P2", [128, NT])
                    GL = LG[:, :, 0:4]
                    EL = LG[:, :, 4:36]
                    V = lambda f, r, w: S.op("dve", f, reads=r, writes=w)
                    V(lambda e: e.tensor_reduce(out=GMAX[:, :], in_=GL, axis=AX.X, op=ALU.max), [LG], [GMAX])
                    V(lambda e: e.tensor_tensor(out=DG_[:, :, :], in0=GL, in1=GMAX[:, :].unsqueeze(2).to_broadcast([128, NT, 4]), op=ALU.subtract), [LG, GMAX], [DG_])
                    S.op("act", lambda e: e.activation(out=GE[:, :, :], in_=DG_[:, :, :], func=AF.Exp), reads=[DG_], writes=[GE])
                    V(lambda e: e.tensor_reduce(out=GS[:, :], in_=GE[:, :, :], axis=AX.X, op=ALU.add), [GE], [GS])
                    V(lambda e: e.reciprocal(out=GW[:, :], in_=GS[:, :]), [GS], [GW])
                    V(lambda e: e.tensor_scalar(out=PEN[:, :, :], in0=DG_[:, :, :], scalar1=0.0, scalar2=-1e30, op0=ALU.is_lt, op1=ALU.mult), [DG_], [PEN])
                    V(lambda e: e.tensor_tensor(out=EM[:, :, :].rearrange("p t (g j) -> p t g j", g=4), in0=EL.rearrange("p t (g j) -> p t g j", g=4),
                                                in1=PEN[:, :, :].unsqueeze(3).to_broadcast([128, NT, 4, 8]), op=ALU.add), [LG, PEN], [EM])
                    V(lambda e: e.tensor_reduce(out=M1[:, :], in_=EM[:, :, :], axis=AX.X, op=ALU.max), [EM], [M1])
                    V(lambda e: e.tensor_tensor(out=OH1[:, :, :], in0=EM[:, :, :], in1=M1[:, :].unsqueeze(2).to_broadcast([128, NT, 32]), op=ALU.is_equal), [EM, M1], [OH1])
                    V(lambda e: e.scalar_tensor_tensor(out=EM2[:, :, :], in0=OH1[:, :, :], scalar=-1e30, in1=EM[:, :, :], op0=ALU.mult, op1=ALU.add), [OH1, EM], [EM2])
                    V(lambda e: e.tensor_reduce(out=M2[:, :], in_=EM2[:, :, :], axis=AX.X, op=ALU.max), [EM2], [M2])
                    V(lambda e: e.tensor_tensor(out=OH2[:, :, :], in0=EM2[:, :, :], in1=M2[:, :].unsqueeze(2).to_broadcast([128, NT, 32]), op=ALU.is_equal), [EM2, M2], [OH2])
                    V(lambda e: e.tensor_tensor(out=DM[:, :], in0=M2[:, :], in1=M1[:, :], op=ALU.subtract), [M1, M2], [DM])
                    S.op("act", lambda e: e.activation(out=DM[:, :], in_=DM[:, :], func=AF.Exp), reads=[DM], writes=[DM])
                    V(lambda e: e.tensor_scalar(out=DM[:, :], in0=DM[:, :], scalar1=1.0, scalar2=None, op0=ALU.add), [DM], [DM])
                    V(lambda e: e.reciprocal(out=P1[:, :], in_=DM[:, :]), [DM], [P1])
                    V(lambda e: e.tensor_scalar(out=P2[:, :], in0=P1[:, :], scalar1=-1.0, scalar2=1.0, op0=ALU.mult, op1=ALU.add), [P1], [P2])
                    V(lambda e: e.tensor_tensor(out=P12[:, 0, :], in0=P1[:, :], in1=GW[:, :], op=ALU.mult), [P1, GW], [P12])
                    V(lambda e: e.tensor_tensor(out=P12[:, 1, :], in0=P2[:, :], in1=GW[:, :], op=ALU.mult), [P2, GW], [P12])
                    M = sb("M", [128, NT, 32]); MB = sb("MB", [128, NT, 32], BF16)
                    EXC = sb("EXC", [128, NT, 32]); TOT = sb("TOT", [128, NT, 32]); OFFS = sb("OFFS", [128, NT + 1, 32])
                    ip = S.psum("ip", [128, 512], F32, st=ph1)
                    tpp = S.psum("tpp", [128, 512], F32, st=ph1)
                    V(lambda e: e.tensor_tensor(out=M[:, :, :], in0=OH1[:, :, :], in1=OH2[:, :, :], op=ALU.add), [OH1, OH2], [M])
                    V(lambda e: e.tensor_copy(out=MB[:, :, :], in_=M[:, :, :]), [M], [MB])
                    S.op("pe", lambda e: e.matmul(ip[:, :], lhsT=TRIB[:, :], rhs=MB[:, :, :].rearrange("p t e -> p (t e)"), start=True, stop=True), reads=[TRIB, MB], writes=[ip])
                    S.op("pe", lambda e: e.matmul(tpp[:, :], lhsT=ONEB[:, :], rhs=MB[:, :, :].rearrange("p t e -> p (t e)"), start=True, stop=True), reads=[ONEB, MB], writes=[tpp])
                    V(lambda e: e.tensor_tensor(out=EXC[:, :, :], in0=ip[:, :].rearrange("p (t e) -> p t e", e=32), in1=M[:, :, :], op=ALU.subtract), [ip, M], [EXC])
                    S.op("act", lambda e: e.activation(out=TOT[:, :, :], in_=tpp[:, :].rearrange("p (t e) -> p t e", e=32), func=AF.Copy), reads=[tpp], writes=[TOT])
                    V(lambda e: e.memset(OFFS[:, 0, :], 0.0), [], [OFFS])
                    for t in range(1, NT + 1):
                        V(lambda e, t=t: e.tensor_tensor(out=OFFS[:, t, :], in0=OFFS[:, t - 1, :], in1=TOT[:, t - 1, :], op=ALU.add), [OFFS, TOT], [OFFS])
                    TMP8 = sb("TMP8", [128, 32, 8]); NVv = sb("NVv", [128, 32]); TMP32 = sb("TMP32", [128, 32, 32]); VB = sb("VB", [128, 32]); VE = sb("VE", [128, 32]); BASE = sb("BASE", [128, 32])
                    V(lambda e: e.tensor_tensor(out=TMP8[:, :, :], in0=C2[:, C2_THR:C2_THR + 8].unsqueeze(1).to_broadcast([128, 32, 8]),
                                                in1=OFFS[:, NT, :].unsqueeze(2).to_broadcast([128, 32, 8]), op=ALU.is_lt), [C2, OFFS], [TMP8])
                    V(lambda e: e.tensor_reduce(out=NVv[:, :], in_=TMP8[:, :, :], axis=AX.X, op=ALU.add), [TMP8], [NVv])
                    V(lambda e: e.tensor_tensor(out=TMP32[:, :, :], in0=C2[:, C2_LT:C2_LT + 1024].rearrange("p (a b) -> p a b", a=32),
                                                in1=NVv[:, :].unsqueeze(1).to_broadcast([128, 32, 32]), op=ALU.mult), [C2, NVv], [TMP32])
                    V(lambda e: e.tensor_reduce(out=VB[:, :], in_=TMP32[:, :, :], axis=AX.X, op=ALU.add), [TMP32], [VB])
                    V(lambda e: e.tensor_tensor(out=VE[:, :], in0=VB[:, :], in1=NVv[:, :], op=ALU.add), [VB, NVv], [VE])
                    V(lambda e: e.tensor_scalar(out=BASE[:, :], in0=VB[:, :], scalar1=256.0, scalar2=None, op0=ALU.mult), [VB], [BASE])
                    T1 = sb("T1r", [128, NT, 32]); POS = sb("POS", [128, 2, NT]); Q = sb("Q", [128, 2, NT]); QI = sb("QI", [128, 2, NT], I32); QF = sb("QF", [128, 2, NT])
                    CORR = sb("CORR", [128, 2, NT]); PM = sb("PM", [128, 2, NT]); DEST = sb("DEST", [128, 2, NT]); DESTI = sb("DESTI", [128, 2, NT], I32)
                    VALI = sb("VALI", [128, 2, NT], I32); FILLF = sb("FILLF", [128, NJ]); FILLI = sb("FILLI", [128, NJ], I32); IF = sb("IF", [128, NJ]); IGE = sb("IGE", [128, NJ])
                    V(lambda e: e.tensor_tensor(out=EXC[:, :, :], in0=EXC[:, :, :], in1=OFFS[:, 0:NT, :], op=ALU.add), [EXC, OFFS], [EXC])
                    V(lambda e: e.tensor_tensor(out=EXC[:, :, :], in0=EXC[:, :, :], in1=BASE[:, :].unsqueeze(1).to_broadcast([128, NT, 32]), op=ALU.add), [EXC, BASE], [EXC])
                    for k, OH in ((0, OH1), (1, OH2)):
                        V(lambda e, OH=OH: e.tensor_tensor(out=T1[:, :, :], in0=OH[:, :, :], in1=EXC[:, :, :], op=ALU.mult), [OH, EXC], [T1])
                        V(lambda e, k=k: e.tensor_reduce(out=POS[:, k, :], in_=T1[:, :, :], axis=AX.X, op=ALU.add), [T1], [POS])
                    V(lambda e: e.tensor_scalar(out=Q[:, :, :], in0=POS[:, :, :], scalar1=1.0 / 128, scalar2=None, op0=ALU.mult), [POS], [Q])
                    V(lambda e: e.tensor_copy(out=QI[:, :, :], in_=Q[:, :, :]), [Q], [QI])
                    V(lambda e: e.tensor_copy(out=QF[:, :, :], in_=QI[:, :, :]), [QI], [QF])
                    V(lambda e: e.tensor_tensor(out=CORR[:, :, :], in0=QF[:, :, :], in1=Q[:, :, :], op=ALU.is_gt), [QF, Q], [CORR])
                    V(lambda e: e.tensor_tensor(out=QF[:, :, :], in0=QF[:, :, :], in1=CORR[:, :, :], op=ALU.subtract), [QF, CORR], [QF])
                    V(lambda e: e.scalar_tensor_tensor(out=PM[:, :, :], in0=QF[:, :, :], scalar=-128.0, in1=POS[:, :, :], op0=ALU.mult, op1=ALU.add), [QF, POS], [PM])
                    V(lambda e: e.scalar_tensor_tensor(out=DEST[:, :, :], in0=PM[:, :, :], scalar=float(NJ), in1=QF[:, :, :], op0=ALU.mult, op1=ALU.add), [PM, QF], [DEST])
                    V(lambda e: e.tensor_copy(out=DESTI[:, :, :], in_=DEST[:, :, :]), [DEST], [DESTI])
                    V(lambda e: e.tensor_copy(out=VALI[:, :, :], in_=C2[:, C2_VAL:C2_VAL + 32].rearrange("p (k t) -> p k t", k=2)), [C2], [VALI])
                    V(lambda e: e.memset(FILLF[:, :], BIG), [], [FILLF])
                    V(lambda e: e.tensor_copy(out=FILLI[:, :], in_=FILLF[:, :]), [FILLF], [FILLI])
                    tab_v = tab_d.rearrange("(p j) o -> p (j o)", p=128)
                    S.dma("sp", lambda e: e.dma_start(out=tab_v, in_=FILLI[:, :]), reads=[FILLI], writes=[TABF], sync=TABF)
                    last_sc = None
                    for k in range(2):
                        for t in range(NT):
                            last_sc = S.dma("pool", lambda e, k=k, t=t: e.indirect_dma_start(out=tab_d[:, :], out_offset=IOA(ap=DESTI[:, k, t:t + 1], axis=0), in_=VALI[:, k, t:t + 1], in_offset=None,
                                                                                            bounds_check=BC[NJ * 128 - 1], oob_is_err=False), reads=[DESTI, VALI, TABF], writes=[], sync=TABS)
                    VA = sb("VA", [128, NV, 32]); VBm = sb("VBm", [128, NV, 32]); WF = sb("WF", [128, NV])
                    vrow = C2[:, C2_VROW:C2_VROW + NV].unsqueeze(2).to_broadcast([128, NV, 32])
                    V(lambda e: e.tensor_tensor(out=VA[:, :, :], in0=vrow, in1=VB[:, :].unsqueeze(1).to_broadcast([128, NV, 32]), op=ALU.is_ge), [C2, VB], [VA])
                    V(lambda e: e.tensor_tensor(out=VBm[:, :, :], in0=vrow, in1=VE[:, :].unsqueeze(1).to_broadcast([128, NV, 32]), op=ALU.is_lt), [C2, VE], [VBm])
                    V(lambda e: e.tensor_tensor(out=VA[:, :, :], in0=VA[:, :, :], in1=VBm[:, :, :], op=ALU.mult), [VA, VBm], [VA])
                    V(lambda e: e.tensor_tensor(out=VA[:, :, :], in0=VA[:, :, :], in1=C2[:, C2_EROW:C2_EROW + 32].unsqueeze(1).to_broadcast([128, NV, 32]), op=ALU.mult), [VA, C2], [VA])
                    V(lambda e: e.tensor_reduce(out=WF[:, :], in_=VA[:, :, :], axis=AX.X, op=ALU.add), [VA], [WF])
                    V(lambda e: e.tensor_scalar(out=WF[:, :], in0=WF[:, :], scalar1=BIGW, scalar2=None, op0=ALU.add), [WF], [WF])
                    V(lambda e: e.tensor_copy(out=WIDX[:, :], in_=WF[:, :]), [WF], [WIDX])
                    TABS.w = last_sc
                    S.dma("sp", lambda e: e.dma_start(out=IDXY[:, :], in_=tab_v), reads=[TABS], writes=[IDXY], sync=IDXY)
                    TABS.w = None
                    V(lambda e: e.tensor_copy(out=IF[:, :], in_=IDXY[:, :]), [IDXY], [IF])
                    V(lambda e: e.tensor_single_scalar(out=IGE[:, :], in_=IF[:, :], scalar=2048.0, op=ALU.is_ge), [IF], [IGE])
                    V(lambda e: e.scalar_tensor_tensor(out=IF[:, :], in0=IGE[:, :], scalar=-2048.0, in1=IF[:, :], op0=ALU.mult, op1=ALU.add), [IGE, IF], [IF])
                    V(lambda e: e.tensor_copy(out=IDXG[:, :], in_=IF[:, :]), [IF], [IDXG])
                    S.fence()
                with ExitStack() as ph2:
                    W = [S.sbuf("W%d" % i, [128, 6144], BF16, st=ph2) for i in range(NW)]
                    HS = [S.sbuf("HS%d" % i, [128, D], BF16, st=ph2) for i in range(NH)]
                    for hs in HS:
                        S.op("dve", lambda e, hs=hs: e.memset(hs[:, :], 0.0), writes=[hs])

                    def issue_w(v):
                        w = W[v % NW]
                        S.dma("pool", lambda e: e.indirect_dma_start(out=w[:, :], out_offset=None, in_=wall_d[:, :], in_offset=IOA(ap=WIDX[:, v:v + 1], axis=0),
                                                                     bounds_check=BC[4095], oob_is_err=False), reads=[DR, WIDX, w], writes=[w], sync=w)

                    for v in range(NW):
                        issue_w(v)
                    HST = Rot([S.sbuf("HST%d" % i, [128, 8, 128], BF16, st=ph2) for i in range(2)])
                    SG = Rot([S.sbuf("SG%d" % i, [128, 256], F32, st=ph2) for i in range(2)])
                    HID = Rot([S.sbuf("HID%d" % i, [128, 256], BF16, st=ph2) for i in range(3)])
                    HIDT = Rot([S.sbuf("HIDT%d" % i, [128, 2, 128], BF16, st=ph2) for i in range(3)])
                    YSB = Rot([S.sbuf("YSB%d" % i, [128, D], F32, st=ph2) for i in range(3)])
                    xT_rot = Rot([S.psum("xTp%d" % i, [128, 8, 128], BF16, st=ph2) for i in range(2)])
                    gu_rot = Rot([S.psum("gu%d" % i, [128, 512], F32, st=ph2) for i in range(2)])
                    hTp = S.psum("hTp", [128, 2, 2, 128], BF16, st=ph2)
                    hT_rot = Rot([Tile(hTp.ap[:, i], "hTp%d" % i) for i in range(2)])
                    y_rot = Rot([S.psum("yp%d" % i, [128, 512], F32, st=ph2) for i in range(3)])
                    state = {}

                    def issue_gather(j):
                        hs = HS[j % NH]
                        S.dma("pool", lambda e: e.indirect_dma_start(out=hs[:, :], out_offset=None, in_=htok_d[:, :], in_offset=IOA(ap=IDXG[:, j:j + 1], axis=0),
                                                                     bounds_check=BC[S_LEN - 1], oob_is_err=False), reads=[DR, IDXG, hs], writes=[hs], sync=hs)

                    def do_T8(j):
                        hs = HS[j % NH]
                        xp = xT_rot.next()
                        for c in range(8):
                            S.op("pe", lambda e, c=c: e.transpose(xp[:, c, :], hs[:, c * 128:(c + 1) * 128], IDB[:, :]), reads=[hs, IDB], writes=[xp])
                        hst = HST.next()
                        S.op("act", lambda e: e.activation(out=hst[:, :, :], in_=xp[:, :, :], func=AF.Copy), reads=[xp], writes=[hst])
                        state[j] = [hst, None, None]

                    def do_GU(j):
                        w = W[(j // 2) % NW]
                        hst = state[j][0]
                        gp = gu_rot.next()
                        for c in range(8):
                            S.op("pe", lambda e, c=c: e.matmul(gp[:, :], lhsT=hst[:, c, :], rhs=w[:, c * 512:(c + 1) * 512], start=(c == 0), stop=(c == 7)),
                                 reads=[hst, w], writes=[gp])
                        sg = SG.next()
                        hid = HID.next()
                        S.op("act", lambda e: e.activation(out=sg[:, :], in_=gp[:, 0:256], func=AF.Silu), reads=[gp], writes=[sg])
                        S.op("dve", lambda e: e.tensor_tensor(out=hid[:, :], in0=gp[:, 256:512], in1=sg[:, :], op=ALU.mult), reads=[gp, sg], writes=[hid])
                        state[j][1] = hid

                    def do_HT(j):
                        hid = state[j][1]
                        hp = hT_rot.next()
                        for c in range(2):
                            S.op("pe", lambda e, c=c: e.transpose(hp[:, c, :], hid[:, c * 128:(c + 1) * 128], IDB[:, :]), reads=[hid, IDB], writes=[hp])
                        hT = HIDT.next()
                        S.op("act", lambda e: e.activation(out=hT[:, :, :], in_=hp[:, :, :], func=AF.Copy), reads=[hp], writes=[hT])
                        state[j][2] = hT

                    def do_D(j):
                        w = W[(j // 2) % NW]
                        hT = state.pop(j)[2]
                        ysb = YSB.next()
                        for hf in range(2):
                            yp = y_rot.next()
                            for c in range(2):
                                S.op("pe", lambda e, hf=hf, c=c, yp=yp: e.matmul(yp[:, :], lhsT=hT[:, c, :], rhs=w[:, 4096 + c * 1024 + hf * 512:4096 + c * 1024 + (hf + 1) * 512],
                                                                                 start=(c == 0), stop=(c == 1)), reads=[hT, w], writes=[yp])
                            S.op("dve", lambda e, yp=yp, hf=hf: e.tensor_tensor(out=ysb[:, hf * 512:(hf + 1) * 512], in0=yp[:, :], in1=G1B[:, hf * 512:(hf + 1) * 512], op=ALU.mult),
                                 reads=[yp, G1B], writes=[ysb])
                        S.dma("pool", lambda e: e.indirect_dma_start(out=ys_d[:, :], out_offset=IOA(ap=IDXY[:, j:j + 1], axis=0), in_=ysb[:, :], in_offset=None,
                                                                     bounds_check=BC[2 * S_LEN - 1], oob_is_err=False), reads=[ysb, IDXY], writes=[], sync=ysb)

                    for j in range(NH):
                        issue_gather(j)
                    do_T8(0)
                    issue_gather(NH)
                    for i in range(NJ + 2):
                        if i + 1 < NJ:
                            do_T8(i + 1)
                            if i + 1 + NH < NJ:
                                issue_gather(i + 1 + NH)
                        if i < NJ:
                            do_GU(i)
                        if 1 <= i <= NJ:
                            do_HT(i - 1)
                        if 2 <= i:
                            do_D(i - 2)
                            if (i - 2) % 2 == 1:
                                nv_ = (i - 2) // 2 + NW
                                if nv_ < NV:
                                    issue_w(nv_)
                    S.fence()
                with ExitStack() as ph3:
                    Y0 = Rot([S.sbuf("Y0_%d" % i, [128, D], F32, st=ph3) for i in range(3)])
                    Y1 = Rot([S.sbuf("Y1_%d" % i, [128, D], F32, st=ph3) for i in range(3)])
                    LJ = S.sbuf("LNJ", [128, D], BF16, st=ph3)
                    lnp = LNPipe(act_stats=True, junk=LJ)
                    for t in range(NT):
                        y0 = Y0.next(); y1 = Y1.next()
                        S.dma("sp", lambda e, t=t, y0=y0: e.dma_start(out=y0[:, :], in_=ys_d[t * 128:(t + 1) * 128, :]), reads=[DR], writes=[y0], sync=y0)
                        S.dma("sp", lambda e, t=t, y1=y1: e.dma_start(out=y1[:, :], in_=ys_d[S_LEN + t * 128:S_LEN + (t + 1) * 128, :]), reads=[DR], writes=[y1], sync=y1)
                        S.op("dve", lambda e, t=t, y0=y0: e.scalar_tensor_tensor(out=XT[t][:, :], in0=y0[:, :], scalar=P12[:, 0, t:t + 1], in1=XT[t][:, :], op0=ALU.mult, op1=ALU.add),
                             reads=[y0, P12, XT[t]], writes=[XT[t]])
                        S.op("dve", lambda e, t=t, y1=y1: e.scalar_tensor_tensor(out=XT[t][:, :], in0=y1[:, :], scalar=P12[:, 1, t:t + 1], in1=XT[t][:, :], op0=ALU.mult, op1=ALU.add),
                             reads=[y1, P12, XT[t]], writes=[XT[t]])
                        lnp.push(t)
                    lnp.flush()
                    S.fence()

        def l0_mixer(s):
            l = 0
            jsh, jsc, jg = 0, 8, 16
            CATA = [Tile(HT.ap[:, 0:4, B * 512:(B + 1) * 512], "CATA%d" % B) for B in range(4)]
            CATB = [Tile(HT.ap[:, 4:8, t * 128:(t + 1) * 128], "CATB%d" % t) for t in range(NT)]
            load_ln(l, 0, True)
            with ExitStack() as p12:
                HCT = S.sbuf("HCT", [128, 4, 2080], BF16, st=p12)
                S.op("pool", lambda e: e.memset(HCT[:, :, 0:30], 0.0), writes=[HCT])
                with ExitStack() as p1:
                    WINC = S.sbuf("WINC", [128, 8, 1024], BF16, st=p1)
                    S.dma("pool", lambda e: e.dma_start(out=WINC[:, :, :], in_=ab_w_in_d.rearrange("(c p) n -> p c n", p=128)[:, :, 0:1024]),
                          reads=[DR], writes=[WINC], sync=WINC)
                    HTB = Rot([S.sbuf("HTB%d" % i, [128, 8, 512], BF16, st=p1) for i in range(2)])
                    SIG = Rot([S.sbuf("SIG%d" % i, [128, 512], F32, st=p1) for i in range(2)])
                    tp_rot = Rot([S.psum("tp%d" % i, [128, 512], F32, st=p1) for i in range(4)])
                    ag_rot = Rot([S.psum("ag%d" % i, [128, 512], F32, st=p1) for i in range(4)])
                    scr = S.sbuf("scr", [128, 8, 128], F32, st=p1)
                    build_g1b(l, jg, s, ag_rot.tiles[0:2], scr)
                    for B in range(4):
                        htb = HTB.next()
                        for i in range(4):
                            transpose_tile(4 * B + i, l, jsc, jsh, s, tp_rot, lambda c, i=i, htb=htb: htb[:, c, i * 128:(i + 1) * 128], htb)
                        for cc in range(4):
                            a_p = ag_rot.next()
                            g_p = ag_rot.next()
                            for kc in range(8):
                                S.op("pe", lambda e, kc=kc, cc=cc, a_p=a_p, htb=htb: e.matmul(a_p[:, :], lhsT=WINC[:, kc, cc * 128:(cc + 1) * 128], rhs=htb[:, kc, :], start=(kc == 0), stop=(kc == 7)),
                                     reads=[WINC, htb], writes=[a_p])
                            for kc in range(8):
                                S.op("pe", lambda e, kc=kc, cc=cc, g_p=g_p, htb=htb: e.matmul(g_p[:, :], lhsT=WINC[:, kc, 512 + cc * 128:512 + (cc + 1) * 128], rhs=htb[:, kc, :], start=(kc == 0), stop=(kc == 7)),
                                     reads=[WINC, htb], writes=[g_p])
                            sig = SIG.next()
                            S.op("act", lambda e, sig=sig, g_p=g_p: e.activation(out=sig[:, :], in_=g_p[:, :], func=AF.Sigmoid), reads=[g_p], writes=[sig])
                            S.op("dve", lambda e, sig=sig, a_p=a_p, cc=cc, B=B: e.tensor_tensor(out=HCT[:, cc, 30 + B * 512:30 + (B + 1) * 512], in0=a_p[:, :], in1=sig[:, :], op=ALU.mult),
                                 reads=[a_p, sig], writes=[HCT])
                    S.fence()
                if stop_after == "l0a":
                    return
                with ExitStack() as p2:
                    CWS = S.sbuf("CWS", [32, 512], F32, st=p2)
                    CW = S.sbuf("CW", [128, 4, 31], F32, st=p2)
                    DG = [S.sbuf("DG%d" % cc, [128, 31, 128], BF16, st=p2) for cc in range(4)]
                    Y = S.sbuf("Y", [128, 4, 512], F32, st=p2)
                    YSQ = S.sbuf("YSQ", [128, 4, 512], F32, st=p2)
                    MEAN = S.sbuf("MEAN", [128, 512], F32, st=p2)
                    MSQ = S.sbuf("MSQ", [128, 512], F32, st=p2)
                    VAR = S.sbuf("VAR", [128, 512], F32, st=p2)
                    RS = S.sbuf("RS", [128, 512], F32, st=p2)
                    T1 = Rot([S.sbuf("T1_%d" % i, [128, 512], F32, st=p2) for i in range(2)])
                    y_ps = [S.psum("yps%d" % i, [128, 512], F32, st=p2) for i in range(4)]
                    st_rot = Rot([S.psum("stp%d" % i, [128, 512], F32, st=p2) for i in range(2)])
                    S.dma("sp", lambda e: e.dma_start(out=CWS[0:31, :], in_=conv_w_d[:, :]), reads=[DR], writes=[CWS], sync=CWS)
                    for cc in range(4):
                        pt = st_rot.next()
                        S.op("pe", lambda e, cc=cc, pt=pt: e.transpose(pt[:, 0:31], CWS[0:31, cc * 128:(cc + 1) * 128], CONST[0:31, C_ID:C_ID + 31]), reads=[CWS, CONST], writes=[pt])
                        S.op("dve", lambda e, cc=cc, pt=pt: e.tensor_copy(out=CW[:, cc, :], in_=pt[:, 0:31]), reads=[pt], writes=[CW])
                    for cc in range(4):
                        for j in range(31):
                            S.op("dve", lambda e, cc=cc, j=j: e.tensor_scalar(out=DG[cc][:, j, :], in0=ident, scalar1=CW[:, cc, j:j + 1], scalar2=None, op0=ALU.mult),
                                 reads=[CONST, CW], writes=[DG[cc]])
                    for B in range(4):
                        for cc in range(4):
                            for j in range(31):
                                S.op("pe", lambda e, cc=cc, j=j, B=B: e.matmul(y_ps[cc][:, :], lhsT=DG[cc][:, j, :], rhs=HCT[:, cc, B * 512 + j:B * 512 + j + 512], start=(j == 0), stop=(j == 30)),
                                     reads=[DG[cc], HCT], writes=[y_ps[cc]])
                            S.op("act", lambda e, cc=cc: e.activation(out=Y[:, cc, :], in_=y_ps[cc][:, :], func=AF.Identity, bias=PVT[:, cc:cc + 1]), reads=[y_ps[cc], PVT], writes=[Y])
                            S.op("act", lambda e, cc=cc: e.activation(out=YSQ[:, cc, :], in_=y_ps[cc][:, :], func=AF.Square, bias=PVT[:, cc:cc + 1]), reads=[y_ps[cc], PVT], writes=[YSQ])
                        mean_ps = st_rot.next()
                        msq_ps = st_rot.next()
                        for cc in range(4):
                            S.op("pe", lambda e, cc=cc, mean_ps=mean_ps: e.matmul(mean_ps[:, :], lhsT=ones, rhs=Y[:, cc, :], start=(cc == 0), stop=(cc == 3)), reads=[CONST, Y], writes=[mean_ps])
                        for cc in range(4):
                            S.op("pe", lambda e, cc=cc, msq_ps=msq_ps: e.matmul(msq_ps[:, :], lhsT=ones, rhs=YSQ[:, cc, :], start=(cc == 0), stop=(cc == 3)), reads=[CONST, YSQ], writes=[msq_ps])
                        S.op("act", lambda e, mean_ps=mean_ps: e.activation(out=MEAN[:, :], in_=mean_ps[:, :], func=AF.Copy, scale=1.0 / 512), reads=[mean_ps], writes=[MEAN])
                        S.op("dve", lambda e: e.tensor_tensor(out=MSQ[:, :], in0=MEAN[:, :], in1=MEAN[:, :], op=ALU.mult), reads=[MEAN], writes=[MSQ])
                        S.op("dve", lambda e, msq_ps=msq_ps: e.scalar_tensor_tensor(out=VAR[:, :], in0=msq_ps[:, :], scalar=1.0 / 512, in1=MSQ[:, :], op0=ALU.mult, op1=ALU.subtract),
                             reads=[msq_ps, MSQ], writes=[VAR])
                        S.op("act", lambda e: e.activation(out=VAR[:, :], in_=VAR[:, :], func=AF.Ln, bias=LN_EPS), reads=[VAR], writes=[VAR])
                        S.op("act", lambda e: e.activation(out=RS[:, :], in_=VAR[:, :], func=AF.Exp, scale=-0.5), reads=[VAR], writes=[RS])
                        for cc in range(4):
                            t1 = T1.next()
                            S.op("dve", lambda e, cc=cc, t1=t1: e.tensor_tensor(out=t1[:, :], in0=Y[:, cc, :], in1=MEAN[:, :], op=ALU.subtract), reads=[Y, MEAN], writes=[t1])
                            S.op("dve", lambda e, cc=cc, t1=t1: e.tensor_tensor(out=t1[:, :], in0=t1[:, :], in1=RS[:, :], op=ALU.mult), reads=[t1, RS], writes=[t1])
                            S.op("act", lambda e, cc=cc, t1=t1, B=B: e.activation(out=CATA[B][:, cc, :], in_=t1[:, :], func=AF.Silu, scale=PVT[:, 4 + cc:5 + cc], bias=PVT[:, 8 + cc:9 + cc]),
                                 reads=[t1, PVT], writes=[CATA[B]])
                    S.fence()
            if stop_after == "l0b":
                return
            with ExitStack() as p3:
                def sb(name, shape, dt=F32):
                    return S.sbuf(name, shape, dt, st=p3)
                WING = sb("WING", [128, 8, 1552], BF16)
                WOUT = sb("WOUT", [128, 8, D], BF16)
                S.dma("pool", lambda e: e.dma_start(out=WING[:, :, :], in_=ab_w_in_d.rearrange("(c p) n -> p c n", p=128)[:, :, 1024:2576]), reads=[DR], writes=[WING], sync=WING)
                S.dma("pool", lambda e: e.dma_start(out=WOUT[:, :, :], in_=ab_w_out_d.rearrange("(c p) n -> p c n", p=128)), reads=[DR], writes=[WOUT], sync=WOUT)
                S.op("pool", lambda e: e.tensor_tensor(out=WOUT[:, :, :], in0=WOUT[:, :, :], in1=G1B[:, :].unsqueeze(1).to_broadcast([128, 8, D]), op=ALU.mult), reads=[WOUT, G1B], writes=[WOUT])
                GWS = sb("GWS", [32, 256])
                GWB = sb("GWB", [32, 256], BF16)
                S.op("dve", lambda e: e.memset(GWS[:, :], 0.0), writes=[GWS])
                S.dma("sp", lambda e: [e.dma_start(out=GWS[0:16, :], in_=gate_w_d[:, :]), e.dma_start(out=GWS[16:17, :], in_=gate_b_d[:, :])], reads=[DR], writes=[GWS], sync=GWS, n=2)
                S.op("dve", lambda e: e.tensor_copy(out=GWB[:, :], in_=GWS[:, :]), reads=[GWS], writes=[GWB])
                GLT = Rot([sb("GLT%d" % i, [32, 512], BF16) for i in range(2)])
                for g_ in GLT.tiles:
                    S.op("dve", lambda e, g_=g_: e.memset(g_[:, :], 1.0), writes=[g_])
                NG = sb("NG", [128, 512])
                S.dma("sp", lambda e: e.dma_start(out=NG[:, :], in_=gnorm_d[0:1, :].partition_broadcast(128)), reads=[DR], writes=[NG], sync=NG)
                S32 = sb("S32", [128, 2, 128])
                SBF = sb("SBF", [128, 2, 128], BF16)
                S.op("dve", lambda e: e.memset(S32[:, :, :], 0.0), writes=[S32])
                S.op("dve", lambda e: e.memset(SBF[:, :, :], 0.0), writes=[SBF])
                S32h = [Tile(S32.ap[(h % 2) * 64:(h % 2) * 64 + 64, h // 2, :], "S32h%d" % h) for h in range(4)]
                SBFh = [Tile(SBF.ap[(h % 2) * 64:(h % 2) * 64 + 64, h // 2, :], "SBFh%d" % h) for h in range(4)]
                for h in range(4):
                    S32h[h].w = S32.w
                    SBFh[h].w = SBF.w
                HTB = sb("HTB", [128, 8, 512], BF16)
                QT = sb("QT", [128, 2, 512])
                KT = sb("KT", [128, 2, 512])
                R2 = lambda name, shape, dt=F32: Rot([sb("%s%d" % (name, i), shape, dt) for i in range(2)])
                VB = R2("VB", [128, 512], BF16); RG = R2("RG", [128, 512]); KTOK = R2("KTOK", [128, 256]); E1 = R2("E1", [128, 256]); SP = R2("SP", [128, 256])
                EB = R2("EB", [128, 2, 128]); ENB = R2("ENB", [128, 2, 128]); EDEC = R2("EDEC", [128, 256])
                QS = R2("QS", [128, 4, 128], BF16); KS = R2("KS", [128, 2, 128], BF16); KDEC = R2("KDEC", [128, 256], BF16)
                ATM = Rot([sb("ATM%d" % i, [128, 128], BF16) for i in range(4)])
                SS = R2("SS", [128, 4]); RS4 = R2("RS4", [128, 4]); YB = R2("YB", [128, 512], BF16)
                JUNK = sb("JUNK", [128, 128], BF16)
                for q_ in QS.tiles:
                    S.op("pool", lambda e, q_=q_: e.memset(q_[:, :, :], 0.0), writes=[q_])
                pool_rot = Rot([S.psum("pl%d" % i, [128, 512], F32, st=p3) for i in range(4)])
                o_ps = S.psum("o_ps", [128, 512], F32, st=p3)
                sn_ps = S.psum("sn_ps", [128, 4, 128], F32, st=p3)
                att_ps = S.psum("att_ps", [128, 4, 128], F32, st=p3)
                ybT_ps = S.psum("ybT", [128, 8, 128], BF16, st=p3)
                tp_rot = pool_rot

                def proj(dst_ps, cols, htb_ap, M=None):
                    pass

                gl_of = {}
                stA = {}

                def gla_prologue(B):
                    for i in range(4):
                        transpose_tile(4 * B + i, l, jsc, jsh, s, tp_rot, lambda c, i=i: HTB[:, c, i * 128:(i + 1) * 128], HTB)
                    for c2 in range(2):
                        for (dst, off) in ((QT, 0), (KT, 256)):
                            pp_ = pool_rot.next()
                            for kc in range(8):
                                S.op("pe", lambda e, kc=kc, c2=c2, off=off, pp_=pp_: e.matmul(pp_[:, :], lhsT=WING[:, kc, off + c2 * 128:off + (c2 + 1) * 128], rhs=HTB[:, kc, :], start=(kc == 0), stop=(kc == 7)),
                                     reads=[WING, HTB], writes=[pp_])
                            S.op("act", lambda e, dst=dst, c2=c2, pp_=pp_: e.activation(out=dst[:, c2, :], in_=pp_[:, :], func=AF.Copy), reads=[pp_], writes=[dst])
                    gl = GLT.next()
                    pp_ = pool_rot.next()
                    for kc in range(8):
                        S.op("pe", lambda e, kc=kc, pp_=pp_: e.matmul(pp_[0:16, :], lhsT=WING[:, kc, 1536:1552], rhs=HTB[:, kc, :], start=(kc == 0), stop=(kc == 7)), reads=[WING, HTB], writes=[pp_])
                    S.op("act", lambda e, gl=gl, pp_=pp_: e.activation(out=gl[0:16, :], in_=pp_[0:16, :], func=AF.Copy), reads=[pp_], writes=[gl])
                    gl_of[B] = gl

                def gla_a0(t):
                    B, i = t // 4, t % 4
                    gl = gl_of[B]
                    tc = slice(i * 128, (i + 1) * 128)
                    d = dict(tc=tc, vb=VB.next(), rg=RG.next(), ktok=KTOK.next(), e1=E1.next(), sp=SP.next(), eb=EB.next(), enb=ENB.next(), edec=EDEC.next(),
                             qs=QS.next(), ks=KS.next(), kdec=KDEC.next(), ss=SS.next(), rs4=RS4.next(), yb=YB.next())
                    stA[t] = d
                    e1, sp = d["e1"], d["sp"]
                    zp = pool_rot.next()
                    S.op("pe", lambda e: e.matmul(zp[:, 0:256], lhsT=gl[0:32, tc], rhs=GWB[0:32, :], start=True, stop=True), reads=[gl, GWB], writes=[zp])
                    S.op("act", lambda e: e.activation(out=e1[:, :], in_=zp[:, 0:256], func=AF.Exp, scale=-1.0), reads=[zp], writes=[e1])
                    S.op("act", lambda e: e.activation(out=sp[:, :], in_=e1[:, :], func=AF.Ln, bias=1.0), reads=[e1], writes=[sp])

                def gla_a1(t):
                    d = stA[t]
                    tc, vb, rg, ktok = d["tc"], d["vb"], d["rg"], d["ktok"]
                    kp = pool_rot.next()
                    for kc in range(8):
                        S.op("pe", lambda e, kc=kc: e.matmul(kp[:, 0:256], lhsT=HTB[:, kc, tc], rhs=WING[:, kc, 256:512], start=(kc == 0), stop=(kc == 7)), reads=[WING, HTB], writes=[kp])
                    S.op("dve", lambda e: e.tensor_copy(out=ktok[:, :], in_=kp[:, 0:256]), reads=[kp], writes=[ktok])
                    vp = pool_rot.next()
                    for kc in range(8):
                        S.op("pe", lambda e, kc=kc: e.matmul(vp[:, :], lhsT=HTB[:, kc, tc], rhs=WING[:, kc, 512:1024], start=(kc == 0), stop=(kc == 7)), reads=[WING, HTB], writes=[vp])
                    S.op("act", lambda e: e.activation(out=vb[:, :], in_=vp[:, :], func=AF.Copy), reads=[vp], writes=[vb])
                    rp = pool_rot.next()
                    for kc in range(8):
                        S.op("pe", lambda e, kc=kc: e.matmul(rp[:, :], lhsT=HTB[:, kc, tc], rhs=WING[:, kc, 1024:1536], start=(kc == 0), stop=(kc == 7)), reads=[WING, HTB], writes=[rp])
                    S.op("act", lambda e: e.activation(out=rg[:, :], in_=rp[:, :], func=AF.Silu), reads=[rp], writes=[rg])
                    S.op("pool", lambda e: e.tensor_tensor(out=rg[:, :], in0=rg[:, :], in1=NG[:, :], op=ALU.mult), reads=[rg, NG], writes=[rg])

                def gla_a2(t):
                    d = stA[t]
                    tc, sp, eb, enb, edec, qs, ks, kdec, ktok = d["tc"], d["sp"], d["eb"], d["enb"], d["edec"], d["qs"], d["ks"], d["kdec"], d["ktok"]
                    revp = pool_rot.next()
                    for c2 in range(2):
                        S.op("pe", lambda e, c2=c2: e.matmul(revp[:, 256 + c2 * 128:256 + (c2 + 1) * 128], lhsT=sp[:, c2 * 128:(c2 + 1) * 128], rhs=tri, start=True, stop=True), reads=[sp, CONST], writes=[revp])
                    S.op("pe", lambda e: e.matmul(revp[:, 0:256], lhsT=su, rhs=sp[:, :], start=True, stop=True), reads=[sp, CONST], writes=[revp])
                    S.op("act", lambda e: e.activation(out=eb[:, :, :], in_=revp[:, 256:512].rearrange("p (a b) -> p a b", a=2), func=AF.Exp, scale=-1.0 / 16), reads=[revp], writes=[eb])
                    S.op("act", lambda e: e.activation(out=enb[:, :, :], in_=revp[:, 256:512].rearrange("p (a b) -> p a b", a=2), func=AF.Exp, scale=1.0 / 16), reads=[revp], writes=[enb])
                    S.op("act", lambda e: e.activation(out=edec[:, :], in_=revp[:, 0:256], func=AF.Exp, scale=-1.0 / 16), reads=[revp], writes=[edec])
                    for h in range(4):
                        c2, hp = h // 2, (h % 2) * 64
                        S.op("dve", lambda e, h=h, c2=c2, hp=hp: e.scalar_tensor_tensor(out=qs[hp:hp + 64, h, :], in0=QT[hp:hp + 64, c2, tc], scalar=0.125, in1=eb[hp:hp + 64, c2, :], op0=ALU.mult, op1=ALU.mult),
                             reads=[QT, eb], writes=[qs])
                    S.op("dve", lambda e: e.tensor_tensor(out=ks[:, :, :], in0=KT[:, :, tc], in1=enb[:, :, :], op=ALU.mult), reads=[KT, enb], writes=[ks])
                    S.op("dve", lambda e: e.tensor_tensor(out=kdec[:, :], in0=ktok[:, :], in1=edec[:, :], op=ALU.mult), reads=[ktok, edec], writes=[kdec])

                def gla_b0(t):
                    d = stA[t]
                    qs, ks = d["qs"], d["ks"]
                    for h in range(4):
                        c2 = h // 2
                        S.op("pe", lambda e, c2=c2, h=h: e.matmul(att_ps[:, h, :], lhsT=ks[:, c2, :], rhs=qs[:, h, :], start=True, stop=True), reads=[ks, qs], writes=[att_ps])
                    atms = []
                    for h in range(4):
                        atm = ATM.next()
                        S.op("dve", lambda e, atm=atm, h=h: e.tensor_tensor(out=atm[:, :], in0=att_ps[:, h, :], in1=tri, op=ALU.mult), reads=[att_ps, CONST], writes=[atm])
                        atms.append(atm)
                    d["atms"] = atms

                def gla_b1(t):
                    d = stA[t]
                    vb, rg, eb, qs, kdec, ss, rs4, yb, atms = d["vb"], d["rg"], d["eb"], d["qs"], d["kdec"], d["ss"], d["rs4"], d["yb"], d["atms"]
                    for h in range(4):
                        c2 = h // 2
                        hc = slice(h * 128, (h + 1) * 128)
                        atm = atms[h]
                        S.op("pe", lambda e, c2=c2, hc=hc, h=h: e.matmul(o_ps[:, hc], lhsT=qs[:, h, :], rhs=SBF[:, c2, :], start=True, stop=False), reads=[qs, SBFh[2 * c2], SBFh[2 * c2 + 1]], writes=[o_ps])
                        S.op("pe", lambda e, atm=atm, hc=hc: e.matmul(o_ps[:, hc], lhsT=atm[:, :], rhs=vb[:, hc], start=False, stop=True), reads=[atm, vb], writes=[o_ps])
                    for h in range(4):
                        c2 = h // 2
                        hc = slice(h * 128, (h + 1) * 128)
                        S.op("pe", lambda e, c2=c2, hc=hc, h=h: e.matmul(sn_ps[:, h, :], lhsT=kdec[:, c2 * 128:(c2 + 1) * 128], rhs=vb[:, hc], start=True, stop=True), reads=[kdec, vb], writes=[sn_ps])
                    for h in range(4):
                        c2, hp = h // 2, (h % 2) * 64
                        S.op("dve", lambda e, c2=c2, hp=hp, h=h: e.scalar_tensor_tensor(out=S32[hp:hp + 64, c2, :], in0=S32[hp:hp + 64, c2, :], scalar=eb[hp:hp + 64, c2, 127:128],
                                                                                         in1=sn_ps[hp:hp + 64, h, :], op0=ALU.mult, op1=ALU.add), reads=[S32h[h], eb, sn_ps], writes=[S32h[h]])
                        S.op("pool", lambda e, c2=c2, hp=hp: e.tensor_copy(out=SBF[hp:hp + 64, c2, :], in_=S32[hp:hp + 64, c2, :]), reads=[S32h[h]], writes=[SBFh[h]])
                    for h in range(4):
                        hc = slice(h * 128, (h + 1) * 128)
                        S.op("act", lambda e, hc=hc, h=h: e.activation(out=JUNK[:, :], in_=o_ps[:, hc], func=AF.Square, accum_out=ss[:, h:h + 1]), reads=[o_ps], writes=[JUNK, ss])
                    S.op("dve", lambda e: e.tensor_scalar(out=ss[:, :], in0=ss[:, :], scalar1=1.0 / 128, scalar2=RMS_EPS, op0=ALU.mult, op1=ALU.add), reads=[ss], writes=[ss])
                    S.op("pool", lambda e: e.tensor_tensor(out=rs4[:, :], in0=ss[:, :], in1=NHALF[:, 0:4], op=ALU.pow), reads=[ss, NHALF], writes=[rs4])
                    for h in range(4):
                        hc = slice(h * 128, (h + 1) * 128)
                        S.op("dve", lambda e, hc=hc, h=h: e.scalar_tensor_tensor(out=yb[:, hc], in0=o_ps[:, hc], scalar=rs4[:, h:h + 1], in1=rg[:, hc], op0=ALU.mult, op1=ALU.mult),
                             reads=[o_ps, rs4, rg], writes=[yb])

                def gla_b2a(t):
                    yb = stA[t]["yb"]
                    for h in range(4):
                        S.op("pe", lambda e, h=h: e.transpose(ybT_ps[:, h, :], yb[:, h * 128:(h + 1) * 128], IDB[:, :]), reads=[yb, IDB], writes=[ybT_ps])
                    S.op("act", lambda e: e.activation(out=CATB[t][:, :, :], in_=ybT_ps[:, 0:4, :], func=AF.Copy), reads=[ybT_ps], writes=[CATB[t]])

                def gla_b2b(t):
                    stA.pop(t)
                    B = t // 4
                    for hf in range(2):
                        mp = pool_rot.next()
                        for kc in range(8):
                            S.op("pe", lambda e, kc=kc, hf=hf, mp=mp: e.matmul(mp[:, :], lhsT=HT[:, kc, t * 128:(t + 1) * 128], rhs=WOUT[:, kc, hf * 512:(hf + 1) * 512], start=(kc == 0), stop=(kc == 7)),
                                 reads=[CATA[B], CATB[t], WOUT], writes=[mp])
                        S.op("dve", lambda e, hf=hf, mp=mp: e.tensor_tensor(out=XT[t][:, hf * 512:(hf + 1) * 512], in0=mp[:, :], in1=XT[t][:, hf * 512:(hf + 1) * 512], op=ALU.add),
                             reads=[mp, XT[t]], writes=[XT[t]])
                    lnp.push(t)

                def gla_a_all(t):
                    gla_a0(t); gla_a1(t); gla_a2(t)

                lnp = LNPipe()
                gla_prologue(0)
                gla_a_all(0)
                for t in range(NT + 1):
                    nxt = t + 1 if t + 1 < NT else None
                    if nxt is not None and nxt % 4 == 0:
                        gla_prologue(nxt // 4)
                    if nxt is not None:
                        gla_a0(nxt)
                    if t < NT:
                        gla_b0(t)
                    if t >= 1:
                        gla_b2a(t - 1)
                    if nxt is not None:
                        gla_a1(nxt)
                        gla_a2(nxt)
                    if t < NT:
                        gla_b1(t)
                    if t >= 1:
                        gla_b2b(t - 1)
                lnp.flush()
                S.fence()

        def l1_mixer(s):
            l = 1
            jsh, jsc, jg = 0, 8, 16
            CQB = [Tile(HT.ap[:, 0:6, B * 512:(B + 1) * 512], "CQB%d" % B) for B in range(4)]
            load_ln(l, 0, True)
            with ExitStack() as pa:
                CS1 = S.sbuf("CS1", [64, S_LEN], F32, st=pa)
                CS2 = S.sbuf("CS2", [64, S_LEN], F32, st=pa)
                with ExitStack() as pr:
                    POSI = S.sbuf("POSI", [64, S_LEN], I32, st=pr)
                    ANG = S.sbuf("ANG", [64, S_LEN], F32, st=pr)
                    TT = S.sbuf("TT", [64, S_LEN], F32, st=pr)
                    TI = S.sbuf("TI", [64, S_LEN], I32, st=pr)
                    FR = S.sbuf("FR", [64, S_LEN], F32, st=pr)
                    MK = S.sbuf("MK", [64, S_LEN], F32, st=pr)
                    S.dma("sp", lambda e: e.dma_start(out=POSI[:, :], in_=pos_d[s:s + 1, :].partition_broadcast(64)), reads=[DR], writes=[POSI], sync=POSI)
                    S.op("dve", lambda e: e.tensor_copy(out=ANG[:, :], in_=POSI[:, :]), reads=[POSI], writes=[ANG])
                    S.op("dve", lambda e: e.tensor_scalar(out=ANG[:, :], in0=ANG[:, :], scalar1=CONST[0:64, C_INVF:C_INVF + 1], scalar2=None, op0=ALU.mult), reads=[ANG, CONST], writes=[ANG])
                    for (dst, shift, sgn) in ((CS1, 0.75, False), (CS2, 0.5, True)):
                        S.op("dve", lambda e, shift=shift: e.tensor_scalar(out=TT[:, :], in0=ANG[:, :], scalar1=1.0 / TWO_PI, scalar2=shift, op0=ALU.mult, op1=ALU.add), reads=[ANG], writes=[TT])
                        S.op("dve", lambda e: e.tensor_copy(out=TI[:, :], in_=TT[:, :]), reads=[TT], writes=[TI])
                        S.op("dve", lambda e: e.tensor_copy(out=FR[:, :], in_=TI[:, :]), reads=[TI], writes=[FR])
                        S.op("dve", lambda e: e.tensor_tensor(out=FR[:, :], in0=TT[:, :], in1=FR[:, :], op=ALU.subtract), reads=[TT, FR], writes=[FR])
                        S.op("dve", lambda e: e.tensor_single_scalar(out=MK[:, :], in_=FR[:, :], scalar=0.0, op=ALU.is_lt), reads=[FR], writes=[MK])
                        S.op("dve", lambda e: e.tensor_tensor(out=FR[:, :], in0=FR[:, :], in1=MK[:, :], op=ALU.add), reads=[FR, MK], writes=[FR])
                        S.op("dve", lambda e: e.tensor_single_scalar(out=MK[:, :], in_=FR[:, :], scalar=1.0, op=ALU.is_ge), reads=[FR], writes=[MK])
                        S.op("dve", lambda e: e.tensor_tensor(out=FR[:, :], in0=FR[:, :], in1=MK[:, :], op=ALU.subtract), reads=[FR, MK], writes=[FR])
                        S.op("act", lambda e, dst=dst: e.activation(out=dst[:, :], in_=FR[:, :], func=AF.Sin, scale=TWO_PI, bias=CONST[0:64, C_NPI:C_NPI + 1]), reads=[FR, CONST], writes=[dst])
                        if sgn:
                            S.op("dve", lambda e, dst=dst: e.tensor_scalar(out=dst[:, :], in0=dst[:, :], scalar1=CONST[0:64, C_SGN:C_SGN + 1], scalar2=None, op0=ALU.mult), reads=[dst, CONST], writes=[dst])
                    S.fence()
                WUQ = S.sbuf("WUQ", [128, 3, 2048], BF16, st=pa)
                WUKV = S.sbuf("WUKV", [128, 2, 2048], BF16, st=pa)
                S.dma("pool", lambda e: [e.dma_start(out=WUQ[:, :, 0:1024], in_=mla_w_uq_d.rearrange("(c p) n -> p c n", p=128)[:, :, 0:1024]),
                                         e.dma_start(out=WUQ[:, :, 1024:2048], in_=mla_w_uq_d.rearrange("(c p) n -> p c n", p=128)[:, :, 1024:2048])],
                      reads=[DR], writes=[WUQ], sync=WUQ, n=2)
                S.dma("pool", lambda e: [e.dma_start(out=WUKV[:, :, 0:1024], in_=mla_w_ukv_d.rearrange("(c p) n -> p c n", p=128)[:, :, 0:1024]),
                                         e.dma_start(out=WUKV[:, :, 1024:2048], in_=mla_w_ukv_d.rearrange("(c p) n -> p c n", p=128)[:, :, 1024:2048])],
                      reads=[DR], writes=[WUKV], sync=WUKV, n=2)
                with ExitStack() as p1:
                    def sb(name, shape, dt=F32):
                        return S.sbuf(name, shape, dt, st=p1)
                    WIN1 = sb("WIN1", [128, 8, 768], BF16)
                    S.dma("pool", lambda e: e.dma_start(out=WIN1[:, :, :], in_=mla_w_in_d.rearrange("(c p) n -> p c n", p=128)), reads=[DR], writes=[WIN1], sync=WIN1)
                    HTB = sb("HTB1", [128, 8, 512], BF16)
                    UT = sb("UT", [128, 5, 512])
                    SQ = Rot([sb("SQ%d" % i, [128, 512]) for i in range(2)])
                    RQ = sb("RQ", [128, 512]); RKV = sb("RKV", [128, 512])
                    T1 = sb("T1a", [64, 512]); T2 = sb("T2a", [64, 512])
                    scr = sb("scr1", [128, 8, 128])
                    pool_rot = Rot([S.psum("pla%d" % i, [128, 512], F32, st=p1) for i in range(6)])
                    ssq = [S.psum("ssq%d" % i, [128, 512], F32, st=p1) for i in range(2)]
                    build_g1b(l, jg, s, pool_rot.tiles[0:2], scr)
                    for B in range(4):
                        bc = slice(B * 512, (B + 1) * 512)
                        for i in range(4):
                            transpose_tile(4 * B + i, l, jsc, jsh, s, pool_rot, lambda c, i=i: HTB[:, c, i * 128:(i + 1) * 128], HTB)
                        for j in range(5):
                            up = pool_rot.next()
                            for kc in range(8):
                                S.op("pe", lambda e, kc=kc, j=j, up=up: e.matmul(up[:, :], lhsT=WIN1[:, kc, j * 128:(j + 1) * 128], rhs=HTB[:, kc, :], start=(kc == 0), stop=(kc == 7)), reads=[WIN1, HTB], writes=[up])
                            S.op("act", lambda e, j=j, up=up: e.activation(out=UT[:, j, :], in_=up[:, :], func=AF.Copy), reads=[up], writes=[UT])
                            sq = SQ.next()
                            S.op("act", lambda e, sq=sq, up=up: e.activation(out=sq[:, :], in_=up[:, :], func=AF.Square), reads=[up], writes=[sq])
                            sp_ = ssq[0] if j < 3 else ssq[1]
                            S.op("pe", lambda e, sq=sq, sp_=sp_, j=j: e.matmul(sp_[:, :], lhsT=ones, rhs=sq[:, :], start=(j in (0, 3)), stop=(j in (2, 4))), reads=[CONST, sq], writes=[sp_])
                        for (dst, sp_, n_) in ((RQ, ssq[0], 384.0), (RKV, ssq[1], 256.0)):
                            S.op("act", lambda e, dst=dst, sp_=sp_, n_=n_: e.activation(out=dst[:, :], in_=sp_[:, :], func=AF.Ln, scale=1.0 / n_, bias=CONST[:, C_REPS:C_REPS + 1]), reads=[sp_, CONST], writes=[dst])
                            S.op("act", lambda e, dst=dst: e.activation(out=dst[:, :], in_=dst[:, :], func=AF.Exp, scale=-0.5), reads=[dst], writes=[dst])
                        for j in range(5):
                            rr = RQ if j < 3 else RKV
                            S.op("dve", lambda e, j=j, rr=rr, bc=bc: e.scalar_tensor_tensor(out=HT[:, j, bc], in0=UT[:, j, :], scalar=PVT[:, 12 + j:13 + j], in1=rr[:, :], op0=ALU.mult, op1=ALU.mult),
                                 reads=[UT, PVT, rr], writes=[CQB[B]])
                        a_p = pool_rot.next()
                        b_p = pool_rot.next()
                        for (pp_, off) in ((a_p, 640), (b_p, 704)):
                            for kc in range(8):
                                S.op("pe", lambda e, kc=kc, pp_=pp_, off=off: e.matmul(pp_[0:64, :], lhsT=WIN1[:, kc, off:off + 64], rhs=HTB[:, kc, :], start=(kc == 0), stop=(kc == 7)), reads=[WIN1, HTB], writes=[pp_])
                        S.op("dve", lambda e, a_p=a_p, bc=bc: e.tensor_tensor(out=T1[:, :], in0=a_p[0:64, :], in1=CS1[:, bc], op=ALU.mult), reads=[a_p, CS1], writes=[T1])
                        S.op("dve", lambda e, b_p=b_p, bc=bc: e.tensor_tensor(out=T2[:, :], in0=b_p[0:64, :], in1=CS2[:, bc], op=ALU.mult), reads=[b_p, CS2], writes=[T2])
                        S.op("pool", lambda e, bc=bc: e.tensor_tensor(out=HT[0:64, 5, bc], in0=T1[:, :], in1=T2[:, :], op=ALU.add), reads=[T1, T2], writes=[CQB[B]])
                    S.fence()
                if stop_after == "l1a":
                    return
                with ExitStack() as p2:
                    def sb(name, shape, dt=F32):
                        return S.sbuf(name, shape, dt, st=p2)
                    WOUT = sb("WOUT1", [128, 8, D], BF16)
                    S.dma("pool", lambda e: e.dma_start(out=WOUT[:, :, :], in_=mla_w_out_d.rearrange("(c p) n -> p c n", p=128)), reads=[DR], writes=[WOUT], sync=WOUT)
                    S.op("pool", lambda e: e.tensor_tensor(out=WOUT[:, :, :], in0=WOUT[:, :, :], in1=G1B[:, :].unsqueeze(1).to_broadcast([128, 8, D]), op=ALU.mult), reads=[WOUT, G1B], writes=[WOUT])
                    QN = sb("QN", [128, S_LEN], BF16); QR = sb("QR", [64, S_LEN], BF16); KN = sb("KN", [128, S_LEN], BF16); VT = sb("VT", [128, NT, 128], BF16)
                    PT = Rot([sb("PT%d" % i, [128, 512], BF16) for i in range(5)])
                    RINV = sb("RINV", [128, 512])
                    OTH = Rot([sb("OTH%d" % i, [128, S_LEN], BF16) for i in range(2)])
                    T1 = sb("T1b", [64, 512]); T2 = sb("T2b", [64, 512])
                    pool_rot = Rot([S.psum("plb%d" % i, [128, 512], F32, st=p2) for i in range(3)])
                    st_rot = Rot([S.psum("stb%d" % i, [128, 512], F32, st=p2) for i in range(3)])
                    o_ps = S.psum("o1_ps", [128, 512], F32, st=p2)
                    r_ps = S.psum("r1_ps", [128, 512], F32, st=p2)
                    lnp = LNPipe()
                    for h in range(8 if dbg >= 2 else 0):
                        hb = h * 256
                        for B in range(4):
                            bc = slice(B * 512, (B + 1) * 512)
                            p_ = pool_rot.next()
                            for k3 in range(3):
                                S.op("pe", lambda e, k3=k3, p_=p_, bc=bc, hb=hb: e.matmul(p_[:, :], lhsT=WUQ[:, k3, hb:hb + 128], rhs=HT[:, k3, bc], start=(k3 == 0), stop=(k3 == 2)), reads=[WUQ, CQB[B]], writes=[p_])
                            S.op("act", lambda e, p_=p_, bc=bc: e.activation(out=QN[:, bc], in_=p_[:, :], func=AF.Copy), reads=[p_], writes=[QN])
                            a_p = pool_rot.next()
                            b_p = pool_rot.next()
                            for (pp_, off) in ((a_p, hb + 128), (b_p, hb + 192)):
                                for k3 in range(3):
                                    S.op("pe", lambda e, k3=k3, pp_=pp_, off=off, bc=bc: e.matmul(pp_[0:64, :], lhsT=WUQ[:, k3, off:off + 64], rhs=HT[:, k3, bc], start=(k3 == 0), stop=(k3 == 2)), reads=[WUQ, CQB[B]], writes=[pp_])
                            S.op("dve", lambda e, a_p=a_p, bc=bc: e.tensor_tensor(out=T1[:, :], in0=a_p[0:64, :], in1=CS1[:, bc], op=ALU.mult), reads=[a_p, CS1], writes=[T1])
                            S.op("dve", lambda e, b_p=b_p, bc=bc: e.tensor_tensor(out=T2[:, :], in0=b_p[0:64, :], in1=CS2[:, bc], op=ALU.mult), reads=[b_p, CS2], writes=[T2])
                            S.op("pool", lambda e, bc=bc: e.tensor_tensor(out=QR[:, bc], in0=T1[:, :], in1=T2[:, :], op=ALU.add), reads=[T1, T2], writes=[QR])
                            p_ = pool_rot.next()
                            for k2 in range(2):
                                S.op("pe", lambda e, k2=k2, p_=p_, bc=bc, hb=hb: e.matmul(p_[:, :], lhsT=WUKV[:, k2, hb:hb + 128], rhs=HT[:, 3 + k2, bc], start=(k2 == 0), stop=(k2 == 1)), reads=[WUKV, CQB[B]], writes=[p_])
                            S.op("act", lambda e, p_=p_, bc=bc: e.activation(out=KN[:, bc], in_=p_[:, :], func=AF.Copy), reads=[p_], writes=[KN])
                            p_ = pool_rot.next()
                            for i in range(4):
                                t = 4 * B + i
                                for k2 in range(2):
                                    S.op("pe", lambda e, k2=k2, p_=p_, i=i, t=t, hb=hb: e.matmul(p_[:, i * 128:(i + 1) * 128], lhsT=HT[:, 3 + k2, t * 128:(t + 1) * 128], rhs=WUKV[:, k2, hb + 128:hb + 256], start=(k2 == 0), stop=(k2 == 1)),
                                         reads=[WUKV, CQB[B]], writes=[p_])
                            S.op("act", lambda e, p_=p_, B=B: e.activation(out=VT[:, 4 * B:4 * B + 4, :], in_=p_[:, :].rearrange("p (a b) -> p a b", a=4), func=AF.Copy), reads=[p_], writes=[VT])
                        oth = OTH.next()
                        steps = [(Q, kt) for Q in range(4) for kt in range(4 * Q + 4)]
                        pend = {}

                        def do_st(Q, kt):
                            m = kt - 4 * Q if kt >= 4 * Q else 0
                            c0 = m * 128
                            st_ = st_rot.next()
                            qc = slice(Q * 512 + c0, (Q + 1) * 512)
                            kc_ = slice(kt * 128, (kt + 1) * 128)
                            S.op("pe", lambda e: e.matmul(st_[:, c0:512], lhsT=KN[:, kc_], rhs=QN[:, qc], start=True, stop=False), reads=[KN, QN], writes=[st_])
                            S.op("pe", lambda e: e.matmul(st_[:, c0:512], lhsT=HT[0:64, 5, kc_], rhs=QR[0:64, qc], start=False, stop=True), reads=[CQB[kt // 4], QR], writes=[st_])
                            pt = PT.next()
                            S.op("act", lambda e: e.activation(out=pt[:, c0:512], in_=st_[:, c0:512], func=AF.Exp, scale=MLA_SCALE), reads=[st_], writes=[pt])
                            if kt >= 4 * Q:
                                S.op("pool", lambda e: e.tensor_tensor(out=pt[:, c0:c0 + 128], in0=pt[:, c0:c0 + 128], in1=TRIB[:, :], op=ALU.mult), reads=[pt, TRIB], writes=[pt])
                            pend[(Q, kt)] = (pt, c0)

                        def do_pv(Q, kt, oth=oth):
                            pt, c0 = pend.pop((Q, kt))
                            last = (kt == 4 * Q + 3)
                            S.op("pe", lambda e: e.matmul(o_ps[:, c0:512], lhsT=VT[:, kt, :], rhs=pt[:, c0:512], start=(kt == 0), stop=last), reads=[VT, pt], writes=[o_ps])
                            S.op("pe", lambda e: e.matmul(r_ps[:, c0:512], lhsT=ONEB[:, :], rhs=pt[:, c0:512], start=(kt == 0), stop=last), reads=[ONEB, pt], writes=[r_ps])
                            if last:
                                qc = slice(Q * 512, (Q + 1) * 512)
                                S.op("dve", lambda e: e.reciprocal(out=RINV[:, :], in_=r_ps[:, :]), reads=[r_ps], writes=[RINV])
                                S.op("dve", lambda e: e.tensor_tensor(out=oth[:, qc], in0=o_ps[:, :], in1=RINV[:, :], op=ALU.mult), reads=[o_ps, RINV], writes=[oth])

                        for i_, (Q, kt) in enumerate(steps):
                            do_st(Q, kt)
                            if i_ >= 2:
                                do_pv(*steps[i_ - 2])
                        do_pv(*steps[-2])
                        do_pv(*steps[-1])
                        for t in range(NT):
                            for hf in range(2):
                                mp = pool_rot.next()
                                S.op("pe", lambda e, t=t, hf=hf, mp=mp, h=h, oth=oth: e.matmul(mp[:, :], lhsT=oth[:, t * 128:(t + 1) * 128], rhs=WOUT[:, h, hf * 512:(hf + 1) * 512], start=True, stop=True), reads=[oth, WOUT], writes=[mp])
                                S.op("dve", lambda e, t=t, hf=hf, mp=mp: e.tensor_tensor(out=XT[t][:, hf * 512:(hf + 1) * 512], in0=mp[:, :], in1=XT[t][:, hf * 512:(hf + 1) * 512], op=ALU.add),
                                     reads=[mp, XT[t]], writes=[XT[t]])
                            if h == 7:
                                lnp.push(t)
                    lnp.flush()
                    S.fence()

        for s in range(nseq):
            for t in range(NT):
                S.dma("sp", lambda e, t=t, s=s: e.dma_start(out=XT[t][:, :], in_=x_d[s, t * 128:(t + 1) * 128, :]), reads=[DR], writes=[XT[t]], sync=XT[t])
                S.op("act", lambda e, t=t: e.activation(out=XT[t][:, :], in_=XT[t][:, :], func=AF.Copy, scale=ALPHA), reads=[XT[t]], writes=[XT[t]])
            if stop_after == "moe0":
                moe_phase(0, s, last=True)
            elif stop_after in ("xm1only", "l1a"):
                l1_mixer(s)
            elif stop_after != "load":
                l0_mixer(s)
                if stop_after not in ("xm0", "l0a", "l0b"):
                    moe_phase(0, s, last=False)
                    if stop_after != "xf0":
                        l1_mixer(s)
                        if stop_after != "xm1":
                            moe_phase(1, s, last=True)
            for t in range(NT):
                S.dma("sp", lambda e, t=t, s=s: e.dma_start(out=out_d[s, t * 128:(t + 1) * 128, :], in_=XT[t][:, :]), reads=[XT[t]], writes=[DO], sync=XT[t])
            S.fence()
        S.emit(final_keys=["X%d" % t for t in range(NT)])
    return nc


def prep_shared(inp):
    f = lambda a: np.ascontiguousarray(np.asarray(a, dtype=np.float32))
    sh = {}
    sh["consts"] = make_consts()
    sh["ada_w"] = f(inp["ada_w"])
    sh["ada_b"] = f(inp["ada_b"])
    sh["ln_gb"] = f(np.stack([inp["ln_mix_g"], inp["ln_mix_b"], inp["ln_ffn_g"], inp["ln_ffn_b"]], axis=1))
    sh["ab_w_in"] = f(inp["ab_w_in"][0])
    sh["conv_w"] = f(inp["conv_w"][0])
    pv = np.zeros((40, 128), np.float32)
    pv[0:4] = np.asarray(inp["conv_b"][0]).reshape(4, 128)
    pv[4:8] = np.asarray(inp["conv_ln_g"][0]).reshape(4, 128)
    pv[8:12] = np.asarray(inp["conv_ln_b"][0]).reshape(4, 128)
    pv[12:15] = np.asarray(inp["mla_q_norm_g"][0]).reshape(3, 128)
    pv[15:17] = np.asarray(inp["mla_kv_norm_g"][0]).reshape(2, 128)
    sh["pvec"] = pv
    sh["gla_gate_w"] = f(inp["gla_gate_w"][0])
    sh["gla_gate_b"] = f(inp["gla_gate_b"][0]).reshape(1, 256)
    sh["gla_norm_g"] = f(inp["gla_norm_g"][0]).reshape(1, 512)
    sh["ab_w_out"] = f(inp["ab_w_out"][0])
    w_in = np.asarray(inp["mla_w_in"][0], dtype=np.float32)
    sh["mla_w_in"] = f(np.concatenate([w_in, w_in[:, 672:704], w_in[:, 640:672]], axis=1))
    wq = np.asarray(inp["mla_w_uq"][0], dtype=np.float32).reshape(384, 8, 192)
    sh["mla_w_uq"] = f(np.concatenate([wq, wq[:, :, 160:192], wq[:, :, 128:160]], axis=2).reshape(384, 2048))
    sh["mla_w_ukv"] = f(inp["mla_w_ukv"][0])
    sh["mla_w_out"] = f(inp["mla_w_out"][0])
    sh["moe_wr"] = f(np.concatenate([inp["moe_w_group"], inp["moe_w_router"]], axis=2))
    sh["moe_br"] = f(np.concatenate([inp["moe_b_group"], inp["moe_b_router"]], axis=1))
    sh["consts2"] = make_consts2()
    wg = np.asarray(inp["moe_w_gate"], dtype=np.float32).reshape(2, 32, 8, 128, 256).transpose(0, 1, 3, 2, 4)
    wu = np.asarray(inp["moe_w_up"], dtype=np.float32).reshape(2, 32, 8, 128, 256).transpose(0, 1, 3, 2, 4)
    wd = np.asarray(inp["moe_w_down"], dtype=np.float32).reshape(2, 32, 2, 128, 1024).transpose(0, 1, 3, 2, 4)
    wall = np.concatenate([np.concatenate([wg, wu], axis=4).reshape(2, 32, 128, 4096), wd.reshape(2, 32, 128, 2048)], axis=3)
    for i in range(2):
        sh["moe_wall%d" % i] = f(wall[i].reshape(32 * 128, 6144))
    return sh


def kernel(**inputs):
    sh = prep_shared(inputs)
    x = np.asarray(inputs["x"], dtype=np.float32)
    c = np.asarray(inputs["c"], dtype=np.float32)
    pos = np.asarray(inputs["positions"], dtype=np.int32)
    nc = build_nc()
    in_maps = []
    for i in range(NCORES):
        m = dict(sh)
        m["x"] = np.ascontiguousarray(x[2 * i:2 * i + 2])
        m["c"] = np.ascontiguousarray(c[2 * i:2 * i + 2])
        m["pos"] = np.ascontiguousarray(pos[2 * i:2 * i + 2])
        in_maps.append(m)
    res = run_bass_kernel_spmd(nc, in_maps, core_ids=list(range(NCORES)))
    return np.concatenate([r["out"] for r in res.results], axis=0).astype(np.float32)
```

```python
from contextlib import ExitStack
import math
import numpy as np
import concourse.bass as bass
import concourse.mybir as mybir
from concourse.bass_utils import run_bass_kernel_spmd

F32 = mybir.dt.float32
BF16 = mybir.dt.bfloat16
I32 = mybir.dt.int32
AF = mybir.ActivationFunctionType
ALU = mybir.AluOpType
AX = mybir.AxisListType

NCORES = 8
D = 1024
S_LEN = 2048
NT = 16
ALPHA = 4.0 ** 0.25
LN_EPS = 1e-5
RMS_EPS = 1e-6
MLA_SCALE = 192.0 ** -0.5
TWO_PI = 2.0 * math.pi

COMPUTE = ("pe", "act", "dve", "pool")


class Tile:
    __slots__ = ("ap", "name", "w", "rs", "semkey")

    def __init__(self, ap, name, semkey=None):
        self.ap = ap
        self.name = name
        self.w = None
        self.rs = []
        self.semkey = semkey or name

    def __getitem__(self, idx):
        return self.ap[idx]


class Op:
    __slots__ = ("eng", "fn", "deps", "signal", "sigidx", "pos", "semkey", "semval", "ndma")

    def __init__(self, eng, fn):
        self.eng = eng
        self.fn = fn
        self.deps = []
        self.signal = False
        self.sigidx = 0
        self.pos = 0
        self.semkey = None
        self.semval = 0
        self.ndma = 0


class Sched:
    def __init__(self, nc, stack):
        self.nc = nc
        self.stack = stack
        self.ops = {e: [] for e in ("pe", "act", "dve", "pool", "sp")}
        self.semcnt = {}
        self.uid = 0
        self.fence_deps = []
        self.fence_pending = set()
        self.dma_since = []

    def sbuf(self, name, shape, dtype, st=None):
        self.uid += 1
        t = (st or self.stack).enter_context(self.nc.sbuf_tensor("%s_%d" % (name, self.uid), list(shape), dtype))
        return Tile(t, name)

    def psum(self, name, shape, dtype=F32, st=None):
        self.uid += 1
        t = (st or self.stack).enter_context(self.nc.psum_tensor("%s_%d" % (name, self.uid), list(shape), dtype))
        return Tile(t, name)

    def _add(self, eng, fn, reads, writes, semkey=None, ndma=0):
        op = Op(eng, fn)
        lst = self.ops[eng]
        op.pos = len(lst)
        deps = []
        if eng in self.fence_pending:
            self.fence_pending.discard(eng)
            deps.extend(self.fence_deps)
        for t in reads:
            if t.w is not None:
                deps.append(t.w)
        for t in writes:
            if t.w is not None:
                deps.append(t.w)
            deps.extend(t.rs)
        seen = set()
        for d in deps:
            if id(d) in seen or d is op:
                continue
            seen.add(id(d))
            if d.semkey is None and d.eng == eng:
                if eng == "pe" or eng == "sp":
                    continue
            op.deps.append(d)
            if d.semkey is None:
                d.signal = True
        for t in reads:
            if semkey is None:
                t.rs = [r for r in t.rs if not (r.semkey is None and r.eng == eng)]
            t.rs.append(op)
        for t in writes:
            t.w = op
            t.rs = []
        if semkey is not None:
            op.semkey = semkey
            op.ndma = ndma
            self.semcnt[semkey] = self.semcnt.get(semkey, 0) + 16 * ndma
            op.semval = self.semcnt[semkey]
            self.dma_since.append(op)
        lst.append(op)
        return op

    def op(self, eng, fn, reads=(), writes=()):
        return self._add(eng, fn, list(reads), list(writes))

    def dma(self, eng, fn, reads=(), writes=(), sync=None, n=1):
        return self._add(eng, fn, list(reads), list(writes), semkey=sync.semkey, ndma=n)

    def fence(self):
        deps = []
        for lst in self.ops.values():
            for d in reversed(lst):
                if d.semkey is None:
                    deps.append(d)
                    break
        deps = deps + self.dma_since
        self.dma_since = []
        self.fence_deps = deps
        self.fence_pending = set(self.ops.keys())

    def emit(self, final_keys=()):
        nc = self.nc
        stack = self.stack
        esem = {e: stack.enter_context(nc.semaphore("es_" + e)) for e in COMPUTE}
        dsem = {k: stack.enter_context(nc.semaphore("ds_%d" % i)) for i, k in enumerate(sorted(self.semcnt))}
        for e in COMPUTE:
            c = 0
            for op in self.ops[e]:
                if op.signal and op.semkey is None:
                    c += 1
                    op.sigidx = c
        block = stack.enter_context(nc.Block())

        def run(ename, engine):
            waited = {}
            for op in self.ops[ename]:
                need = {}
                for d in op.deps:
                    if d.semkey is not None:
                        s, v, key = dsem[d.semkey], d.semval, "d" + d.semkey
                    else:
                        s, v, key = esem[d.eng], d.sigidx, "e" + d.eng
                    if key not in need or need[key][1] < v:
                        need[key] = (s, v)
                for key, (s, v) in need.items():
                    if waited.get(key, 0) >= v:
                        continue
                    waited[key] = v
                    engine.wait_ge(s, v)
                r = op.fn(engine)
                if op.semkey is not None:
                    if not isinstance(r, (list, tuple)):
                        r = [r]
                    assert len(r) == op.ndma, (len(r), op.ndma)
                    for ins in r:
                        ins.then_inc(dsem[op.semkey], 16)
                elif op.signal:
                    r.then_inc(esem[ename], 1)
            if ename == "sp":
                for k in final_keys:
                    engine.wait_ge(dsem[k], self.semcnt[k])

        @block.sync
        def _(sync):
            run("sp", sync)

        @block.tensor
        def _(tensor):
            run("pe", tensor)

        @block.scalar
        def _(scalar):
            run("act", scalar)

        @block.vector
        def _(vector):
            run("dve", vector)

        @block.gpsimd
        def _(gpsimd):
            run("pool", gpsimd)


class Rot:
    def __init__(self, tiles):
        self.tiles = tiles
        self.i = 0

    def next(self):
        t = self.tiles[self.i % len(self.tiles)]
        self.i += 1
        return t


C_ID, C_TRI, C_SU, C_ONE, C_INVF, C_SGN, C_NHALF, C_NPI, C_REPS, NCONST = 0, 128, 256, 384, 512, 513, 514, 515, 516, 517


C2_LT, C2_THR, C2_VROW, C2_EROW, C2_VAL, NC2 = 0, 1024, 1032, 1080, 1112, 1144


def make_consts2():
    c = np.zeros((128, NC2), np.float32)
    p = np.arange(128)
    e = np.arange(32)
    c[:, C2_LT:C2_LT + 1024] = (e[None, :] < e[:, None]).astype(np.float32).reshape(1, 1024)
    c[:, C2_THR:C2_THR + 8] = 256.0 * np.arange(8)[None, :]
    c[:, C2_VROW:C2_VROW + 48] = np.arange(48)[None, :]
    c[:, C2_EROW:C2_EROW + 32] = e[None, :] * 128.0 + p[:, None] - 8192.0
    kt = np.arange(32)
    c[:, C2_VAL:C2_VAL + 32] = (kt // 16)[None, :] * 2048.0 + (kt % 16)[None, :] * 128.0 + p[:, None]
    return c


def make_consts():
    c = np.zeros((128, NCONST), np.float32)
    p = np.arange(128)
    c[:, C_ID:C_ID + 128] = np.eye(128, dtype=np.float32)
    c[:, C_TRI:C_TRI + 128] = (p[:, None] <= p[None, :]).astype(np.float32)
    c[:, C_SU:C_SU + 128] = (p[:, None] > p[None, :]).astype(np.float32)
    c[:, C_ONE:C_ONE + 128] = 1.0
    inv_freq = (1.0 / (np.float32(10000.0) ** (np.arange(0, 64, 2, dtype=np.float32) / np.float32(64)))).astype(np.float32)
    c[:64, C_INVF] = inv_freq[p[:64] % 32]
    c[:, C_SGN] = np.where(p < 32, -1.0, 1.0)
    c[:, C_NHALF] = -0.5
    c[:, C_NPI] = -math.pi
    c[:, C_REPS] = RMS_EPS
    return c


def build_nc(nseq=2, stop_after=None, dbg=9):
    nc = bass.Bass("TRN2", target_bir_lowering=False)

    def din(name, shape, dt=F32):
        return nc.dram_tensor(name, list(shape), dt, kind="ExternalInput").ap()

    x_d = din("x", [2, S_LEN, D])
    c_d = din("c", [2, D])
    pos_d = din("pos", [2, S_LEN], I32)
    const_d = din("consts", [128, NCONST])
    ada_w_d = din("ada_w", [2, D, 6 * D])
    ada_b_d = din("ada_b", [2, 6 * D])
    ln_d = din("ln_gb", [2, 4, D])
    ab_w_in_d = din("ab_w_in", [D, 2576])
    conv_w_d = din("conv_w", [31, 512])
    pv_d = din("pvec", [40, 128])
    gate_w_d = din("gla_gate_w", [16, 256])
    gate_b_d = din("gla_gate_b", [1, 256])
    gnorm_d = din("gla_norm_g", [1, 512])
    ab_w_out_d = din("ab_w_out", [D, D])
    mla_w_in_d = din("mla_w_in", [D, 768])
    mla_w_uq_d = din("mla_w_uq", [384, 2048])
    mla_w_ukv_d = din("mla_w_ukv", [256, 2048])
    mla_w_out_d = din("mla_w_out", [D, D])
    wr_d = din("moe_wr", [2, D, 36])
    br_d = din("moe_br", [2, 36])
    wall_ds = [din("moe_wall%d" % i, [32 * 128, 6144]) for i in range(2)]
    const2_d = din("consts2", [128, NC2])
    htok_d = nc.dram_tensor("htok_scr", [S_LEN, D], BF16).ap()
    ys_d = nc.dram_tensor("ys_scr", [2 * S_LEN, D], F32).ap()
    tab_d = nc.dram_tensor("tab_scr", [96 * 128, 1], I32).ap()
    out_d = nc.dram_tensor("out", [2, S_LEN, D], F32, kind="ExternalOutput").ap()

    with ExitStack() as st:
        S = Sched(nc, st)
        global LAST_SCHED
        LAST_SCHED = S
        DR = Tile(None, "dram_in")
        DO = Tile(None, "dram_out")
        TABF = Tile(None, "tabf")
        TABS = Tile(None, "tabs")
        BC = {}
        for bv in (4095, 2047, 96 * 128 - 1):
            BC[bv] = nc.alloc_register(mybir.EngineType.Pool, "bc%d" % bv)
            S.op("pool", lambda e, bv=bv: e.reg_mov(BC[bv], bv))

        X = S.sbuf("X", [128, NT, D], F32)
        XT = [Tile(X.ap[:, t, :], "X%d" % t) for t in range(NT)]
        HT = S.sbuf("HT", [128, 8, S_LEN], BF16)
        CONST = S.sbuf("CONST", [128, NCONST], F32)
        IDB = S.sbuf("IDB", [128, 128], BF16)
        TRIB = S.sbuf("TRIB", [128, 128], BF16)
        ONEB = S.sbuf("ONEB", [128, 128], BF16)
        MOD = S.sbuf("MOD", [128, 2, 48, 2], F32)
        G1B = S.sbuf("G1B", [128, D], F32)
        LNG = S.sbuf("LNG", [128, D], F32)
        LNB = S.sbuf("LNB", [128, D], F32)
        PVT = S.sbuf("PVT", [128, 40], F32)
        MV = S.sbuf("MV", [128, NT, 2], F32)
        RSTD = S.sbuf("RSTD", [128, NT], F32)
        NHALF = S.sbuf("NHALF", [128, 512], F32)

        ident = CONST[:, C_ID:C_ID + 128]
        tri = CONST[:, C_TRI:C_TRI + 128]
        su = CONST[:, C_SU:C_SU + 128]
        ones = CONST[:, C_ONE:C_ONE + 128]

        with ExitStack() as ph:
            S.dma("sp", lambda e: e.dma_start(out=CONST[:, :], in_=const_d[:, :]), reads=[DR], writes=[CONST], sync=CONST)
            S.op("dve", lambda e: e.tensor_copy(out=IDB[:, :], in_=ident), reads=[CONST], writes=[IDB])
            S.op("dve", lambda e: e.tensor_copy(out=TRIB[:, :], in_=tri), reads=[CONST], writes=[TRIB])
            S.op("dve", lambda e: e.tensor_copy(out=ONEB[:, :], in_=ones), reads=[CONST], writes=[ONEB])
            S.op("pool", lambda e: e.memset(NHALF[:, :], -0.5), writes=[NHALF])
            STG = S.sbuf("STG", [128, 128], F32, st=ph)
            S.op("dve", lambda e: e.memset(STG[:, :], 0.0), writes=[STG])
            S.dma("sp", lambda e: [e.dma_start(out=STG[0:40, :], in_=pv_d[:, :]),
                                   e.dma_start(out=STG[64:80, :], in_=c_d.rearrange("s (c p) -> (s c) p", p=128)),
                                   ], reads=[DR], writes=[STG], sync=STG, n=2)
            pp = S.psum("pp_setup", [128, 512], F32, st=ph)
            pp2 = S.psum("pp_setup2", [128, 512], F32, st=ph)
            S.op("pe", lambda e: e.transpose(pp[:, 0:128], STG[:, :], ident), reads=[STG, CONST], writes=[pp])
            S.op("dve", lambda e: e.tensor_copy(out=PVT[:, :], in_=pp[:, 0:40]), reads=[pp], writes=[PVT])
            CACT = S.sbuf("CACT", [128, 2, 8], BF16, st=ph)
            S.op("act", lambda e: e.activation(out=CACT[:, :, :].rearrange("p s c -> p (s c)"), in_=pp[:, 64:80], func=AF.Silu), reads=[pp], writes=[CACT])
            STB = S.sbuf("STB", [128, 128], F32, st=ph)
            S.op("dve", lambda e: e.memset(STB[:, :], 0.0), writes=[STB])
            S.dma("sp", lambda e: e.dma_start(out=STB[0:96, :], in_=ada_b_d.rearrange("l (j p) -> (l j) p", p=128)), reads=[DR], writes=[STB], sync=STB)
            S.op("pe", lambda e: e.transpose(pp2[:, 0:128], STB[:, :], ident), reads=[STB, CONST], writes=[pp2])
            ADABT = S.sbuf("ADABT", [128, 96], F32, st=ph)
            S.op("dve", lambda e: e.tensor_copy(out=ADABT[:, :], in_=pp2[:, 0:96]), reads=[pp2], writes=[ADABT])
            AW = [S.sbuf("AW%d" % i, [128, 8, 512], BF16, st=ph) for i in range(3)]
            modp = S.psum("modp", [128, 2, 48, 2], F32, st=ph)
            for l in range(2):
                for blk in range(12):
                    aw = AW[(l * 12 + blk) % 3]
                    src = ada_w_d[l].rearrange("(c p) n -> p c n", p=128)[:, :, blk * 512:(blk + 1) * 512]
                    S.dma("pool", lambda e, aw=aw, src=src: e.dma_start(out=aw[:, :, :], in_=src), reads=[DR], writes=[aw], sync=aw)
                    for jj in range(4):
                        j = blk * 4 + jj
                        for kc in range(8):
                            S.op("pe", lambda e, aw=aw, jj=jj, kc=kc, l=l, j=j: e.matmul(
                                modp[:, l, j, :], lhsT=aw[:, kc, jj * 128:(jj + 1) * 128], rhs=CACT[:, :, kc],
                                start=(kc == 0), stop=(kc == 7)), reads=[aw, CACT], writes=[modp])
            for l in range(2):
                for s in range(2):
                    S.op("dve", lambda e, l=l, s=s: e.tensor_tensor(out=MOD[:, l, :, s], in0=modp[:, l, :, s], in1=ADABT[:, l * 48:(l + 1) * 48], op=ALU.add),
                         reads=[modp, ADABT], writes=[MOD])
            for l in range(2):
                for j0 in (8, 32):
                    S.op("dve", lambda e, l=l, j0=j0: e.tensor_scalar(out=MOD[:, l, j0:j0 + 8, :], in0=MOD[:, l, j0:j0 + 8, :], scalar1=1.0, scalar2=1.0 / ALPHA, op0=ALU.add, op1=ALU.mult),
                         reads=[MOD], writes=[MOD])
                for j0 in (16, 40):
                    S.op("dve", lambda e, l=l, j0=j0: e.tensor_scalar(out=MOD[:, l, j0:j0 + 8, :], in0=MOD[:, l, j0:j0 + 8, :], scalar1=1.0, scalar2=None, op0=ALU.add),
                         reads=[MOD], writes=[MOD])
            S.fence()

        def load_ln(l, which, scaled):
            S.dma("sp", lambda e: [e.dma_start(out=LNG[:, :], in_=ln_d[l, 2 * which:2 * which + 1, :].partition_broadcast(128)),
                                   e.dma_start(out=LNB[:, :], in_=ln_d[l, 2 * which + 1:2 * which + 2, :].partition_broadcast(128))],
                  reads=[DR], writes=[LNG, LNB], sync=LNG, n=2)
            if scaled:
                S.op("dve", lambda e: e.tensor_scalar(out=LNG[:, :], in0=LNG[:, :], scalar1=ALPHA, scalar2=None, op0=ALU.mult), reads=[LNG], writes=[LNG])
                S.op("dve", lambda e: e.tensor_scalar(out=LNB[:, :], in0=LNB[:, :], scalar1=ALPHA, scalar2=None, op0=ALU.mult), reads=[LNB], writes=[LNB])

        def build_g1b(l, j0, s, pp_ts, scratch, dst=None):
            dst = G1B if dst is None else dst
            for c in range(8):
                S.op("dve", lambda e, c=c: e.tensor_scalar(out=scratch[:, c, :], in0=ident, scalar1=MOD[:, l, j0 + c, s:s + 1], scalar2=None, op0=ALU.mult),
                     reads=[CONST, MOD], writes=[scratch])
            for hf in range(2):
                pp_t = pp_ts[hf]
                for c4 in range(4):
                    S.op("pe", lambda e, hf=hf, c4=c4, pp_t=pp_t: e.matmul(pp_t[:, c4 * 128:(c4 + 1) * 128], lhsT=ones, rhs=scratch[:, hf * 4 + c4, :], start=True, stop=True),
                         reads=[CONST, scratch], writes=[pp_t])
                S.op("act", lambda e, hf=hf, pp_t=pp_t: e.activation(out=dst[:, hf * 512:(hf + 1) * 512], in_=pp_t[:, :], func=AF.Copy), reads=[pp_t], writes=[dst])

        def transpose_tile(t, l, jsc, jsh, s, tp_rot, dst_fn, dst_tile, f32_dst=None):
            for hlf in range(2):
                tp = tp_rot.next()
                for c4 in range(4):
                    c = hlf * 4 + c4
                    S.op("pe", lambda e, tp=tp, c=c, c4=c4: e.transpose(tp[:, c4 * 128:(c4 + 1) * 128], XT[t][:, c * 128:(c + 1) * 128], ident),
                         reads=[XT[t], CONST], writes=[tp])
                for c4 in range(4):
                    c = hlf * 4 + c4
                    o_ap = dst_fn(c) if f32_dst is None else f32_dst[:, c, :]
                    o_t = dst_tile if f32_dst is None else f32_dst
                    if c % 2 == 0:
                        S.op("dve", lambda e, tp=tp, c=c, c4=c4, o_ap=o_ap: e.tensor_scalar(
                            out=o_ap, in0=tp[:, c4 * 128:(c4 + 1) * 128], scalar1=MOD[:, l, jsc + c, s:s + 1], scalar2=MOD[:, l, jsh + c, s:s + 1],
                            op0=ALU.mult, op1=ALU.add), reads=[tp, MOD], writes=[o_t])
                    else:
                        S.op("act", lambda e, tp=tp, c=c, c4=c4, o_ap=o_ap: e.activation(
                            out=o_ap, in_=tp[:, c4 * 128:(c4 + 1) * 128], func=AF.Identity, scale=MOD[:, l, jsc + c, s:s + 1], bias=MOD[:, l, jsh + c, s:s + 1]),
                            reads=[tp, MOD], writes=[o_t])

        LN_STATS = S.sbuf("STATS", [128, NT, 2, 6], F32)
        LN_VE = S.sbuf("VE", [128, NT], F32)
        LN_NMR = S.sbuf("NMR", [128, NT], F32)

        def layer_norm_all():
            STATS, VE, NMR = LN_STATS, LN_VE, LN_NMR
            for t in range(NT):
                for h2 in range(2):
                    S.op("dve", lambda e, t=t, h2=h2: e.bn_stats(out=STATS[:, t, h2, :], in_=XT[t][:, h2 * 512:(h2 + 1) * 512]), reads=[XT[t]], writes=[STATS])
                S.op("dve", lambda e, t=t: e.bn_aggr(out=MV[:, t, :], in_=STATS[:, t, :, :].rearrange("p a b -> p (a b)")), reads=[STATS], writes=[MV])
            S.op("dve", lambda e: e.tensor_scalar(out=VE[:, :], in0=MV[:, :, 1], scalar1=LN_EPS, scalar2=None, op0=ALU.add), reads=[MV], writes=[VE])
            S.op("pool", lambda e: e.tensor_tensor(out=RSTD[:, :], in0=VE[:, :], in1=NHALF[:, 0:NT], op=ALU.pow), reads=[VE, NHALF], writes=[RSTD])
            S.op("dve", lambda e: e.scalar_tensor_tensor(out=NMR[:, :], in0=MV[:, :, 0], scalar=-1.0, in1=RSTD[:, :], op0=ALU.mult, op1=ALU.mult), reads=[MV, RSTD], writes=[NMR])
            for t in range(NT):
                S.op("act", lambda e, t=t: e.activation(out=XT[t][:, :], in_=XT[t][:, :], func=AF.Identity, scale=RSTD[:, t:t + 1], bias=NMR[:, t:t + 1]),
                     reads=[XT[t], NMR, RSTD], writes=[XT[t]])
                S.op("dve", lambda e, t=t: e.tensor_tensor(out=XT[t][:, :], in0=XT[t][:, :], in1=LNG[:, :], op=ALU.mult), reads=[XT[t], LNG], writes=[XT[t]])
                S.op("pool", lambda e, t=t: e.tensor_tensor(out=XT[t][:, :], in0=XT[t][:, :], in1=LNB[:, :], op=ALU.add), reads=[XT[t], LNB], writes=[XT[t]])

        LN_STt = [Tile(LN_STATS.ap[:, t], "LNST%d" % t) for t in range(NT)]
        MVt = [Tile(MV.ap[:, t, :], "MV%d" % t) for t in range(NT)]
        VEt = [Tile(LN_VE.ap[:, t:t + 1], "VE%d" % t) for t in range(NT)]
        RSTDt = [Tile(RSTD.ap[:, t:t + 1], "RSTD%d" % t) for t in range(NT)]
        NMRt = [Tile(LN_NMR.ap[:, t:t + 1], "NMR%d" % t) for t in range(NT)]

        class LNPipe:
            def __init__(self, act_stats=False, junk=None):
                self.q = []
                self.act_stats = act_stats
                self.junk = junk

            def s1(self, t):
                st_, mv, ve, rstd = LN_STt[t], MVt[t], VEt[t], RSTDt[t]
                if self.act_stats:
                    junk = self.junk
                    S.op("act", lambda e: e.activation(out=junk[:, :], in_=XT[t][:, :], func=AF.Copy, accum_out=st_[:, 0, 0:1]), reads=[XT[t]], writes=[junk, st_])
                    S.op("act", lambda e: e.activation(out=junk[:, :], in_=XT[t][:, :], func=AF.Square, accum_out=st_[:, 0, 1:2]), reads=[XT[t]], writes=[junk, st_])
                    S.op("dve", lambda e: e.tensor_scalar(out=mv[:, 0:1], in0=st_[:, 0, 0:1], scalar1=1.0 / D, scalar2=None, op0=ALU.mult), reads=[st_], writes=[mv])
                    S.op("dve", lambda e: e.tensor_tensor(out=mv[:, 1:2], in0=mv[:, 0:1], in1=mv[:, 0:1], op=ALU.mult), reads=[mv], writes=[mv])
                    S.op("dve", lambda e: e.scalar_tensor_tensor(out=ve[:, :], in0=st_[:, 0, 1:2], scalar=1.0 / D, in1=mv[:, 1:2], op0=ALU.mult, op1=ALU.subtract), reads=[st_, mv], writes=[ve])
                    S.op("dve", lambda e: e.tensor_scalar(out=ve[:, :], in0=ve[:, :], scalar1=LN_EPS, scalar2=None, op0=ALU.add), reads=[ve], writes=[ve])
                else:
                    for h2 in range(2):
                        S.op("dve", lambda e, h2=h2: e.bn_stats(out=st_[:, h2, :], in_=XT[t][:, h2 * 512:(h2 + 1) * 512]), reads=[XT[t]], writes=[st_])
                    S.op("dve", lambda e: e.bn_aggr(out=mv[:, :], in_=st_[:, :, :].rearrange("p a b -> p (a b)")), reads=[st_], writes=[mv])
                    S.op("dve", lambda e: e.tensor_scalar(out=ve[:, :], in0=mv[:, 1:2], scalar1=LN_EPS, scalar2=None, op0=ALU.add), reads=[mv], writes=[ve])
                S.op("pool", lambda e: e.tensor_tensor(out=rstd[:, :], in0=ve[:, :], in1=NHALF[:, 0:1], op=ALU.pow), reads=[ve, NHALF], writes=[rstd])

            def s2(self, t):
                mv, rstd, nmr = MVt[t], RSTDt[t], NMRt[t]
                S.op("dve", lambda e: e.scalar_tensor_tensor(out=nmr[:, :], in0=mv[:, 0:1], scalar=-1.0, in1=rstd[:, :], op0=ALU.mult, op1=ALU.mult), reads=[mv, rstd], writes=[nmr])
                S.op("act", lambda e: e.activation(out=XT[t][:, :], in_=XT[t][:, :], func=AF.Identity, scale=rstd[:, :], bias=nmr[:, :]), reads=[XT[t], nmr, rstd], writes=[XT[t]])

            def s3(self, t):
                S.op("dve", lambda e: e.tensor_tensor(out=XT[t][:, :], in0=XT[t][:, :], in1=LNG[:, :], op=ALU.mult), reads=[XT[t], LNG], writes=[XT[t]])
                S.op("pool", lambda e: e.tensor_tensor(out=XT[t][:, :], in0=XT[t][:, :], in1=LNB[:, :], op=ALU.add), reads=[XT[t], LNB], writes=[XT[t]])

            def push(self, t):
                self.q.append([t, 0])
                self.step()

            def step(self):
                for ent in list(self.q):
                    if ent[1] == 0:
                        self.s1(ent[0])
                    elif ent[1] == 1:
                        self.s2(ent[0])
                    else:
                        self.s3(ent[0])
                    ent[1] += 1
                self.q = [en for en in self.q if en[1] < 3]

            def flush(self):
                while self.q:
                    self.step()

        NV, NJ = 48, 96
        BIG = 65536.0
        BIGW = 8192.0

        def moe_phase(l, s, last):
            jsh, jsc, jg = 24, 32, 40
            wall_d = wall_ds[l]
            IOA = bass.IndirectOffsetOnAxis
            with ExitStack() as ph:
                LG = S.sbuf("LG", [128, NT, 36], F32, st=ph)
                P12 = S.sbuf("P12", [128, 2, NT], F32, st=ph)
                IDXY = S.sbuf("IDXY", [128, NJ], I32, st=ph)
                IDXG = S.sbuf("IDXG", [128, NJ], I32, st=ph)
                WIDX = S.sbuf("WIDX", [128, NV], I32, st=ph)
                NW, NH = 4, 4

                with ExitStack() as ph1:
                    WR = S.sbuf("WR", [128, 8, 36], F32, st=ph1)
                    RB = S.sbuf("RB", [128, 36], F32, st=ph1)
                    H32 = Rot([S.sbuf("H32_%d" % i, [128, 8, 128], F32, st=ph1) for i in range(3)])
                    SCB = S.sbuf("SCB", [128, D], F32, st=ph1)
                    SHB = S.sbuf("SHB", [128, D], F32, st=ph1)
                    TM32 = Rot([S.sbuf("TM32_%d" % i, [128, D], F32, st=ph1) for i in range(2)])
                    HB = Rot([S.sbuf("HB_%d" % i, [128, D], BF16, st=ph1) for i in range(2)])
                    tp_rot = Rot([S.psum("tp%d" % i, [128, 512], F32, st=ph1) for i in range(4)])
                    lg_rot = Rot([S.psum("lgp%d" % i, [128, 512], F32, st=ph1) for i in range(2)])
                    scrs = [S.sbuf("scr%d" % i, [128, 8, 128], F32, st=ph1) for i in range(2)]
                    S.dma("sp", lambda e: [e.dma_start(out=WR[:, :, :], in_=wr_d[l].rearrange("(c p) n -> p c n", p=128)),
                                           e.dma_start(out=RB[:, :], in_=br_d[l:l + 1, :].partition_broadcast(128))],
                          reads=[DR], writes=[WR, RB], sync=WR, n=2)
                    build_g1b(l, jsc, s, tp_rot.tiles[0:2], scrs[0], dst=SCB)
                    build_g1b(l, jsh, s, tp_rot.tiles[2:4], scrs[1], dst=SHB)
                    build_g1b(l, jg, s, lg_rot.tiles, scrs[0])
                    load_ln(l, 1, not last)
                    h32s = {}

                    def router(t):
                        h32 = h32s.pop(t)
                        lgp = lg_rot.next()
                        for c in range(8):
                            S.op("pe", lambda e, c=c: e.matmul(lgp[:, 0:36], lhsT=h32[:, c, :], rhs=WR[:, c, :], start=(c == 0), stop=(c == 7)),
                                 reads=[h32, WR], writes=[lgp])
                        S.op("dve", lambda e: e.tensor_tensor(out=LG[:, t, :], in0=lgp[:, 0:36], in1=RB[:, :], op=ALU.add), reads=[lgp, RB], writes=[LG])

                    for t in range(NT):
                        h32 = H32.next()
                        h32s[t] = h32
                        transpose_tile(t, l, jsc, jsh, s, tp_rot, None, None, f32_dst=h32)
                        tm = TM32.next()
                        hb = HB.next()
                        S.op("dve", lambda e, t=t, tm=tm: e.tensor_tensor(out=tm[:, :], in0=XT[t][:, :], in1=SCB[:, :], op=ALU.mult), reads=[XT[t], SCB], writes=[tm])
                        S.op("pool", lambda e, tm=tm, hb=hb: e.tensor_tensor(out=hb[:, :], in0=tm[:, :], in1=SHB[:, :], op=ALU.add), reads=[tm, SHB], writes=[hb])
                        S.dma("sp", lambda e, t=t, hb=hb: e.dma_start(out=htok_d[t * 128:(t + 1) * 128, :], in_=hb[:, :]), reads=[hb], writes=[], sync=hb)
                        if t >= 1:
                            router(t - 1)
                    router(NT - 1)
                    S.fence()
                with ExitStack() as ph1:
                    def sb(name, shape, dt=F32):
                        return S.sbuf(name, shape, dt, st=ph1)
                    C2 = sb("C2", [128, NC2])
                    S.dma("sp", lambda e: e.dma_start(out=C2[:, :], in_=const2_d[:, :]), reads=[DR], writes=[C2], sync=C2)
                    GMAX = sb("GMAX", [128, NT]); DG_ = sb("DGL", [128, NT, 4]); GE = sb("GE", [128, NT, 4]); GS = sb("GS", [128, NT])
                    GW = sb("GW", [128, NT]); PEN = sb("PEN", [128, NT, 4]); EM = sb("EM", [128, NT, 32]); M1 = sb("M1", [128, NT])
                    OH1 = sb("OH1", [128, NT, 32]); EM2 = sb("EM2", [128, NT, 32]); M2 = sb("M2", [128, NT]); OH2 = sb("OH2", [128, NT, 32])
                    DM = sb("DM", [128, NT]); P1 = sb("P1", [128, NT]); P2 = sb("P2", [128, NT])
                    GL = LG[:, :, 0:4]
                    EL = LG[:, :, 4:36]
                    V = lambda f, r, w: S.op("dve", f, reads=r, writes=w)
                    V(lambda e: e.tensor_reduce(out=GMAX[:, :], in_=GL, axis=AX.X, op=ALU.max), [LG], [GMAX])
                    V(lambda e: e.tensor_tensor(out=DG_[:, :, :], in0=GL, in1=GMAX[:, :].unsqueeze(2).to_broadcast([128, NT, 4]), op=ALU.subtract), [LG, GMAX], [DG_])
                    S.op("act", lambda e: e.activation(out=GE[:, :, :], in_=DG_[:, :, :], func=AF.Exp), reads=[DG_], writes=[GE])
                    V(lambda e: e.tensor_reduce(out=GS[:, :], in_=GE[:, :, :], axis=AX.X, op=ALU.add), [GE], [GS])
                    V(lambda e: e.reciprocal(out=GW[:, :], in_=GS[:, :]), [GS], [GW])
                    V(lambda e: e.tensor_scalar(out=PEN[:, :, :], in0=DG_[:, :, :], scalar1=0.0, scalar2=-1e30, op0=ALU.is_lt, op1=ALU.mult), [DG_], [PEN])
                    V(lambda e: e.tensor_tensor(out=EM[:, :, :].rearrange("p t (g j) -> p t g j", g=4), in0=EL.rearrange("p t (g j) -> p t g j", g=4),
                                                in1=PEN[:, :, :].unsqueeze(3).to_broadcast([128, NT, 4, 8]), op=ALU.add), [LG, PEN], [EM])
                    V(lambda e: e.tensor_reduce(out=M1[:, :], in_=EM[:, :, :], axis=AX.X, op=ALU.max), [EM], [M1])
                    V(lambda e: e.tensor_tensor(out=OH1[:, :, :], in0=EM[:, :, :], in1=M1[:, :].unsqueeze(2).to_broadcast([128, NT, 32]), op=ALU.is_equal), [EM, M1], [OH1])
                    V(lambda e: e.scalar_tensor_tensor(out=EM2[:, :, :], in0=OH1[:, :, :], scalar=-1e30, in1=EM[:, :, :], op0=ALU.mult, op1=ALU.add), [OH1, EM], [EM2])
                    V(lambda e: e.tensor_reduce(out=M2[:, :], in_=EM2[:, :, :], axis=AX.X, op=ALU.max), [EM2], [M2])
                    V(lambda e: e.tensor_tensor(out=OH2[:, :, :], in0=EM2[:, :, :], in1=M2[:, :].unsqueeze(2).to_broadcast([128, NT, 32]), op=ALU.is_equal), [EM2, M2], [OH2])
                    V(lambda e: e.tensor_tensor(out=DM[:, :], in0=M2[:, :], in1=M1[:, :], op=ALU.subtract), [M1, M2], [DM])
                    S.op("act", lambda e: e.activation(out=DM[:, :], in_=DM[:, :], func=AF.Exp), reads=[DM], writes=[DM])
                    V(lambda e: e.tensor_scalar(out=DM[:, :], in0=DM[:, :], scalar1=1.0, scalar2=None, op0=ALU.add), [DM], [DM])
                    V(lambda e: e.reciprocal(out=P1[:, :], in_=DM[:, :]), [DM], [P1])
                    V(lambda e: e.tensor_scalar(out=P2[:, :], in0=P1[:, :], scalar1=-1.0, scalar2=1.0, op0=ALU.mult, op1=ALU.add), [P1], [P2])
                    V(lambda e: e.tensor_tensor(out=P12[:, 0, :], in0=P1[:, :], in1=GW[:, :], op=ALU.mult), [P1, GW], [P12])
                    V(lambda e: e.tensor_tensor(out=P12[:, 1, :], in0=P2[:, :], in1=GW[:, :], op=ALU.mult), [P2, GW], [P12])
                    M = sb("M", [128, NT, 32]); MB = sb("MB", [128, NT, 32], BF16)
                    EXC = sb("EXC", [128, NT, 32]); TOT = sb("TOT", [128, NT, 32]); OFFS = sb("OFFS", [128, NT + 1, 32])
                    ip = S.psum("ip", [128, 512], F32, st=ph1)
                    tpp = S.psum("tpp", [128, 512], F32, st=ph1)
                    V(lambda e: e.tensor_tensor(out=M[:, :, :], in0=OH1[:, :, :], in1=OH2[:, :, :], op=ALU.add), [OH1, OH2], [M])
                    V(lambda e: e.tensor_copy(out=MB[:, :, :], in_=M[:, :, :]), [M], [MB])
                    S.op("pe", lambda e: e.matmul(ip[:, :], lhsT=TRIB[:, :], rhs=MB[:, :, :].rearrange("p t e -> p (t e)"), start=True, stop=True), reads=[TRIB, MB], writes=[ip])
                    S.op("pe", lambda e: e.matmul(tpp[:, :], lhsT=ONEB[:, :], rhs=MB[:, :, :].rearrange("p t e -> p (t e)"), start=True, stop=True), reads=[ONEB, MB], writes=[tpp])
                    V(lambda e: e.tensor_tensor(out=EXC[:, :, :], in0=ip[:, :].rearrange("p (t e) -> p t e", e=32), in1=M[:, :, :], op=ALU.subtract), [ip, M], [EXC])
                    S.op("act", lambda e: e.activation(out=TOT[:, :, :], in_=tpp[:, :].rearrange("p (t e) -> p t e", e=32), func=AF.Copy), reads=[tpp], writes=[TOT])
                    V(lambda e: e.memset(OFFS[:, 0, :], 0.0), [], [OFFS])
                    for t in range(1, NT + 1):
                        V(lambda e, t=t: e.tensor_tensor(out=OFFS[:, t, :], in0=OFFS[:, t - 1, :], in1=TOT[:, t - 1, :], op=ALU.add), [OFFS, TOT], [OFFS])
                    TMP8 = sb("TMP8", [128, 32, 8]); NVv = sb("NVv", [128, 32]); TMP32 = sb("TMP32", [128, 32, 32]); VB = sb("VB", [128, 32]); VE = sb("VE", [128, 32]); BASE = sb("BASE", [128, 32])
                    V(lambda e: e.tensor_tensor(out=TMP8[:, :, :], in0=C2[:, C2_THR:C2_THR + 8].unsqueeze(1).to_broadcast([128, 32, 8]),
                                                in1=OFFS[:, NT, :].unsqueeze(2).to_broadcast([128, 32, 8]), op=ALU.is_lt), [C2, OFFS], [TMP8])
                    V(lambda e: e.tensor_reduce(out=NVv[:, :], in_=TMP8[:, :, :], axis=AX.X, op=ALU.add), [TMP8], [NVv])
                    V(lambda e: e.tensor_tensor(out=TMP32[:, :, :], in0=C2[:, C2_LT:C2_LT + 1024].rearrange("p (a b) -> p a b", a=32),
                                                in1=NVv[:, :].unsqueeze(1).to_broadcast([128, 32, 32]), op=ALU.mult), [C2, NVv], [TMP32])
                    V(lambda e: e.tensor_reduce(out=VB[:, :], in_=TMP32[:, :, :], axis=AX.X, op=ALU.add), [TMP32], [VB])
                    V(lambda e: e.tensor_tensor(out=VE[:, :], in0=VB[:, :], in1=NVv[:, :], op=ALU.add), [VB, NVv], [VE])
                    V(lambda e: e.tensor_scalar(out=BASE[:, :], in0=VB[:, :], scalar1=256.0, scalar2=None, op0=ALU.mult), [VB], [BASE])
                    T1 = sb("T1r", [128, NT, 32]); POS = sb("POS", [128, 2, NT]); Q = sb("Q", [128, 2, NT]); QI = sb("QI", [128, 2, NT], I32); QF = sb("QF", [128, 2, NT])
                    CORR = sb("CORR", [128, 2, NT]); PM = sb("PM", [128, 2, NT]); DEST = sb("DEST", [128, 2, NT]); DESTI = sb("DESTI", [128, 2, NT], I32)
                    VALI = sb("VALI", [128, 2, NT], I32); FILLF = sb("FILLF", [128, NJ]); FILLI = sb("FILLI", [128, NJ], I32); IF = sb("IF", [128, NJ]); IGE = sb("IGE", [128, NJ])
                    V(lambda e: e.tensor_tensor(out=EXC[:, :, :], in0=EXC[:, :, :], in1=OFFS[:, 0:NT, :], op=ALU.add), [EXC, OFFS], [EXC])
                    V(lambda e: e.tensor_tensor(out=EXC[:, :, :], in0=EXC[:, :, :], in1=BASE[:, :].unsqueeze(1).to_broadcast([128, NT, 32]), op=ALU.add), [EXC, BASE], [EXC])
                    for k, OH in ((0, OH1), (1, OH2)):
                        V(lambda e, OH=OH: e.tensor_tensor(out=T1[:, :, :], in0=OH[:, :, :], in1=EXC[:, :, :], op=ALU.mult), [OH, EXC], [T1])
                        V(lambda e, k=k: e.tensor_reduce(out=POS[:, k, :], in_=T1[:, :, :], axis=AX.X, op=ALU.add), [T1], [POS])
                    V(lambda e: e.tensor_scalar(out=Q[:, :, :], in0=POS[:, :, :], scalar1=1.0 / 128, scalar2=None, op0=ALU.mult), [POS], [Q])
                    V(lambda e: e.tensor_copy(out=QI[:, :, :], in_=Q[:, :, :]), [Q], [QI])
                    V(lambda e: e.tensor_copy(out=QF[:, :, :], in_=QI[:, :, :]), [QI], [QF])
                    V(lambda e: e.tensor_tensor(out=CORR[:, :, :], in0=QF[:, :, :], in1=Q[:, :, :], op=ALU.is_gt), [QF, Q], [CORR])
                    V(lambda e: e.tensor_tensor(out=QF[:, :, :], in0=QF[:, :, :], in1=CORR[:, :, :], op=ALU.subtract), [QF, CORR], [QF])
                    V(lambda e: e.scalar_tensor_tensor(out=PM[:, :, :], in0=QF[:, :, :], scalar=-128.0, in1=POS[:, :, :], op0=ALU.mult, op1=ALU.add), [QF, POS], [PM])
                    V(lambda e: e.scalar_tensor_tensor(out=DEST[:, :, :], in0=PM[:, :, :], scalar=float(NJ), in1=QF[:, :, :], op0=ALU.mult, op1=ALU.add), [PM, QF], [DEST])
                    V(lambda e: e.tensor_copy(out=DESTI[:, :, :], in_=DEST[:, :, :]), [DEST], [DESTI])
                    V(lambda e: e.tensor_copy(out=VALI[:, :, :], in_=C2[:, C2_VAL:C2_VAL + 32].rearrange("p (k t) -> p k t", k=2)), [C2], [VALI])
                    V(lambda e: e.memset(FILLF[:, :], BIG), [], [FILLF])
                    V(lambda e: e.tensor_copy(out=FILLI[:, :], in_=FILLF[:, :]), [FILLF], [FILLI])
                    tab_v = tab_d.rearrange("(p j) o -> p (j o)", p=128)
                    S.dma("sp", lambda e: e.dma_start(out=tab_v, in_=FILLI[:, :]), reads=[FILLI], writes=[TABF], sync=TABF)
                    last_sc = None
                    for k in range(2):
                        for t in range(NT):
                            last_sc = S.dma("pool", lambda e, k=k, t=t: e.indirect_dma_start(out=tab_d[:, :], out_offset=IOA(ap=DESTI[:, k, t:t + 1], axis=0), in_=VALI[:, k, t:t + 1], in_offset=None,
                                                                                            bounds_check=BC[NJ * 128 - 1], oob_is_err=False), reads=[DESTI, VALI, TABF], writes=[], sync=TABS)
                    VA = sb("VA", [128, NV, 32]); VBm = sb("VBm", [128, NV, 32]); WF = sb("WF", [128, NV])
                    vrow = C2[:, C2_VROW:C2_VROW + NV].unsqueeze(2).to_broadcast([128, NV, 32])
                    V(lambda e: e.tensor_tensor(out=VA[:, :, :], in0=vrow, in1=VB[:, :].unsqueeze(1).to_broadcast([128, NV, 32]), op=ALU.is_ge), [C2, VB], [VA])
                    V(lambda e: e.tensor_tensor(out=VBm[:, :, :], in0=vrow, in1=VE[:, :].unsqueeze(1).to_broadcast([128, NV, 32]), op=ALU.is_lt), [C2, VE], [VBm])
                    V(lambda e: e.tensor_tensor(out=VA[:, :, :], in0=VA[:, :, :], in1=VBm[:, :, :], op=ALU.mult), [VA, VBm], [VA])
                    V(lambda e: e.tensor_tensor(out=VA[:, :, :], in0=VA[:, :, :], in1=C2[:, C2_EROW:C2_EROW + 32].unsqueeze(1).to_broadcast([128, NV, 32]), op=ALU.mult), [VA, C2], [VA])
                    V(lambda e: e.tensor_reduce(out=WF[:, :], in_=VA[:, :, :], axis=AX.X, op=ALU.add), [VA], [WF])
                    V(lambda e: e.tensor_scalar(out=WF[:, :], in0=WF[:, :], scalar1=BIGW, scalar2=None, op0=ALU.add), [WF], [WF])
                    V(lambda e: e.tensor_copy(out=WIDX[:, :], in_=WF[:, :]), [WF], [WIDX])
                    TABS.w = last_sc
                    S.dma("sp", lambda e: e.dma_start(out=IDXY[:, :], in_=tab_v), reads=[TABS], writes=[IDXY], sync=IDXY)
                    TABS.w = None
                    V(lambda e: e.tensor_copy(out=IF[:, :], in_=IDXY[:, :]), [IDXY], [IF])
                    V(lambda e: e.tensor_single_scalar(out=IGE[:, :], in_=IF[:, :], scalar=2048.0, op=ALU.is_ge), [IF], [IGE])
                    V(lambda e: e.scalar_tensor_tensor(out=IF[:, :], in0=IGE[:, :], scalar=-2048.0, in1=IF[:, :], op0=ALU.mult, op1=ALU.add), [IGE, IF], [IF])
                    V(lambda e: e.tensor_copy(out=IDXG[:, :], in_=IF[:, :]), [IF], [IDXG])
                    S.fence()
                with ExitStack() as ph2:
                    W = [S.sbuf("W%d" % i, [128, 6144], BF16, st=ph2) for i in range(NW)]
                    HS = [S.sbuf("HS%d" % i, [128, D], BF16, st=ph2) for i in range(NH)]
                    for hs in HS:
                        S.op("dve", lambda e, hs=hs: e.memset(hs[:, :], 0.0), writes=[hs])

                    def issue_w(v):
                        w = W[v % NW]
                        S.dma("pool", lambda e: e.indirect_dma_start(out=w[:, :], out_offset=None, in_=wall_d[:, :], in_offset=IOA(ap=WIDX[:, v:v + 1], axis=0),
                                                                     bounds_check=BC[4095], oob_is_err=False), reads=[DR, WIDX, w], writes=[w], sync=w)

                    for v in range(NW):
                        issue_w(v)
                    HST = Rot([S.sbuf("HST%d" % i, [128, 8, 128], BF16, st=ph2) for i in range(2)])
                    SG = Rot([S.sbuf("SG%d" % i, [128, 256], F32, st=ph2) for i in range(2)])
                    HID = Rot([S.sbuf("HID%d" % i, [128, 256], BF16, st=ph2) for i in range(3)])
                    HIDT = Rot([S.sbuf("HIDT%d" % i, [128, 2, 128], BF16, st=ph2) for i in range(3)])
                    YSB = Rot([S.sbuf("YSB%d" % i, [128, D], F32, st=ph2) for i in range(3)])
                    xT_rot = Rot([S.psum("xTp%d" % i, [128, 8, 128], BF16, st=ph2) for i in range(2)])
                    gu_rot = Rot([S.psum("gu%d" % i, [128, 512], F32, st=ph2) for i in range(2)])
                    hTp = S.psum("hTp", [128, 2, 2, 128], BF16, st=ph2)
                    hT_rot = Rot([Tile(hTp.ap[:, i], "hTp%d" % i) for i in range(2)])
                    y_rot = Rot([S.psum("yp%d" % i, [128, 512], F32, st=ph2) for i in range(3)])
                    state = {}

                    def issue_gather(j):
                        hs = HS[j % NH]
                        S.dma("pool", lambda e: e.indirect_dma_start(out=hs[:, :], out_offset=None, in_=htok_d[:, :], in_offset=IOA(ap=IDXG[:, j:j + 1], axis=0),
                                                                     bounds_check=BC[S_LEN - 1], oob_is_err=False), reads=[DR, IDXG, hs], writes=[hs], sync=hs)

                    def do_T8(j):
                        hs = HS[j % NH]
                        xp = xT_rot.next()
                        for c in range(8):
                            S.op("pe", lambda e, c=c: e.transpose(xp[:, c, :], hs[:, c * 128:(c + 1) * 128], IDB[:, :]), reads=[hs, IDB], writes=[xp])
                        hst = HST.next()
                        S.op("act", lambda e: e.activation(out=hst[:, :, :], in_=xp[:, :, :], func=AF.Copy), reads=[xp], writes=[hst])
                        state[j] = [hst, None, None]

                    def do_GU(j):
                        w = W[(j // 2) % NW]
                        hst = state[j][0]
                        gp = gu_rot.next()
                        for c in range(8):
                            S.op("pe", lambda e, c=c: e.matmul(gp[:, :], lhsT=hst[:, c, :], rhs=w[:, c * 512:(c + 1) * 512], start=(c == 0), stop=(c == 7)),
                                 reads=[hst, w], writes=[gp])
                        sg = SG.next()
                        hid = HID.next()
                        S.op("act", lambda e: e.activation(out=sg[:, :], in_=gp[:, 0:256], func=AF.Silu), reads=[gp], writes=[sg])
                        S.op("dve", lambda e: e.tensor_tensor(out=hid[:, :], in0=gp[:, 256:512], in1=sg[:, :], op=ALU.mult), reads=[gp, sg], writes=[hid])
                        state[j][1] = hid

                    def do_HT(j):
                        hid = state[j][1]
                        hp = hT_rot.next()
                        for c in range(2):
                            S.op("pe", lambda e, c=c: e.transpose(hp[:, c, :], hid[:, c * 128:(c + 1) * 128], IDB[:, :]), reads=[hid, IDB], writes=[hp])
                        hT = HIDT.next()
                        S.op("act", lambda e: e.activation(out=hT[:, :, :], in_=hp[:, :, :], func=AF.Copy), reads=[hp], writes=[hT])
                        state[j][2] = hT

                    def do_D(j):
                        w = W[(j // 2) % NW]
                        hT = state.pop(j)[2]
                        ysb = YSB.next()
                        for hf in range(2):
                            yp = y_rot.next()
                            for c in range(2):
                                S.op("pe", lambda e, hf=hf, c=c, yp=yp: e.matmul(yp[:, :], lhsT=hT[:, c, :], rhs=w[:, 4096 + c * 1024 + hf * 512:4096 + c * 1024 + (hf + 1) * 512],
                                                                                 start=(c == 0), stop=(c == 1)), reads=[hT, w], writes=[yp])
                            S.op("dve", lambda e, yp=yp, hf=hf: e.tensor_tensor(out=ysb[:, hf * 512:(hf + 1) * 512], in0=yp[:, :], in1=G1B[:, hf * 512:(hf + 1) * 512], op=ALU.mult),
                                 reads=[yp, G1B], writes=[ysb])
                        S.dma("pool", lambda e: e.indirect_dma_start(out=ys_d[:, :], out_offset=IOA(ap=IDXY[:, j:j + 1], axis=0), in_=ysb[:, :], in_offset=None,
                                                                     bounds_check=BC[2 * S_LEN - 1], oob_is_err=False), reads=[ysb, IDXY], writes=[], sync=ysb)

                    for j in range(NH):
                        issue_gather(j)
                    do_T8(0)
                    issue_gather(NH)
                    for i in range(NJ + 2):
                        if i + 1 < NJ:
                            do_T8(i + 1)
                            if i + 1 + NH < NJ:
                                issue_gather(i + 1 + NH)
                        if i < NJ:
                            do_GU(i)
                        if 1 <= i <= NJ:
                            do_HT(i - 1)
                        if 2 <= i:
                            do_D(i - 2)
                            if (i - 2) % 2 == 1:
                                nv_ = (i - 2) // 2 + NW
                                if nv_ < NV:
                                    issue_w(nv_)
                    S.fence()
                with ExitStack() as ph3:
                    Y0 = Rot([S.sbuf("Y0_%d" % i, [128, D], F32, st=ph3) for i in range(3)])
                    Y1 = Rot([S.sbuf("Y1_%d" % i, [128, D], F32, st=ph3) for i in range(3)])
                    LJ = S.sbuf("LNJ", [128, D], BF16, st=ph3)
                    lnp = LNPipe(act_stats=True, junk=LJ)
                    for t in range(NT):
                        y0 = Y0.next(); y1 = Y1.next()
                        S.dma("sp", lambda e, t=t, y0=y0: e.dma_start(out=y0[:, :], in_=ys_d[t * 128:(t + 1) * 128, :]), reads=[DR], writes=[y0], sync=y0)
                        S.dma("sp", lambda e, t=t, y1=y1: e.dma_start(out=y1[:, :], in_=ys_d[S_LEN + t * 128:S_LEN + (t + 1) * 128, :]), reads=[DR], writes=[y1], sync=y1)
                        S.op("dve", lambda e, t=t, y0=y0: e.scalar_tensor_tensor(out=XT[t][:, :], in0=y0[:, :], scalar=P12[:, 0, t:t + 1], in1=XT[t][:, :], op0=ALU.mult, op1=ALU.add),
                             reads=[y0, P12, XT[t]], writes=[XT[t]])
                        S.op("dve", lambda e, t=t, y1=y1: e.scalar_tensor_tensor(out=XT[t][:, :], in0=y1[:, :], scalar=P12[:, 1, t:t + 1], in1=XT[t][:, :], op0=ALU.mult, op1=ALU.add),
                             reads=[y1, P12, XT[t]], writes=[XT[t]])
                        lnp.push(t)
                    lnp.flush()
                    S.fence()

        def l0_mixer(s):
            l = 0
            jsh, jsc, jg = 0, 8, 16
            CATA = [Tile(HT.ap[:, 0:4, B * 512:(B + 1) * 512], "CATA%d" % B) for B in range(4)]
            CATB = [Tile(HT.ap[:, 4:8, t * 128:(t + 1) * 128], "CATB%d" % t) for t in range(NT)]
            load_ln(l, 0, True)
            with ExitStack() as p12:
                HCT = S.sbuf("HCT", [128, 4, 2080], BF16, st=p12)
                S.op("pool", lambda e: e.memset(HCT[:, :, 0:30], 0.0), writes=[HCT])
                with ExitStack() as p1:
                    WINC = S.sbuf("WINC", [128, 8, 1024], BF16, st=p1)
                    S.dma("pool", lambda e: e.dma_start(out=WINC[:, :, :], in_=ab_w_in_d.rearrange("(c p) n -> p c n", p=128)[:, :, 0:1024]),
                          reads=[DR], writes=[WINC], sync=WINC)
                    HTB = Rot([S.sbuf("HTB%d" % i, [128, 8, 512], BF16, st=p1) for i in range(2)])
                    SIG = Rot([S.sbuf("SIG%d" % i, [128, 512], F32, st=p1) for i in range(2)])
                    tp_rot = Rot([S.psum("tp%d" % i, [128, 512], F32, st=p1) for i in range(4)])
                    ag_rot = Rot([S.psum("ag%d" % i, [128, 512], F32, st=p1) for i in range(4)])
                    scr = S.sbuf("scr", [128, 8, 128], F32, st=p1)
                    build_g1b(l, jg, s, ag_rot.tiles[0:2], scr)
                    for B in range(4):
                        htb = HTB.next()
                        for i in range(4):
                            transpose_tile(4 * B + i, l, jsc, jsh, s, tp_rot, lambda c, i=i, htb=htb: htb[:, c, i * 128:(i + 1) * 128], htb)
                        for cc in range(4):
                            a_p = ag_rot.next()
                            g_p = ag_rot.next()
                            for kc in range(8):
                                S.op("pe", lambda e, kc=kc, cc=cc, a_p=a_p, htb=htb: e.matmul(a_p[:, :], lhsT=WINC[:, kc, cc * 128:(cc + 1) * 128], rhs=htb[:, kc, :], start=(kc == 0), stop=(kc == 7)),
                                     reads=[WINC, htb], writes=[a_p])
                            for kc in range(8):
                                S.op("pe", lambda e, kc=kc, cc=cc, g_p=g_p, htb=htb: e.matmul(g_p[:, :], lhsT=WINC[:, kc, 512 + cc * 128:512 + (cc + 1) * 128], rhs=htb[:, kc, :], start=(kc == 0), stop=(kc == 7)),
                                     reads=[WINC, htb], writes=[g_p])
                            sig = SIG.next()
                            S.op("act", lambda e, sig=sig, g_p=g_p: e.activation(out=sig[:, :], in_=g_p[:, :], func=AF.Sigmoid), reads=[g_p], writes=[sig])
                            S.op("dve", lambda e, sig=sig, a_p=a_p, cc=cc, B=B: e.tensor_tensor(out=HCT[:, cc, 30 + B * 512:30 + (B + 1) * 512], in0=a_p[:, :], in1=sig[:, :], op=ALU.mult),
                                 reads=[a_p, sig], writes=[HCT])
                    S.fence()
                if stop_after == "l0a":
                    return
                with ExitStack() as p2:
                    CWS = S.sbuf("CWS", [32, 512], F32, st=p2)
                    CW = S.sbuf("CW", [128, 4, 31], F32, st=p2)
                    DG = [S.sbuf("DG%d" % cc, [128, 31, 128], BF16, st=p2) for cc in range(4)]
                    Y = S.sbuf("Y", [128, 4, 512], F32, st=p2)
                    YSQ = S.sbuf("YSQ", [128, 4, 512], F32, st=p2)
                    MEAN = S.sbuf("MEAN", [128, 512], F32, st=p2)
                    MSQ = S.sbuf("MSQ", [128, 512], F32, st=p2)
                    VAR = S.sbuf("VAR", [128, 512], F32, st=p2)
                    RS = S.sbuf("RS", [128, 512], F32, st=p2)
                    T1 = Rot([S.sbuf("T1_%d" % i, [128, 512], F32, st=p2) for i in range(2)])
                    y_ps = [S.psum("yps%d" % i, [128, 512], F32, st=p2) for i in range(4)]
                    st_rot = Rot([S.psum("stp%d" % i, [128, 512], F32, st=p2) for i in range(2)])
                    S.dma("sp", lambda e: e.dma_start(out=CWS[0:31, :], in_=conv_w_d[:, :]), reads=[DR], writes=[CWS], sync=CWS)
                    for cc in range(4):
                        pt = st_rot.next()
                        S.op("pe", lambda e, cc=cc, pt=pt: e.transpose(pt[:, 0:31], CWS[0:31, cc * 128:(cc + 1) * 128], CONST[0:31, C_ID:C_ID + 31]), reads=[CWS, CONST], writes=[pt])
                        S.op("dve", lambda e, cc=cc, pt=pt: e.tensor_copy(out=CW[:, cc, :], in_=pt[:, 0:31]), reads=[pt], writes=[CW])
                    for cc in range(4):
                        for j in range(31):
                            S.op("dve", lambda e, cc=cc, j=j: e.tensor_scalar(out=DG[cc][:, j, :], in0=ident, scalar1=CW[:, cc, j:j + 1], scalar2=None, op0=ALU.mult),
                                 reads=[CONST, CW], writes=[DG[cc]])
                    for B in range(4):
                        for cc in range(4):
                            for j in range(31):
                                S.op("pe", lambda e, cc=cc, j=j, B=B: e.matmul(y_ps[cc][:, :], lhsT=DG[cc][:, j, :], rhs=HCT[:, cc, B * 512 + j:B * 512 + j + 512], start=(j == 0), stop=(j == 30)),
                                     reads=[DG[cc], HCT], writes=[y_ps[cc]])
                            S.op("act", lambda e, cc=cc: e.activation(out=Y[:, cc, :], in_=y_ps[cc][:, :], func=AF.Identity, bias=PVT[:, cc:cc + 1]), reads=[y_ps[cc], PVT], writes=[Y])
                            S.op("act", lambda e, cc=cc: e.activation(out=YSQ[:, cc, :], in_=y_ps[cc][:, :], func=AF.Square, bias=PVT[:, cc:cc + 1]), reads=[y_ps[cc], PVT], writes=[YSQ])
                        mean_ps = st_rot.next()
                        msq_ps = st_rot.next()
                        for cc in range(4):
                            S.op("pe", lambda e, cc=cc, mean_ps=mean_ps: e.matmul(mean_ps[:, :], lhsT=ones, rhs=Y[:, cc, :], start=(cc == 0), stop=(cc == 3)), reads=[CONST, Y], writes=[mean_ps])
                        for cc in range(4):
                            S.op("pe", lambda e, cc=cc, msq_ps=msq_ps: e.matmul(msq_ps[:, :], lhsT=ones, rhs=YSQ[:, cc, :], start=(cc == 0), stop=(cc == 3)), reads=[CONST, YSQ], writes=[msq_ps])
                        S.op("act", lambda e, mean_ps=mean_ps: e.activation(out=MEAN[:, :], in_=mean_ps[:, :], func=AF.Copy, scale=1.0 / 512), reads=[mean_ps], writes=[MEAN])
                        S.op("dve", lambda e: e.tensor_tensor(out=MSQ[:, :], in0=MEAN[:, :], in1=MEAN[:, :], op=ALU.mult), reads=[MEAN], writes=[MSQ])
                        S.op("dve", lambda e, msq_ps=msq_ps: e.scalar_tensor_tensor(out=VAR[:, :], in0=msq_ps[:, :], scalar=1.0 / 512, in1=MSQ[:, :], op0=ALU.mult, op1=ALU.subtract),
                             reads=[msq_ps, MSQ], writes=[VAR])
                        S.op("act", lambda e: e.activation(out=VAR[:, :], in_=VAR[:, :], func=AF.Ln, bias=LN_EPS), reads=[VAR], writes=[VAR])
                        S.op("act", lambda e: e.activation(out=RS[:, :], in_=VAR[:, :], func=AF.Exp, scale=-0.5), reads=[VAR], writes=[RS])
                        for cc in range(4):
                            t1 = T1.next()
                            S.op("dve", lambda e, cc=cc, t1=t1: e.tensor_tensor(out=t1[:, :], in0=Y[:, cc, :], in1=MEAN[:, :], op=ALU.subtract), reads=[Y, MEAN], writes=[t1])
                            S.op("dve", lambda e, cc=cc, t1=t1: e.tensor_tensor(out=t1[:, :], in0=t1[:, :], in1=RS[:, :], op=ALU.mult), reads=[t1, RS], writes=[t1])
                            S.op("act", lambda e, cc=cc, t1=t1, B=B: e.activation(out=CATA[B][:, cc, :], in_=t1[:, :], func=AF.Silu, scale=PVT[:, 4 + cc:5 + cc], bias=PVT[:, 8 + cc:9 + cc]),
                                 reads=[t1, PVT], writes=[CATA[B]])
                    S.fence()
            if stop_after == "l0b":
                return
            with ExitStack() as p3:
                def sb(name, shape, dt=F32):
                    return S.sbuf(name, shape, dt, st=p3)
                WING = sb("WING", [128, 8, 1552], BF16)
                WOUT = sb("WOUT", [128, 8, D], BF16)
                S.dma("pool", lambda e: e.dma_start(out=WING[:, :, :], in_=ab_w_in_d.rearrange("(c p) n -> p c n", p=128)[:, :, 1024:2576]), reads=[DR], writes=[WING], sync=WING)
                S.dma("pool", lambda e: e.dma_start(out=WOUT[:, :, :], in_=ab_w_out_d.rearrange("(c p) n -> p c n", p=128)), reads=[DR], writes=[WOUT], sync=WOUT)
                S.op("pool", lambda e: e.tensor_tensor(out=WOUT[:, :, :], in0=WOUT[:, :, :], in1=G1B[:, :].unsqueeze(1).to_broadcast([128, 8, D]), op=ALU.mult), reads=[WOUT, G1B], writes=[WOUT])
                GWS = sb("GWS", [32, 256])
                GWB = sb("GWB", [32, 256], BF16)
                S.op("dve", lambda e: e.memset(GWS[:, :], 0.0), writes=[GWS])
                S.dma("sp", lambda e: [e.dma_start(out=GWS[0:16, :], in_=gate_w_d[:, :]), e.dma_start(out=GWS[16:17, :], in_=gate_b_d[:, :])], reads=[DR], writes=[GWS], sync=GWS, n=2)
                S.op("dve", lambda e: e.tensor_copy(out=GWB[:, :], in_=GWS[:, :]), reads=[GWS], writes=[GWB])
                GLT = Rot([sb("GLT%d" % i, [32, 512], BF16) for i in range(2)])
                for g_ in GLT.tiles:
                    S.op("dve", lambda e, g_=g_: e.memset(g_[:, :], 1.0), writes=[g_])
                NG = sb("NG", [128, 512])
                S.dma("sp", lambda e: e.dma_start(out=NG[:, :], in_=gnorm_d[0:1, :].partition_broadcast(128)), reads=[DR], writes=[NG], sync=NG)
                S32 = sb("S32", [128, 2, 128])
                SBF = sb("SBF", [128, 2, 128], BF16)
                S.op("dve", lambda e: e.memset(S32[:, :, :], 0.0), writes=[S32])
                S.op("dve", lambda e: e.memset(SBF[:, :, :], 0.0), writes=[SBF])
                S32h = [Tile(S32.ap[(h % 2) * 64:(h % 2) * 64 + 64, h // 2, :], "S32h%d" % h) for h in range(4)]
                SBFh = [Tile(SBF.ap[(h % 2) * 64:(h % 2) * 64 + 64, h // 2, :], "SBFh%d" % h) for h in range(4)]
                for h in range(4):
                    S32h[h].w = S32.w
                    SBFh[h].w = SBF.w
                HTB = sb("HTB", [128, 8, 512], BF16)
                QT = sb("QT", [128, 2, 512])
                KT = sb("KT", [128, 2, 512])
                R2 = lambda name, shape, dt=F32: Rot([sb("%s%d" % (name, i), shape, dt) for i in range(2)])
                VB = R2("VB", [128, 512], BF16); RG = R2("RG", [128, 512]); KTOK = R2("KTOK", [128, 256]); E1 = R2("E1", [128, 256]); SP = R2("SP", [128, 256])
                EB = R2("EB", [128, 2, 128]); ENB = R2("ENB", [128, 2, 128]); EDEC = R2("EDEC", [128, 256])
                QS = R2("QS", [128, 4, 128], BF16); KS = R2("KS", [128, 2, 128], BF16); KDEC = R2("KDEC", [128, 256], BF16)
                ATM = Rot([sb("ATM%d" % i, [128, 128], BF16) for i in range(4)])
                SS = R2("SS", [128, 4]); RS4 = R2("RS4", [128, 4]); YB = R2("YB", [128, 512], BF16)
                JUNK = sb("JUNK", [128, 128], BF16)
                for q_ in QS.tiles:
                    S.op("pool", lambda e, q_=q_: e.memset(q_[:, :, :], 0.0), writes=[q_])
                pool_rot = Rot([S.psum("pl%d" % i, [128, 512], F32, st=p3) for i in range(4)])
                o_ps = S.psum("o_ps", [128, 512], F32, st=p3)
                sn_ps = S.psum("sn_ps", [128, 4, 128], F32, st=p3)
                att_ps = S.psum("att_ps", [128, 4, 128], F32, st=p3)
                ybT_ps = S.psum("ybT", [128, 8, 128], BF16, st=p3)
                tp_rot = pool_rot

                def proj(dst_ps, cols, htb_ap, M=None):
                    pass

                gl_of = {}
                stA = {}

                def gla_prologue(B):
                    for i in range(4):
                        transpose_tile(4 * B + i, l, jsc, jsh, s, tp_rot, lambda c, i=i: HTB[:, c, i * 128:(i + 1) * 128], HTB)
                    for c2 in range(2):
                        for (dst, off) in ((QT, 0), (KT, 256)):
                            pp_ = pool_rot.next()
                            for kc in range(8):
                                S.op("pe", lambda e, kc=kc, c2=c2, off=off, pp_=pp_: e.matmul(pp_[:, :], lhsT=WING[:, kc, off + c2 * 128:off + (c2 + 1) * 128], rhs=HTB[:, kc, :], start=(kc == 0), stop=(kc == 7)),
                                     reads=[WING, HTB], writes=[pp_])
                            S.op("act", lambda e, dst=dst, c2=c2, pp_=pp_: e.activation(out=dst[:, c2, :], in_=pp_[:, :], func=AF.Copy), reads=[pp_], writes=[dst])
                    gl = GLT.next()
                    pp_ = pool_rot.next()
                    for kc in range(8):
                        S.op("pe", lambda e, kc=kc, pp_=pp_: e.matmul(pp_[0:16, :], lhsT=WING[:, kc, 1536:1552], rhs=HTB[:, kc, :], start=(kc == 0), stop=(kc == 7)), reads=[WING, HTB], writes=[pp_])
                    S.op("act", lambda e, gl=gl, pp_=pp_: e.activation(out=gl[0:16, :], in_=pp_[0:16, :], func=AF.Copy), reads=[pp_], writes=[gl])
                    gl_of[B] = gl

                def gla_a0(t):
                    B, i = t // 4, t % 4
                    gl = gl_of[B]
                    tc = slice(i * 128, (i + 1) * 128)
                    d = dict(tc=tc, vb=VB.next(), rg=RG.next(), ktok=KTOK.next(), e1=E1.next(), sp=SP.next(), eb=EB.next(), enb=ENB.next(), edec=EDEC.next(),
                             qs=QS.next(), ks=KS.next(), kdec=KDEC.next(), ss=SS.next(), rs4=RS4.next(), yb=YB.next())
                    stA[t] = d
                    e1, sp = d["e1"], d["sp"]
                    zp = pool_rot.next()
                    S.op("pe", lambda e: e.matmul(zp[:, 0:256], lhsT=gl[0:32, tc], rhs=GWB[0:32, :], start=True, stop=True), reads=[gl, GWB], writes=[zp])
                    S.op("act", lambda e: e.activation(out=e1[:, :], in_=zp[:, 0:256], func=AF.Exp, scale=-1.0), reads=[zp], writes=[e1])
                    S.op("act", lambda e: e.activation(out=sp[:, :], in_=e1[:, :], func=AF.Ln, bias=1.0), reads=[e1], writes=[sp])

                def gla_a1(t):
                    d = stA[t]
                    tc, vb, rg, ktok = d["tc"], d["vb"], d["rg"], d["ktok"]
                    kp = pool_rot.next()
                    for kc in range(8):
                        S.op("pe", lambda e, kc=kc: e.matmul(kp[:, 0:256], lhsT=HTB[:, kc, tc], rhs=WING[:, kc, 256:512], start=(kc == 0), stop=(kc == 7)), reads=[WING, HTB], writes=[kp])
                    S.op("dve", lambda e: e.tensor_copy(out=ktok[:, :], in_=kp[:, 0:256]), reads=[kp], writes=[ktok])
                    vp = pool_rot.next()
                    for kc in range(8):
                        S.op("pe", lambda e, kc=kc: e.matmul(vp[:, :], lhsT=HTB[:, kc, tc], rhs=WING[:, kc, 512:1024], start=(kc == 0), stop=(kc == 7)), reads=[WING, HTB], writes=[vp])
                    S.op("act", lambda e: e.activation(out=vb[:, :], in_=vp[:, :], func=AF.Copy), reads=[vp], writes=[vb])
                    rp = pool_rot.next()
                    for kc in range(8):
                        S.op("pe", lambda e, kc=kc: e.matmul(rp[:, :], lhsT=HTB[:, kc, tc], rhs=WING[:, kc, 1024:1536], start=(kc == 0), stop=(kc == 7)), reads=[WING, HTB], writes=[rp])
                    S.op("act", lambda e: e.activation(out=rg[:, :], in_=rp[:, :], func=AF.Silu), reads=[rp], writes=[rg])
                    S.op("pool", lambda e: e.tensor_tensor(out=rg[:, :], in0=rg[:, :], in1=NG[:, :], op=ALU.mult), reads=[rg, NG], writes=[rg])

                def gla_a2(t):
                    d = stA[t]
                    tc, sp, eb, enb, edec, qs, ks, kdec, ktok = d["tc"], d["sp"], d["eb"], d["enb"], d["edec"], d["qs"], d["ks"], d["kdec"], d["ktok"]
                    revp = pool_rot.next()
                    for c2 in range(2):
                        S.op("pe", lambda e, c2=c2: e.matmul(revp[:, 256 + c2 * 128:256 + (c2 + 1) * 128], lhsT=sp[:, c2 * 128:(c2 + 1) * 128], rhs=tri, start=True, stop=True), reads=[sp, CONST], writes=[revp])
                    S.op("pe", lambda e: e.matmul(revp[:, 0:256], lhsT=su, rhs=sp[:, :], start=True, stop=True), reads=[sp, CONST], writes=[revp])
                    S.op("act", lambda e: e.activation(out=eb[:, :, :], in_=revp[:, 256:512].rearrange("p (a b) -> p a b", a=2), func=AF.Exp, scale=-1.0 / 16), reads=[revp], writes=[eb])
                    S.op("act", lambda e: e.activation(out=enb[:, :, :], in_=revp[:, 256:512].rearrange("p (a b) -> p a b", a=2), func=AF.Exp, scale=1.0 / 16), reads=[revp], writes=[enb])
                    S.op("act", lambda e: e.activation(out=edec[:, :], in_=revp[:, 0:256], func=AF.Exp, scale=-1.0 / 16), reads=[revp], writes=[edec])
                    for h in range(4):
                        c2, hp = h // 2, (h % 2) * 64
                        S.op("dve", lambda e, h=h, c2=c2, hp=hp: e.scalar_tensor_tensor(out=qs[hp:hp + 64, h, :], in0=QT[hp:hp + 64, c2, tc], scalar=0.125, in1=eb[hp:hp + 64, c2, :], op0=ALU.mult, op1=ALU.mult),
                             reads=[QT, eb], writes=[qs])
                    S.op("dve", lambda e: e.tensor_tensor(out=ks[:, :, :], in0=KT[:, :, tc], in1=enb[:, :, :], op=ALU.mult), reads=[KT, enb], writes=[ks])
                    S.op("dve", lambda e: e.tensor_tensor(out=kdec[:, :], in0=ktok[:, :], in1=edec[:, :], op=ALU.mult), reads=[ktok, edec], writes=[kdec])

                def gla_b0(t):
                    d = stA[t]
                    qs, ks = d["qs"], d["ks"]
                    for h in range(4):
                        c2 = h // 2
                        S.op("pe", lambda e, c2=c2, h=h: e.matmul(att_ps[:, h, :], lhsT=ks[:, c2, :], rhs=qs[:, h, :], start=True, stop=True), reads=[ks, qs], writes=[att_ps])
                    atms = []
                    for h in range(4):
                        atm = ATM.next()
                        S.op("dve", lambda e, atm=atm, h=h: e.tensor_tensor(out=atm[:, :], in0=att_ps[:, h, :], in1=tri, op=ALU.mult), reads=[att_ps, CONST], writes=[atm])
                        atms.append(atm)
                    d["atms"] = atms

                def gla_b1(t):
                    d = stA[t]
                    vb, rg, eb, qs, kdec, ss, rs4, yb, atms = d["vb"], d["rg"], d["eb"], d["qs"], d["kdec"], d["ss"], d["rs4"], d["yb"], d["atms"]
                    for h in range(4):
                        c2 = h // 2
                        hc = slice(h * 128, (h + 1) * 128)
                        atm = atms[h]
                        S.op("pe", lambda e, c2=c2, hc=hc, h=h: e.matmul(o_ps[:, hc], lhsT=qs[:, h, :], rhs=SBF[:, c2, :], start=True, stop=False), reads=[qs, SBFh[2 * c2], SBFh[2 * c2 + 1]], writes=[o_ps])
                        S.op("pe", lambda e, atm=atm, hc=hc: e.matmul(o_ps[:, hc], lhsT=atm[:, :], rhs=vb[:, hc], start=False, stop=True), reads=[atm, vb], writes=[o_ps])
                    for h in range(4):
                        c2 = h // 2
                        hc = slice(h * 128, (h + 1) * 128)
                        S.op("pe", lambda e, c2=c2, hc=hc, h=h: e.matmul(sn_ps[:, h, :], lhsT=kdec[:, c2 * 128:(c2 + 1) * 128], rhs=vb[:, hc], start=True, stop=True), reads=[kdec, vb], writes=[sn_ps])
                    for h in range(4):
                        c2, hp = h // 2, (h % 2) * 64
                        S.op("dve", lambda e, c2=c2, hp=hp, h=h: e.scalar_tensor_tensor(out=S32[hp:hp + 64, c2, :], in0=S32[hp:hp + 64, c2, :], scalar=eb[hp:hp + 64, c2, 127:128],
                                                                                         in1=sn_ps[hp:hp + 64, h, :], op0=ALU.mult, op1=ALU.add), reads=[S32h[h], eb, sn_ps], writes=[S32h[h]])
                        S.op("pool", lambda e, c2=c2, hp=hp: e.tensor_copy(out=SBF[hp:hp + 64, c2, :], in_=S32[hp:hp + 64, c2, :]), reads=[S32h[h]], writes=[SBFh[h]])
                    for h in range(4):
                        hc = slice(h * 128, (h + 1) * 128)
                        S.op("act", lambda e, hc=hc, h=h: e.activation(out=JUNK[:, :], in_=o_ps[:, hc], func=AF.Square, accum_out=ss[:, h:h + 1]), reads=[o_ps], writes=[JUNK, ss])
                    S.op("dve", lambda e: e.tensor_scalar(out=ss[:, :], in0=ss[:, :], scalar1=1.0 / 128, scalar2=RMS_EPS, op0=ALU.mult, op1=ALU.add), reads=[ss], writes=[ss])
                    S.op("pool", lambda e: e.tensor_tensor(out=rs4[:, :], in0=ss[:, :], in1=NHALF[:, 0:4], op=ALU.pow), reads=[ss, NHALF], writes=[rs4])
                    for h in range(4):
                        hc = slice(h * 128, (h + 1) * 128)
                        S.op("dve", lambda e, hc=hc, h=h: e.scalar_tensor_tensor(out=yb[:, hc], in0=o_ps[:, hc], scalar=rs4[:, h:h + 1], in1=rg[:, hc], op0=ALU.mult, op1=ALU.mult),
                             reads=[o_ps, rs4, rg], writes=[yb])

                def gla_b2a(t):
                    yb = stA[t]["yb"]
                    for h in range(4):
                        S.op("pe", lambda e, h=h: e.transpose(ybT_ps[:, h, :], yb[:, h * 128:(h + 1) * 128], IDB[:, :]), reads=[yb, IDB], writes=[ybT_ps])
                    S.op("act", lambda e: e.activation(out=CATB[t][:, :, :], in_=ybT_ps[:, 0:4, :], func=AF.Copy), reads=[ybT_ps], writes=[CATB[t]])

                def gla_b2b(t):
                    stA.pop(t)
                    B = t // 4
                    for hf in range(2):
                        mp = pool_rot.next()
                        for kc in range(8):
                            S.op("pe", lambda e, kc=kc, hf=hf, mp=mp: e.matmul(mp[:, :], lhsT=HT[:, kc, t * 128:(t + 1) * 128], rhs=WOUT[:, kc, hf * 512:(hf + 1) * 512], start=(kc == 0), stop=(kc == 7)),
                                 reads=[CATA[B], CATB[t], WOUT], writes=[mp])
                        S.op("dve", lambda e, hf=hf, mp=mp: e.tensor_tensor(out=XT[t][:, hf * 512:(hf + 1) * 512], in0=mp[:, :], in1=XT[t][:, hf * 512:(hf + 1) * 512], op=ALU.add),
                             reads=[mp, XT[t]], writes=[XT[t]])
                    lnp.push(t)

                def gla_a_all(t):
                    gla_a0(t); gla_a1(t); gla_a2(t)

                lnp = LNPipe()
                gla_prologue(0)
                gla_a_all(0)
                for t in range(NT + 1):
                    nxt = t + 1 if t + 1 < NT else None
                    if nxt is not None and nxt % 4 == 0:
                        gla_prologue(nxt // 4)
                    if nxt is not None:
                        gla_a0(nxt)
                    if t < NT:
                        gla_b0(t)
                    if t >= 1:
                        gla_b2a(t - 1)
                    if nxt is not None:
                        gla_a1(nxt)
                        gla_a2(nxt)
                    if t < NT:
                        gla_b1(t)
                    if t >= 1:
                        gla_b2b(t - 1)
                lnp.flush()
                S.fence()

        def l1_mixer(s):
            l = 1
            jsh, jsc, jg = 0, 8, 16
            CQB = [Tile(HT.ap[:, 0:6, B * 512:(B + 1) * 512], "CQB%d" % B) for B in range(4)]
            load_ln(l, 0, True)
            with ExitStack() as pa:
                CS1 = S.sbuf("CS1", [64, S_LEN], F32, st=pa)
                CS2 = S.sbuf("CS2", [64, S_LEN], F32, st=pa)
                with ExitStack() as pr:
                    POSI = S.sbuf("POSI", [64, S_LEN], I32, st=pr)
                    ANG = S.sbuf("ANG", [64, S_LEN], F32, st=pr)
                    TT = S.sbuf("TT", [64, S_LEN], F32, st=pr)
                    TI = S.sbuf("TI", [64, S_LEN], I32, st=pr)
                    FR = S.sbuf("FR", [64, S_LEN], F32, st=pr)
                    MK = S.sbuf("MK", [64, S_LEN], F32, st=pr)
                    S.dma("sp", lambda e: e.dma_start(out=POSI[:, :], in_=pos_d[s:s + 1, :].partition_broadcast(64)), reads=[DR], writes=[POSI], sync=POSI)
                    S.op("dve", lambda e: e.tensor_copy(out=ANG[:, :], in_=POSI[:, :]), reads=[POSI], writes=[ANG])
                    S.op("dve", lambda e: e.tensor_scalar(out=ANG[:, :], in0=ANG[:, :], scalar1=CONST[0:64, C_INVF:C_INVF + 1], scalar2=None, op0=ALU.mult), reads=[ANG, CONST], writes=[ANG])
                    for (dst, shift, sgn) in ((CS1, 0.75, False), (CS2, 0.5, True)):
                        S.op("dve", lambda e, shift=shift: e.tensor_scalar(out=TT[:, :], in0=ANG[:, :], scalar1=1.0 / TWO_PI, scalar2=shift, op0=ALU.mult, op1=ALU.add), reads=[ANG], writes=[TT])
                        S.op("dve", lambda e: e.tensor_copy(out=TI[:, :], in_=TT[:, :]), reads=[TT], writes=[TI])
                        S.op("dve", lambda e: e.tensor_copy(out=FR[:, :], in_=TI[:, :]), reads=[TI], writes=[FR])
                        S.op("dve", lambda e: e.tensor_tensor(out=FR[:, :], in0=TT[:, :], in1=FR[:, :], op=ALU.subtract), reads=[TT, FR], writes=[FR])
                        S.op("dve", lambda e: e.tensor_single_scalar(out=MK[:, :], in_=FR[:, :], scalar=0.0, op=ALU.is_lt), reads=[FR], writes=[MK])
                        S.op("dve", lambda e: e.tensor_tensor(out=FR[:, :], in0=FR[:, :], in1=MK[:, :], op=ALU.add), reads=[FR, MK], writes=[FR])
                        S.op("dve", lambda e: e.tensor_single_scalar(out=MK[:, :], in_=FR[:, :], scalar=1.0, op=ALU.is_ge), reads=[FR], writes=[MK])
                        S.op("dve", lambda e: e.tensor_tensor(out=FR[:, :], in0=FR[:, :], in1=MK[:, :], op=ALU.subtract), reads=[FR, MK], writes=[FR])
                        S.op("act", lambda e, dst=dst: e.activation(out=dst[:, :], in_=FR[:, :], func=AF.Sin, scale=TWO_PI, bias=CONST[0:64, C_NPI:C_NPI + 1]), reads=[FR, CONST], writes=[dst])
                        if sgn:
                            S.op("dve", lambda e, dst=dst: e.tensor_scalar(out=dst[:, :], in0=dst[:, :], scalar1=CONST[0:64, C_SGN:C_SGN + 1], scalar2=None, op0=ALU.mult), reads=[dst, CONST], writes=[dst])
                    S.fence()
                WUQ = S.sbuf("WUQ", [128, 3, 2048], BF16, st=pa)
                WUKV = S.sbuf("WUKV", [128, 2, 2048], BF16, st=pa)
                S.dma("pool", lambda e: [e.dma_start(out=WUQ[:, :, 0:1024], in_=mla_w_uq_d.rearrange("(c p) n -> p c n", p=128)[:, :, 0:1024]),
                                         e.dma_start(out=WUQ[:, :, 1024:2048], in_=mla_w_uq_d.rearrange("(c p) n -> p c n", p=128)[:, :, 1024:2048])],
                      reads=[DR], writes=[WUQ], sync=WUQ, n=2)
                S.dma("pool", lambda e: [e.dma_start(out=WUKV[:, :, 0:1024], in_=mla_w_ukv_d.rearrange("(c p) n -> p c n", p=128)[:, :, 0:1024]),
                                         e.dma_start(out=WUKV[:, :, 1024:2048], in_=mla_w_ukv_d.rearrange("(c p) n -> p c n", p=128)[:, :, 1024:2048])],
                      reads=[DR], writes=[WUKV], sync=WUKV, n=2)
                with ExitStack() as p1:
                    def sb(name, shape, dt=F32):
                        return S.sbuf(name, shape, dt, st=p1)
                    WIN1 = sb("WIN1", [128, 8, 768], BF16)
                    S.dma("pool", lambda e: e.dma_start(out=WIN1[:, :, :], in_=mla_w_in_d.rearrange("(c p) n -> p c n", p=128)), reads=[DR], writes=[WIN1], sync=WIN1)
                    HTB = sb("HTB1", [128, 8, 512], BF16)
                    UT = sb("UT", [128, 5, 512])
                    SQ = Rot([sb("SQ%d" % i, [128, 512]) for i in range(2)])
                    RQ = sb("RQ", [128, 512]); RKV = sb("RKV", [128, 512])
                    T1 = sb("T1a", [64, 512]); T2 = sb("T2a", [64, 512])
                    scr = sb("scr1", [128, 8, 128])
                    pool_rot = Rot([S.psum("pla%d" % i, [128, 512], F32, st=p1) for i in range(6)])
                    ssq = [S.psum("ssq%d" % i, [128, 512], F32, st=p1) for i in range(2)]
                    build_g1b(l, jg, s, pool_rot.tiles[0:2], scr)
                    for B in range(4):
                        bc = slice(B * 512, (B + 1) * 512)
                        for i in range(4):
                            transpose_tile(4 * B + i, l, jsc, jsh, s, pool_rot, lambda c, i=i: HTB[:, c, i * 128:(i + 1) * 128], HTB)
                        for j in range(5):
                            up = pool_rot.next()
                            for kc in range(8):
                                S.op("pe", lambda e, kc=kc, j=j, up=up: e.matmul(up[:, :], lhsT=WIN1[:, kc, j * 128:(j + 1) * 128], rhs=HTB[:, kc, :], start=(kc == 0), stop=(kc == 7)), reads=[WIN1, HTB], writes=[up])
                            S.op("act", lambda e, j=j, up=up: e.activation(out=UT[:, j, :], in_=up[:, :], func=AF.Copy), reads=[up], writes=[UT])
                            sq = SQ.next()
                            S.op("act", lambda e, sq=sq, up=up: e.activation(out=sq[:, :], in_=up[:, :], func=AF.Square), reads=[up], writes=[sq])
                            sp_ = ssq[0] if j < 3 else ssq[1]
                            S.op("pe", lambda e, sq=sq, sp_=sp_, j=j: e.matmul(sp_[:, :], lhsT=ones, rhs=sq[:, :], start=(j in (0, 3)), stop=(j in (2, 4))), reads=[CONST, sq], writes=[sp_])
                        for (dst, sp_, n_) in ((RQ, ssq[0], 384.0), (RKV, ssq[1], 256.0)):
                            S.op("act", lambda e, dst=dst, sp_=sp_, n_=n_: e.activation(out=dst[:, :], in_=sp_[:, :], func=AF.Ln, scale=1.0 / n_, bias=CONST[:, C_REPS:C_REPS + 1]), reads=[sp_, CONST], writes=[dst])
                            S.op("act", lambda e, dst=dst: e.activation(out=dst[:, :], in_=dst[:, :], func=AF.Exp, scale=-0.5), reads=[dst], writes=[dst])
                        for j in range(5):
                            rr = RQ if j < 3 else RKV
                            S.op("dve", lambda e, j=j, rr=rr, bc=bc: e.scalar_tensor_tensor(out=HT[:, j, bc], in0=UT[:, j, :], scalar=PVT[:, 12 + j:13 + j], in1=rr[:, :], op0=ALU.mult, op1=ALU.mult),
                                 reads=[UT, PVT, rr], writes=[CQB[B]])
                        a_p = pool_rot.next()
                        b_p = pool_rot.next()
                        for (pp_, off) in ((a_p, 640), (b_p, 704)):
                            for kc in range(8):
                                S.op("pe", lambda e, kc=kc, pp_=pp_, off=off: e.matmul(pp_[0:64, :], lhsT=WIN1[:, kc, off:off + 64], rhs=HTB[:, kc, :], start=(kc == 0), stop=(kc == 7)), reads=[WIN1, HTB], writes=[pp_])
                        S.op("dve", lambda e, a_p=a_p, bc=bc: e.tensor_tensor(out=T1[:, :], in0=a_p[0:64, :], in1=CS1[:, bc], op=ALU.mult), reads=[a_p, CS1], writes=[T1])
                        S.op("dve", lambda e, b_p=b_p, bc=bc: e.tensor_tensor(out=T2[:, :], in0=b_p[0:64, :], in1=CS2[:, bc], op=ALU.mult), reads=[b_p, CS2], writes=[T2])
                        S.op("pool", lambda e, bc=bc: e.tensor_tensor(out=HT[0:64, 5, bc], in0=T1[:, :], in1=T2[:, :], op=ALU.add), reads=[T1, T2], writes=[CQB[B]])
                    S.fence()
                if stop_after == "l1a":
                    return
                with ExitStack() as p2:
                    def sb(name, shape, dt=F32):
                        return S.sbuf(name, shape, dt, st=p2)
                    WOUT = sb("WOUT1", [128, 8, D], BF16)
                    S.dma("pool", lambda e: e.dma_start(out=WOUT[:, :, :], in_=mla_w_out_d.rearrange("(c p) n -> p c n", p=128)), reads=[DR], writes=[WOUT], sync=WOUT)
                    S.op("pool", lambda e: e.tensor_tensor(out=WOUT[:, :, :], in0=WOUT[:, :, :], in1=G1B[:, :].unsqueeze(1).to_broadcast([128, 8, D]), op=ALU.mult), reads=[WOUT, G1B], writes=[WOUT])
                    QN = sb("QN", [128, S_LEN], BF16); QR = sb("QR", [64, S_LEN], BF16); KN = sb("KN", [128, S_LEN], BF16); VT = sb("VT", [128, NT, 128], BF16)
                    PT = Rot([sb("PT%d" % i, [128, 512], BF16) for i in range(5)])
                    RINV = sb("RINV", [128, 512])
                    OTH = Rot([sb("OTH%d" % i, [128, S_LEN], BF16) for i in range(2)])
                    T1 = sb("T1b", [64, 512]); T2 = sb("T2b", [64, 512])
                    pool_rot = Rot([S.psum("plb%d" % i, [128, 512], F32, st=p2) for i in range(3)])
                    st_rot = Rot([S.psum("stb%d" % i, [128, 512], F32, st=p2) for i in range(3)])
                    o_ps = S.psum("o1_ps", [128, 512], F32, st=p2)
                    r_ps = S.psum("r1_ps", [128, 512], F32, st=p2)
                    lnp = LNPipe()
                    for h in range(8 if dbg >= 2 else 0):
                        hb = h * 256
                        for B in range(4):
                            bc = slice(B * 512, (B + 1) * 512)
                            p_ = pool_rot.next()
                            for k3 in range(3):
                                S.op("pe", lambda e, k3=k3, p_=p_, bc=bc, hb=hb: e.matmul(p_[:, :], lhsT=WUQ[:, k3, hb:hb + 128], rhs=HT[:, k3, bc], start=(k3 == 0), stop=(k3 == 2)), reads=[WUQ, CQB[B]], writes=[p_])
                            S.op("act", lambda e, p_=p_, bc=bc: e.activation(out=QN[:, bc], in_=p_[:, :], func=AF.Copy), reads=[p_], writes=[QN])
                            a_p = pool_rot.next()
                            b_p = pool_rot.next()
                            for (pp_, off) in ((a_p, hb + 128), (b_p, hb + 192)):
                                for k3 in range(3):
                                    S.op("pe", lambda e, k3=k3, pp_=pp_, off=off, bc=bc: e.matmul(pp_[0:64, :], lhsT=WUQ[:, k3, off:off + 64], rhs=HT[:, k3, bc], start=(k3 == 0), stop=(k3 == 2)), reads=[WUQ, CQB[B]], writes=[pp_])
                            S.op("dve", lambda e, a_p=a_p, bc=bc: e.tensor_tensor(out=T1[:, :], in0=a_p[0:64, :], in1=CS1[:, bc], op=ALU.mult), reads=[a_p, CS1], writes=[T1])
                            S.op("dve", lambda e, b_p=b_p, bc=bc: e.tensor_tensor(out=T2[:, :], in0=b_p[0:64, :], in1=CS2[:, bc], op=ALU.mult), reads=[b_p, CS2], writes=[T2])
                            S.op("pool", lambda e, bc=bc: e.tensor_tensor(out=QR[:, bc], in0=T1[:, :], in1=T2[:, :], op=ALU.add), reads=[T1, T2], writes=[QR])
                            p_ = pool_rot.next()
                            for k2 in range(2):
                                S.op("pe", lambda e, k2=k2, p_=p_, bc=bc, hb=hb: e.matmul(p_[:, :], lhsT=WUKV[:, k2, hb:hb + 128], rhs=HT[:, 3 + k2, bc], start=(k2 == 0), stop=(k2 == 1)), reads=[WUKV, CQB[B]], writes=[p_])
                            S.op("act", lambda e, p_=p_, bc=bc: e.activation(out=KN[:, bc], in_=p_[:, :], func=AF.Copy), reads=[p_], writes=[KN])
                            p_ = pool_rot.next()
                            for i in range(4):
                                t = 4 * B + i
                                for k2 in range(2):
                                    S.op("pe", lambda e, k2=k2, p_=p_, i=i, t=t, hb=hb: e.matmul(p_[:, i * 128:(i + 1) * 128], lhsT=HT[:, 3 + k2, t * 128:(t + 1) * 128], rhs=WUKV[:, k2, hb + 128:hb + 256], start=(k2 == 0), stop=(k2 == 1)),
                                         reads=[WUKV, CQB[B]], writes=[p_])
                            S.op("act", lambda e, p_=p_, B=B: e.activation(out=VT[:, 4 * B:4 * B + 4, :], in_=p_[:, :].rearrange("p (a b) -> p a b", a=4), func=AF.Copy), reads=[p_], writes=[VT])
                        oth = OTH.next()
                        steps = [(Q, kt) for Q in range(4) for kt in range(4 * Q + 4)]
                        pend = {}

                        def do_st(Q, kt):
                            m = kt - 4 * Q if kt >= 4 * Q else 0
                            c0 = m * 128
                            st_ = st_rot.next()
                            qc = slice(Q * 512 + c0, (Q + 1) * 512)
                            kc_ = slice(kt * 128, (kt + 1) * 128)
                            S.op("pe", lambda e: e.matmul(st_[:, c0:512], lhsT=KN[:, kc_], rhs=QN[:, qc], start=True, stop=False), reads=[KN, QN], writes=[st_])
                            S.op("pe", lambda e: e.matmul(st_[:, c0:512], lhsT=HT[0:64, 5, kc_], rhs=QR[0:64, qc], start=False, stop=True), reads=[CQB[kt // 4], QR], writes=[st_])
                            pt = PT.next()
                            S.op("act", lambda e: e.activation(out=pt[:, c0:512], in_=st_[:, c0:512], func=AF.Exp, scale=MLA_SCALE), reads=[st_], writes=[pt])
                            if kt >= 4 * Q:
                                S.op("pool", lambda e: e.tensor_tensor(out=pt[:, c0:c0 + 128], in0=pt[:, c0:c0 + 128], in1=TRIB[:, :], op=ALU.mult), reads=[pt, TRIB], writes=[pt])
                            pend[(Q, kt)] = (pt, c0)

                        def do_pv(Q, kt, oth=oth):
                            pt, c0 = pend.pop((Q, kt))
                            last = (kt == 4 * Q + 3)
                            S.op("pe", lambda e: e.matmul(o_ps[:, c0:512], lhsT=VT[:, kt, :], rhs=pt[:, c0:512], start=(kt == 0), stop=last), reads=[VT, pt], writes=[o_ps])
                            S.op("pe", lambda e: e.matmul(r_ps[:, c0:512], lhsT=ONEB[:, :], rhs=pt[:, c0:512], start=(kt == 0), stop=last), reads=[ONEB, pt], writes=[r_ps])
                            if last:
                                qc = slice(Q * 512, (Q + 1) * 512)
                                S.op("dve", lambda e: e.reciprocal(out=RINV[:, :], in_=r_ps[:, :]), reads=[r_ps], writes=[RINV])
                                S.op("dve", lambda e: e.tensor_tensor(out=oth[:, qc], in0=o_ps[:, :], in1=RINV[:, :], op=ALU.mult), reads=[o_ps, RINV], writes=[oth])

                        for i_, (Q, kt) in enumerate(steps):
                            do_st(Q, kt)
                            if i_ >= 2:
                                do_pv(*steps[i_ - 2])
                        do_pv(*steps[-2])
                        do_pv(*steps[-1])
                        for t in range(NT):
                            for hf in range(2):
                                mp = pool_rot.next()
                                S.op("pe", lambda e, t=t, hf=hf, mp=mp, h=h, oth=oth: e.matmul(mp[:, :], lhsT=oth[:, t * 128:(t + 1) * 128], rhs=WOUT[:, h, hf * 512:(hf + 1) * 512], start=True, stop=True), reads=[oth, WOUT], writes=[mp])
                                S.op("dve", lambda e, t=t, hf=hf, mp=mp: e.tensor_tensor(out=XT[t][:, hf * 512:(hf + 1) * 512], in0=mp[:, :], in1=XT[t][:, hf * 512:(hf + 1) * 512], op=ALU.add),
                                     reads=[mp, XT[t]], writes=[XT[t]])
                            if h == 7:
                                lnp.push(t)
                    lnp.flush()
                    S.fence()

        for s in range(nseq):
            for t in range(NT):
                S.dma("sp", lambda e, t=t, s=s: e.dma_start(out=XT[t][:, :], in_=x_d[s, t * 128:(t + 1) * 128, :]), reads=[DR], writes=[XT[t]], sync=XT[t])
                S.op("act", lambda e, t=t: e.activation(out=XT[t][:, :], in_=XT[t][:, :], func=AF.Copy, scale=ALPHA), reads=[XT[t]], writes=[XT[t]])
            if stop_after == "moe0":
                moe_phase(0, s, last=True)
            elif stop_after in ("xm1only", "l1a"):
                l1_mixer(s)
            elif stop_after != "load":
                l0_mixer(s)
                if stop_after not in ("xm0", "l0a", "l0b"):
                    moe_phase(0, s, last=False)
                    if stop_after != "xf0":
                        l1_mixer(s)
                        if stop_after != "xm1":
                            moe_phase(1, s, last=True)
            for t in range(NT):
                S.dma("sp", lambda e, t=t, s=s: e.dma_start(out=out_d[s, t * 128:(t + 1) * 128, :], in_=XT[t][:, :]), reads=[XT[t]], writes=[DO], sync=XT[t])
            S.fence()
        S.emit(final_keys=["X%d" % t for t in range(NT)])
    return nc


def prep_shared(inp):
    f = lambda a: np.ascontiguousarray(np.asarray(a, dtype=np.float32))
    sh = {}
    sh["consts"] = make_consts()
    sh["ada_w"] = f(inp["ada_w"])
    sh["ada_b"] = f(inp["ada_b"])
    sh["ln_gb"] = f(np.stack([inp["ln_mix_g"], inp["ln_mix_b"], inp["ln_ffn_g"], inp["ln_ffn_b"]], axis=1))
    sh["ab_w_in"] = f(inp["ab_w_in"][0])
    sh["conv_w"] = f(inp["conv_w"][0])
    pv = np.zeros((40, 128), np.float32)
    pv[0:4] = np.asarray(inp["conv_b"][0]).reshape(4, 128)
    pv[4:8] = np.asarray(inp["conv_ln_g"][0]).reshape(4, 128)
    pv[8:12] = np.asarray(inp["conv_ln_b"][0]).reshape(4, 128)
    pv[12:15] = np.asarray(inp["mla_q_norm_g"][0]).reshape(3, 128)
    pv[15:17] = np.asarray(inp["mla_kv_norm_g"][0]).reshape(2, 128)
    sh["pvec"] = pv
    sh["gla_gate_w"] = f(inp["gla_gate_w"][0])
    sh["gla_gate_b"] = f(inp["gla_gate_b"][0]).reshape(1, 256)
    sh["gla_norm_g"] = f(inp["gla_norm_g"][0]).reshape(1, 512)
    sh["ab_w_out"] = f(inp["ab_w_out"][0])
    w_in = np.asarray(inp["mla_w_in"][0], dtype=np.float32)
    sh["mla_w_in"] = f(np.concatenate([w_in, w_in[:, 672:704], w_in[:, 640:672]], axis=1))
    wq = np.asarray(inp["mla_w_uq"][0], dtype=np.float32).reshape(384, 8, 192)
    sh["mla_w_uq"] = f(np.concatenate([wq, wq[:, :, 160:192], wq[:, :, 128:160]], axis=2).reshape(384, 2048))
    sh["mla_w_ukv"] = f(inp["mla_w_ukv"][0])
    sh["mla_w_out"] = f(inp["mla_w_out"][0])
    sh["moe_wr"] = f(np.concatenate([inp["moe_w_group"], inp["moe_w_router"]], axis=2))
    sh["moe_br"] = f(np.concatenate([inp["moe_b_group"], inp["moe_b_router"]], axis=1))
    sh["consts2"] = make_consts2()
    wg = np.asarray(inp["moe_w_gate"], dtype=np.float32).reshape(2, 32, 8, 128, 256).transpose(0, 1, 3, 2, 4)
    wu = np.asarray(inp["moe_w_up"], dtype=np.float32).reshape(2, 32, 8, 128, 256).transpose(0, 1, 3, 2, 4)
    wd = np.asarray(inp["moe_w_down"], dtype=np.float32).reshape(2, 32, 2, 128, 1024).transpose(0, 1, 3, 2, 4)
    wall = np.concatenate([np.concatenate([wg, wu], axis=4).reshape(2, 32, 128, 4096), wd.reshape(2, 32, 128, 2048)], axis=3)
    for i in range(2):
        sh["moe_wall%d" % i] = f(wall[i].reshape(32 * 128, 6144))
    return sh


def kernel(**inputs):
    sh = prep_shared(inputs)
    x = np.asarray(inputs["x"], dtype=np.float32)
    c = np.asarray(inputs["c"], dtype=np.float32)
    pos = np.asarray(inputs["positions"], dtype=np.int32)
    nc = build_nc()
    in_maps = []
    for i in range(NCORES):
        m = dict(sh)
        m["x"] = np.ascontiguousarray(x[2 * i:2 * i + 2])
        m["c"] = np.ascontiguousarray(c[2 * i:2 * i + 2])
        m["pos"] = np.ascontiguousarray(pos[2 * i:2 * i + 2])
        in_maps.append(m)
    res = run_bass_kernel_spmd(nc, in_maps, core_ids=list(range(NCORES)))
    return np.concatenate([r["out"] for r in res.results], axis=0).astype(np.float32)
```

```python
from contextlib import ExitStack
import math
import numpy as np
import concourse.bass as bass
import concourse.mybir as mybir
from concourse.bass_utils import run_bass_kernel_spmd

F32 = mybir.dt.float32
BF16 = mybir.dt.bfloat16
I32 = mybir.dt.int32
AF = mybir.ActivationFunctionType
ALU = mybir.AluOpType
AX = mybir.AxisListType

NCORES = 8
D = 1024
S_LEN = 2048
NT = 16
ALPHA = 4.0 ** 0.25
LN_EPS = 1e-5
RMS_EPS = 1e-6
MLA_SCALE = 192.0 ** -0.5
TWO_PI = 2.0 * math.pi

COMPUTE = ("pe", "act", "dve", "pool")


class Tile:
    __slots__ = ("ap", "name", "w", "rs", "semkey")

    def __init__(self, ap, name, semkey=None):
        self.ap = ap
        self.name = name
        self.w = None
        self.rs = []
        self.semkey = semkey or name

    def __getitem__(self, idx):
        return self.ap[idx]


class Op:
    __slots__ = ("eng", "fn", "deps", "signal", "sigidx", "pos", "semkey", "semval", "ndma")

    def __init__(self, eng, fn):
        self.eng = eng
        self.fn = fn
        self.deps = []
        self.signal = False
        self.sigidx = 0
        self.pos = 0
        self.semkey = None
        self.semval = 0
        self.ndma = 0


class Sched:
    def __init__(self, nc, stack):
        self.nc = nc
        self.stack = stack
        self.ops = {e: [] for e in ("pe", "act", "dve", "pool", "sp")}
        self.semcnt = {}
        self.uid = 0
        self.fence_deps = []
        self.fence_pending = set()
        self.dma_since = []

    def sbuf(self, name, shape, dtype, st=None):
        self.uid += 1
        t = (st or self.stack).enter_context(self.nc.sbuf_tensor("%s_%d" % (name, self.uid), list(shape), dtype))
        return Tile(t, name)

    def psum(self, name, shape, dtype=F32, st=None):
        self.uid += 1
        t = (st or self.stack).enter_context(self.nc.psum_tensor("%s_%d" % (name, self.uid), list(shape), dtype))
        return Tile(t, name)

    def _add(self, eng, fn, reads, writes, semkey=None, ndma=0):
        op = Op(eng, fn)
        lst = self.ops[eng]
        op.pos = len(lst)
        deps = []
        if eng in self.fence_pending:
            self.fence_pending.discard(eng)
            deps.extend(self.fence_deps)
        for t in reads:
            if t.w is not None:
                deps.append(t.w)
        for t in writes:
            if t.w is not None:
                deps.append(t.w)
            deps.extend(t.rs)
        seen = set()
        for d in deps:
            if id(d) in seen or d is op:
                continue
            seen.add(id(d))
            if d.semkey is None and d.eng == eng:
                if eng == "pe" or eng == "sp":
                    continue
            op.deps.append(d)
            if d.semkey is None:
                d.signal = True
        for t in reads:
            if semkey is None:
                t.rs = [r for r in t.rs if not (r.semkey is None and r.eng == eng)]
            t.rs.append(op)
        for t in writes:
            t.w = op
            t.rs = []
        if semkey is not None:
            op.semkey = semkey
            op.ndma = ndma
            self.semcnt[semkey] = self.semcnt.get(semkey, 0) + 16 * ndma
            op.semval = self.semcnt[semkey]
            self.dma_since.append(op)
        lst.append(op)
        return op

    def op(self, eng, fn, reads=(), writes=()):
        return self._add(eng, fn, list(reads), list(writes))

    def dma(self, eng, fn, reads=(), writes=(), sync=None, n=1):
        return self._add(eng, fn, list(reads), list(writes), semkey=sync.semkey, ndma=n)

    def fence(self):
        deps = []
        for lst in self.ops.values():
            for d in reversed(lst):
                if d.semkey is None:
                    deps.append(d)
                    break
        deps = deps + self.dma_since
        self.dma_since = []
        self.fence_deps = deps
        self.fence_pending = set(self.ops.keys())

    def emit(self, final_keys=()):
        nc = self.nc
        stack = self.stack
        esem = {e: stack.enter_context(nc.semaphore("es_" + e)) for e in COMPUTE}
        dsem = {k: stack.enter_context(nc.semaphore("ds_%d" % i)) for i, k in enumerate(sorted(self.semcnt))}
        for e in COMPUTE:
            c = 0
            for op in self.ops[e]:
                if op.signal and op.semkey is None:
                    c += 1
                    op.sigidx = c
        block = stack.enter_context(nc.Block())

        def run(ename, engine):
            waited = {}
            for op in self.ops[ename]:
                need = {}
                for d in op.deps:
                    if d.semkey is not None:
                        s, v, key = dsem[d.semkey], d.semval, "d" + d.semkey
                    else:
                        s, v, key = esem[d.eng], d.sigidx, "e" + d.eng
                    if key not in need or need[key][1] < v:
                        need[key] = (s, v)
                for key, (s, v) in need.items():
                    if waited.get(key, 0) >= v:
                        continue
                    waited[key] = v
                    engine.wait_ge(s, v)
                r = op.fn(engine)
                if op.semkey is not None:
                    if not isinstance(r, (list, tuple)):
                        r = [r]
                    assert len(r) == op.ndma, (len(r), op.ndma)
                    for ins in r:
                        ins.then_inc(dsem[op.semkey], 16)
                elif op.signal:
                    r.then_inc(esem[ename], 1)
            if ename == "sp":
                for k in final_keys:
                    engine.wait_ge(dsem[k], self.semcnt[k])

        @block.sync
        def _(sync):
            run("sp", sync)

        @block.tensor
        def _(tensor):
            run("pe", tensor)

        @block.scalar
        def _(scalar):
            run("act", scalar)

        @block.vector
        def _(vector):
            run("dve", vector)

        @block.gpsimd
        def _(gpsimd):
            run("pool", gpsimd)


class Rot:
    def __init__(self, tiles):
        self.tiles = tiles
        self.i = 0

    def next(self):
        t = self.tiles[self.i % len(self.tiles)]
        self.i += 1
        return t


C_ID, C_TRI, C_SU, C_ONE, C_INVF, C_SGN, C_NHALF, C_NPI, C_REPS, NCONST = 0, 128, 256, 384, 512, 513, 514, 515, 516, 517


C2_LT, C2_THR, C2_VROW, C2_EROW, C2_VAL, NC2 = 0, 1024, 1032, 1080, 1112, 1144


def make_consts2():
    c = np.zeros((128, NC2), np.float32)
    p = np.arange(128)
    e = np.arange(32)
    c[:, C2_LT:C2_LT + 1024] = (e[None, :] < e[:, None]).astype(np.float32).reshape(1, 1024)
    c[:, C2_THR:C2_THR + 8] = 256.0 * np.arange(8)[None, :]
    c[:, C2_VROW:C2_VROW + 48] = np.arange(48)[None, :]
    c[:, C2_EROW:C2_EROW + 32] = e[None, :] * 128.0 + p[:, None] - 8192.0
    kt = np.arange(32)
    c[:, C2_VAL:C2_VAL + 32] = (kt // 16)[None, :] * 2048.0 + (kt % 16)[None, :] * 128.0 + p[:, None]
    return c


def make_consts():
    c = np.zeros((128, NCONST), np.float32)
    p = np.arange(128)
    c[:, C_ID:C_ID + 128] = np.eye(128, dtype=np.float32)
    c[:, C_TRI:C_TRI + 128] = (p[:, None] <= p[None, :]).astype(np.float32)
    c[:, C_SU:C_SU + 128] = (p[:, None] > p[None, :]).astype(np.float32)
    c[:, C_ONE:C_ONE + 128] = 1.0
    inv_freq = (1.0 / (np.float32(10000.0) ** (np.arange(0, 64, 2, dtype=np.float32) / np.float32(64)))).astype(np.float32)
    c[:64, C_INVF] = inv_freq[p[:64] % 32]
    c[:, C_SGN] = np.where(p < 32, -1.0, 1.0)
    c[:, C_NHALF] = -0.5
    c[:, C_NPI] = -math.pi
    c[:, C_REPS] = RMS_EPS
    return c


def build_nc(nseq=2, stop_after=None, dbg=9):
    nc = bass.Bass("TRN2", target_bir_lowering=False)

    def din(name, shape, dt=F32):
        return nc.dram_tensor(name, list(shape), dt, kind="ExternalInput").ap()

    x_d = din("x", [2, S_LEN, D])
    c_d = din("c", [2, D])
    pos_d = din("pos", [2, S_LEN], I32)
    const_d = din("consts", [128, NCONST])
    ada_w_d = din("ada_w", [2, D, 6 * D])
    ada_b_d = din("ada_b", [2, 6 * D])
    ln_d = din("ln_gb", [2, 4, D])
    ab_w_in_d = din("ab_w_in", [D, 2576])
    conv_w_d = din("conv_w", [31, 512])
    pv_d = din("pvec", [40, 128])
    gate_w_d = din("gla_gate_w", [16, 256])
    gate_b_d = din("gla_gate_b", [1, 256])
    gnorm_d = din("gla_norm_g", [1, 512])
    ab_w_out_d = din("ab_w_out", [D, D])
    mla_w_in_d = din("mla_w_in", [D, 768])
    mla_w_uq_d = din("mla_w_uq", [384, 2048])
    mla_w_ukv_d = din("mla_w_ukv", [256, 2048])
    mla_w_out_d = din("mla_w_out", [D, D])
    wr_d = din("moe_wr", [2, D, 36])
    br_d = din("moe_br", [2, 36])
    wall_ds = [din("moe_wall%d" % i, [32 * 128, 6144]) for i in range(2)]
    const2_d = din("consts2", [128, NC2])
    htok_d = nc.dram_tensor("htok_scr", [S_LEN, D], BF16).ap()
    ys_d = nc.dram_tensor("ys_scr", [2 * S_LEN, D], F32).ap()
    tab_d = nc.dram_tensor("tab_scr", [96 * 128, 1], I32).ap()
    out_d = nc.dram_tensor("out", [2, S_LEN, D], F32, kind="ExternalOutput").ap()

    with ExitStack() as st:
        S = Sched(nc, st)
        global LAST_SCHED
        LAST_SCHED = S
        DR = Tile(None, "dram_in")
        DO = Tile(None, "dram_out")
        TABF = Tile(None, "tabf")
        TABS = Tile(None, "tabs")
        BC = {}
        for bv in (4095, 2047, 96 * 128 - 1):
            BC[bv] = nc.alloc_register(mybir.EngineType.Pool, "bc%d" % bv)
            S.op("pool", lambda e, bv=bv: e.reg_mov(BC[bv], bv))

        X = S.sbuf("X", [128, NT, D], F32)
        XT = [Tile(X.ap[:, t, :], "X%d" % t) for t in range(NT)]
        HT = S.sbuf("HT", [128, 8, S_LEN], BF16)
        CONST = S.sbuf("CONST", [128, NCONST], F32)
        IDB = S.sbuf("IDB", [128, 128], BF16)
        TRIB = S.sbuf("TRIB", [128, 128], BF16)
        ONEB = S.sbuf("ONEB", [128, 128], BF16)
        MOD = S.sbuf("MOD", [128, 2, 48, 2], F32)
        G1B = S.sbuf("G1B", [128, D], F32)
        LNG = S.sbuf("LNG", [128, D], F32)
        LNB = S.sbuf("LNB", [128, D], F32)
        PVT = S.sbuf("PVT", [128, 40], F32)
        MV = S.sbuf("MV", [128, NT, 2], F32)
        RSTD = S.sbuf("RSTD", [128, NT], F32)
        NHALF = S.sbuf("NHALF", [128, 512], F32)

        ident = CONST[:, C_ID:C_ID + 128]
        tri = CONST[:, C_TRI:C_TRI + 128]
        su = CONST[:, C_SU:C_SU + 128]
        ones = CONST[:, C_ONE:C_ONE + 128]

        with ExitStack() as ph:
            S.dma("sp", lambda e: e.dma_start(out=CONST[:, :], in_=const_d[:, :]), reads=[DR], writes=[CONST], sync=CONST)
            S.op("dve", lambda e: e.tensor_copy(out=IDB[:, :], in_=ident), reads=[CONST], writes=[IDB])
            S.op("dve", lambda e: e.tensor_copy(out=TRIB[:, :], in_=tri), reads=[CONST], writes=[TRIB])
            S.op("dve", lambda e: e.tensor_copy(out=ONEB[:, :], in_=ones), reads=[CONST], writes=[ONEB])
            S.op("pool", lambda e: e.memset(NHALF[:, :], -0.5), writes=[NHALF])
            STG = S.sbuf("STG", [128, 128], F32, st=ph)
            S.op("dve", lambda e: e.memset(STG[:, :], 0.0), writes=[STG])
            S.dma("sp", lambda e: [e.dma_start(out=STG[0:40, :], in_=pv_d[:, :]),
                                   e.dma_start(out=STG[64:80, :], in_=c_d.rearrange("s (c p) -> (s c) p", p=128)),
                                   ], reads=[DR], writes=[STG], sync=STG, n=2)
            pp = S.psum("pp_setup", [128, 512], F32, st=ph)
            pp2 = S.psum("pp_setup2", [128, 512], F32, st=ph)
            S.op("pe", lambda e: e.transpose(pp[:, 0:128], STG[:, :], ident), reads=[STG, CONST], writes=[pp])
            S.op("dve", lambda e: e.tensor_copy(out=PVT[:, :], in_=pp[:, 0:40]), reads=[pp], writes=[PVT])
            CACT = S.sbuf("CACT", [128, 2, 8], BF16, st=ph)
            S.op("act", lambda e: e.activation(out=CACT[:, :, :].rearrange("p s c -> p (s c)"), in_=pp[:, 64:80], func=AF.Silu), reads=[pp], writes=[CACT])
            STB = S.sbuf("STB", [128, 128], F32, st=ph)
            S.op("dve", lambda e: e.memset(STB[:, :], 0.0), writes=[STB])
            S.dma("sp", lambda e: e.dma_start(out=STB[0:96, :], in_=ada_b_d.rearrange("l (j p) -> (l j) p", p=128)), reads=[DR], writes=[STB], sync=STB)
            S.op("pe", lambda e: e.transpose(pp2[:, 0:128], STB[:, :], ident), reads=[STB, CONST], writes=[pp2])
            ADABT = S.sbuf("ADABT", [128, 96], F32, st=ph)
            S.op("dve", lambda e: e.tensor_copy(out=ADABT[:, :], in_=pp2[:, 0:96]), reads=[pp2], writes=[ADABT])
            AW = [S.sbuf("AW%d" % i, [128, 8, 512], BF16, st=ph) for i in range(3)]
            modp = S.psum("modp", [128, 2, 48, 2], F32, st=ph)
            for l in range(2):
                for blk in range(12):
                    aw = AW[(l * 12 + blk) % 3]
                    src = ada_w_d[l].rearrange("(c p) n -> p c n", p=128)[:, :, blk * 512:(blk + 1) * 512]
                    S.dma("pool", lambda e, aw=aw, src=src: e.dma_start(out=aw[:, :, :], in_=src), reads=[DR], writes=[aw], sync=aw)
                    for jj in range(4):
                        j = blk * 4 + jj
                        for kc in range(8):
                            S.op("pe", lambda e, aw=aw, jj=jj, kc=kc, l=l, j=j: e.matmul(
                                modp[:, l, j, :], lhsT=aw[:, kc, jj * 128:(jj + 1) * 128], rhs=CACT[:, :, kc],
                                start=(kc == 0), stop=(kc == 7)), reads=[aw, CACT], writes=[modp])
            for l in range(2):
                for s in range(2):
                    S.op("dve", lambda e, l=l, s=s: e.tensor_tensor(out=MOD[:, l, :, s], in0=modp[:, l, :, s], in1=ADABT[:, l * 48:(l + 1) * 48], op=ALU.add),
                         reads=[modp, ADABT], writes=[MOD])
            for l in range(2):
                for j0 in (8, 32):
                    S.op("dve", lambda e, l=l, j0=j0: e.tensor_scalar(out=MOD[:, l, j0:j0 + 8, :], in0=MOD[:, l, j0:j0 + 8, :], scalar1=1.0, scalar2=1.0 / ALPHA, op0=ALU.add, op1=ALU.mult),
                         reads=[MOD], writes=[MOD])
                for j0 in (16, 40):
                    S.op("dve", lambda e, l=l, j0=j0: e.tensor_scalar(out=MOD[:, l, j0:j0 + 8, :], in0=MOD[:, l, j0:j0 + 8, :], scalar1=1.0, scalar2=None, op0=ALU.add),
                         reads=[MOD], writes=[MOD])
            S.fence()

        def load_ln(l, which, scaled):
            S.dma("sp", lambda e: [e.dma_start(out=LNG[:, :], in_=ln_d[l, 2 * which:2 * which + 1, :].partition_broadcast(128)),
                                   e.dma_start(out=LNB[:, :], in_=ln_d[l, 2 * which + 1:2 * which + 2, :].partition_broadcast(128))],
                  reads=[DR], writes=[LNG, LNB], sync=LNG, n=2)
            if scaled:
                S.op("dve", lambda e: e.tensor_scalar(out=LNG[:, :], in0=LNG[:, :], scalar1=ALPHA, scalar2=None, op0=ALU.mult), reads=[LNG], writes=[LNG])
                S.op("dve", lambda e: e.tensor_scalar(out=LNB[:, :], in0=LNB[:, :], scalar1=ALPHA, scalar2=None, op0=ALU.mult), reads=[LNB], writes=[LNB])

        def build_g1b(l, j0, s, pp_ts, scratch, dst=None):
            dst = G1B if dst is None else dst
            for c in range(8):
                S.op("dve", lambda e, c=c: e.tensor_scalar(out=scratch[:, c, :], in0=ident, scalar1=MOD[:, l, j0 + c, s:s + 1], scalar2=None, op0=ALU.mult),
                     reads=[CONST, MOD], writes=[scratch])
            for hf in range(2):
                pp_t = pp_ts[hf]
                for c4 in range(4):
                    S.op("pe", lambda e, hf=hf, c4=c4, pp_t=pp_t: e.matmul(pp_t[:, c4 * 128:(c4 + 1) * 128], lhsT=ones, rhs=scratch[:, hf * 4 + c4, :], start=True, stop=True),
                         reads=[CONST, scratch], writes=[pp_t])
                S.op("act", lambda e, hf=hf, pp_t=pp_t: e.activation(out=dst[:, hf * 512:(hf + 1) * 512], in_=pp_t[:, :], func=AF.Copy), reads=[pp_t], writes=[dst])

        def transpose_tile(t, l, jsc, jsh, s, tp_rot, dst_fn, dst_tile, f32_dst=None):
            for hlf in range(2):
                tp = tp_rot.next()
                for c4 in range(4):
                    c = hlf * 4 + c4
                    S.op("pe", lambda e, tp=tp, c=c, c4=c4: e.transpose(tp[:, c4 * 128:(c4 + 1) * 128], XT[t][:, c * 128:(c + 1) * 128], ident),
                         reads=[XT[t], CONST], writes=[tp])
                for c4 in range(4):
                    c = hlf * 4 + c4
                    o_ap = dst_fn(c) if f32_dst is None else f32_dst[:, c, :]
                    o_t = dst_tile if f32_dst is None else f32_dst
                    if c % 2 == 0:
                        S.op("dve", lambda e, tp=tp, c=c, c4=c4, o_ap=o_ap: e.tensor_scalar(
                            out=o_ap, in0=tp[:, c4 * 128:(c4 + 1) * 128], scalar1=MOD[:, l, jsc + c, s:s + 1], scalar2=MOD[:, l, jsh + c, s:s + 1],
                            op0=ALU.mult, op1=ALU.add), reads=[tp, MOD], writes=[o_t])
                    else:
                        S.op("act", lambda e, tp=tp, c=c, c4=c4, o_ap=o_ap: e.activation(
                            out=o_ap, in_=tp[:, c4 * 128:(c4 + 1) * 128], func=AF.Identity, scale=MOD[:, l, jsc + c, s:s + 1], bias=MOD[:, l, jsh + c, s:s + 1]),
                            reads=[tp, MOD], writes=[o_t])

        LN_STATS = S.sbuf("STATS", [128, NT, 2, 6], F32)
        LN_VE = S.sbuf("VE", [128, NT], F32)
        LN_NMR = S.sbuf("NMR", [128, NT], F32)

        def layer_norm_all():
            STATS, VE, NMR = LN_STATS, LN_VE, LN_NMR
            for t in range(NT):
                for h2 in range(2):
                    S.op("dve", lambda e, t=t, h2=h2: e.bn_stats(out=STATS[:, t, h2, :], in_=XT[t][:, h2 * 512:(h2 + 1) * 512]), reads=[XT[t]], writes=[STATS])
                S.op("dve", lambda e, t=t: e.bn_aggr(out=MV[:, t, :], in_=STATS[:, t, :, :].rearrange("p a b -> p (a b)")), reads=[STATS], writes=[MV])
            S.op("dve", lambda e: e.tensor_scalar(out=VE[:, :], in0=MV[:, :, 1], scalar1=LN_EPS, scalar2=None, op0=ALU.add), reads=[MV], writes=[VE])
            S.op("pool", lambda e: e.tensor_tensor(out=RSTD[:, :], in0=VE[:, :], in1=NHALF[:, 0:NT], op=ALU.pow), reads=[VE, NHALF], writes=[RSTD])
            S.op("dve", lambda e: e.scalar_tensor_tensor(out=NMR[:, :], in0=MV[:, :, 0], scalar=-1.0, in1=RSTD[:, :], op0=ALU.mult, op1=ALU.mult), reads=[MV, RSTD], writes=[NMR])
            for t in range(NT):
                S.op("act", lambda e, t=t: e.activation(out=XT[t][:, :], in_=XT[t][:, :], func=AF.Identity, scale=RSTD[:, t:t + 1], bias=NMR[:, t:t + 1]),
                     reads=[XT[t], NMR, RSTD], writes=[XT[t]])
                S.op("dve", lambda e, t=t: e.tensor_tensor(out=XT[t][:, :], in0=XT[t][:, :], in1=LNG[:, :], op=ALU.mult), reads=[XT[t], LNG], writes=[XT[t]])
                S.op("pool", lambda e, t=t: e.tensor_tensor(out=XT[t][:, :], in0=XT[t][:, :], in1=LNB[:, :], op=ALU.add), reads=[XT[t], LNB], writes=[XT[t]])

        LN_STt = [Tile(LN_STATS.ap[:, t], "LNST%d" % t) for t in range(NT)]
        MVt = [Tile(MV.ap[:, t, :], "MV%d" % t) for t in range(NT)]
        VEt = [Tile(LN_VE.ap[:, t:t + 1], "VE%d" % t) for t in range(NT)]
        RSTDt = [Tile(RSTD.ap[:, t:t + 1], "RSTD%d" % t) for t in range(NT)]
        NMRt = [Tile(LN_NMR.ap[:, t:t + 1], "NMR%d" % t) for t in range(NT)]

        class LNPipe:
            def __init__(self, act_stats=False, junk=None):
                self.q = []
                self.act_stats = act_stats
                self.junk = junk

            def s1(self, t):
                st_, mv, ve, rstd = LN_STt[t], MVt[t], VEt[t], RSTDt[t]
                if self.act_stats:
                    junk = self.junk
                    S.op("act", lambda e: e.activation(out=junk[:, :], in_=XT[t][:, :], func=AF.Copy, accum_out=st_[:, 0, 0:1]), reads=[XT[t]], writes=[junk, st_])
                    S.op("act", lambda e: e.activation(out=junk[:, :], in_=XT[t][:, :], func=AF.Square, accum_out=st_[:, 0, 1:2]), reads=[XT[t]], writes=[junk, st_])
                    S.op("dve", lambda e: e.tensor_scalar(out=mv[:, 0:1], in0=st_[:, 0, 0:1], scalar1=1.0 / D, scalar2=None, op0=ALU.mult), reads=[st_], writes=[mv])
                    S.op("dve", lambda e: e.tensor_tensor(out=mv[:, 1:2], in0=mv[:, 0:1], in1=mv[:, 0:1], op=ALU.mult), reads=[mv], writes=[mv])
                    S.op("dve", lambda e: e.scalar_tensor_tensor(out=ve[:, :], in0=st_[:, 0, 1:2], scalar=1.0 / D, in1=mv[:, 1:2], op0=ALU.mult, op1=ALU.subtract), reads=[st_, mv], writes=[ve])
                    S.op("dve", lambda e: e.tensor_scalar(out=ve[:, :], in0=ve[:, :], scalar1=LN_EPS, scalar2=None, op0=ALU.add), reads=[ve], writes=[ve])
                else:
                    for h2 in range(2):
                        S.op("dve", lambda e, h2=h2: e.bn_stats(out=st_[:, h2, :], in_=XT[t][:, h2 * 512:(h2 + 1) * 512]), reads=[XT[t]], writes=[st_])
                    S.op("dve", lambda e: e.bn_aggr(out=mv[:, :], in_=st_[:, :, :].rearrange("p a b -> p (a b)")), reads=[st_], writes=[mv])
                    S.op("dve", lambda e: e.tensor_scalar(out=ve[:, :], in0=mv[:, 1:2], scalar1=LN_EPS, scalar2=None, op0=ALU.add), reads=[mv], writes=[ve])
                S.op("pool", lambda e: e.tensor_tensor(out=rstd[:, :], in0=ve[:, :], in1=NHALF[:, 0:1], op=ALU.pow), reads=[ve, NHALF], writes=[rstd])

            def s2(self, t):
                mv, rstd, nmr = MVt[t], RSTDt[t], NMRt[t]
                S.op("dve", lambda e: e.scalar_tensor_tensor(out=nmr[:, :], in0=mv[:, 0:1], scalar=-1.0, in1=rstd[:, :], op0=ALU.mult, op1=ALU.mult), reads=[mv, rstd], writes=[nmr])
                S.op("act", lambda e: e.activation(out=XT[t][:, :], in_=XT[t][:, :], func=AF.Identity, scale=rstd[:, :], bias=nmr[:, :]), reads=[XT[t], nmr, rstd], writes=[XT[t]])

            def s3(self, t):
                S.op("dve", lambda e: e.tensor_tensor(out=XT[t][:, :], in0=XT[t][:, :], in1=LNG[:, :], op=ALU.mult), reads=[XT[t], LNG], writes=[XT[t]])
                S.op("pool", lambda e: e.tensor_tensor(out=XT[t][:, :], in0=XT[t][:, :], in1=LNB[:, :], op=ALU.add), reads=[XT[t], LNB], writes=[XT[t]])

            def push(self, t):
                self.q.append([t, 0])
                self.step()

            def step(self):
                for ent in list(self.q):
                    if ent[1] == 0:
                        self.s1(ent[0])
                    elif ent[1] == 1:
                        self.s2(ent[0])
                    else:
                        self.s3(ent[0])
                    ent[1] += 1
                self.q = [en for en in self.q if en[1] < 3]

            def flush(self):
                while self.q:
                    self.step()

        NV, NJ = 48, 96
        BIG = 65536.0
        BIGW = 8192.0

        def moe_phase(l, s, last):
            jsh, jsc, jg = 24, 32, 40
            wall_d = wall_ds[l]
            IOA = bass.IndirectOffsetOnAxis
            with ExitStack() as ph:
                LG = S.sbuf("LG", [128, NT, 36], F32, st=ph)
                P12 = S.sbuf("P12", [128, 2, NT], F32, st=ph)
                IDXY = S.sbuf("IDXY", [128, NJ], I32, st=ph)
                IDXG = S.sbuf("IDXG", [128, NJ], I32, st=ph)
                WIDX = S.sbuf("WIDX", [128, NV], I32, st=ph)
                NW, NH = 5, 4

                with ExitStack() as ph1:
                    WR = S.sbuf("WR", [128, 8, 36], F32, st=ph1)
                    RB = S.sbuf("RB", [128, 36], F32, st=ph1)
                    H32 = Rot([S.sbuf("H32_%d" % i, [128, 8, 128], F32, st=ph1) for i in range(3)])
                    SCB = S.sbuf("SCB", [128, D], F32, st=ph1)
                    SHB = S.sbuf("SHB", [128, D], F32, st=ph1)
                    TM32 = Rot([S.sbuf("TM32_%d" % i, [128, D], F32, st=ph1) for i in range(2)])
                    HB = Rot([S.sbuf("HB_%d" % i, [128, D], BF16, st=ph1) for i in range(2)])
                    tp_rot = Rot([S.psum("tp%d" % i, [128, 512], F32, st=ph1) for i in range(4)])
                    lg_rot = Rot([S.psum("lgp%d" % i, [128, 512], F32, st=ph1) for i in range(2)])
                    scrs = [S.sbuf("scr%d" % i, [128, 8, 128], F32, st=ph1) for i in range(2)]
                    S.dma("sp", lambda e: [e.dma_start(out=WR[:, :, :], in_=wr_d[l].rearrange("(c p) n -> p c n", p=128)),
                                           e.dma_start(out=RB[:, :], in_=br_d[l:l + 1, :].partition_broadcast(128))],
                          reads=[DR], writes=[WR, RB], sync=WR, n=2)
                    build_g1b(l, jsc, s, tp_rot.tiles[0:2], scrs[0], dst=SCB)
                    build_g1b(l, jsh, s, tp_rot.tiles[2:4], scrs[1], dst=SHB)
                    build_g1b(l, jg, s, lg_rot.tiles, scrs[0])
                    load_ln(l, 1, not last)
                    h32s = {}

                    def router(t):
                        h32 = h32s.pop(t)
                        lgp = lg_rot.next()
                        for c in range(8):
                            S.op("pe", lambda e, c=c: e.matmul(lgp[:, 0:36], lhsT=h32[:, c, :], rhs=WR[:, c, :], start=(c == 0), stop=(c == 7)),
                                 reads=[h32, WR], writes=[lgp])
                        S.op("dve", lambda e: e.tensor_tensor(out=LG[:, t, :], in0=lgp[:, 0:36], in1=RB[:, :], op=ALU.add), reads=[lgp, RB], writes=[LG])

                    for t in range(NT):
                        h32 = H32.next()
                        h32s[t] = h32
                        transpose_tile(t, l, jsc, jsh, s, tp_rot, None, None, f32_dst=h32)
                        tm = TM32.next()
                        hb = HB.next()
                        S.op("dve", lambda e, t=t, tm=tm: e.tensor_tensor(out=tm[:, :], in0=XT[t][:, :], in1=SCB[:, :], op=ALU.mult), reads=[XT[t], SCB], writes=[tm])
                        S.op("pool", lambda e, tm=tm, hb=hb: e.tensor_tensor(out=hb[:, :], in0=tm[:, :], in1=SHB[:, :], op=ALU.add), reads=[tm, SHB], writes=[hb])
                        S.dma("sp", lambda e, t=t, hb=hb: e.dma_start(out=htok_d[t * 128:(t + 1) * 128, :], in_=hb[:, :]), reads=[hb], writes=[], sync=hb)
                        if t >= 1:
                            router(t - 1)
                    router(NT - 1)
                    S.fence()
                with ExitStack() as ph1:
                    def sb(name, shape, dt=F32):
                        return S.sbuf(name, shape, dt, st=ph1)
                    C2 = sb("C2", [128, NC2])
                    S.dma("sp", lambda e: e.dma_start(out=C2[:, :], in_=const2_d[:, :]), reads=[DR], writes=[C2], sync=C2)
                    GMAX = sb("GMAX", [128, NT]); DG_ = sb("DGL", [128, NT, 4]); GE = sb("GE", [128, NT, 4]); GS = sb("GS", [128, NT])
                    GW = sb("GW", [128, NT]); PEN = sb("PEN", [128, NT, 4]); EM = sb("EM", [128, NT, 32]); M1 = sb("M1", [128, NT])
                    OH1 = sb("OH1", [128, NT, 32]); EM2 = sb("EM2", [128, NT, 32]); M2 = sb("M2", [128, NT]); OH2 = sb("OH2", [128, NT, 32])
                    DM = sb("DM", [128, NT]); P1 = sb("P1", [128, NT]); P2 = sb("P2", [128, NT])
                    GL = LG[:, :, 0:4]
                    EL = LG[:, :, 4:36]
                    V = lambda f, r, w: S.op("dve", f, reads=r, writes=w)
                    V(lambda e: e.tensor_reduce(out=GMAX[:, :], in_=GL, axis=AX.X, op=ALU.max), [LG], [GMAX])
                    V(lambda e: e.tensor_tensor(out=DG_[:, :, :], in0=GL, in1=GMAX[:, :].unsqueeze(2).to_broadcast([128, NT, 4]), op=ALU.subtract), [LG, GMAX], [DG_])
                    S.op("act", lambda e: e.activation(out=GE[:, :, :], in_=DG_[:, :, :], func=AF.Exp), reads=[DG_], writes=[GE])
                    V(lambda e: e.tensor_reduce(out=GS[:, :], in_=GE[:, :, :], axis=AX.X, op=ALU.add), [GE], [GS])
                    V(lambda e: e.reciprocal(out=GW[:, :], in_=GS[:, :]), [GS], [GW])
                    V(lambda e: e.tensor_scalar(out=PEN[:, :, :], in0=DG_[:, :, :], scalar1=0.0, scalar2=-1e30, op0=ALU.is_lt, op1=ALU.mult), [DG_], [PEN])
                    V(lambda e: e.tensor_tensor(out=EM[:, :, :].rearrange("p t (g j) -> p t g j", g=4), in0=EL.rearrange("p t (g j) -> p t g j", g=4),
                                                in1=PEN[:, :, :].unsqueeze(3).to_broadcast([128, NT, 4, 8]), op=ALU.add), [LG, PEN], [EM])
                    V(lambda e: e.tensor_reduce(out=M1[:, :], in_=EM[:, :, :], axis=AX.X, op=ALU.max), [EM], [M1])
                    V(lambda e: e.tensor_tensor(out=OH1[:, :, :], in0=EM[:, :, :], in1=M1[:, :].unsqueeze(2).to_broadcast([128, NT, 32]), op=ALU.is_equal), [EM, M1], [OH1])
                    V(lambda e: e.scalar_tensor_tensor(out=EM2[:, :, :], in0=OH1[:, :, :], scalar=-1e30, in1=EM[:, :, :], op0=ALU.mult, op1=ALU.add), [OH1, EM], [EM2])
                    V(lambda e: e.tensor_reduce(out=M2[:, :], in_=EM2[:, :, :], axis=AX.X, op=ALU.max), [EM2], [M2])
                    V(lambda e: e.tensor_tensor(out=OH2[:, :, :], in0=EM2[:, :, :], in1=M2[:, :].unsqueeze(2).to_broadcast([128, NT, 32]), op=ALU.is_equal), [EM2, M2], [OH2])
                    V(lambda e: e.tensor_tensor(out=DM[:, :], in0=M2[:, :], in1=M1[:, :], op=ALU.subtract), [M1, M2], [DM])
                    S.op("act", lambda e: e.activation(out=DM[:, :], in_=DM[:, :], func=AF.Exp), reads=[DM], writes=[DM])
                    V(lambda e: e.tensor_scalar(out=DM[:, :], in0=DM[:, :], scalar1=1.0, scalar2=None, op0=ALU.add), [DM], [DM])
                    V(lambda e: e.reciprocal(out=P1[:, :], in_=DM[:, :]), [DM], [P1])
                    V(lambda e: e.tensor_scalar(out=P2[:, :], in0=P1[:, :], scalar1=-1.0, scalar2=1.0, op0=ALU.mult, op1=ALU.add), [P1], [P2])
                    V(lambda e: e.tensor_tensor(out=P12[:, 0, :], in0=P1[:, :], in1=GW[:, :], op=ALU.mult), [P1, GW], [P12])
                    V(lambda e: e.tensor_tensor(out=P12[:, 1, :], in0=P2[:, :], in1=GW[:, :], op=ALU.mult), [P2, GW], [P12])
                    M = sb("M", [128, NT, 32]); MB = sb("MB", [128, NT, 32], BF16)
                    EXC = sb("EXC", [128, NT, 32]); TOT = sb("TOT", [128, NT, 32]); OFFS = sb("OFFS", [128, NT + 1, 32])
                    ip = S.psum("ip", [128, 512], F32, st=ph1)
                    tpp = S.psum("tpp", [128, 512], F32, st=ph1)
                    V(lambda e: e.tensor_tensor(out=M[:, :, :], in0=OH1[:, :, :], in1=OH2[:, :, :], op=ALU.add), [OH1, OH2], [M])
                    V(lambda e: e.tensor_copy(out=MB[:, :, :], in_=M[:, :, :]), [M], [MB])
                    S.op("pe", lambda e: e.matmul(ip[:, :], lhsT=TRIB[:, :], rhs=MB[:, :, :].rearrange("p t e -> p (t e)"), start=True, stop=True), reads=[TRIB, MB], writes=[ip])
                    S.op("pe", lambda e: e.matmul(tpp[:, :], lhsT=ONEB[:, :], rhs=MB[:, :, :].rearrange("p t e -> p (t e)"), start=True, stop=True), reads=[ONEB, MB], writes=[tpp])
                    V(lambda e: e.tensor_tensor(out=EXC[:, :, :], in0=ip[:, :].rearrange("p (t e) -> p t e", e=32), in1=M[:, :, :], op=ALU.subtract), [ip, M], [EXC])
                    S.op("act", lambda e: e.activation(out=TOT[:, :, :], in_=tpp[:, :].rearrange("p (t e) -> p t e", e=32), func=AF.Copy), reads=[tpp], writes=[TOT])
                    V(lambda e: e.memset(OFFS[:, 0, :], 0.0), [], [OFFS])
                    for t in range(1, NT + 1):
                        V(lambda e, t=t: e.tensor_tensor(out=OFFS[:, t, :], in0=OFFS[:, t - 1, :], in1=TOT[:, t - 1, :], op=ALU.add), [OFFS, TOT], [OFFS])
                    TMP8 = sb("TMP8", [128, 32, 8]); NVv = sb("NVv", [128, 32]); TMP32 = sb("TMP32", [128, 32, 32]); VB = sb("VB", [128, 32]); VE = sb("VE", [128, 32]); BASE = sb("BASE", [128, 32])
                    V(lambda e: e.tensor_tensor(out=TMP8[:, :, :], in0=C2[:, C2_THR:C2_THR + 8].unsqueeze(1).to_broadcast([128, 32, 8]),
                                                in1=OFFS[:, NT, :].unsqueeze(2).to_broadcast([128, 32, 8]), op=ALU.is_lt), [C2, OFFS], [TMP8])
                    V(lambda e: e.tensor_reduce(out=NVv[:, :], in_=TMP8[:, :, :], axis=AX.X, op=ALU.add), [TMP8], [NVv])
                    V(lambda e: e.tensor_tensor(out=TMP32[:, :, :], in0=C2[:, C2_LT:C2_LT + 1024].rearrange("p (a b) -> p a b", a=32),
                                                in1=NVv[:, :].unsqueeze(1).to_broadcast([128, 32, 32]), op=ALU.mult), [C2, NVv], [TMP32])
                    V(lambda e: e.tensor_reduce(out=VB[:, :], in_=TMP32[:, :, :], axis=AX.X, op=ALU.add), [TMP32], [VB])
                    V(lambda e: e.tensor_tensor(out=VE[:, :], in0=VB[:, :], in1=NVv[:, :], op=ALU.add), [VB, NVv], [VE])
                    V(lambda e: e.tensor_scalar(out=BASE[:, :], in0=VB[:, :], scalar1=256.0, scalar2=None, op0=ALU.mult), [VB], [BASE])
                    T1 = sb("T1r", [128, NT, 32]); POS = sb("POS", [128, 2, NT]); Q = sb("Q", [128, 2, NT]); QI = sb("QI", [128, 2, NT], I32); QF = sb("QF", [128, 2, NT])
                    CORR = sb("CORR", [128, 2, NT]); PM = sb("PM", [128, 2, NT]); DEST = sb("DEST", [128, 2, NT]); DESTI = sb("DESTI", [128, 2, NT], I32)
                    VALI = sb("VALI", [128, 2, NT], I32); FILLF = sb("FILLF", [128, NJ]); FILLI = sb("FILLI", [128, NJ], I32); IF = sb("IF", [128, NJ]); IGE = sb("IGE", [128, NJ])
                    V(lambda e: e.tensor_tensor(out=EXC[:, :, :], in0=EXC[:, :, :], in1=OFFS[:, 0:NT, :], op=ALU.add), [EXC, OFFS], [EXC])
                    V(lambda e: e.tensor_tensor(out=EXC[:, :, :], in0=EXC[:, :, :], in1=BASE[:, :].unsqueeze(1).to_broadcast([128, NT, 32]), op=ALU.add), [EXC, BASE], [EXC])
                    for k, OH in ((0, OH1), (1, OH2)):
                        V(lambda e, OH=OH: e.tensor_tensor(out=T1[:, :, :], in0=OH[:, :, :], in1=EXC[:, :, :], op=ALU.mult), [OH, EXC], [T1])
                        V(lambda e, k=k: e.tensor_reduce(out=POS[:, k, :], in_=T1[:, :, :], axis=AX.X, op=ALU.add), [T1], [POS])
                    V(lambda e: e.tensor_scalar(out=Q[:, :, :], in0=POS[:, :, :], scalar1=1.0 / 128, scalar2=None, op0=ALU.mult), [POS], [Q])
                    V(lambda e: e.tensor_copy(out=QI[:, :, :], in_=Q[:, :, :]), [Q], [QI])
                    V(lambda e: e.tensor_copy(out=QF[:, :, :], in_=QI[:, :, :]), [QI], [QF])
                    V(lambda e: e.tensor_tensor(out=CORR[:, :, :], in0=QF[:, :, :], in1=Q[:, :, :], op=ALU.is_gt), [QF, Q], [CORR])
                    V(lambda e: e.tensor_tensor(out=QF[:, :, :], in0=QF[:, :, :], in1=CORR[:, :, :], op=ALU.subtract), [QF, CORR], [QF])
                    V(lambda e: e.scalar_tensor_tensor(out=PM[:, :, :], in0=QF[:, :, :], scalar=-128.0, in1=POS[:, :, :], op0=ALU.mult, op1=ALU.add), [QF, POS], [PM])
                    V(lambda e: e.scalar_tensor_tensor(out=DEST[:, :, :], in0=PM[:, :, :], scalar=float(NJ), in1=QF[:, :, :], op0=ALU.mult, op1=ALU.add), [PM, QF], [DEST])
                    V(lambda e: e.tensor_copy(out=DESTI[:, :, :], in_=DEST[:, :, :]), [DEST], [DESTI])
                    V(lambda e: e.tensor_copy(out=VALI[:, :, :], in_=C2[:, C2_VAL:C2_VAL + 32].rearrange("p (k t) -> p k t", k=2)), [C2], [VALI])
                    V(lambda e: e.memset(FILLF[:, :], BIG), [], [FILLF])
                    V(lambda e: e.tensor_copy(out=FILLI[:, :], in_=FILLF[:, :]), [FILLF], [FILLI])
                    tab_v = tab_d.rearrange("(p j) o -> p (j o)", p=128)
                    S.dma("sp", lambda e: e.dma_start(out=tab_v, in_=FILLI[:, :]), reads=[FILLI], writes=[TABF], sync=TABF)
                    last_sc = None
                    for k in range(2):
                        for t in range(NT):
                            last_sc = S.dma("pool", lambda e, k=k, t=t: e.indirect_dma_start(out=tab_d[:, :], out_offset=IOA(ap=DESTI[:, k, t:t + 1], axis=0), in_=VALI[:, k, t:t + 1], in_offset=None,
                                                                                            bounds_check=BC[NJ * 128 - 1], oob_is_err=False), reads=[DESTI, VALI, TABF], writes=[], sync=TABS)
                    VA = sb("VA", [128, NV, 32]); VBm = sb("VBm", [128, NV, 32]); WF = sb("WF", [128, NV])
                    vrow = C2[:, C2_VROW:C2_VROW + NV].unsqueeze(2).to_broadcast([128, NV, 32])
                    V(lambda e: e.tensor_tensor(out=VA[:, :, :], in0=vrow, in1=VB[:, :].unsqueeze(1).to_broadcast([128, NV, 32]), op=ALU.is_ge), [C2, VB], [VA])
                    V(lambda e: e.tensor_tensor(out=VBm[:, :, :], in0=vrow, in1=VE[:, :].unsqueeze(1).to_broadcast([128, NV, 32]), op=ALU.is_lt), [C2, VE], [VBm])
                    V(lambda e: e.tensor_tensor(out=VA[:, :, :], in0=VA[:, :, :], in1=VBm[:, :, :], op=ALU.mult), [VA, VBm], [VA])
                    V(lambda e: e.tensor_tensor(out=VA[:, :, :], in0=VA[:, :, :], in1=C2[:, C2_EROW:C2_EROW + 32].unsqueeze(1).to_broadcast([128, NV, 32]), op=ALU.mult), [VA, C2], [VA])
                    V(lambda e: e.tensor_reduce(out=WF[:, :], in_=VA[:, :, :], axis=AX.X, op=ALU.add), [VA], [WF])
                    V(lambda e: e.tensor_scalar(out=WF[:, :], in0=WF[:, :], scalar1=BIGW, scalar2=None, op0=ALU.add), [WF], [WF])
                    V(lambda e: e.tensor_copy(out=WIDX[:, :], in_=WF[:, :]), [WF], [WIDX])
                    TABS.w = last_sc
                    S.dma("sp", lambda e: e.dma_start(out=IDXY[:, :], in_=tab_v), reads=[TABS], writes=[IDXY], sync=IDXY)
                    TABS.w = None
                    V(lambda e: e.tensor_copy(out=IF[:, :], in_=IDXY[:, :]), [IDXY], [IF])
                    V(lambda e: e.tensor_single_scalar(out=IGE[:, :], in_=IF[:, :], scalar=2048.0, op=ALU.is_ge), [IF], [IGE])
                    V(lambda e: e.scalar_tensor_tensor(out=IF[:, :], in0=IGE[:, :], scalar=-2048.0, in1=IF[:, :], op0=ALU.mult, op1=ALU.add), [IGE, IF], [IF])
                    V(lambda e: e.tensor_copy(out=IDXG[:, :], in_=IF[:, :]), [IF], [IDXG])
                    S.fence()
                with ExitStack() as ph2:
                    W = [S.sbuf("W%d" % i, [128, 6144], BF16, st=ph2) for i in range(NW)]
                    HS = [S.sbuf("HS%d" % i, [128, D], BF16, st=ph2) for i in range(NH)]
                    for hs in HS:
                        S.op("dve", lambda e, hs=hs: e.memset(hs[:, :], 0.0), writes=[hs])

                    def issue_w(v):
                        w = W[v % NW]
                        S.dma("pool", lambda e: e.indirect_dma_start(out=w[:, :], out_offset=None, in_=wall_d[:, :], in_offset=IOA(ap=WIDX[:, v:v + 1], axis=0),
                                                                     bounds_check=BC[4095], oob_is_err=False), reads=[DR, WIDX, w], writes=[w], sync=w)

                    for v in range(NW):
                        issue_w(v)
                    HST = Rot([S.sbuf("HST%d" % i, [128, 8, 128], BF16, st=ph2) for i in range(2)])
                    SG = Rot([S.sbuf("SG%d" % i, [128, 256], F32, st=ph2) for i in range(2)])
                    HID = Rot([S.sbuf("HID%d" % i, [128, 256], BF16, st=ph2) for i in range(3)])
                    HIDT = Rot([S.sbuf("HIDT%d" % i, [128, 2, 128], BF16, st=ph2) for i in range(3)])
                    YSB = Rot([S.sbuf("YSB%d" % i, [128, D], F32, st=ph2) for i in range(3)])
                    xT_rot = Rot([S.psum("xTp%d" % i, [128, 8, 128], BF16, st=ph2) for i in range(2)])
                    gu_rot = Rot([S.psum("gu%d" % i, [128, 512], F32, st=ph2) for i in range(2)])
                    hTp = S.psum("hTp", [128, 2, 2, 128], BF16, st=ph2)
                    hT_rot = Rot([Tile(hTp.ap[:, i], "hTp%d" % i) for i in range(2)])
                    y_rot = Rot([S.psum("yp%d" % i, [128, 512], F32, st=ph2) for i in range(3)])
                    state = {}

                    def issue_gather(j):
                        hs = HS[j % NH]
                        S.dma("pool", lambda e: e.indirect_dma_start(out=hs[:, :], out_offset=None, in_=htok_d[:, :], in_offset=IOA(ap=IDXG[:, j:j + 1], axis=0),
                                                                     bounds_check=BC[S_LEN - 1], oob_is_err=False), reads=[DR, IDXG, hs], writes=[hs], sync=hs)

                    def do_T8(j):
                        hs = HS[j % NH]
                        xp = xT_rot.next()
                        for c in range(8):
                            S.op("pe", lambda e, c=c: e.transpose(xp[:, c, :], hs[:, c * 128:(c + 1) * 128], IDB[:, :]), reads=[hs, IDB], writes=[xp])
                        hst = HST.next()
                        S.op("act", lambda e: e.activation(out=hst[:, :, :], in_=xp[:, :, :], func=AF.Copy), reads=[xp], writes=[hst])
                        state[j] = [hst, None, None]

                    def do_GU(j):
                        w = W[(j // 2) % NW]
                        hst = state[j][0]
                        gp = gu_rot.next()
                        for c in range(8):
                            S.op("pe", lambda e, c=c: e.matmul(gp[:, :], lhsT=hst[:, c, :], rhs=w[:, c * 512:(c + 1) * 512], start=(c == 0), stop=(c == 7)),
                                 reads=[hst, w], writes=[gp])
                        sg = SG.next()
                        hid = HID.next()
                        S.op("act", lambda e: e.activation(out=sg[:, :], in_=gp[:, 0:256], func=AF.Silu), reads=[gp], writes=[sg])
                        S.op("dve", lambda e: e.tensor_tensor(out=hid[:, :], in0=gp[:, 256:512], in1=sg[:, :], op=ALU.mult), reads=[gp, sg], writes=[hid])
                        state[j][1] = hid

                    def do_HT(j):
                        hid = state[j][1]
                        hp = hT_rot.next()
                        for c in range(2):
                            S.op("pe", lambda e, c=c: e.transpose(hp[:, c, :], hid[:, c * 128:(c + 1) * 128], IDB[:, :]), reads=[hid, IDB], writes=[hp])
                        hT = HIDT.next()
                        S.op("act", lambda e: e.activation(out=hT[:, :, :], in_=hp[:, :, :], func=AF.Copy), reads=[hp], writes=[hT])
                        state[j][2] = hT

                    def do_D(j):
                        w = W[(j // 2) % NW]
                        hT = state.pop(j)[2]
                        ysb = YSB.next()
                        for hf in range(2):
                            yp = y_rot.next()
                            for c in range(2):
                                S.op("pe", lambda e, hf=hf, c=c, yp=yp: e.matmul(yp[:, :], lhsT=hT[:, c, :], rhs=w[:, 4096 + c * 1024 + hf * 512:4096 + c * 1024 + (hf + 1) * 512],
                                                                                 start=(c == 0), stop=(c == 1)), reads=[hT, w], writes=[yp])
                            S.op("dve", lambda e, yp=yp, hf=hf: e.tensor_tensor(out=ysb[:, hf * 512:(hf + 1) * 512], in0=yp[:, :], in1=G1B[:, hf * 512:(hf + 1) * 512], op=ALU.mult),
                                 reads=[yp, G1B], writes=[ysb])
                        S.dma("pool", lambda e: e.indirect_dma_start(out=ys_d[:, :], out_offset=IOA(ap=IDXY[:, j:j + 1], axis=0), in_=ysb[:, :], in_offset=None,
                                                                     bounds_check=BC[2 * S_LEN - 1], oob_is_err=False), reads=[ysb, IDXY], writes=[], sync=ysb)

                    for j in range(NH):
                        issue_gather(j)
                    do_T8(0)
                    issue_gather(NH)
                    for i in range(NJ + 2):
                        if i + 1 < NJ:
                            do_T8(i + 1)
                            if i + 1 + NH < NJ:
                                issue_gather(i + 1 + NH)
                        if i < NJ:
                            do_GU(i)
                        if 1 <= i <= NJ:
                            do_HT(i - 1)
                        if 2 <= i:
                            do_D(i - 2)
                            if (i - 2) % 2 == 1:
                                nv_ = (i - 2) // 2 + NW
                                if nv_ < NV:
                                    issue_w(nv_)
                    S.fence()
                with ExitStack() as ph3:
                    Y0 = Rot([S.sbuf("Y0_%d" % i, [128, D], F32, st=ph3) for i in range(3)])
                    Y1 = Rot([S.sbuf("Y1_%d" % i, [128, D], F32, st=ph3) for i in range(3)])
                    LJ = S.sbuf("LNJ", [128, D], BF16, st=ph3)
                    lnp = LNPipe(act_stats=True, junk=LJ)
                    for t in range(NT):
                        y0 = Y0.next(); y1 = Y1.next()
                        S.dma("sp", lambda e, t=t, y0=y0: e.dma_start(out=y0[:, :], in_=ys_d[t * 128:(t + 1) * 128, :]), reads=[DR], writes=[y0], sync=y0)
                        S.dma("sp", lambda e, t=t, y1=y1: e.dma_start(out=y1[:, :], in_=ys_d[S_LEN + t * 128:S_LEN + (t + 1) * 128, :]), reads=[DR], writes=[y1], sync=y1)
                        S.op("dve", lambda e, t=t, y0=y0: e.scalar_tensor_tensor(out=XT[t][:, :], in0=y0[:, :], scalar=P12[:, 0, t:t + 1], in1=XT[t][:, :], op0=ALU.mult, op1=ALU.add),
                             reads=[y0, P12, XT[t]], writes=[XT[t]])
                        S.op("dve", lambda e, t=t, y1=y1: e.scalar_tensor_tensor(out=XT[t][:, :], in0=y1[:, :], scalar=P12[:, 1, t:t + 1], in1=XT[t][:, :], op0=ALU.mult, op1=ALU.add),
                             reads=[y1, P12, XT[t]], writes=[XT[t]])
                        lnp.push(t)
                    lnp.flush()
                    S.fence()

        def l0_mixer(s):
            l = 0
            jsh, jsc, jg = 0, 8, 16
            CATA = [Tile(HT.ap[:, 0:4, B * 512:(B + 1) * 512], "CATA%d" % B) for B in range(4)]
            CATB = [Tile(HT.ap[:, 4:8, t * 128:(t + 1) * 128], "CATB%d" % t) for t in range(NT)]
            load_ln(l, 0, True)
            with ExitStack() as p12:
                HCT = S.sbuf("HCT", [128, 4, 2080], BF16, st=p12)
                S.op("pool", lambda e: e.memset(HCT[:, :, 0:30], 0.0), writes=[HCT])
                with ExitStack() as p1:
                    WINC = S.sbuf("WINC", [128, 8, 1024], BF16, st=p1)
                    S.dma("pool", lambda e: e.dma_start(out=WINC[:, :, :], in_=ab_w_in_d.rearrange("(c p) n -> p c n", p=128)[:, :, 0:1024]),
                          reads=[DR], writes=[WINC], sync=WINC)
                    HTB = Rot([S.sbuf("HTB%d" % i, [128, 8, 512], BF16, st=p1) for i in range(2)])
                    SIG = Rot([S.sbuf("SIG%d" % i, [128, 512], F32, st=p1) for i in range(2)])
                    tp_rot = Rot([S.psum("tp%d" % i, [128, 512], F32, st=p1) for i in range(4)])
                    ag_rot = Rot([S.psum("ag%d" % i, [128, 512], F32, st=p1) for i in range(4)])
                    scr = S.sbuf("scr", [128, 8, 128], F32, st=p1)
                    build_g1b(l, jg, s, ag_rot.tiles[0:2], scr)
                    for B in range(4):
                        htb = HTB.next()
                        for i in range(4):
                            transpose_tile(4 * B + i, l, jsc, jsh, s, tp_rot, lambda c, i=i, htb=htb: htb[:, c, i * 128:(i + 1) * 128], htb)
                        for cc in range(4):
                            a_p = ag_rot.next()
                            g_p = ag_rot.next()
                            for kc in range(8):
                                S.op("pe", lambda e, kc=kc, cc=cc, a_p=a_p, htb=htb: e.matmul(a_p[:, :], lhsT=WINC[:, kc, cc * 128:(cc + 1) * 128], rhs=htb[:, kc, :], start=(kc == 0), stop=(kc == 7)),
                                     reads=[WINC, htb], writes=[a_p])
                            for kc in range(8):
                                S.op("pe", lambda e, kc=kc, cc=cc, g_p=g_p, htb=htb: e.matmul(g_p[:, :], lhsT=WINC[:, kc, 512 + cc * 128:512 + (cc + 1) * 128], rhs=htb[:, kc, :], start=(kc == 0), stop=(kc == 7)),
                                     reads=[WINC, htb], writes=[g_p])
                            sig = SIG.next()
                            S.op("act", lambda e, sig=sig, g_p=g_p: e.activation(out=sig[:, :], in_=g_p[:, :], func=AF.Sigmoid), reads=[g_p], writes=[sig])
                            S.op("dve", lambda e, sig=sig, a_p=a_p, cc=cc, B=B: e.tensor_tensor(out=HCT[:, cc, 30 + B * 512:30 + (B + 1) * 512], in0=a_p[:, :], in1=sig[:, :], op=ALU.mult),
                                 reads=[a_p, sig], writes=[HCT])
                    S.fence()
                if stop_after == "l0a":
                    return
                with ExitStack() as p2:
                    CWS = S.sbuf("CWS", [32, 512], F32, st=p2)
                    CW = S.sbuf("CW", [128, 4, 31], F32, st=p2)
                    DG = [S.sbuf("DG%d" % cc, [128, 31, 128], BF16, st=p2) for cc in range(4)]
                    Y = S.sbuf("Y", [128, 4, 512], F32, st=p2)
                    YSQ = S.sbuf("YSQ", [128, 4, 512], F32, st=p2)
                    MEAN = S.sbuf("MEAN", [128, 512], F32, st=p2)
                    MSQ = S.sbuf("MSQ", [128, 512], F32, st=p2)
                    VAR = S.sbuf("VAR", [128, 512], F32, st=p2)
                    RS = S.sbuf("RS", [128, 512], F32, st=p2)
                    T1 = Rot([S.sbuf("T1_%d" % i, [128, 512], F32, st=p2) for i in range(2)])
                    y_ps = [S.psum("yps%d" % i, [128, 512], F32, st=p2) for i in range(4)]
                    st_rot = Rot([S.psum("stp%d" % i, [128, 512], F32, st=p2) for i in range(2)])
                    S.dma("sp", lambda e: e.dma_start(out=CWS[0:31, :], in_=conv_w_d[:, :]), reads=[DR], writes=[CWS], sync=CWS)
                    for cc in range(4):
                        pt = st_rot.next()
                        S.op("pe", lambda e, cc=cc, pt=pt: e.transpose(pt[:, 0:31], CWS[0:31, cc * 128:(cc + 1) * 128], CONST[0:31, C_ID:C_ID + 31]), reads=[CWS, CONST], writes=[pt])
                        S.op("dve", lambda e, cc=cc, pt=pt: e.tensor_copy(out=CW[:, cc, :], in_=pt[:, 0:31]), reads=[pt], writes=[CW])
                    for cc in range(4):
                        for j in range(31):
                            S.op("dve", lambda e, cc=cc, j=j: e.tensor_scalar(out=DG[cc][:, j, :], in0=ident, scalar1=CW[:, cc, j:j + 1], scalar2=None, op0=ALU.mult),
                                 reads=[CONST, CW], writes=[DG[cc]])
                    for B in range(4):
                        for cc in range(4):
                            for j in range(31):
                                S.op("pe", lambda e, cc=cc, j=j, B=B: e.matmul(y_ps[cc][:, :], lhsT=DG[cc][:, j, :], rhs=HCT[:, cc, B * 512 + j:B * 512 + j + 512], start=(j == 0), stop=(j == 30)),
                                     reads=[DG[cc], HCT], writes=[y_ps[cc]])
                            S.op("act", lambda e, cc=cc: e.activation(out=Y[:, cc, :], in_=y_ps[cc][:, :], func=AF.Identity, bias=PVT[:, cc:cc + 1]), reads=[y_ps[cc], PVT], writes=[Y])
                            S.op("act", lambda e, cc=cc: e.activation(out=YSQ[:, cc, :], in_=y_ps[cc][:, :], func=AF.Square, bias=PVT[:, cc:cc + 1]), reads=[y_ps[cc], PVT], writes=[YSQ])
                        mean_ps = st_rot.next()
                        msq_ps = st_rot.next()
                        for cc in range(4):
                            S.op("pe", lambda e, cc=cc, mean_ps=mean_ps: e.matmul(mean_ps[:, :], lhsT=ones, rhs=Y[:, cc, :], start=(cc == 0), stop=(cc == 3)), reads=[CONST, Y], writes=[mean_ps])
                        for cc in range(4):
                            S.op("pe", lambda e, cc=cc, msq_ps=msq_ps: e.matmul(msq_ps[:, :], lhsT=ones, rhs=YSQ[:, cc, :], start=(cc == 0), stop=(cc == 3)), reads=[CONST, YSQ], writes=[msq_ps])
                        S.op("act", lambda e, mean_ps=mean_ps: e.activation(out=MEAN[:, :], in_=mean_ps[:, :], func=AF.Copy, scale=1.0 / 512), reads=[mean_ps], writes=[MEAN])
                        S.op("dve", lambda e: e.tensor_tensor(out=MSQ[:, :], in0=MEAN[:, :], in1=MEAN[:, :], op=ALU.mult), reads=[MEAN], writes=[MSQ])
                        S.op("dve", lambda e, msq_ps=msq_ps: e.scalar_tensor_tensor(out=VAR[:, :], in0=msq_ps[:, :], scalar=1.0 / 512, in1=MSQ[:, :], op0=ALU.mult, op1=ALU.subtract),
                             reads=[msq_ps, MSQ], writes=[VAR])
                        S.op("act", lambda e: e.activation(out=VAR[:, :], in_=VAR[:, :], func=AF.Ln, bias=LN_EPS), reads=[VAR], writes=[VAR])
                        S.op("act", lambda e: e.activation(out=RS[:, :], in_=VAR[:, :], func=AF.Exp, scale=-0.5), reads=[VAR], writes=[RS])
                        for cc in range(4):
                            t1 = T1.next()
                            S.op("dve", lambda e, cc=cc, t1=t1: e.tensor_tensor(out=t1[:, :], in0=Y[:, cc, :], in1=MEAN[:, :], op=ALU.subtract), reads=[Y, MEAN], writes=[t1])
                            S.op("dve", lambda e, cc=cc, t1=t1: e.tensor_tensor(out=t1[:, :], in0=t1[:, :], in1=RS[:, :], op=ALU.mult), reads=[t1, RS], writes=[t1])
                            S.op("act", lambda e, cc=cc, t1=t1, B=B: e.activation(out=CATA[B][:, cc, :], in_=t1[:, :], func=AF.Silu, scale=PVT[:, 4 + cc:5 + cc], bias=PVT[:, 8 + cc:9 + cc]),
                                 reads=[t1, PVT], writes=[CATA[B]])
                    S.fence()
            if stop_after == "l0b":
                return
            with ExitStack() as p3:
                def sb(name, shape, dt=F32):
                    return S.sbuf(name, shape, dt, st=p3)
                WING = sb("WING", [128, 8, 1552], BF16)
                WOUT = sb("WOUT", [128, 8, D], BF16)
                S.dma("pool", lambda e: e.dma_start(out=WING[:, :, :], in_=ab_w_in_d.rearrange("(c p) n -> p c n", p=128)[:, :, 1024:2576]), reads=[DR], writes=[WING], sync=WING)
                S.dma("pool", lambda e: e.dma_start(out=WOUT[:, :, :], in_=ab_w_out_d.rearrange("(c p) n -> p c n", p=128)), reads=[DR], writes=[WOUT], sync=WOUT)
                S.op("pool", lambda e: e.tensor_tensor(out=WOUT[:, :, :], in0=WOUT[:, :, :], in1=G1B[:, :].unsqueeze(1).to_broadcast([128, 8, D]), op=ALU.mult), reads=[WOUT, G1B], writes=[WOUT])
                GWS = sb("GWS", [32, 256])
                GWB = sb("GWB", [32, 256], BF16)
                S.op("dve", lambda e: e.memset(GWS[:, :], 0.0), writes=[GWS])
                S.dma("sp", lambda e: [e.dma_start(out=GWS[0:16, :], in_=gate_w_d[:, :]), e.dma_start(out=GWS[16:17, :], in_=gate_b_d[:, :])], reads=[DR], writes=[GWS], sync=GWS, n=2)
                S.op("dve", lambda e: e.tensor_copy(out=GWB[:, :], in_=GWS[:, :]), reads=[GWS], writes=[GWB])
                GLT = Rot([sb("GLT%d" % i, [32, 512], BF16) for i in range(2)])
                for g_ in GLT.tiles:
                    S.op("dve", lambda e, g_=g_: e.memset(g_[:, :], 1.0), writes=[g_])
                NG = sb("NG", [128, 512])
                S.dma("sp", lambda e: e.dma_start(out=NG[:, :], in_=gnorm_d[0:1, :].partition_broadcast(128)), reads=[DR], writes=[NG], sync=NG)
                S32 = sb("S32", [128, 2, 128])
                SBF = sb("SBF", [128, 2, 128], BF16)
                S.op("dve", lambda e: e.memset(S32[:, :, :], 0.0), writes=[S32])
                S.op("dve", lambda e: e.memset(SBF[:, :, :], 0.0), writes=[SBF])
                S32h = [Tile(S32.ap[(h % 2) * 64:(h % 2) * 64 + 64, h // 2, :], "S32h%d" % h) for h in range(4)]
                SBFh = [Tile(SBF.ap[(h % 2) * 64:(h % 2) * 64 + 64, h // 2, :], "SBFh%d" % h) for h in range(4)]
                for h in range(4):
                    S32h[h].w = S32.w
                    SBFh[h].w = SBF.w
                HTB = sb("HTB", [128, 8, 512], BF16)
                QT = sb("QT", [128, 2, 512])
                KT = sb("KT", [128, 2, 512])
                R2 = lambda name, shape, dt=F32: Rot([sb("%s%d" % (name, i), shape, dt) for i in range(2)])
                VB = R2("VB", [128, 512], BF16); RG = R2("RG", [128, 512]); KTOK = R2("KTOK", [128, 256]); E1 = R2("E1", [128, 256]); SP = R2("SP", [128, 256])
                EB = R2("EB", [128, 2, 128]); ENB = R2("ENB", [128, 2, 128]); EDEC = R2("EDEC", [128, 256])
                QS = R2("QS", [128, 4, 128], BF16); KS = R2("KS", [128, 2, 128], BF16); KDEC = R2("KDEC", [128, 256], BF16)
                ATM = Rot([sb("ATM%d" % i, [128, 128], BF16) for i in range(4)])
                SS = R2("SS", [128, 4]); RS4 = R2("RS4", [128, 4]); YB = R2("YB", [128, 512], BF16)
                JUNK = sb("JUNK", [128, 128], BF16)
                for q_ in QS.tiles:
                    S.op("pool", lambda e, q_=q_: e.memset(q_[:, :, :], 0.0), writes=[q_])
                pool_rot = Rot([S.psum("pl%d" % i, [128, 512], F32, st=p3) for i in range(4)])
                o_ps = S.psum("o_ps", [128, 512], F32, st=p3)
                sn_ps = S.psum("sn_ps", [128, 4, 128], F32, st=p3)
                att_ps = S.psum("att_ps", [128, 4, 128], F32, st=p3)
                ybT_ps = S.psum("ybT", [128, 8, 128], BF16, st=p3)
                tp_rot = pool_rot

                def proj(dst_ps, cols, htb_ap, M=None):
                    pass

                gl_of = {}
                stA = {}

                def gla_prologue(B):
                    for i in range(4):
                        transpose_tile(4 * B + i, l, jsc, jsh, s, tp_rot, lambda c, i=i: HTB[:, c, i * 128:(i + 1) * 128], HTB)
                    for c2 in range(2):
                        for (dst, off) in ((QT, 0), (KT, 256)):
                            pp_ = pool_rot.next()
                            for kc in range(8):
                                S.op("pe", lambda e, kc=kc, c2=c2, off=off, pp_=pp_: e.matmul(pp_[:, :], lhsT=WING[:, kc, off + c2 * 128:off + (c2 + 1) * 128], rhs=HTB[:, kc, :], start=(kc == 0), stop=(kc == 7)),
                                     reads=[WING, HTB], writes=[pp_])
                            S.op("act", lambda e, dst=dst, c2=c2, pp_=pp_: e.activation(out=dst[:, c2, :], in_=pp_[:, :], func=AF.Copy), reads=[pp_], writes=[dst])
                    gl = GLT.next()
                    pp_ = pool_rot.next()
                    for kc in range(8):
                        S.op("pe", lambda e, kc=kc, pp_=pp_: e.matmul(pp_[0:16, :], lhsT=WING[:, kc, 1536:1552], rhs=HTB[:, kc, :], start=(kc == 0), stop=(kc == 7)), reads=[WING, HTB], writes=[pp_])
                    S.op("act", lambda e, gl=gl, pp_=pp_: e.activation(out=gl[0:16, :], in_=pp_[0:16, :], func=AF.Copy), reads=[pp_], writes=[gl])
                    gl_of[B] = gl

                def gla_a0(t):
                    B, i = t // 4, t % 4
                    gl = gl_of[B]
                    tc = slice(i * 128, (i + 1) * 128)
                    d = dict(tc=tc, vb=VB.next(), rg=RG.next(), ktok=KTOK.next(), e1=E1.next(), sp=SP.next(), eb=EB.next(), enb=ENB.next(), edec=EDEC.next(),
                             qs=QS.next(), ks=KS.next(), kdec=KDEC.next(), ss=SS.next(), rs4=RS4.next(), yb=YB.next())
                    stA[t] = d
                    e1, sp = d["e1"], d["sp"]
                    zp = pool_rot.next()
                    S.op("pe", lambda e: e.matmul(zp[:, 0:256], lhsT=gl[0:32, tc], rhs=GWB[0:32, :], start=True, stop=True), reads=[gl, GWB], writes=[zp])
                    S.op("act", lambda e: e.activation(out=e1[:, :], in_=zp[:, 0:256], func=AF.Exp, scale=-1.0), reads=[zp], writes=[e1])
                    S.op("act", lambda e: e.activation(out=sp[:, :], in_=e1[:, :], func=AF.Ln, bias=1.0), reads=[e1], writes=[sp])

                def gla_a1(t):
                    d = stA[t]
                    tc, vb, rg, ktok = d["tc"], d["vb"], d["rg"], d["ktok"]
                    kp = pool_rot.next()
                    for kc in range(8):
                        S.op("pe", lambda e, kc=kc: e.matmul(kp[:, 0:256], lhsT=HTB[:, kc, tc], rhs=WING[:, kc, 256:512], start=(kc == 0), stop=(kc == 7)), reads=[WING, HTB], writes=[kp])
                    S.op("dve", lambda e: e.tensor_copy(out=ktok[:, :], in_=kp[:, 0:256]), reads=[kp], writes=[ktok])
                    vp = pool_rot.next()
                    for kc in range(8):
                        S.op("pe", lambda e, kc=kc: e.matmul(vp[:, :], lhsT=HTB[:, kc, tc], rhs=WING[:, kc, 512:1024], start=(kc == 0), stop=(kc == 7)), reads=[WING, HTB], writes=[vp])
                    S.op("act", lambda e: e.activation(out=vb[:, :], in_=vp[:, :], func=AF.Copy), reads=[vp], writes=[vb])
                    rp = pool_rot.next()
                    for kc in range(8):
                        S.op("pe", lambda e, kc=kc: e.matmul(rp[:, :], lhsT=HTB[:, kc, tc], rhs=WING[:, kc, 1024:1536], start=(kc == 0), stop=(kc == 7)), reads=[WING, HTB], writes=[rp])
                    S.op("act", lambda e: e.activation(out=rg[:, :], in_=rp[:, :], func=AF.Silu), reads=[rp], writes=[rg])
                    S.op("pool", lambda e: e.tensor_tensor(out=rg[:, :], in0=rg[:, :], in1=NG[:, :], op=ALU.mult), reads=[rg, NG], writes=[rg])

                def gla_a2(t):
                    d = stA[t]
                    tc, sp, eb, enb, edec, qs, ks, kdec, ktok = d["tc"], d["sp"], d["eb"], d["enb"], d["edec"], d["qs"], d["ks"], d["kdec"], d["ktok"]
                    revp = pool_rot.next()
                    for c2 in range(2):
                        S.op("pe", lambda e, c2=c2: e.matmul(revp[:, 256 + c2 * 128:256 + (c2 + 1) * 128], lhsT=sp[:, c2 * 128:(c2 + 1) * 128], rhs=tri, start=True, stop=True), reads=[sp, CONST], writes=[revp])
                    S.op("pe", lambda e: e.matmul(revp[:, 0:256], lhsT=su, rhs=sp[:, :], start=True, stop=True), reads=[sp, CONST], writes=[revp])
                    S.op("act", lambda e: e.activation(out=eb[:, :, :], in_=revp[:, 256:512].rearrange("p (a b) -> p a b", a=2), func=AF.Exp, scale=-1.0 / 16), reads=[revp], writes=[eb])
                    S.op("act", lambda e: e.activation(out=enb[:, :, :], in_=revp[:, 256:512].rearrange("p (a b) -> p a b", a=2), func=AF.Exp, scale=1.0 / 16), reads=[revp], writes=[enb])
                    S.op("act", lambda e: e.activation(out=edec[:, :], in_=revp[:, 0:256], func=AF.Exp, scale=-1.0 / 16), reads=[revp], writes=[edec])
                    for h in range(4):
                        c2, hp = h // 2, (h % 2) * 64
                        S.op("dve", lambda e, h=h, c2=c2, hp=hp: e.scalar_tensor_tensor(out=qs[hp:hp + 64, h, :], in0=QT[hp:hp + 64, c2, tc], scalar=0.125, in1=eb[hp:hp + 64, c2, :], op0=ALU.mult, op1=ALU.mult),
                             reads=[QT, eb], writes=[qs])
                    S.op("dve", lambda e: e.tensor_tensor(out=ks[:, :, :], in0=KT[:, :, tc], in1=enb[:, :, :], op=ALU.mult), reads=[KT, enb], writes=[ks])
                    S.op("dve", lambda e: e.tensor_tensor(out=kdec[:, :], in0=ktok[:, :], in1=edec[:, :], op=ALU.mult), reads=[ktok, edec], writes=[kdec])

                def gla_b0(t):
                    d = stA[t]
                    qs, ks = d["qs"], d["ks"]
                    for h in range(4):
                        c2 = h // 2
                        S.op("pe", lambda e, c2=c2, h=h: e.matmul(att_ps[:, h, :], lhsT=ks[:, c2, :], rhs=qs[:, h, :], start=True, stop=True), reads=[ks, qs], writes=[att_ps])
                    atms = []
                    for h in range(4):
                        atm = ATM.next()
                        S.op("dve", lambda e, atm=atm, h=h: e.tensor_tensor(out=atm[:, :], in0=att_ps[:, h, :], in1=tri, op=ALU.mult), reads=[att_ps, CONST], writes=[atm])
                        atms.append(atm)
                    d["atms"] = atms

                def gla_b1(t):
                    d = stA[t]
                    vb, rg, eb, qs, kdec, ss, rs4, yb, atms = d["vb"], d["rg"], d["eb"], d["qs"], d["kdec"], d["ss"], d["rs4"], d["yb"], d["atms"]
                    for h in range(4):
                        c2 = h // 2
                        hc = slice(h * 128, (h + 1) * 128)
                        atm = atms[h]
                        S.op("pe", lambda e, c2=c2, hc=hc, h=h: e.matmul(o_ps[:, hc], lhsT=qs[:, h, :], rhs=SBF[:, c2, :], start=True, stop=False), reads=[qs, SBFh[2 * c2], SBFh[2 * c2 + 1]], writes=[o_ps])
                        S.op("pe", lambda e, atm=atm, hc=hc: e.matmul(o_ps[:, hc], lhsT=atm[:, :], rhs=vb[:, hc], start=False, stop=True), reads=[atm, vb], writes=[o_ps])
                    for h in range(4):
                        c2 = h // 2
                        hc = slice(h * 128, (h + 1) * 128)
                        S.op("pe", lambda e, c2=c2, hc=hc, h=h: e.matmul(sn_ps[:, h, :], lhsT=kdec[:, c2 * 128:(c2 + 1) * 128], rhs=vb[:, hc], start=True, stop=True), reads=[kdec, vb], writes=[sn_ps])
                    for h in range(4):
                        c2, hp = h // 2, (h % 2) * 64
                        S.op("dve", lambda e, c2=c2, hp=hp, h=h: e.scalar_tensor_tensor(out=S32[hp:hp + 64, c2, :], in0=S32[hp:hp + 64, c2, :], scalar=eb[hp:hp + 64, c2, 127:128],
                                                                                         in1=sn_ps[hp:hp + 64, h, :], op0=ALU.mult, op1=ALU.add), reads=[S32h[h], eb, sn_ps], writes=[S32h[h]])
                        S.op("pool", lambda e, c2=c2, hp=hp: e.tensor_copy(out=SBF[hp:hp + 64, c2, :], in_=S32[hp:hp + 64, c2, :]), reads=[S32h[h]], writes=[SBFh[h]])
                    for h in range(4):
                        hc = slice(h * 128, (h + 1) * 128)
                        S.op("act", lambda e, hc=hc, h=h: e.activation(out=JUNK[:, :], in_=o_ps[:, hc], func=AF.Square, accum_out=ss[:, h:h + 1]), reads=[o_ps], writes=[JUNK, ss])
                    S.op("dve", lambda e: e.tensor_scalar(out=ss[:, :], in0=ss[:, :], scalar1=1.0 / 128, scalar2=RMS_EPS, op0=ALU.mult, op1=ALU.add), reads=[ss], writes=[ss])
                    S.op("pool", lambda e: e.tensor_tensor(out=rs4[:, :], in0=ss[:, :], in1=NHALF[:, 0:4], op=ALU.pow), reads=[ss, NHALF], writes=[rs4])
                    for h in range(4):
                        hc = slice(h * 128, (h + 1) * 128)
                        S.op("dve", lambda e, hc=hc, h=h: e.scalar_tensor_tensor(out=yb[:, hc], in0=o_ps[:, hc], scalar=rs4[:, h:h + 1], in1=rg[:, hc], op0=ALU.mult, op1=ALU.mult),
                             reads=[o_ps, rs4, rg], writes=[yb])

                def gla_b2a(t):
                    yb = stA[t]["yb"]
                    for h in range(4):
                        S.op("pe", lambda e, h=h: e.transpose(ybT_ps[:, h, :], yb[:, h * 128:(h + 1) * 128], IDB[:, :]), reads=[yb, IDB], writes=[ybT_ps])
                    S.op("act", lambda e: e.activation(out=CATB[t][:, :, :], in_=ybT_ps[:, 0:4, :], func=AF.Copy), reads=[ybT_ps], writes=[CATB[t]])

                def gla_b2b(t):
                    stA.pop(t)
                    B = t // 4
                    for hf in range(2):
                        mp = pool_rot.next()
                        for kc in range(8):
                            S.op("pe", lambda e, kc=kc, hf=hf, mp=mp: e.matmul(mp[:, :], lhsT=HT[:, kc, t * 128:(t + 1) * 128], rhs=WOUT[:, kc, hf * 512:(hf + 1) * 512], start=(kc == 0), stop=(kc == 7)),
                                 reads=[CATA[B], CATB[t], WOUT], writes=[mp])
                        S.op("dve", lambda e, hf=hf, mp=mp: e.tensor_tensor(out=XT[t][:, hf * 512:(hf + 1) * 512], in0=mp[:, :], in1=XT[t][:, hf * 512:(hf + 1) * 512], op=ALU.add),
                             reads=[mp, XT[t]], writes=[XT[t]])
                    lnp.push(t)

                def gla_a_all(t):
                    gla_a0(t); gla_a1(t); gla_a2(t)

                lnp = LNPipe()
                gla_prologue(0)
                gla_a_all(0)
                for t in range(NT + 1):
                    nxt = t + 1 if t + 1 < NT else None
                    if nxt is not None and nxt % 4 == 0:
                        gla_prologue(nxt // 4)
                    if nxt is not None:
                        gla_a0(nxt)
                    if t < NT:
                        gla_b0(t)
                    if t >= 1:
                        gla_b2a(t - 1)
                    if nxt is not None:
                        gla_a1(nxt)
                        gla_a2(nxt)
                    if t < NT:
                        gla_b1(t)
                    if t >= 1:
                        gla_b2b(t - 1)
                lnp.flush()
                S.fence()

        def l1_mixer(s):
            l = 1
            jsh, jsc, jg = 0, 8, 16
            CQB = [Tile(HT.ap[:, 0:6, B * 512:(B + 1) * 512], "CQB%d" % B) for B in range(4)]
            load_ln(l, 0, True)
            with ExitStack() as pa:
                CS1 = S.sbuf("CS1", [64, S_LEN], F32, st=pa)
                CS2 = S.sbuf("CS2", [64, S_LEN], F32, st=pa)
                with ExitStack() as pr:
                    POSI = S.sbuf("POSI", [64, S_LEN], I32, st=pr)
                    ANG = S.sbuf("ANG", [64, S_LEN], F32, st=pr)
                    TT = S.sbuf("TT", [64, S_LEN], F32, st=pr)
                    TI = S.sbuf("TI", [64, S_LEN], I32, st=pr)
                    FR = S.sbuf("FR", [64, S_LEN], F32, st=pr)
                    MK = S.sbuf("MK", [64, S_LEN], F32, st=pr)
                    S.dma("sp", lambda e: e.dma_start(out=POSI[:, :], in_=pos_d[s:s + 1, :].partition_broadcast(64)), reads=[DR], writes=[POSI], sync=POSI)
                    S.op("dve", lambda e: e.tensor_copy(out=ANG[:, :], in_=POSI[:, :]), reads=[POSI], writes=[ANG])
                    S.op("dve", lambda e: e.tensor_scalar(out=ANG[:, :], in0=ANG[:, :], scalar1=CONST[0:64, C_INVF:C_INVF + 1], scalar2=None, op0=ALU.mult), reads=[ANG, CONST], writes=[ANG])
                    for (dst, shift, sgn) in ((CS1, 0.75, False), (CS2, 0.5, True)):
                        S.op("dve", lambda e, shift=shift: e.tensor_scalar(out=TT[:, :], in0=ANG[:, :], scalar1=1.0 / TWO_PI, scalar2=shift, op0=ALU.mult, op1=ALU.add), reads=[ANG], writes=[TT])
                        S.op("dve", lambda e: e.tensor_copy(out=TI[:, :], in_=TT[:, :]), reads=[TT], writes=[TI])
                        S.op("dve", lambda e: e.tensor_copy(out=FR[:, :], in_=TI[:, :]), reads=[TI], writes=[FR])
                        S.op("dve", lambda e: e.tensor_tensor(out=FR[:, :], in0=TT[:, :], in1=FR[:, :], op=ALU.subtract), reads=[TT, FR], writes=[FR])
                        S.op("dve", lambda e: e.tensor_single_scalar(out=MK[:, :], in_=FR[:, :], scalar=0.0, op=ALU.is_lt), reads=[FR], writes=[MK])
                        S.op("dve", lambda e: e.tensor_tensor(out=FR[:, :], in0=FR[:, :], in1=MK[:, :], op=ALU.add), reads=[FR, MK], writes=[FR])
                        S.op("dve", lambda e: e.tensor_single_scalar(out=MK[:, :], in_=FR[:, :], scalar=1.0, op=ALU.is_ge), reads=[FR], writes=[MK])
                        S.op("dve", lambda e: e.tensor_tensor(out=FR[:, :], in0=FR[:, :], in1=MK[:, :], op=ALU.subtract), reads=[FR, MK], writes=[FR])
                        S.op("act", lambda e, dst=dst: e.activation(out=dst[:, :], in_=FR[:, :], func=AF.Sin, scale=TWO_PI, bias=CONST[0:64, C_NPI:C_NPI + 1]), reads=[FR, CONST], writes=[dst])
                        if sgn:
                            S.op("dve", lambda e, dst=dst: e.tensor_scalar(out=dst[:, :], in0=dst[:, :], scalar1=CONST[0:64, C_SGN:C_SGN + 1], scalar2=None, op0=ALU.mult), reads=[dst, CONST], writes=[dst])
                    S.fence()
                WUQ = S.sbuf("WUQ", [128, 3, 2048], BF16, st=pa)
                WUKV = S.sbuf("WUKV", [128, 2, 2048], BF16, st=pa)
                S.dma("pool", lambda e: [e.dma_start(out=WUQ[:, :, 0:1024], in_=mla_w_uq_d.rearrange("(c p) n -> p c n", p=128)[:, :, 0:1024]),
                                         e.dma_start(out=WUQ[:, :, 1024:2048], in_=mla_w_uq_d.rearrange("(c p) n -> p c n", p=128)[:, :, 1024:2048])],
                      reads=[DR], writes=[WUQ], sync=WUQ, n=2)
                S.dma("pool", lambda e: [e.dma_start(out=WUKV[:, :, 0:1024], in_=mla_w_ukv_d.rearrange("(c p) n -> p c n", p=128)[:, :, 0:1024]),
                                         e.dma_start(out=WUKV[:, :, 1024:2048], in_=mla_w_ukv_d.rearrange("(c p) n -> p c n", p=128)[:, :, 1024:2048])],
                      reads=[DR], writes=[WUKV], sync=WUKV, n=2)
                with ExitStack() as p1:
                    def sb(name, shape, dt=F32):
                        return S.sbuf(name, shape, dt, st=p1)
                    WIN1 = sb("WIN1", [128, 8, 768], BF16)
                    S.dma("pool", lambda e: e.dma_start(out=WIN1[:, :, :], in_=mla_w_in_d.rearrange("(c p) n -> p c n", p=128)), reads=[DR], writes=[WIN1], sync=WIN1)
                    HTB = sb("HTB1", [128, 8, 512], BF16)
                    UT = sb("UT", [128, 5, 512])
                    SQ = Rot([sb("SQ%d" % i, [128, 512]) for i in range(2)])
                    RQ = sb("RQ", [128, 512]); RKV = sb("RKV", [128, 512])
                    T1 = sb("T1a", [64, 512]); T2 = sb("T2a", [64, 512])
                    scr = sb("scr1", [128, 8, 128])
                    pool_rot = Rot([S.psum("pla%d" % i, [128, 512], F32, st=p1) for i in range(6)])
                    ssq = [S.psum("ssq%d" % i, [128, 512], F32, st=p1) for i in range(2)]
                    build_g1b(l, jg, s, pool_rot.tiles[0:2], scr)
                    for B in range(4):
                        bc = slice(B * 512, (B + 1) * 512)
                        for i in range(4):
                            transpose_tile(4 * B + i, l, jsc, jsh, s, pool_rot, lambda c, i=i: HTB[:, c, i * 128:(i + 1) * 128], HTB)
                        for j in range(5):
                            up = pool_rot.next()
                            for kc in range(8):
                                S.op("pe", lambda e, kc=kc, j=j, up=up: e.matmul(up[:, :], lhsT=WIN1[:, kc, j * 128:(j + 1) * 128], rhs=HTB[:, kc, :], start=(kc == 0), stop=(kc == 7)), reads=[WIN1, HTB], writes=[up])
                            S.op("act", lambda e, j=j, up=up: e.activation(out=UT[:, j, :], in_=up[:, :], func=AF.Copy), reads=[up], writes=[UT])
                            sq = SQ.next()
                            S.op("act", lambda e, sq=sq, up=up: e.activation(out=sq[:, :], in_=up[:, :], func=AF.Square), reads=[up], writes=[sq])
                            sp_ = ssq[0] if j < 3 else ssq[1]
                            S.op("pe", lambda e, sq=sq, sp_=sp_, j=j: e.matmul(sp_[:, :], lhsT=ones, rhs=sq[:, :], start=(j in (0, 3)), stop=(j in (2, 4))), reads=[CONST, sq], writes=[sp_])
                        for (dst, sp_, n_) in ((RQ, ssq[0], 384.0), (RKV, ssq[1], 256.0)):
                            S.op("act", lambda e, dst=dst, sp_=sp_, n_=n_: e.activation(out=dst[:, :], in_=sp_[:, :], func=AF.Ln, scale=1.0 / n_, bias=CONST[:, C_REPS:C_REPS + 1]), reads=[sp_, CONST], writes=[dst])
                            S.op("act", lambda e, dst=dst: e.activation(out=dst[:, :], in_=dst[:, :], func=AF.Exp, scale=-0.5), reads=[dst], writes=[dst])
                        for j in range(5):
                            rr = RQ if j < 3 else RKV
                            S.op("dve", lambda e, j=j, rr=rr, bc=bc: e.scalar_tensor_tensor(out=HT[:, j, bc], in0=UT[:, j, :], scalar=PVT[:, 12 + j:13 + j], in1=rr[:, :], op0=ALU.mult, op1=ALU.mult),
                                 reads=[UT, PVT, rr], writes=[CQB[B]])
                        a_p = pool_rot.next()
                        b_p = pool_rot.next()
                        for (pp_, off) in ((a_p, 640), (b_p, 704)):
                            for kc in range(8):
                                S.op("pe", lambda e, kc=kc, pp_=pp_, off=off: e.matmul(pp_[0:64, :], lhsT=WIN1[:, kc, off:off + 64], rhs=HTB[:, kc, :], start=(kc == 0), stop=(kc == 7)), reads=[WIN1, HTB], writes=[pp_])
                        S.op("dve", lambda e, a_p=a_p, bc=bc: e.tensor_tensor(out=T1[:, :], in0=a_p[0:64, :], in1=CS1[:, bc], op=ALU.mult), reads=[a_p, CS1], writes=[T1])
                        S.op("dve", lambda e, b_p=b_p, bc=bc: e.tensor_tensor(out=T2[:, :], in0=b_p[0:64, :], in1=CS2[:, bc], op=ALU.mult), reads=[b_p, CS2], writes=[T2])
                        S.op("pool", lambda e, bc=bc: e.tensor_tensor(out=HT[0:64, 5, bc], in0=T1[:, :], in1=T2[:, :], op=ALU.add), reads=[T1, T2], writes=[CQB[B]])
                    S.fence()
                if stop_after == "l1a":
                    return
                with ExitStack() as p2:
                    def sb(name, shape, dt=F32):
                        return S.sbuf(name, shape, dt, st=p2)
                    WOUT = sb("WOUT1", [128, 8, D], BF16)
                    S.dma("pool", lambda e: e.dma_start(out=WOUT[:, :, :], in_=mla_w_out_d.rearrange("(c p) n -> p c n", p=128)), reads=[DR], writes=[WOUT], sync=WOUT)
                    S.op("pool", lambda e: e.tensor_tensor(out=WOUT[:, :, :], in0=WOUT[:, :, :], in1=G1B[:, :].unsqueeze(1).to_broadcast([128, 8, D]), op=ALU.mult), reads=[WOUT, G1B], writes=[WOUT])
                    QN = sb("QN", [128, S_LEN], BF16); QR = sb("QR", [64, S_LEN], BF16); KN = sb("KN", [128, S_LEN], BF16); VT = sb("VT", [128, NT, 128], BF16)
                    PT = Rot([sb("PT%d" % i, [128, 512], BF16) for i in range(5)])
                    RINV = sb("RINV", [128, 512])
                    OTH = Rot([sb("OTH%d" % i, [128, S_LEN], BF16) for i in range(2)])
                    T1 = sb("T1b", [64, 512]); T2 = sb("T2b", [64, 512])
                    pool_rot = Rot([S.psum("plb%d" % i, [128, 512], F32, st=p2) for i in range(3)])
                    st_rot = Rot([S.psum("stb%d" % i, [128, 512], F32, st=p2) for i in range(3)])
                    o_ps = S.psum("o1_ps", [128, 512], F32, st=p2)
                    r_ps = S.psum("r1_ps", [128, 512], F32, st=p2)
                    lnp = LNPipe()
                    for h in range(8 if dbg >= 2 else 0):
                        hb = h * 256
                        for B in range(4):
                            bc = slice(B * 512, (B + 1) * 512)
                            p_ = pool_rot.next()
                            for k3 in range(3):
                                S.op("pe", lambda e, k3=k3, p_=p_, bc=bc, hb=hb: e.matmul(p_[:, :], lhsT=WUQ[:, k3, hb:hb + 128], rhs=HT[:, k3, bc], start=(k3 == 0), stop=(k3 == 2)), reads=[WUQ, CQB[B]], writes=[p_])
                            S.op("act", lambda e, p_=p_, bc=bc: e.activation(out=QN[:, bc], in_=p_[:, :], func=AF.Copy), reads=[p_], writes=[QN])
                            a_p = pool_rot.next()
                            b_p = pool_rot.next()
                            for (pp_, off) in ((a_p, hb + 128), (b_p, hb + 192)):
                                for k3 in range(3):
                                    S.op("pe", lambda e, k3=k3, pp_=pp_, off=off, bc=bc: e.matmul(pp_[0:64, :], lhsT=WUQ[:, k3, off:off + 64], rhs=HT[:, k3, bc], start=(k3 == 0), stop=(k3 == 2)), reads=[WUQ, CQB[B]], writes=[pp_])
                            S.op("dve", lambda e, a_p=a_p, bc=bc: e.tensor_tensor(out=T1[:, :], in0=a_p[0:64, :], in1=CS1[:, bc], op=ALU.mult), reads=[a_p, CS1], writes=[T1])
                            S.op("dve", lambda e, b_p=b_p, bc=bc: e.tensor_tensor(out=T2[:, :], in0=b_p[0:64, :], in1=CS2[:, bc], op=ALU.mult), reads=[b_p, CS2], writes=[T2])
                            S.op("pool", lambda e, bc=bc: e.tensor_tensor(out=QR[:, bc], in0=T1[:, :], in1=T2[:, :], op=ALU.add), reads=[T1, T2], writes=[QR])
                            p_ = pool_rot.next()
                            for k2 in range(2):
                                S.op("pe", lambda e, k2=k2, p_=p_, bc=bc, hb=hb: e.matmul(p_[:, :], lhsT=WUKV[:, k2, hb:hb + 128], rhs=HT[:, 3 + k2, bc], start=(k2 == 0), stop=(k2 == 1)), reads=[WUKV, CQB[B]], writes=[p_])
                            S.op("act", lambda e, p_=p_, bc=bc: e.activation(out=KN[:, bc], in_=p_[:, :], func=AF.Copy), reads=[p_], writes=[KN])
                            p_ = pool_rot.next()
                            for i in range(4):
                                t = 4 * B + i
                                for k2 in range(2):
                                    S.op("pe", lambda e, k2=k2, p_=p_, i=i, t=t, hb=hb: e.matmul(p_[:, i * 128:(i + 1) * 128], lhsT=HT[:, 3 + k2, t * 128:(t + 1) * 128], rhs=WUKV[:, k2, hb + 128:hb + 256], start=(k2 == 0), stop=(k2 == 1)),
                                         reads=[WUKV, CQB[B]], writes=[p_])
                            S.op("act", lambda e, p_=p_, B=B: e.activation(out=VT[:, 4 * B:4 * B + 4, :], in_=p_[:, :].rearrange("p (a b) -> p a b", a=4), func=AF.Copy), reads=[p_], writes=[VT])
                        oth = OTH.next()
                        steps = [(Q, kt) for Q in range(4) for kt in range(4 * Q + 4)]
                        pend = {}

                        def do_st(Q, kt):
                            m = kt - 4 * Q if kt >= 4 * Q else 0
                            c0 = m * 128
                            st_ = st_rot.next()
                            qc = slice(Q * 512 + c0, (Q + 1) * 512)
                            kc_ = slice(kt * 128, (kt + 1) * 128)
                            S.op("pe", lambda e: e.matmul(st_[:, c0:512], lhsT=KN[:, kc_], rhs=QN[:, qc], start=True, stop=False), reads=[KN, QN], writes=[st_])
                            S.op("pe", lambda e: e.matmul(st_[:, c0:512], lhsT=HT[0:64, 5, kc_], rhs=QR[0:64, qc], start=False, stop=True), reads=[CQB[kt // 4], QR], writes=[st_])
                            pt = PT.next()
                            S.op("act", lambda e: e.activation(out=pt[:, c0:512], in_=st_[:, c0:512], func=AF.Exp, scale=MLA_SCALE), reads=[st_], writes=[pt])
                            if kt >= 4 * Q:
                                S.op("pool", lambda e: e.tensor_tensor(out=pt[:, c0:c0 + 128], in0=pt[:, c0:c0 + 128], in1=TRIB[:, :], op=ALU.mult), reads=[pt, TRIB], writes=[pt])
                            pend[(Q, kt)] = (pt, c0)

                        def do_pv(Q, kt, oth=oth):
                            pt, c0 = pend.pop((Q, kt))
                            last = (kt == 4 * Q + 3)
                            S.op("pe", lambda e: e.matmul(o_ps[:, c0:512], lhsT=VT[:, kt, :], rhs=pt[:, c0:512], start=(kt == 0), stop=last), reads=[VT, pt], writes=[o_ps])
                            S.op("pe", lambda e: e.matmul(r_ps[:, c0:512], lhsT=ONEB[:, :], rhs=pt[:, c0:512], start=(kt == 0), stop=last), reads=[ONEB, pt], writes=[r_ps])
                            if last:
                                qc = slice(Q * 512, (Q + 1) * 512)
                                S.op("dve", lambda e: e.reciprocal(out=RINV[:, :], in_=r_ps[:, :]), reads=[r_ps], writes=[RINV])
                                S.op("dve", lambda e: e.tensor_tensor(out=oth[:, qc], in0=o_ps[:, :], in1=RINV[:, :], op=ALU.mult), reads=[o_ps, RINV], writes=[oth])

                        for i_, (Q, kt) in enumerate(steps):
                            do_st(Q, kt)
                            if i_ >= 2:
                                do_pv(*steps[i_ - 2])
                        do_pv(*steps[-2])
                        do_pv(*steps[-1])
                        for t in range(NT):
                            for hf in range(2):
                                mp = pool_rot.next()
                                S.op("pe", lambda e, t=t, hf=hf, mp=mp, h=h, oth=oth: e.matmul(mp[:, :], lhsT=oth[:, t * 128:(t + 1) * 128], rhs=WOUT[:, h, hf * 512:(hf + 1) * 512], start=True, stop=True), reads=[oth, WOUT], writes=[mp])
                                S.op("dve", lambda e, t=t, hf=hf, mp=mp: e.tensor_tensor(out=XT[t][:, hf * 512:(hf + 1) * 512], in0=mp[:, :], in1=XT[t][:, hf * 512:(hf + 1) * 512], op=ALU.add),
                                     reads=[mp, XT[t]], writes=[XT[t]])
                            if h == 7:
                                lnp.push(t)
                    lnp.flush()
                    S.fence()

        for s in range(nseq):
            for t in range(NT):
                S.dma("sp", lambda e, t=t, s=s: e.dma_start(out=XT[t][:, :], in_=x_d[s, t * 128:(t + 1) * 128, :]), reads=[DR], writes=[XT[t]], sync=XT[t])
                S.op("act", lambda e, t=t: e.activation(out=XT[t][:, :], in_=XT[t][:, :], func=AF.Copy, scale=ALPHA), reads=[XT[t]], writes=[XT[t]])
            if stop_after == "moe0":
                moe_phase(0, s, last=True)
            elif stop_after in ("xm1only", "l1a"):
                l1_mixer(s)
            elif stop_after != "load":
                l0_mixer(s)
                if stop_after not in ("xm0", "l0a", "l0b"):
                    moe_phase(0, s, last=False)
                    if stop_after != "xf0":
                        l1_mixer(s)
                        if stop_after != "xm1":
                            moe_phase(1, s, last=True)
            for t in range(NT):
                S.dma("sp", lambda e, t=t, s=s: e.dma_start(out=out_d[s, t * 128:(t + 1) * 128, :], in_=XT[t][:, :]), reads=[XT[t]], writes=[DO], sync=XT[t])
            S.fence()
        S.emit(final_keys=["X%d" % t for t in range(NT)])
    return nc


def prep_shared(inp):
    f = lambda a: np.ascontiguousarray(np.asarray(a, dtype=np.float32))
    sh = {}
    sh["consts"] = make_consts()
    sh["ada_w"] = f(inp["ada_w"])
    sh["ada_b"] = f(inp["ada_b"])
    sh["ln_gb"] = f(np.stack([inp["ln_mix_g"], inp["ln_mix_b"], inp["ln_ffn_g"], inp["ln_ffn_b"]], axis=1))
    sh["ab_w_in"] = f(inp["ab_w_in"][0])
    sh["conv_w"] = f(inp["conv_w"][0])
    pv = np.zeros((40, 128), np.float32)
    pv[0:4] = np.asarray(inp["conv_b"][0]).reshape(4, 128)
    pv[4:8] = np.asarray(inp["conv_ln_g"][0]).reshape(4, 128)
    pv[8:12] = np.asarray(inp["conv_ln_b"][0]).reshape(4, 128)
    pv[12:15] = np.asarray(inp["mla_q_norm_g"][0]).reshape(3, 128)
    pv[15:17] = np.asarray(inp["mla_kv_norm_g"][0]).reshape(2, 128)
    sh["pvec"] = pv
    sh["gla_gate_w"] = f(inp["gla_gate_w"][0])
    sh["gla_gate_b"] = f(inp["gla_gate_b"][0]).reshape(1, 256)
    sh["gla_norm_g"] = f(inp["gla_norm_g"][0]).reshape(1, 512)
    sh["ab_w_out"] = f(inp["ab_w_out"][0])
    w_in = np.asarray(inp["mla_w_in"][0], dtype=np.float32)
    sh["mla_w_in"] = f(np.concatenate([w_in, w_in[:, 672:704], w_in[:, 640:672]], axis=1))
    wq = np.asarray(inp["mla_w_uq"][0], dtype=np.float32).reshape(384, 8, 192)
    sh["mla_w_uq"] = f(np.concatenate([wq, wq[:, :, 160:192], wq[:, :, 128:160]], axis=2).reshape(384, 2048))
    sh["mla_w_ukv"] = f(inp["mla_w_ukv"][0])
    sh["mla_w_out"] = f(inp["mla_w_out"][0])
    sh["moe_wr"] = f(np.concatenate([inp["moe_w_group"], inp["moe_w_router"]], axis=2))
    sh["moe_br"] = f(np.concatenate([inp["moe_b_group"], inp["moe_b_router"]], axis=1))
    sh["consts2"] = make_consts2()
    wg = np.asarray(inp["moe_w_gate"], dtype=np.float32).reshape(2, 32, 8, 128, 256).transpose(0, 1, 3, 2, 4)
    wu = np.asarray(inp["moe_w_up"], dtype=np.float32).reshape(2, 32, 8, 128, 256).transpose(0, 1, 3, 2, 4)
    wd = np.asarray(inp["moe_w_down"], dtype=np.float32).reshape(2, 32, 2, 128, 1024).transpose(0, 1, 3, 2, 4)
    wall = np.concatenate([np.concatenate([wg, wu], axis=4).reshape(2, 32, 128, 4096), wd.reshape(2, 32, 128, 2048)], axis=3)
    for i in range(2):
        sh["moe_wall%d" % i] = f(wall[i].reshape(32 * 128, 6144))
    return sh


def kernel(**inputs):
    sh = prep_shared(inputs)
    x = np.asarray(inputs["x"], dtype=np.float32)
    c = np.asarray(inputs["c"], dtype=np.float32)
    pos = np.asarray(inputs["positions"], dtype=np.int32)
    nc = build_nc()
    in_maps = []
    for i in range(NCORES):
        m = dict(sh)
        m["x"] = np.ascontiguousarray(x[2 * i:2 * i + 2])
        m["c"] = np.ascontiguousarray(c[2 * i:2 * i + 2])
        m["pos"] = np.ascontiguousarray(pos[2 * i:2 * i + 2])
        in_maps.append(m)
    res = run_bass_kernel_spmd(nc, in_maps, core_ids=list(range(NCORES)))
    return np.concatenate([r["out"] for r in res.results], axis=0).astype(np.float32)
```
